# Optimizing a Trainium2 kernel written in Bass

```python
import math
import jax, jax.numpy as jnp
from jax import lax
import numpy as np

D_MODEL = 1024
BATCH = 2
SEQ = 8192
DEPTH = 4

CHUNK = 64
D_MIX = D_MODEL
W_GROUP = D_MIX // 4
RET_HEADS = 4
RET_DV = W_GROUP // RET_HEADS
RET_DK = RET_DV // 2
ROPE_BASE = 10000.0
CONF_CH = W_GROUP
CONF_KERNEL = 31
SC_CH = W_GROUP
SC_KERNEL = 3
FOX_HEADS = 4
FOX_DH = W_GROUP // FOX_HEADS
FOX_FORGET_BIAS = 3.0
Q_BLOCK = 128
N_EXPERTS = 32
TOP_K = 4
D_FF_EXPERT = D_MODEL
SWIGLU_ALPHA = 1.702
SWIGLU_LIMIT = 7.0
MOE_BLOCK = 128
DEEPNORM_ALPHA = (2 * DEPTH) ** 0.25
DEEPNORM_BETA = (8 * DEPTH) ** -0.25
LN_EPS = 1e-5
SPLIT_SIZES = (
    RET_HEADS * RET_DK, RET_HEADS * RET_DK, RET_HEADS * RET_DV, RET_HEADS * RET_DV,
    CONF_CH, CONF_CH,
    SC_CH, SC_CH, SC_CH,
    W_GROUP, W_GROUP, W_GROUP, FOX_HEADS,
)
D_IN = 2820

kernel_name = 'hybrid_ret_conv_fox_moe_deepnorm'


def layer_norm(x, g, b=None):
    xf = x.astype(jnp.float32)
    mu = jnp.mean(xf, axis=-1, keepdims=True)
    var = jnp.mean(jnp.square(xf - mu), axis=-1, keepdims=True)
    y = (xf - mu) * lax.rsqrt(var + LN_EPS) * g.astype(jnp.float32)
    if b is not None:
        y = y + b.astype(jnp.float32)
    return y.astype(x.dtype)


def causal_depthwise_conv(x, w):
    width = w.shape[0]
    return lax.conv_general_dilated(
        x, w[:, None, :].astype(x.dtype), window_strides=(1,), padding=((width - 1, 0),),
        dimension_numbers=('NWC', 'WIO', 'NWC'), feature_group_count=x.shape[-1])


def rope(x, pos):
    half = x.shape[-1] // 2
    freqs = ROPE_BASE ** (-jnp.arange(half, dtype=jnp.float32) / half)
    ang = pos.astype(jnp.float32)[:, None] * freqs[None, :]
    cos = jnp.cos(ang)[:, None, :].astype(x.dtype)
    sin = jnp.sin(ang)[:, None, :].astype(x.dtype)
    x1, x2 = x[..., :half], x[..., half:]
    return jnp.concatenate([x1 * cos - x2 * sin, x1 * sin + x2 * cos], axis=-1)


def chunk_retention(q, k, v):
    bsz, seq, heads, dk = q.shape
    dv = v.shape[-1]
    nc = seq // CHUNK
    log_g = jnp.log1p(-(2.0 ** (-5.0 - jnp.arange(heads, dtype=jnp.float32))))
    qc = q.reshape(bsz, nc, CHUNK, heads, dk)
    kc = k.reshape(bsz, nc, CHUNK, heads, dk)
    vc = v.reshape(bsz, nc, CHUNK, heads, dv)
    idx = jnp.arange(CHUNK, dtype=jnp.float32)
    dist = jnp.abs(idx[:, None] - idx[None, :])
    intra_decay = jnp.exp(log_g[:, None, None] * dist[None]).astype(q.dtype)
    scores = jnp.einsum('bcnhd,bcmhd->bchnm', qc, kc) * intra_decay
    o_intra = jnp.einsum('bchnm,bcmhe->bcnhe', scores, vc)
    k_decay = jnp.exp(log_g[None, :] * (CHUNK - 1 - idx)[:, None]).astype(k.dtype)
    kv = jnp.einsum('bcmhd,bcmhe->bchde', kc * k_decay[:, :, None], vc)
    chunk_decay = jnp.exp(log_g * CHUNK).astype(kv.dtype)[:, None, None]

    def step(state, kv_c):
        return chunk_decay * state + kv_c, state

    _, prev = lax.scan(step, jnp.zeros_like(kv[:, 0]), jnp.moveaxis(kv, 1, 0))
    prev = jnp.moveaxis(prev, 0, 1)
    q_decay = jnp.exp(log_g[None, :] * (idx + 1.0)[:, None]).astype(q.dtype)
    o_cross = jnp.einsum('bcnhd,bchde->bcnhe', qc * q_decay[:, :, None], prev)
    return (o_intra + o_cross).reshape(bsz, seq, heads, dv)


def forgetting_attention(q, k, v, log_f):
    bsz, seq, heads, dh = q.shape
    nb = seq // Q_BLOCK
    scale = dh ** -0.5
    F = jnp.cumsum(log_f, axis=1)
    Fk = jnp.transpose(F, (0, 2, 1))
    kpos = jnp.arange(seq)
    qb = jnp.moveaxis(q.reshape(bsz, nb, Q_BLOCK, heads, dh), 1, 0)
    Fb = jnp.moveaxis(F.reshape(bsz, nb, Q_BLOCK, heads), 1, 0)
    posb = kpos.reshape(nb, Q_BLOCK)

    def block(args):
        qi, Fi, pi = args
        s = jnp.einsum('bqhd,bkhd->bhqk', qi, k).astype(jnp.float32) * scale
        s = s + jnp.transpose(Fi, (0, 2, 1))[..., None] - Fk[:, :, None, :]
        s = jnp.where(kpos[None, None, None, :] <= pi[None, None, :, None], s, -jnp.inf)
        p = jax.nn.softmax(s, axis=-1).astype(v.dtype)
        return jnp.einsum('bhqk,bkhd->bqhd', p, v)

    out = lax.map(block, (qb, Fb, posb))
    return jnp.moveaxis(out, 0, 1).reshape(bsz, seq, heads, dh)


def hybrid_mixer(h, w_in, fox_b_f, conf_dw, conf_dw_b, conf_ln_g, conf_ln_b, sc_dw, ret_gn_g, w_o):
    bsz, seq, _ = h.shape
    proj = h @ w_in
    (ret_q, ret_k, ret_v, ret_g, conf_a, conf_b, sc_b, sc_c, sc_h,
     fox_q, fox_k, fox_v, fox_f) = jnp.split(proj, [int(i) for i in np.cumsum(SPLIT_SIZES)[:-1]], axis=-1)
    pos = jnp.arange(seq)
    rq = rope(ret_q.reshape(bsz, seq, RET_HEADS, RET_DK), pos)
    rk = rope(ret_k.reshape(bsz, seq, RET_HEADS, RET_DK), pos) * (RET_DK ** -0.5)
    rv = ret_v.reshape(bsz, seq, RET_HEADS, RET_DV)
    ro = layer_norm(chunk_retention(rq, rk, rv), ret_gn_g.reshape(RET_HEADS, RET_DV))
    ret_out = jax.nn.silu(ret_g) * ro.reshape(bsz, seq, W_GROUP)
    u = conf_a * jax.nn.sigmoid(conf_b)
    u = causal_depthwise_conv(u, conf_dw) + conf_dw_b
    conf_out = jax.nn.silu(layer_norm(u, conf_ln_g, conf_ln_b))
    sc_out = sc_b * causal_depthwise_conv(sc_c * sc_h, sc_dw)
    log_f = jax.nn.log_sigmoid((fox_f + fox_b_f).astype(jnp.float32))
    fo = forgetting_attention(fox_q.reshape(bsz, seq, FOX_HEADS, FOX_DH),
                              fox_k.reshape(bsz, seq, FOX_HEADS, FOX_DH),
                              fox_v.reshape(bsz, seq, FOX_HEADS, FOX_DH), log_f)
    fox_out = fo.reshape(bsz, seq, W_GROUP)
    mixed = jnp.concatenate([ret_out, conf_out, sc_out, fox_out], axis=-1)
    return mixed @ w_o


def moe_ffn(x, router_w, router_b, w1, b1, w2, b2):
    bsz, seq, d = x.shape
    n_tok = bsz * seq
    nk = n_tok * TOP_K
    xf = x.reshape(n_tok, d)
    logits = (xf @ router_w + router_b).astype(jnp.float32)
    top_v, top_i = lax.top_k(logits, TOP_K)
    gates = jax.nn.softmax(top_v, axis=-1)
    flat_e = top_i.reshape(-1).astype(jnp.int32)
    flat_tok = jnp.arange(nk, dtype=jnp.int32) // TOP_K
    order = jnp.argsort(flat_e)
    se, stok, sg = flat_e[order], flat_tok[order], gates.reshape(-1)[order]
    counts = jnp.bincount(flat_e, length=N_EXPERTS).astype(jnp.int32)
    padded = (counts + MOE_BLOCK - 1) // MOE_BLOCK * MOE_BLOCK
    start_sorted = jnp.cumsum(counts) - counts
    cum_pad = jnp.cumsum(padded)
    start_pad = cum_pad - padded
    dest = start_pad[se] + jnp.arange(nk, dtype=jnp.int32) - start_sorted[se]
    n_blocks = -(-nk // MOE_BLOCK) + N_EXPERTS
    n_rows = n_blocks * MOE_BLOCK
    row_tok = jnp.full((n_rows,), n_tok, jnp.int32).at[dest].set(stok)
    row_gate = jnp.zeros((n_rows,), jnp.float32).at[dest].set(sg)
    block_e = jnp.minimum(jnp.searchsorted(cum_pad, jnp.arange(n_blocks, dtype=jnp.int32) * MOE_BLOCK,
                                           side='right'), N_EXPERTS - 1).astype(jnp.int32)
    x_pad = jnp.concatenate([xf, jnp.zeros((1, d), xf.dtype)], axis=0)
    xb = x_pad[row_tok].reshape(n_blocks, MOE_BLOCK, d)

    def expert_block(args):
        xblk, e = args
        hdn = xblk @ w1[e] + b1[e]
        glu, lin = hdn[:, :D_FF_EXPERT], hdn[:, D_FF_EXPERT:]
        glu = jnp.minimum(glu, SWIGLU_LIMIT)
        lin = jnp.clip(lin, -SWIGLU_LIMIT, SWIGLU_LIMIT)
        act = glu * jax.nn.sigmoid(SWIGLU_ALPHA * glu) * (lin + 1.0)
        return act @ w2[e] + b2[e]

    yb = lax.map(expert_block, (xb, block_e))
    y = yb.reshape(n_rows, d) * row_gate[:, None].astype(yb.dtype)
    out = jnp.zeros((n_tok + 1, d), y.dtype).at[row_tok].add(y)[:n_tok]
    return out.reshape(bsz, seq, d)


def setup_inputs(seed: int = 0) -> dict:
    key = jax.random.key(seed)
    ks = jax.random.split(key, 20)
    L = DEPTH

    def nrm(k, shape, scale):
        return scale * jax.random.normal(k, shape, jnp.float32)

    return {
        'x': nrm(ks[0], (BATCH, SEQ, D_MODEL), 1.0),
        'w_in': nrm(ks[1], (L, D_MODEL, D_IN), D_MODEL ** -0.5),
        'fox_b_f': FOX_FORGET_BIAS + nrm(ks[2], (L, FOX_HEADS), 0.1),
        'conf_dw': nrm(ks[3], (L, CONF_KERNEL, CONF_CH), CONF_KERNEL ** -0.5),
        'conf_dw_b': nrm(ks[4], (L, CONF_CH), 0.02),
        'conf_ln_g': 1.0 + nrm(ks[5], (L, CONF_CH), 0.05),
        'conf_ln_b': nrm(ks[6], (L, CONF_CH), 0.02),
        'sc_dw': nrm(ks[7], (L, SC_KERNEL, SC_CH), SC_KERNEL ** -0.5),
        'ret_gn_g': 1.0 + nrm(ks[8], (L, W_GROUP), 0.05),
        'w_o': nrm(ks[9], (L, D_MIX, D_MODEL), DEEPNORM_BETA * D_MIX ** -0.5),
        'ln1_g': 1.0 + nrm(ks[10], (L, D_MODEL), 0.05),
        'ln1_b': nrm(ks[11], (L, D_MODEL), 0.02),
        'router_w': nrm(ks[12], (L, D_MODEL, N_EXPERTS), D_MODEL ** -0.5),
        'router_b': nrm(ks[13], (L, N_EXPERTS), 0.01),
        'w1': nrm(ks[14], (L, N_EXPERTS, D_MODEL, 2 * D_FF_EXPERT), D_MODEL ** -0.5),
        'b1': nrm(ks[15], (L, N_EXPERTS, 2 * D_FF_EXPERT), 0.02),
        'w2': nrm(ks[16], (L, N_EXPERTS, D_FF_EXPERT, D_MODEL), DEEPNORM_BETA * D_FF_EXPERT ** -0.5),
        'b2': nrm(ks[17], (L, N_EXPERTS, D_MODEL), 0.02),
        'ln2_g': 1.0 + nrm(ks[18], (L, D_MODEL), 0.05),
        'ln2_b': nrm(ks[19], (L, D_MODEL), 0.02),
    }


def reference(x, w_in, fox_b_f, conf_dw, conf_dw_b, conf_ln_g, conf_ln_b, sc_dw, ret_gn_g, w_o,
              ln1_g, ln1_b, router_w, router_b, w1, b1, w2, b2, ln2_g, ln2_b):
    for l in range(DEPTH):
        mix = hybrid_mixer(x, w_in[l], fox_b_f[l], conf_dw[l], conf_dw_b[l], conf_ln_g[l], conf_ln_b[l],
                           sc_dw[l], ret_gn_g[l], w_o[l])
        x = layer_norm(DEEPNORM_ALPHA * x + mix, ln1_g[l], ln1_b[l])
        ffn = moe_ffn(x, router_w[l], router_b[l], w1[l], b1[l], w2[l], b2[l])
        x = layer_norm(DEEPNORM_ALPHA * x + ffn, ln2_g[l], ln2_b[l])
    return x
```

```python
import math
import os
KN = int(os.environ.get('KN', '99'))
from contextlib import ExitStack
import numpy as np
import concourse.bass as bass
import concourse.mybir as mybir
from concourse.bass_utils import run_bass_kernel_spmd

F32 = mybir.dt.float32
BF16 = mybir.dt.bfloat16
ALU = mybir.AluOpType
AF = mybir.ActivationFunctionType
AX = mybir.AxisListType

D = 1024
CHUNK = 64
ALPHA = 8 ** 0.25
EPS = 1e-5
NCV = 110
WA = 2820 + 256


class Sched:
    def __init__(s, nc, es):
        s.nc = nc
        s.eng = {'pe': nc.tensor, 'act': nc.scalar, 'dve': nc.vector, 'pool': nc.gpsimd, 'sp': nc.sync}
        s.sem = {k: es.enter_context(nc.semaphore('s_' + k)) for k in ('pe', 'act', 'dve', 'pool')}
        s.cnt = {k: 0 for k in s.sem}
        s.seen = {k: {} for k in s.eng}
        s.dq = {}
        for q in ('sp', 'pool', 'act'):
            s.dq[q] = dict(sems=[es.enter_context(nc.semaphore(f'd_{q}{i}')) for i in range(8)], n=0)
        s.res = {}

    def _need(s, en, deps):
        best = {}
        for (k, h, v) in deps:
            if v > best.get(k, (None, 0))[1]:
                best[k] = (h, v)
        for k, (h, v) in best.items():
            if s.seen[en].get(k, 0) < v:
                s.eng[en].wait_ge(h, v)
                s.seen[en][k] = v

    def _collect(s, en, r, w):
        deps = []
        for k in r:
            st = s.res.get(k)
            if st and st[0]:
                deps.append(st[0])
        for k in w:
            st = s.res.get(k)
            if st:
                if st[0]:
                    deps.append(st[0])
                deps.extend(st[1].values())
        if en == 'pe':
            deps = [d for d in deps if d[0] != 'pe']
        return deps

    def _record(s, dep, r, w):
        for k in r:
            st = s.res.setdefault(k, [None, {}])
            o = st[1].get(dep[0])
            if o is None or o[2] < dep[2]:
                st[1][dep[0]] = dep
        for k in w:
            s.res[k] = [dep, {}]

    def op(s, en, fn, r=(), w=(), inc=True):
        assert inc or en == 'pe'
        s._need(en, s._collect(en, r, w))
        ins = fn(s.eng[en])
        if inc:
            s.cnt[en] += 1
            ins.then_inc(s.sem[en], 1)
            dep = (en, s.sem[en], s.cnt[en])
        else:
            dep = (en, s.sem[en], s.cnt[en] + 1)
        s._record(dep, r, w)

    def dma(s, q, out, in_, r=(), w=(), **kw):
        Q = s.dq[q]
        i = Q['n']
        Q['n'] += 1
        K = len(Q['sems'])
        h = Q['sems'][i % K]
        key = f'd_{q}{i % K}'
        deps = s._collect(q, r, w)
        if i >= K:
            deps.append((key, h, 16 * (i // K)))
        s._need(q, deps)
        s.eng[q].dma_start(out=out, in_=in_, **kw).then_inc(h, 16)
        s._record((key, h, 16 * (i // K + 1)), r, w)

    def alldeps(s):
        deps = [(k, s.sem[k], s.cnt[k]) for k in s.sem if s.cnt[k] > 0]
        for q, Q in s.dq.items():
            K = len(Q['sems'])
            for j in range(min(K, Q['n'])):
                tot = (Q['n'] - 1 - j) // K + 1
                deps.append((f'd_{q}{j}', Q['sems'][j], 16 * tot))
        return deps

    def barrier(s):
        deps = s.alldeps()
        for en in s.eng:
            s._need(en, [d for d in deps if not (en == 'pe' and d[0] == 'pe')])
        s.res = {}


class _Stop(Exception):
    pass


def build(S, DEPTH, NE, SBK, dbg=None, upto=99):
    NB = S // 512
    NT = S // 128
    NCH = S // 64
    NSB = S // SBK
    nc = bass.Bass("TRN2", target_bir_lowering=False)
    dt = nc.dram_tensor
    xT_in = dt("xT", [D, S], F32, kind="ExternalInput").ap()
    w_in_d = dt("w_in", [DEPTH, D, WA], F32, kind="ExternalInput").ap()
    w_o_d = dt("w_o", [DEPTH, D, D], F32, kind="ExternalInput").ap()
    cvec_d = dt("cvec", [DEPTH, 128, NCV], F32, kind="ExternalInput").ap()
    foxb_d = dt("foxb", [DEPTH, 1, 4], F32, kind="ExternalInput").ap()
    rw_d = dt("rw", [DEPTH, D, NE], F32, kind="ExternalInput").ap()
    rb_d = dt("rb", [DEPTH, 1, NE], F32, kind="ExternalInput").ap()
    w1_d = dt("w1", [DEPTH, NE, D, 2048], F32, kind="ExternalInput").ap()
    b1_d = dt("b1T", [DEPTH, 128, NE * 16], F32, kind="ExternalInput").ap()
    w2_d = dt("w2", [DEPTH, NE, D, D], F32, kind="ExternalInput").ap()
    b2_d = dt("b2", [DEPTH, NE, D], F32, kind="ExternalInput").ap()
    cos_d = dt("ropec", [128, S], F32, kind="ExternalInput").ap()
    sin_d = dt("ropes", [128, S], F32, kind="ExternalInput").ap()
    cst_d = dt("cst", [128, 4 * 128 + 512 + 4 + 128], F32, kind="ExternalInput").ap()
    msk_d = dt("msk", [128, 4 * 512], F32, kind="ExternalInput").ap()
    outT = dt("outT", [D, S], F32, kind="ExternalOutput").ap()
    okind = {} if dbg is None else {"kind": "ExternalOutput"}
    XT = dt("XT", [D, S], F32).ap()
    XB = dt("XB", [D, S], BF16).ap()
    X1T = dt("X1T", [D, S], F32).ap()
    X1B = dt("X1B", [D, S], BF16).ap()
    MIXT = dt("MIXT", [D, S], BF16, **okind).ap()
    RQ = dt("RQ", [128, S], BF16).ap()
    RK = dt("RK", [128, S], BF16).ap()
    RV = dt("RV", [S, 256], BF16).ap()
    GATE = dt("GATE", [256, S], BF16).ap()
    FQ = dt("FQ", [256, S], BF16).ap()
    FK = dt("FK", [256, S], BF16).ap()
    FV = dt("FV", [S, 256], BF16).ap()
    FF = dt("FF", [4, S], F32).ap()
    FRD = dt("FRD", [7, S], BF16).ap()
    GT = dt("GT", [NE, S], F32, **okind).ap()

    def chunked(ap):
        return ap.rearrange("(kc p) t -> p kc t", p=128)

    with ExitStack() as es:
        sc = Sched(nc, es)
        uid = [0]

        def sb(name, shape, dtp, st=es):
            uid[0] += 1
            return st.enter_context(nc.sbuf_tensor(f"{name}_s{uid[0]}", shape, dtp))
        PS = [es.enter_context(nc.psum_tensor(f"ps{i}", [128, 512], F32)) for i in range(7)]
        PSB = es.enter_context(nc.psum_tensor("psb", [128, 1024], BF16))
        pctr = [0]

        def bank():
            pctr[0] = (pctr[0] + 1) % 7
            return PS[pctr[0]], f"ps{pctr[0]}"

        cst = sb("cst", [128, 4 * 128 + 512 + 4 + 128], F32)
        sc.dma('sp', cst[:], cst_d[:, :], w=['cst'])
        identb = sb("identb", [128, 128], BF16)
        sc.op('dve', lambda e: e.tensor_copy(identb[:], cst[:, 1028:1156]), r=['cst'], w=['identb'])
        ident = cst[:, 1028:1156]
        onesD = sb("onesD", [128, 128], F32)
        sc.op('pool', lambda e: e.memset(onesD[:], 1.0 / D), w=['onesD'])
        ones256 = sb("ones256", [128, 128], F32)
        sc.op('pool', lambda e: e.memset(ones256[:], 1.0 / 256), w=['ones256'])
        ones64 = sb("ones64", [64, 64], F32)
        sc.op('pool', lambda e: e.memset(ones64[:], 1.0 / 64), w=['ones64'])
        onesb = sb("onesb", [128, 64], BF16)
        sc.op('pool', lambda e: e.memset(onesb[:], 1.0), w=['onesb'])
        cv = sb("cv", [128, NCV], F32)

        def mm(out, lhsT, rhs, start, stop, r, w, inc=None):
            sc.op('pe', lambda e: e.matmul(out, lhsT, rhs, start=start, stop=stop), r=r, w=w,
                  inc=(stop if inc is None else inc))

        def ln_block(z, zk, gcol, bcol, st):
            pm, pmk = bank()
            for dc in range(8):
                mm(pm[:, :], onesD[:], z[:, dc, :], dc == 0, dc == 7, r=['onesD', zk], w=[pmk])
            pe2, pe2k = bank()
            for dc in range(8):
                sq = st['sq'][dc % 2]
                sqk = st['sqk'] + str(dc % 2)
                sc.op('act', lambda e, dc=dc: e.activation(sq[:], z[:, dc, :], AF.Square), r=[zk], w=[sqk])
                mm(pe2[:, :], onesD[:], sq[:], dc == 0, dc == 7, r=['onesD', sqk], w=[pe2k], inc=True)
            mean, rstd, tmp = st['mean'], st['rstd'], st['tmp']
            mk_, rk_, tk_ = st['sqk'] + 'mean', st['sqk'] + 'rstd', st['sqk'] + 'tmp'
            sc.op('act', lambda e: e.copy(mean[:], pm[:, :]), r=[pmk], w=[mk_])
            sc.op('dve', lambda e: e.tensor_tensor(rstd[:], mean[:], mean[:], ALU.mult), r=[mk_], w=[rk_])
            sc.op('dve', lambda e: e.tensor_tensor(rstd[:], pe2[:, :], rstd[:], ALU.subtract), r=[pe2k, rk_], w=[rk_])
            sc.op('dve', lambda e: e.tensor_scalar(rstd[:], rstd[:], 0.0, EPS, ALU.max, ALU.add), r=[rk_], w=[rk_])
            sc.op('act', lambda e: e.activation(rstd[:], rstd[:], AF.Ln), r=[rk_], w=[rk_])
            sc.op('act', lambda e: e.activation(rstd[:], rstd[:], AF.Exp, scale=-0.5), r=[rk_], w=[rk_])
            for dc in range(8):
                sc.op('dve', lambda e, dc=dc: e.tensor_tensor(tmp[:], z[:, dc, :], mean[:], ALU.subtract), r=[zk, mk_], w=[tk_])
                sc.op('dve', lambda e, dc=dc: e.tensor_tensor(tmp[:], tmp[:], rstd[:], ALU.mult), r=[tk_, rk_], w=[tk_])
                sc.op('act', lambda e, dc=dc: e.activation(z[:, dc, :], tmp[:], AF.Identity,
                                                          bias=cv[:, bcol + dc:bcol + dc + 1], scale=cv[:, gcol + dc:gcol + dc + 1]),
                      r=[tk_, 'cv'], w=[zk])

        with ExitStack() as st0:
            xf = [sb(f"xf{i}", [128, 8, 512], F32, st0) for i in range(2)]
            xh = [sb(f"xh{i}", [128, 8, 512], BF16, st0) for i in range(2)]
            for tb in range(NB):
                p = tb % 2
                sl = slice(tb * 512, tb * 512 + 512)
                sc.dma('sp', xf[p][:], chunked(xT_in)[:, :, sl], w=[f'xf{p}'])
                sc.op('dve', lambda e, p=p: e.tensor_copy(xh[p][:], xf[p][:]), r=[f'xf{p}'], w=[f'xh{p}'])
                sc.dma('sp', chunked(XT)[:, :, sl], xf[p][:], r=[f'xf{p}'], w=[('XT', tb)])
                sc.dma('sp', chunked(XB)[:, :, sl], xh[p][:], r=[f'xh{p}'], w=[('XB', tb)])
            sc.barrier()

        stop = [False]
        try:
          for l in range(DEPTH):
            last = l == DEPTH - 1
            sc.dma('sp', cv[:], cvec_d[l, :, :], w=['cv'])
            with ExitStack() as st1:
              for _once in ([0] if not stop[0] else []):
                win = sb("win", [128, 8, WA], BF16, st1)
                for kc in range(8):
                    sc.dma('pool', win[:, kc, :], w_in_d[l, kc * 128:(kc + 1) * 128, :], w=[('win', kc)])
                WK = [('win', kc) for kc in range(8)]
                xbt = [sb(f"xbt{i}", [128, 8, 544], BF16, st1) for i in range(2)]
                cs_t = sb("cs_t", [128, 512], F32, st1)
                sn_t = sb("sn_t", [128, 512], F32, st1)
                t1 = sb("t1", [128, 512], F32, st1)
                t2 = sb("t2", [128, 512], F32, st1)
                ob = [sb(f"ob{i}", [128, 512], BF16, st1) for i in range(4)]
                obc = [0]
                u_t = [sb(f"u{i}", [128, 544], F32, st1) for i in range(2)]
                sg_t = sb("sg_t", [128, 544], F32, st1)
                ca_t = [sb(f"ca{i}", [128, 512], F32, st1) for i in range(2)]
                sq1 = sb("sq1", [128, 512], F32, st1)
                mean1 = sb("mean1", [128, 512], F32, st1)
                rstd1 = sb("rstd1", [128, 512], F32, st1)
                ff_t = sb("ff_t", [4, 512], F32, st1)
                vt = [sb(f"vt{i}", [128, 256], BF16, st1) for i in range(2)]
                vtc = [0]

                def nob():
                    obc[0] = (obc[0] + 1) % 4
                    return ob[obc[0]], f"ob{obc[0]}"

                for tb in range(NB):
                    t0 = tb * 512
                    p = tb % 2
                    xk = f'xbt{p}'
                    x_ = xbt[p]
                    if tb == 0:
                        sc.op('pool', lambda e: e.memset(x_[:, :, 0:32], 0.0), w=[xk])
                        sc.dma('sp', x_[:, :, 32:544], chunked(XB)[:, :, 0:512], r=[('XB', 0)], w=[xk])
                    else:
                        sc.dma('sp', x_[:, :, :], chunked(XB)[:, :, t0 - 32:t0 + 512], r=[('XB', tb - 1), ('XB', tb)], w=[xk])
                    sc.dma('sp', cs_t[:], cos_d[:, t0:t0 + 512], w=['cs_t'])
                    sc.dma('sp', sn_t[:], sin_d[:, t0:t0 + 512], w=['sn_t'])

                    def fm(c0, ncols=128, halo=False):
                        pm, pmk = bank()
                        for kc in range(8):
                            mm(pm[0:ncols, :], win[:, kc, c0:c0 + ncols], x_[:, kc, 32:544], kc == 0, kc == 7, r=[WK[kc], xk], w=[pmk])
                        if not halo:
                            return pm, pmk
                        ph, phk = bank()
                        for kc in range(8):
                            mm(ph[0:ncols, 0:32], win[:, kc, c0:c0 + ncols], x_[:, kc, 0:32], kc == 0, kc == 7, r=[WK[kc], xk], w=[phk])
                        return pm, pmk, ph, phk

                    for (c0, cp, dst, dk) in ((0, 2820, RQ, 'RQ'), (128, 2948, RK, 'RK')):
                        pa, pak = fm(c0)
                        pp, ppk = fm(cp)
                        sc.op('dve', lambda e: e.tensor_tensor(t1[:], pa[:, :], cs_t[:], ALU.mult), r=[pak, 'cs_t'], w=['t1'])
                        sc.op('dve', lambda e: e.tensor_tensor(t2[:], pp[:, :], sn_t[:], ALU.mult), r=[ppk, 'sn_t'], w=['t2'])
                        o, ok = nob()
                        sc.op('dve', lambda e: e.tensor_tensor(o[:], t1[:], t2[:], ALU.add), r=['t1', 't2'], w=[ok])
                        sc.dma('sp', dst[:, t0:t0 + 512], o[:], r=[ok], w=[(dk, tb)])
                    for c in range(2):
                        pa, pak = fm(512 + 128 * c)
                        o, ok = nob()
                        sc.op('act', lambda e: e.activation(o[:], pa[:, :], AF.Silu), r=[pak], w=[ok])
                        sc.dma('sp', GATE[128 * c:128 * c + 128, t0:t0 + 512], o[:], r=[ok], w=[('GATE', tb)])
                    for c in range(2):
                        pa, pak, pah, pahk = fm(768 + 128 * c, halo=True)
                        pb_, pbk, pbh, pbhk = fm(1024 + 128 * c, halo=True)
                        u = u_t[c]
                        uk = f'u{c}'
                        sc.op('act', lambda e: e.activation(sg_t[:, 32:544], pb_[:, :], AF.Sigmoid), r=[pbk], w=['sg_t'])
                        sc.op('act', lambda e: e.activation(sg_t[:, 0:32], pbh[:, 0:32], AF.Sigmoid), r=[pbhk, 'sg_t'], w=['sg_t'])
                        sc.op('dve', lambda e: e.tensor_tensor(u[:, 32:544], pa[:, :], sg_t[:, 32:544], ALU.mult), r=[pak, 'sg_t'], w=[uk])
                        sc.op('dve', lambda e: e.tensor_tensor(u[:, 0:32], pah[:, 0:32], sg_t[:, 0:32], ALU.mult), r=[pahk, 'sg_t', uk], w=[uk])
                        ce = 'dve'
                        ca = ca_t[c]
                        cak = f'ca{c}'
                        wc0 = 31 * c
                        sc.op(ce, lambda e: e.tensor_scalar(ca[:], u[:, 2:514], cv[:, wc0:wc0 + 1], cv[:, 68 + c:69 + c], ALU.mult, ALU.add),
                              r=[uk, 'cv'], w=[cak])
                        for j in range(1, 31):
                            sc.op(ce, lambda e, j=j: e.scalar_tensor_tensor(ca[:], u[:, 2 + j:514 + j], cv[:, wc0 + j:wc0 + j + 1], ca[:], ALU.mult, ALU.add),
                                  r=[uk, 'cv', cak], w=[cak])
                    pm, pmk = bank()
                    pe2, pe2k = bank()
                    for c in range(2):
                        mm(pm[:, :], ones256[:], ca_t[c][:], c == 0, c == 1, r=['ones256', f'ca{c}'], w=[pmk])
                    for c in range(2):
                        sc.op('act', lambda e, c=c: e.activation(sq1[:], ca_t[c][:], AF.Square), r=[f'ca{c}'], w=['sq1'])
                        mm(pe2[:, :], ones256[:], sq1[:], c == 0, c == 1, r=['ones256', 'sq1'], w=[pe2k], inc=True)
                    sc.op('act', lambda e: e.copy(mean1[:], pm[:, :]), r=[pmk], w=['mean1'])
                    sc.op('dve', lambda e: e.tensor_tensor(rstd1[:], mean1[:], mean1[:], ALU.mult), r=['mean1'], w=['rstd1'])
                    sc.op('dve', lambda e: e.tensor_tensor(rstd1[:], pe2[:, :], rstd1[:], ALU.subtract), r=[pe2k, 'rstd1'], w=['rstd1'])
                    sc.op('dve', lambda e: e.tensor_scalar(rstd1[:], rstd1[:], 0.0, EPS, ALU.max, ALU.add), r=['rstd1'], w=['rstd1'])
                    sc.op('act', lambda e: e.activation(rstd1[:], rstd1[:], AF.Ln), r=['rstd1'], w=['rstd1'])
                    sc.op('act', lambda e: e.activation(rstd1[:], rstd1[:], AF.Exp, scale=-0.5), r=['rstd1'], w=['rstd1'])
                    for c in range(2):
                        sc.op('dve', lambda e, c=c: e.tensor_tensor(t1[:], ca_t[c][:], mean1[:], ALU.subtract), r=[f'ca{c}', 'mean1'], w=['t1'])
                        sc.op('dve', lambda e: e.tensor_tensor(t1[:], t1[:], rstd1[:], ALU.mult), r=['t1', 'rstd1'], w=['t1'])
                        o, ok = nob()
                        sc.op('act', lambda e, c=c: e.activation(o[:], t1[:], AF.Silu, bias=cv[:, 72 + c:73 + c], scale=cv[:, 70 + c:71 + c]),
                              r=['t1', 'cv'], w=[ok])
                        sc.dma('sp', MIXT[256 + 128 * c:384 + 128 * c, t0:t0 + 512], o[:], r=[ok], w=[('MIXT', tb)])
                    for c in range(2):
                        pc, pck, pch, pchk = fm(1536 + 128 * c, halo=True)
                        ph_, phk_, phh, phhk = fm(1792 + 128 * c, halo=True)
                        u = u_t[c]
                        uk = f'u{c}'
                        sc.op('act', lambda e: e.copy(sg_t[:, 32:544], pc[:, :]), r=[pck], w=['sg_t'])
                        sc.op('act', lambda e: e.copy(sg_t[:, 0:32], pch[:, 0:32]), r=[pchk, 'sg_t'], w=['sg_t'])
                        sc.op('dve', lambda e: e.tensor_tensor(u[:, 32:544], ph_[:, :], sg_t[:, 32:544], ALU.mult), r=[phk_, 'sg_t'], w=[uk])
                        sc.op('dve', lambda e: e.tensor_tensor(u[:, 0:32], phh[:, 0:32], sg_t[:, 0:32], ALU.mult), r=[phhk, 'sg_t', uk], w=[uk])
                        wc0 = 62 + 3 * c
                        sc.op('dve', lambda e: e.tensor_scalar(t1[:], u[:, 30:542], cv[:, wc0:wc0 + 1], None, ALU.mult), r=[uk, 'cv'], w=['t1'])
                        for j in (1, 2):
                            sc.op('dve', lambda e, j=j: e.scalar_tensor_tensor(t1[:], u[:, 30 + j:542 + j], cv[:, wc0 + j:wc0 + j + 1], t1[:], ALU.mult, ALU.add),
                                  r=[uk, 'cv', 't1'], w=['t1'])
                        pbb, pbbk = fm(1280 + 128 * c)
                        o, ok = nob()
                        sc.op('dve', lambda e: e.tensor_tensor(o[:], pbb[:, :], t1[:], ALU.mult), r=[pbbk, 't1'], w=[ok])
                        sc.dma('sp', MIXT[512 + 128 * c:640 + 128 * c, t0:t0 + 512], o[:], r=[ok], w=[('MIXT', tb)])
                    for c in range(2):
                        pa, pak = fm(2048 + 128 * c)
                        o, ok = nob()
                        sc.op('act', lambda e: e.mul(o[:], pa[:, :], 0.125), r=[pak], w=[ok])
                        sc.dma('sp', FQ[128 * c:128 * c + 128, t0:t0 + 512], o[:], r=[ok], w=[('FQ', tb)])
                        pa, pak = fm(2304 + 128 * c)
                        o, ok = nob()
                        sc.op('act', lambda e: e.copy(o[:], pa[:, :]), r=[pak], w=[ok])
                        sc.dma('sp', FK[128 * c:128 * c + 128, t0:t0 + 512], o[:], r=[ok], w=[('FK', tb)])
                    pa, pak = fm(2816, ncols=4)
                    sc.op('act', lambda e: e.copy(ff_t[:], pa[0:4, :]), r=[pak], w=['ff_t'])
                    sc.dma('sp', FF[:, t0:t0 + 512], ff_t[:], r=['ff_t'], w=[('FF', tb)])
                    for sub in range(4):
                        for (c0, dst, dk) in ((256, RV, 'RV'), (2560, FV, 'FV')):
                            pm, pmk = bank()
                            for kc in range(8):
                                mm(pm[:, 0:256], x_[:, kc, 32 + 128 * sub:160 + 128 * sub], win[:, kc, c0:c0 + 256], kc == 0, kc == 7, r=[WK[kc], xk], w=[pmk])
                            vtc[0] ^= 1
                            v = vt[vtc[0]]
                            vk = f'vt{vtc[0]}'
                            sc.op('act', lambda e: e.copy(v[:], pm[:, 0:256]), r=[pmk], w=[vk])
                            sc.dma('sp', dst[t0 + 128 * sub:t0 + 128 * sub + 128, :], v[:], r=[vk], w=[(dk, tb)])
                sc.barrier()
                if upto == 1:
                    stop[0] = True

            with ExitStack() as st2:
              for _once in ([0] if not stop[0] else []):
                rq_t = sb("rq_t", [32, S], BF16, st2)
                rk_t = sb("rk_t", [32, S], BF16, st2)
                qd_t = sb("qd_t", [32, S], BF16, st2)
                v_t = sb("v_t", [128, NT, 64], BF16, st2)
                v64 = sb("v64", [64, NCH, 64], BF16, st2)
                qdec = sb("qdec", [32, 512], F32, st2)
                stt = sb("stt", [32, 64], F32, st2)
                prevb = sb("prevb", [32, NCH, 64], BF16, st2)
                kd = [sb(f"kd{i}", [128, 32], BF16, st2) for i in range(2)]
                sd = [sb(f"sd{i}", [128, 128], BF16, st2) for i in range(2)]
                o_sb = sb("o_sb", [64, 512], F32, st2)
                sq2 = sb("sq2", [64, 512], F32, st2)
                mean2 = sb("mean2", [64, 512], F32, st2)
                rstd2 = sb("rstd2", [64, 512], F32, st2)
                g_t = sb("g_t", [64, 512], BF16, st2)
                ro = [sb(f"ro{i}", [64, 512], BF16, st2) for i in range(2)]
                for h in range(4):
                    gam = 1.0 - 2.0 ** (-5.0 - h)
                    cd = gam ** CHUNK
                    sc.dma('sp', rq_t[:], RQ[32 * h:32 * h + 32, :], w=['rq_t'])
                    sc.dma('sp', rk_t[:], RK[32 * h:32 * h + 32, :], w=['rk_t'])
                    sc.dma('sp', v_t[:], RV[:, 64 * h:64 * h + 64].rearrange("(j p) e -> p j e", p=128), w=['v_t'])
                    sc.dma('sp', qdec[:], cst_d[32 * h:32 * h + 32, 512:1024], w=['qdec'])
                    for tb in range(NB):
                        sc.op('dve', lambda e, tb=tb: e.tensor_tensor(qd_t[:, tb * 512:tb * 512 + 512], rq_t[:, tb * 512:tb * 512 + 512], qdec[:], ALU.mult),
                              r=['rq_t', 'qdec'], w=['qd_t'])
                    sc.op('dve', lambda e: e.memset(stt[:], 0.0), w=['stt'])
                    sc.dma('sp', v64[:], RV[:, 64 * h:64 * h + 64].rearrange("(c p) e -> p c e", p=64), w=['v64'])
                    for c in range(NCH):
                        sc.op('pe', lambda e, c=c: e.transpose(PSB[0:64, 0:32], rk_t[:, 64 * c:64 * c + 64], identb[0:32, 0:32]),
                              r=['rk_t', 'identb'], w=['psb'])
                        k_ = kd[c % 2]
                        kk = f'kd{c % 2}'
                        sc.op('dve', lambda e: e.tensor_scalar(k_[0:64, :], PSB[0:64, 0:32], cst[0:64, 1024 + h:1025 + h], None, ALU.mult), r=['psb', 'cst'], w=[kk])
                        pkv, pkvk = bank()
                        mm(pkv[0:32, 0:64], k_[0:64, :], v64[:, c, :], True, True, r=[kk, 'v64'], w=[pkvk])
                        sc.op('dve', lambda e, c=c: e.tensor_copy(prevb[:, c, :], stt[:]), r=['stt'], w=['prevb'])
                        sc.op('dve', lambda e: e.scalar_tensor_tensor(stt[:], stt[:], cd, pkv[0:32, 0:64], ALU.mult, ALU.add),
                              r=['stt', pkvk], w=['stt'])
                    for tb in range(NB if KN >= 5 else 0):
                        t0 = tb * 512
                        po, pok = bank()
                        for jj in range(4):
                            j = tb * 4 + jj
                            ps_, psk = bank()
                            mm(ps_[:, 0:128], rk_t[:, 128 * j:128 * j + 128], rq_t[:, 128 * j:128 * j + 128], True, True, r=['rk_t', 'rq_t'], w=[psk])
                            s_ = sd[j % 2]
                            sk = f'sd{j % 2}'
                            sc.op('dve', lambda e: e.tensor_tensor(s_[:], ps_[:, 0:128], cst[:, 128 * h:128 * h + 128], ALU.mult), r=[psk, 'cst'], w=[sk])
                            mm(po[0:64, 128 * jj:128 * jj + 128], v_t[:, j, :], s_[:], True, False, r=['v_t', sk], w=[pok], inc=False)
                            for hf in range(2):
                                c0 = 128 * jj + 64 * hf
                                mm(po[0:64, c0:c0 + 64], prevb[:, 2 * j + hf, :], qd_t[:, 128 * j + 64 * hf:128 * j + 64 * hf + 64], False, hf == 1,
                                   r=['prevb', 'qd_t'], w=[pok], inc=(hf == 1))
                        sc.op('act', lambda e: e.copy(o_sb[:], po[0:64, :]), r=[pok], w=['o_sb'])
                        sc.op('act', lambda e: e.activation(sq2[:], po[0:64, :], AF.Square), r=[pok], w=['sq2'])
                        pm, pmk = bank()
                        mm(pm[0:64, :], ones64[:], o_sb[:], True, True, r=['ones64', 'o_sb'], w=[pmk])
                        pe2, pe2k = bank()
                        mm(pe2[0:64, :], ones64[:], sq2[:], True, True, r=['ones64', 'sq2'], w=[pe2k])
                        sc.op('act', lambda e: e.copy(mean2[:], pm[0:64, :]), r=[pmk], w=['mean2'])
                        sc.op('dve', lambda e: e.tensor_tensor(rstd2[:], mean2[:], mean2[:], ALU.mult), r=['mean2'], w=['rstd2'])
                        sc.op('dve', lambda e: e.tensor_tensor(rstd2[:], pe2[0:64, :], rstd2[:], ALU.subtract), r=[pe2k, 'rstd2'], w=['rstd2'])
                        sc.op('dve', lambda e: e.tensor_scalar(rstd2[:], rstd2[:], 0.0, EPS, ALU.max, ALU.add), r=['rstd2'], w=['rstd2'])
                        sc.op('act', lambda e: e.activation(rstd2[:], rstd2[:], AF.Ln), r=['rstd2'], w=['rstd2'])
                        sc.op('act', lambda e: e.activation(rstd2[:], rstd2[:], AF.Exp, scale=-0.5), r=['rstd2'], w=['rstd2'])
                        sc.dma('sp', g_t[:], GATE[64 * h:64 * h + 64, t0:t0 + 512], w=['g_t'])
                        sc.op('dve', lambda e: e.tensor_tensor(o_sb[:], o_sb[:], mean2[:], ALU.subtract), r=['o_sb', 'mean2'], w=['o_sb'])
                        sc.op('dve', lambda e: e.tensor_tensor(o_sb[:], o_sb[:], rstd2[:], ALU.mult), r=['o_sb', 'rstd2'], w=['o_sb'])
                        sc.op('dve', lambda e: e.tensor_scalar(o_sb[:], o_sb[:], cv[0:64, 74 + h:75 + h], None, ALU.mult), r=['o_sb', 'cv'], w=['o_sb'])
                        r_ = ro[tb % 2]
                        rk_ = f'ro{tb % 2}'
                        sc.op('dve', lambda e: e.tensor_tensor(r_[:], o_sb[:], g_t[:], ALU.mult), r=['o_sb', 'g_t'], w=[rk_])
                        sc.dma('sp', MIXT[64 * h:64 * h + 64, t0:t0 + 512], r_[:], r=[rk_], w=[('MIXT2', h, tb)])
                sc.barrier()
                if upto == 2:
                    stop[0] = True

            with ExitStack() as st3:
              for _once in ([0] if not stop[0] else []):
                qa = sb("qa", [70, S], BF16, st3)
                ka = sb("ka", [70, S], BF16, st3)
                fv_t = sb("fv_t", [128, NT, 64], BF16, st3)
                msk = sb("msk", [128, 4 * 512], F32, st3)
                sc.dma('sp', msk[:], msk_d[:, :], w=['msk'])
                fb = sb("fb", [1, 4], F32, st3)
                sc.dma('sp', fb[:], foxb_d[l, :, :], w=['fb'])
                sc.op('dve', lambda e: e.tensor_scalar(fb[:], fb[:], -1.0, None, ALU.mult), r=['fb'], w=['fb'])
                FW = min(S, 2048)
                f_r = sb("f_r", [1, FW], F32, st3)
                F_r = sb("F_r", [1, FW], F32, st3)
                one_r = sb("one_r", [1, FW], F32, st3)
                sc.op('pool', lambda e: e.memset(one_r[:], 1.0), w=['one_r'])
                fr = sb("fr", [1, 7, FW], BF16, st3)
                r1 = sb("r1", [1, FW], F32, st3)
                r2 = sb("r2", [1, FW], F32, st3)
                Fl = sb("Fl", [1, 1], F32, st3)
                pt = [sb(f"pt{i}", [128, 512], BF16, st3) for i in range(3)]
                rden = sb("rden", [64, 512], F32, st3)
                fo = [sb(f"fo{i}", [64, 512], BF16, st3) for i in range(2)]
                for h in range(4):
                    sc.op('dve', lambda e: e.memset(Fl[:], 0.0), w=['Fl'])
                    for pc in range(S // FW):
                        sl = slice(pc * FW, pc * FW + FW)
                        sc.dma('sp', f_r[:], FF[h:h + 1, sl], w=['f_r'])
                        sc.op('act', lambda e: e.activation(f_r[:], f_r[:], AF.Exp, bias=fb[0:1, h:h + 1], scale=-1.0), r=['f_r', 'fb'], w=['f_r'])
                        sc.op('act', lambda e: e.activation(f_r[:], f_r[:], AF.Ln, bias=1.0), r=['f_r'], w=['f_r'])
                        sc.op('dve', lambda e: e.tensor_scalar(f_r[:], f_r[:], -1.0, None, ALU.mult), r=['f_r'], w=['f_r'])
                        sc.op('dve', lambda e: e.tensor_tensor_scan(F_r[:], one_r[:], f_r[:], Fl[0:1, 0:1], ALU.mult, ALU.add), r=['one_r', 'f_r', 'Fl'], w=['F_r'])
                        sc.op('dve', lambda e: e.tensor_copy(Fl[:], F_r[0:1, FW - 1:FW]), r=['F_r'], w=['Fl'])
                        sc.op('dve', lambda e: e.tensor_copy(fr[:, 0, :], F_r[:]), r=['F_r'], w=['fr'])
                        sc.op('dve', lambda e: e.tensor_tensor(r1[:], F_r[:], fr[:, 0, :], ALU.subtract), r=['F_r', 'fr'], w=['r1'])
                        sc.op('dve', lambda e: e.tensor_copy(fr[:, 1, :], r1[:]), r=['r1', 'fr'], w=['fr'])
                        sc.op('dve', lambda e: e.tensor_tensor(r2[:], r1[:], fr[:, 1, :], ALU.subtract), r=['r1', 'fr'], w=['r2'])
                        sc.op('dve', lambda e: e.tensor_copy(fr[:, 2, :], r2[:]), r=['r2', 'fr'], w=['fr'])
                        sc.op('dve', lambda e: e.tensor_copy(fr[:, 3, :], one_r[:]), r=['one_r', 'fr'], w=['fr'])
                        for i in range(3):
                            sc.op('dve', lambda e, i=i: e.tensor_scalar(fr[:, 4 + i, :], fr[:, i, :], -1.0, None, ALU.mult), r=['fr'], w=['fr'])
                        sc.dma('sp', FRD[:, sl].rearrange("(o r) t -> o r t", o=1), fr[:], r=['fr'], w=[('FRD', pc)])
                    FRk = [('FRD', pc) for pc in range(S // FW)]
                    sc.dma('sp', qa[0:64, :], FQ[64 * h:64 * h + 64, :], w=['qa'])
                    sc.dma('sp', qa[64:67, :], FRD[0:3, :], r=FRk + ['qa'], w=['qa'])
                    for i in range(3):
                        sc.dma('sp', qa[67 + i:68 + i, :], FRD[3:4, :], r=FRk + ['qa'], w=['qa'])
                    sc.dma('sp', ka[0:64, :], FK[64 * h:64 * h + 64, :], w=['ka'])
                    for i in range(3):
                        sc.dma('sp', ka[64 + i:65 + i, :], FRD[3:4, :], r=FRk + ['ka'], w=['ka'])
                    sc.dma('sp', ka[67:70, :], FRD[4:7, :], r=FRk + ['ka'], w=['ka'])
                    sc.dma('sp', fv_t[:], FV[:, 64 * h:64 * h + 64].rearrange("(j p) e -> p j e", p=128), w=['fv_t'])
                    pi = 0
                    for qb in range(NB):
                        q0 = qb * 512
                        pn, pnk = PS[5], 'ps5'
                        pd, pdk = PS[6], 'ps6'
                        nk = 4 * qb + 4
                        for j in range(nk):
                            ps_ = PS[j % 4]
                            psk = f'ps{j % 4}'
                            mm(ps_[:, :], ka[:, 128 * j:128 * j + 128], qa[:, q0:q0 + 512], True, True, r=['ka', 'qa'], w=[psk])
                            p_ = pt[pi % 3]
                            pk = f'pt{pi % 3}'
                            pi += 1
                            sc.op('act', lambda e: e.activation(p_[:], ps_[:, :], AF.Exp), r=[psk], w=[pk])
                            if j >= 4 * qb:
                                m = j - 4 * qb
                                sc.op('dve', lambda e: e.tensor_tensor(p_[:], p_[:], msk[:, 512 * m:512 * m + 512], ALU.mult), r=[pk, 'msk'], w=[pk])
                            mm(pn[0:64, :], fv_t[:, j, :], p_[:], j == 0, j == nk - 1, r=['fv_t', pk], w=[pnk], inc=False)
                            mm(pd[0:64, :], onesb[:], p_[:], j == 0, j == nk - 1, r=['onesb', pk], w=[pdk], inc=True)
                        sc.op('dve', lambda e: e.reciprocal(rden[:], pd[0:64, :]), r=[pdk], w=['rden'])
                        f_ = fo[qb % 2]
                        fk_ = f'fo{qb % 2}'
                        sc.op('dve', lambda e: e.tensor_tensor(f_[:], pn[0:64, :], rden[:], ALU.mult), r=[pnk, 'rden'], w=[fk_])
                        sc.dma('sp', MIXT[768 + 64 * h:832 + 64 * h, q0:q0 + 512], f_[:], r=[fk_], w=[('MIXT3', h, qb)])
                sc.barrier()
                if upto == 3:
                    stop[0] = True

            with ExitStack() as st4:
              for _once in ([0] if not stop[0] else []):
                wo = sb("wo", [128, 8, D], BF16, st4)
                for kc in range(8):
                    sc.dma('pool', wo[:, kc, :], w_o_d[l, kc * 128:(kc + 1) * 128, :], w=[('wo', kc)])
                rwt = sb("rwt", [128, 8, NE], F32, st4)
                sc.dma('sp', rwt[:], rw_d[l].rearrange("(kc p) e -> p kc e", p=128), w=['rwt'])
                rbt = sb("rbt", [128, NE], F32, st4)
                sc.dma('sp', rbt[:], rb_d[l, 0:1, :].partition_broadcast(128), w=['rbt'])
                mx = [sb(f"mx{i}", [128, 8, 512], BF16, st4) for i in range(2)]
                xr = [sb(f"xr{i}", [128, 8, 512], F32, st4) for i in range(2)]
                z = sb("z", [128, 8, 512], F32, st4)
                stl = dict(sq=[sb(f"sq4{i}", [128, 512], F32, st4) for i in range(2)], sqk='s4', mean=sb("mean", [128, 512], F32, st4),
                           rstd=sb("rstd", [128, 512], F32, st4), tmp=sb("tmp", [128, 512], F32, st4))
                x1f = z
                x1h = sb("x1h", [128, 8, 512], BF16, st4)
                lg = sb("lg", [128, NE], F32, st4)
                m8 = sb("m8", [128, 8], F32, st4)
                nmx = sb("nmx", [128, 1], F32, st4)
                mk = sb("mk", [128, NE], F32, st4)
                ex = sb("ex", [128, NE], F32, st4)
                ssum = sb("ssum", [128, 1], F32, st4)
                gtt = sb("gtt", [NE, 512], F32, st4)
                for tb in range(NB):
                    t0 = tb * 512
                    p = tb % 2
                    sl = slice(t0, t0 + 512)
                    sc.dma('sp', mx[p][:], chunked(MIXT)[:, :, sl], w=[f'mx{p}'])
                    sc.dma('sp', xr[p][:], chunked(XT)[:, :, sl], w=[f'xr{p}'])
                    for dc in range(8):
                        pm, pmk = bank()
                        for kc in range(8):
                            mm(pm[:, :], wo[:, kc, 128 * dc:128 * dc + 128], mx[p][:, kc, :], kc == 0, kc == 7, r=[('wo', kc), f'mx{p}'], w=[pmk])
                        sc.op('dve', lambda e, dc=dc: e.scalar_tensor_tensor(z[:, dc, :], xr[p][:, dc, :], ALPHA, pm[:, :], ALU.mult, ALU.add),
                              r=[f'xr{p}', pmk], w=['z'])
                    ln_block(z, 'z', 78, 86, stl)
                    sc.op('dve', lambda e: e.tensor_copy(x1h[:], z[:]), r=['z'], w=['x1h'])
                    sc.dma('sp', chunked(X1T)[:, :, sl], x1f[:], r=['z'], w=[('X1T', tb)])
                    sc.dma('sp', chunked(X1B)[:, :, sl], x1h[:], r=['x1h'], w=[('X1B', tb)])
                    for sub in range(4):
                        pm, pmk = bank()
                        for kc in range(8):
                            mm(pm[:, 0:NE], x1f[:, kc, 128 * sub:128 * sub + 128], rwt[:, kc, :], kc == 0, kc == 7, r=['z', 'rwt'], w=[pmk])
                        sc.op('dve', lambda e: e.tensor_tensor(lg[:], pm[:, 0:NE], rbt[:], ALU.add), r=[pmk, 'rbt'], w=['lg'])
                        sc.op('dve', lambda e: e.max(m8[:], lg[:]), r=['lg'], w=['m8'])
                        sc.op('dve', lambda e: e.tensor_scalar(mk[:], lg[:], m8[:, 3:4], None, ALU.is_ge), r=['lg', 'm8'], w=['mk'])
                        sc.op('dve', lambda e: e.tensor_scalar(nmx[:], m8[:, 0:1], -1.0, None, ALU.mult), r=['m8'], w=['nmx'])
                        sc.op('act', lambda e: e.activation(ex[:], lg[:], AF.Exp, bias=nmx[:, 0:1], scale=1.0), r=['lg', 'nmx'], w=['ex'])
                        sc.op('dve', lambda e: e.tensor_tensor(ex[:], ex[:], mk[:], ALU.mult), r=['ex', 'mk'], w=['ex'])
                        sc.op('dve', lambda e: e.tensor_reduce(ssum[:], ex[:], AX.X, ALU.add), r=['ex'], w=['ssum'])
                        sc.op('dve', lambda e: e.reciprocal(ssum[:], ssum[:]), r=['ssum'], w=['ssum'])
                        sc.op('dve', lambda e: e.tensor_scalar(ex[:], ex[:], ssum[:, 0:1], None, ALU.mult), r=['ex', 'ssum'], w=['ex'])
                        pg, pgk = bank()
                        mm(pg[0:NE, 0:128], ex[:], ident, True, True, r=['ex', 'cst'], w=[pgk])
                        sc.op('act', lambda e, sub=sub: e.copy(gtt[:, 128 * sub:128 * sub + 128], pg[0:NE, 0:128]), r=[pgk], w=['gtt'])
                    sc.dma('sp', GT[:, sl], gtt[:], r=['gtt'], w=[('GT', tb)])
                sc.barrier()
                if upto == 4:
                    stop[0] = True

            with ExitStack() as st5:
              for _once in ([0] if not stop[0] else []):
                NBS = SBK // 512
                acc = sb("acc", [128, 8, SBK], F32, st5)
                xs = sb("xs", [128, 8, SBK], BF16, st5)
                w1t = [sb(f"w1t{i}", [128, 8, 2048], BF16, st5) for i in range(2)]
                w2t = [sb(f"w2t{i}", [128, 8, D], BF16, st5) for i in range(2)]
                gb = [sb(f"gb{i}", [128, SBK], F32, st5) for i in range(2)]
                b1t = sb("b1t", [128, NE * 16], F32, st5)
                sc.dma('sp', b1t[:], b1_d[l, :, :], w=['b1t'])
                b2t = sb("b2t", [NE, D], F32, st5)
                sc.dma('sp', b2t[:], b2_d[l, :, :], w=['b2t'])
                gts = sb("gts", [NE, SBK], F32, st5)
                actt = sb("actt", [128, 8, 512], BF16, st5)
                ga = [sb(f"ga{i}", [128, 512], F32, st5) for i in range(2)]
                sgm = [sb(f"sgm{i}", [128, 512], F32, st5) for i in range(2)]
                li = [sb(f"li{i}", [128, 512], F32, st5) for i in range(2)]
                xr5 = [sb(f"xr5{i}", [128, 512], F32, st5) for i in range(2)]
                hb5 = [sb(f"hb5{i}", [128, 512], BF16, st5) for i in range(2)]
                stl = dict(sq=[sb(f"sq5{i}", [128, 512], F32, st5) for i in range(2)], sqk='s5', mean=sb("mean5", [128, 512], F32, st5),
                           rstd=sb("rstd5", [128, 512], F32, st5), tmp=sb("tmp5", [128, 512], F32, st5))
                wn = 0
                for sbi in range(NSB):
                    s0 = sbi * SBK
                    sc.dma('sp', xs[:], chunked(X1B)[:, :, s0:s0 + SBK], w=['xs'])
                    sc.dma('sp', gts[:], GT[0:NE, s0:s0 + SBK], w=['gts'])
                    for tb in range(NBS):
                        for dc in range(8):
                            pm, pmk = bank()
                            mm(pm[:, :], b2t[:, 128 * dc:128 * dc + 128], gts[:, 512 * tb:512 * tb + 512], True, True, r=['b2t', 'gts'], w=[pmk])
                            sc.op('act', lambda e, dc=dc, tb=tb: e.copy(acc[:, dc, 512 * tb:512 * tb + 512], pm[:, :]), r=[pmk], w=['acc'])
                    for ex_ in range(NE):
                        wp = wn % 2
                        wn += 1
                        for kc in range(8):
                            sc.dma('pool', w1t[wp][:, kc, :], w1_d[l, ex_, kc * 128:(kc + 1) * 128, :], w=[(f'w1t{wp}', kc)])
                        for kc in range(8):
                            sc.dma('pool', w2t[wp][:, kc, :], w2_d[l, ex_, kc * 128:(kc + 1) * 128, :], w=[(f'w2t{wp}', kc)])
                        sc.dma('sp', gb[wp][:], GT[ex_:ex_ + 1, s0:s0 + SBK].partition_broadcast(128), w=[f'gb{wp}'])
                        for tb in range(NBS):
                            for fc in range(8):
                                pg, pgk = bank()
                                for kc in range(8):
                                    mm(pg[:, :], w1t[wp][:, kc, 128 * fc:128 * fc + 128], xs[:, kc, 512 * tb:512 * tb + 512], kc == 0, kc == 7,
                                       r=[(f'w1t{wp}', kc), 'xs'], w=[pgk])
                                pl, plk = bank()
                                for kc in range(8):
                                    mm(pl[:, :], w1t[wp][:, kc, 1024 + 128 * fc:1152 + 128 * fc], xs[:, kc, 512 * tb:512 * tb + 512], kc == 0, kc == 7,
                                       r=[(f'w1t{wp}', kc), 'xs'], w=[plk])
                                q = fc % 2
                                bg = b1t[:, ex_ * 16 + fc:ex_ * 16 + fc + 1]
                                bl = b1t[:, ex_ * 16 + 8 + fc:ex_ * 16 + 8 + fc + 1]
                                sc.op('dve', lambda e: e.tensor_scalar(ga[q][:], pg[:, :], bg, 7.0, ALU.add, ALU.min), r=[pgk, 'b1t'], w=[f'ga{q}'])
                                sc.op('act', lambda e: e.activation(sgm[q][:], ga[q][:], AF.Sigmoid, scale=1.702), r=[f'ga{q}'], w=[f'sgm{q}'])
                                sc.op('dve', lambda e: e.tensor_scalar(li[q][:], pl[:, :], bl, 7.0, ALU.add, ALU.min), r=[plk, 'b1t'], w=[f'li{q}'])
                                sc.op('pool', lambda e: e.tensor_scalar(li[q][:], li[q][:], -7.0, 1.0, ALU.max, ALU.add), r=[f'li{q}'], w=[f'li{q}'])
                                sc.op('pool', lambda e: e.tensor_tensor(ga[q][:], ga[q][:], sgm[q][:], ALU.mult), r=[f'ga{q}', f'sgm{q}'], w=[f'ga{q}'])
                                sc.op('pool', lambda e: e.tensor_tensor(ga[q][:], ga[q][:], li[q][:], ALU.mult), r=[f'ga{q}', f'li{q}'], w=[f'ga{q}'])
                                sc.op('dve', lambda e, fc=fc, tb=tb: e.tensor_tensor(actt[:, fc, :], ga[q][:], gb[wp][:, 512 * tb:512 * tb + 512], ALU.mult),
                                      r=[f'ga{q}', f'gb{wp}'], w=['actt'])
                            for dc in range(8):
                                pm, pmk = bank()
                                for fc in range(8):
                                    mm(pm[:, :], w2t[wp][:, fc, 128 * dc:128 * dc + 128], actt[:, fc, :], fc == 0, fc == 7, r=[(f'w2t{wp}', fc), 'actt'], w=[pmk])
                                sc.op('dve', lambda e, dc=dc, tb=tb: e.tensor_tensor(acc[:, dc, 512 * tb:512 * tb + 512], acc[:, dc, 512 * tb:512 * tb + 512], pm[:, :], ALU.add),
                                      r=['acc', pmk], w=['acc'])
                    for tb in range(NBS):
                        gtb = sbi * NBS + tb
                        sl = slice(gtb * 512, gtb * 512 + 512)
                        zs = acc[:, :, 512 * tb:512 * tb + 512]
                        for dc in range(8):
                            xr_ = xr5[dc % 2]
                            xk_ = f'xr5{dc % 2}'
                            sc.dma('sp', xr_[:], X1T[128 * dc:128 * dc + 128, sl], r=[('X1T', gtb)], w=[xk_])
                            sc.op('dve', lambda e, dc=dc: e.scalar_tensor_tensor(zs[:, dc, :], xr_[:], ALPHA, zs[:, dc, :], ALU.mult, ALU.add),
                                  r=[xk_, 'acc'], w=['acc'])
                        ln_block(zs, 'acc', 94, 102, stl)
                        if last:
                            sc.dma('sp', chunked(outT)[:, :, sl], zs, r=['acc'], w=[('outT', gtb)])
                        else:
                            sc.dma('sp', chunked(XT)[:, :, sl], zs, r=['acc'], w=[('XT', gtb)])
                            for dc in range(8):
                                hb_ = hb5[dc % 2]
                                hk_ = f'hb5{dc % 2}'
                                sc.op('act', lambda e, dc=dc: e.copy(hb_[:], zs[:, dc, :]), r=['acc'], w=[hk_])
                                sc.dma('sp', XB[128 * dc:128 * dc + 128, sl], hb_[:], r=[hk_], w=[('XB', gtb, dc)])
                sc.barrier()
                if upto == 5:
                    stop[0] = True
        except _Stop:
            pass
    return nc


def host_consts(S):
    half = 16
    freqs = (10000.0 ** (-np.arange(half, dtype=np.float32) / half)).astype(np.float32)
    pos = np.arange(S, dtype=np.float32)
    ang = pos[None, :] * freqs[:, None]
    cos = np.cos(ang).astype(np.float32)
    sin = np.sin(ang).astype(np.float32)
    ropec = np.tile(np.concatenate([cos, cos], 0), (4, 1))
    ropes = np.tile(np.concatenate([-sin, sin], 0), (4, 1))
    cst = np.zeros((128, 4 * 128 + 512 + 4 + 128), np.float32)
    idx = np.arange(128)
    same = (idx[:, None] // 64) == (idx[None, :] // 64)
    scale = 32 ** -0.5
    for h in range(4):
        g = 1.0 - 2.0 ** (-5.0 - h)
        cst[:, 128 * h:128 * h + 128] = np.where(same, g ** np.abs(idx[:, None] - idx[None, :]), 0.0) * scale
        cst[32 * h:32 * h + 32, 512:1024] = (g ** ((np.arange(512) % 64) + 1.0))[None, :]
        cst[:, 1024 + h] = g ** (63 - (idx % 64)) * scale
    cst[:, 1028:1156] = np.eye(128, dtype=np.float32)
    msk = np.zeros((128, 4 * 512), np.float32)
    s_ = np.arange(128)[:, None]
    t_ = np.arange(512)[None, :]
    for m in range(4):
        msk[:, 512 * m:512 * m + 512] = (t_ >= 128 * m + s_)
    return dict(ropec=np.ascontiguousarray(ropec), ropes=np.ascontiguousarray(ropes), cst=cst, msk=msk)


def host_layout(inp, S, DEPTH, NE):
    L = DEPTH
    perm = np.concatenate([np.arange(16, 32), np.arange(0, 16)])
    pq = np.concatenate([h * 32 + perm for h in range(4)])
    w_in = np.asarray(inp['w_in'])
    w_aug = np.concatenate([w_in, w_in[:, :, pq], w_in[:, :, 128 + pq]], axis=2)
    cv = np.zeros((L, 128, NCV), np.float32)
    cdw = np.asarray(inp['conf_dw'])
    sdw = np.asarray(inp['sc_dw'])
    for c in range(2):
        cv[:, :, 31 * c:31 * c + 31] = cdw[:, :, 128 * c:128 * c + 128].transpose(0, 2, 1)
        cv[:, :, 62 + 3 * c:65 + 3 * c] = sdw[:, :, 128 * c:128 * c + 128].transpose(0, 2, 1)
        cv[:, :, 68 + c] = np.asarray(inp['conf_dw_b'])[:, 128 * c:128 * c + 128]
        cv[:, :, 70 + c] = np.asarray(inp['conf_ln_g'])[:, 128 * c:128 * c + 128]
        cv[:, :, 72 + c] = np.asarray(inp['conf_ln_b'])[:, 128 * c:128 * c + 128]
    cv[:, 0:64, 74:78] = np.asarray(inp['ret_gn_g']).reshape(L, 4, 64).transpose(0, 2, 1)
    for name, c0 in (('ln1_g', 78), ('ln1_b', 86), ('ln2_g', 94), ('ln2_b', 102)):
        cv[:, :, c0:c0 + 8] = np.asarray(inp[name]).reshape(L, 8, 128).transpose(0, 2, 1)
    b1T = np.ascontiguousarray(np.asarray(inp['b1']).reshape(L, NE, 16, 128).transpose(0, 3, 1, 2).reshape(L, 128, NE * 16))
    rw = np.asarray(inp['router_w'])
    rb = np.asarray(inp['router_b'])
    common = dict(w_in=np.ascontiguousarray(w_aug), w_o=np.asarray(inp['w_o']), cvec=cv,
                  foxb=np.asarray(inp['fox_b_f']).reshape(L, 1, 4), rw=rw, rb=rb.reshape(L, 1, -1),
                  w1=np.asarray(inp['w1']), b1T=b1T, w2=np.asarray(inp['w2']), b2=np.asarray(inp['b2']))
    common.update(host_consts(S))
    return common


def run(inp, S, DEPTH, NE, SBK, dbg=None, upto=99):
    x = np.asarray(inp['x'])
    B = x.shape[0]
    common = host_layout(inp, S, DEPTH, NE)
    nc = build(S, DEPTH, NE, SBK, dbg, upto)
    in_maps = []
    for b in range(B):
        m = dict(common)
        m['xT'] = np.ascontiguousarray(x[b].T)
        in_maps.append(m)
    res = run_bass_kernel_spmd(nc, in_maps, core_ids=list(range(B)))
    out = np.stack([np.ascontiguousarray(r['outT'].T) for r in res.results], 0).astype(np.float32)
    if dbg is not None:
        return out, res.results
    return out


def kernel(**inputs):
    return run(inputs, 8192, 4, 32, 1024)
```

```python
import math
import os
KN = int(os.environ.get('KN', '99'))
KW = int(os.environ.get('KW', '1'))
from contextlib import ExitStack
import numpy as np
import concourse.bass as bass
import concourse.mybir as mybir
from concourse.bass_utils import run_bass_kernel_spmd

F32 = mybir.dt.float32
BF16 = mybir.dt.bfloat16
ALU = mybir.AluOpType
AF = mybir.ActivationFunctionType
AX = mybir.AxisListType

D = 1024
CHUNK = 64
ALPHA = 8 ** 0.25
EPS = 1e-5
NCV = 110
WA = 2820 + 256


class Sched:
    def __init__(s, nc, es):
        s.nc = nc
        s.eng = {'pe': nc.tensor, 'act': nc.scalar, 'dve': nc.vector, 'pool': nc.gpsimd, 'sp': nc.sync}
        s.sem = {k: es.enter_context(nc.semaphore('s_' + k)) for k in ('pe', 'act', 'dve', 'pool')}
        s.cnt = {k: 0 for k in s.sem}
        s.seen = {k: {} for k in s.eng}
        s.dq = {}
        for q in ('sp', 'pool', 'act'):
            s.dq[q] = dict(sems=[es.enter_context(nc.semaphore(f'd_{q}{i}')) for i in range(8)], n=0)
        s.res = {}

    def _need(s, en, deps):
        best = {}
        for (k, h, v) in deps:
            if v > best.get(k, (None, 0))[1]:
                best[k] = (h, v)
        for k, (h, v) in best.items():
            if s.seen[en].get(k, 0) < v:
                s.eng[en].wait_ge(h, v)
                s.seen[en][k] = v

    def _collect(s, en, r, w):
        deps = []
        for k in r:
            st = s.res.get(k)
            if st and st[0]:
                deps.append(st[0])
        for k in w:
            st = s.res.get(k)
            if st:
                if st[0]:
                    deps.append(st[0])
                deps.extend(st[1].values())
        if en == 'pe':
            deps = [d for d in deps if d[0] != 'pe']
        return deps

    def _record(s, dep, r, w):
        for k in r:
            st = s.res.setdefault(k, [None, {}])
            o = st[1].get(dep[0])
            if o is None or o[2] < dep[2]:
                st[1][dep[0]] = dep
        for k in w:
            s.res[k] = [dep, {}]

    def op(s, en, fn, r=(), w=(), inc=True):
        assert inc or en == 'pe'
        s._need(en, s._collect(en, r, w))
        ins = fn(s.eng[en])
        if inc:
            s.cnt[en] += 1
            ins.then_inc(s.sem[en], 1)
            dep = (en, s.sem[en], s.cnt[en])
        else:
            dep = (en, s.sem[en], s.cnt[en] + 1)
        s._record(dep, r, w)

    def dma(s, q, out, in_, r=(), w=(), **kw):
        Q = s.dq[q]
        i = Q['n']
        Q['n'] += 1
        K = len(Q['sems'])
        h = Q['sems'][i % K]
        key = f'd_{q}{i % K}'
        deps = s._collect(q, r, w)
        if i >= K:
            deps.append((key, h, 16 * (i // K)))
        s._need(q, deps)
        s.eng[q].dma_start(out=out, in_=in_, **kw).then_inc(h, 16)
        s._record((key, h, 16 * (i // K + 1)), r, w)

    def alldeps(s):
        deps = [(k, s.sem[k], s.cnt[k]) for k in s.sem if s.cnt[k] > 0]
        for q, Q in s.dq.items():
            K = len(Q['sems'])
            for j in range(min(K, Q['n'])):
                tot = (Q['n'] - 1 - j) // K + 1
                deps.append((f'd_{q}{j}', Q['sems'][j], 16 * tot))
        return deps

    def barrier(s):
        deps = s.alldeps()
        for en in s.eng:
            s._need(en, [d for d in deps if not (en == 'pe' and d[0] == 'pe')])
        s.res = {}


class _Stop(Exception):
    pass


def build(S, DEPTH, NE, SBK, dbg=None, upto=99):
    NB = S // 512
    NT = S // 128
    NCH = S // 64
    NSB = S // SBK
    nc = bass.Bass("TRN2", target_bir_lowering=False)
    dt = nc.dram_tensor
    xT_in = dt("xT", [D, S], F32, kind="ExternalInput").ap()
    w_in_d = dt("w_in", [DEPTH, D, WA], F32, kind="ExternalInput").ap()
    w_o_d = dt("w_o", [DEPTH, D, D], F32, kind="ExternalInput").ap()
    cvec_d = dt("cvec", [DEPTH, 128, NCV], F32, kind="ExternalInput").ap()
    foxb_d = dt("foxb", [DEPTH, 1, 4], F32, kind="ExternalInput").ap()
    rw_d = dt("rw", [DEPTH, D, NE], F32, kind="ExternalInput").ap()
    rb_d = dt("rb", [DEPTH, 1, NE], F32, kind="ExternalInput").ap()
    w1_d = dt("w1", [DEPTH, NE, D, 2048], F32, kind="ExternalInput").ap()
    b1_d = dt("b1T", [DEPTH, 128, NE * 16], F32, kind="ExternalInput").ap()
    w2_d = dt("w2", [DEPTH, NE, D, D], F32, kind="ExternalInput").ap()
    b2_d = dt("b2", [DEPTH, NE, D], F32, kind="ExternalInput").ap()
    cos_d = dt("ropec", [128, S], F32, kind="ExternalInput").ap()
    sin_d = dt("ropes", [128, S], F32, kind="ExternalInput").ap()
    cst_d = dt("cst", [128, 4 * 128 + 512 + 4 + 128], F32, kind="ExternalInput").ap()
    msk_d = dt("msk", [128, 4 * 512], F32, kind="ExternalInput").ap()
    outT = dt("outT", [D, S], F32, kind="ExternalOutput").ap()
    okind = {} if dbg is None else {"kind": "ExternalOutput"}
    XT = dt("XT", [D, S], F32).ap()
    XB = dt("XB", [D, S], BF16).ap()
    X1T = dt("X1T", [D, S], F32).ap()
    X1B = dt("X1B", [D, S], BF16).ap()
    MIXT = dt("MIXT", [D, S], BF16, **okind).ap()
    RQ = dt("RQ", [128, S], BF16).ap()
    RK = dt("RK", [128, S], BF16).ap()
    RV = dt("RV", [S, 256], BF16).ap()
    GATE = dt("GATE", [256, S], BF16).ap()
    FQ = dt("FQ", [256, S], BF16).ap()
    FK = dt("FK", [256, S], BF16).ap()
    FV = dt("FV", [S, 256], BF16).ap()
    FF = dt("FF", [4, S], F32).ap()
    FRD = dt("FRD", [7, S], BF16).ap()
    GT = dt("GT", [NE, S], F32, **okind).ap()

    def chunked(ap):
        return ap.rearrange("(kc p) t -> p kc t", p=128)

    with ExitStack() as es:
        sc = Sched(nc, es)
        uid = [0]

        def sb(name, shape, dtp, st=es):
            uid[0] += 1
            return st.enter_context(nc.sbuf_tensor(f"{name}_s{uid[0]}", shape, dtp))
        PS = [es.enter_context(nc.psum_tensor(f"ps{i}", [128, 512], F32)) for i in range(7)]
        PSB = es.enter_context(nc.psum_tensor("psb", [128, 1024], BF16))
        pctr = [0]

        def bank():
            pctr[0] = (pctr[0] + 1) % 7
            return PS[pctr[0]], f"ps{pctr[0]}"

        cst = sb("cst", [128, 4 * 128 + 512 + 4 + 128], F32)
        sc.dma('sp', cst[:], cst_d[:, :], w=['cst'])
        identb = sb("identb", [128, 128], BF16)
        sc.op('dve', lambda e: e.tensor_copy(identb[:], cst[:, 1028:1156]), r=['cst'], w=['identb'])
        ident = cst[:, 1028:1156]
        onesD = sb("onesD", [128, 128], F32)
        sc.op('pool', lambda e: e.memset(onesD[:], 1.0 / D), w=['onesD'])
        ones256 = sb("ones256", [128, 128], F32)
        sc.op('pool', lambda e: e.memset(ones256[:], 1.0 / 256), w=['ones256'])
        ones64 = sb("ones64", [64, 64], F32)
        sc.op('pool', lambda e: e.memset(ones64[:], 1.0 / 64), w=['ones64'])
        onesb = sb("onesb", [128, 64], BF16)
        sc.op('pool', lambda e: e.memset(onesb[:], 1.0), w=['onesb'])
        cv = sb("cv", [128, NCV], F32)

        def mm(out, lhsT, rhs, start, stop, r, w, inc=None):
            sc.op('pe', lambda e: e.matmul(out, lhsT, rhs, start=start, stop=stop), r=r, w=w,
                  inc=(stop if inc is None else inc))

        def ln_block(z, zk, gcol, bcol, st):
            pm, pmk = bank()
            for dc in range(8):
                mm(pm[:, :], onesD[:], z[:, dc, :], dc == 0, dc == 7, r=['onesD', zk], w=[pmk])
            pe2, pe2k = bank()
            for dc in range(8):
                sq = st['sq'][dc % 2]
                sqk = st['sqk'] + str(dc % 2)
                sc.op('act', lambda e, dc=dc: e.activation(sq[:], z[:, dc, :], AF.Square), r=[zk], w=[sqk])
                mm(pe2[:, :], onesD[:], sq[:], dc == 0, dc == 7, r=['onesD', sqk], w=[pe2k], inc=True)
            mean, rstd, tmp = st['mean'], st['rstd'], st['tmp']
            mk_, rk_, tk_ = st.get('keys', (st['sqk'] + 'mean', st['sqk'] + 'rstd', st['sqk'] + 'tmp'))
            sc.op('act', lambda e: e.copy(mean[:], pm[:, :]), r=[pmk], w=[mk_])
            sc.op('dve', lambda e: e.tensor_tensor(rstd[:], mean[:], mean[:], ALU.mult), r=[mk_], w=[rk_])
            sc.op('dve', lambda e: e.tensor_tensor(rstd[:], pe2[:, :], rstd[:], ALU.subtract), r=[pe2k, rk_], w=[rk_])
            sc.op('dve', lambda e: e.tensor_scalar(rstd[:], rstd[:], 0.0, EPS, ALU.max, ALU.add), r=[rk_], w=[rk_])
            sc.op('act', lambda e: e.activation(rstd[:], rstd[:], AF.Ln), r=[rk_], w=[rk_])
            sc.op('act', lambda e: e.activation(rstd[:], rstd[:], AF.Exp, scale=-0.5), r=[rk_], w=[rk_])
            for dc in range(8):
                sc.op('dve', lambda e, dc=dc: e.tensor_tensor(tmp[:], z[:, dc, :], mean[:], ALU.subtract), r=[zk, mk_], w=[tk_])
                sc.op('dve', lambda e, dc=dc: e.tensor_tensor(tmp[:], tmp[:], rstd[:], ALU.mult), r=[tk_, rk_], w=[tk_])
                sc.op('act', lambda e, dc=dc: e.activation(z[:, dc, :], tmp[:], AF.Identity,
                                                          bias=cv[:, bcol + dc:bcol + dc + 1], scale=cv[:, gcol + dc:gcol + dc + 1]),
                      r=[tk_, 'cv'], w=[zk])

        with ExitStack() as st0:
            xf = [sb(f"xf{i}", [128, 8, 512], F32, st0) for i in range(2)]
            xh = [sb(f"xh{i}", [128, 8, 512], BF16, st0) for i in range(2)]
            for tb in range(NB):
                p = tb % 2
                sl = slice(tb * 512, tb * 512 + 512)
                sc.dma('sp', xf[p][:], chunked(xT_in)[:, :, sl], w=[f'xf{p}'])
                sc.op('dve', lambda e, p=p: e.tensor_copy(xh[p][:], xf[p][:]), r=[f'xf{p}'], w=[f'xh{p}'])
                sc.dma('sp', chunked(XT)[:, :, sl], xf[p][:], r=[f'xf{p}'], w=[('XT', tb)])
                sc.dma('sp', chunked(XB)[:, :, sl], xh[p][:], r=[f'xh{p}'], w=[('XB', tb)])
            sc.barrier()

        stop = [False]
        try:
          for l in range(DEPTH):
            last = l == DEPTH - 1
            sc.dma('sp', cv[:], cvec_d[l, :, :], w=['cv'])
            with ExitStack() as st1:
              for _once in ([0] if not stop[0] else []):
                win = sb("win", [128, 8, WA], BF16, st1)
                for kc in range(8):
                    sc.dma('pool', win[:, kc, :], w_in_d[l, kc * 128:(kc + 1) * 128, :], w=[('win', kc)])
                WK = [('win', kc) for kc in range(8)]
                xbt = [sb(f"xbt{i}", [128, 8, 544], BF16, st1) for i in range(2)]
                cs_t = sb("cs_t", [128, 512], F32, st1)
                sn_t = sb("sn_t", [128, 512], F32, st1)
                t1 = sb("t1", [128, 512], F32, st1)
                t2 = sb("t2", [128, 512], F32, st1)
                ob = [sb(f"ob{i}", [128, 512], BF16, st1) for i in range(4)]
                obc = [0]
                u_t = [sb(f"u{i}", [128, 544], F32, st1) for i in range(2)]
                sg_t = sb("sg_t", [128, 544], F32, st1)
                ca_t = [sb(f"ca{i}", [128, 512], F32, st1) for i in range(2)]
                sq1 = sb("sq1", [128, 512], F32, st1)
                mean1 = sb("mean1", [128, 512], F32, st1)
                rstd1 = sb("rstd1", [128, 512], F32, st1)
                ff_t = sb("ff_t", [4, 512], F32, st1)
                vt = [sb(f"vt{i}", [128, 256], BF16, st1) for i in range(2)]
                vtc = [0]

                def nob():
                    obc[0] = (obc[0] + 1) % 4
                    return ob[obc[0]], f"ob{obc[0]}"

                for tb in range(NB):
                    t0 = tb * 512
                    p = tb % 2
                    xk = f'xbt{p}'
                    x_ = xbt[p]
                    if tb == 0:
                        sc.op('pool', lambda e: e.memset(x_[:, :, 0:32], 0.0), w=[xk])
                        sc.dma('sp', x_[:, :, 32:544], chunked(XB)[:, :, 0:512], r=[('XB', 0)], w=[xk])
                    else:
                        sc.dma('sp', x_[:, :, :], chunked(XB)[:, :, t0 - 32:t0 + 512], r=[('XB', tb - 1), ('XB', tb)], w=[xk])
                    sc.dma('sp', cs_t[:], cos_d[:, t0:t0 + 512], w=['cs_t'])
                    sc.dma('sp', sn_t[:], sin_d[:, t0:t0 + 512], w=['sn_t'])

                    def fm(c0, ncols=128, halo=False):
                        pm, pmk = bank()
                        for kc in range(8):
                            mm(pm[0:ncols, :], win[:, kc, c0:c0 + ncols], x_[:, kc, 32:544], kc == 0, kc == 7, r=[WK[kc], xk], w=[pmk])
                        if not halo:
                            return pm, pmk
                        ph, phk = bank()
                        for kc in range(8):
                            mm(ph[0:ncols, 0:32], win[:, kc, c0:c0 + ncols], x_[:, kc, 0:32], kc == 0, kc == 7, r=[WK[kc], xk], w=[phk])
                        return pm, pmk, ph, phk

                    for (c0, cp, dst, dk) in ((0, 2820, RQ, 'RQ'), (128, 2948, RK, 'RK')):
                        pa, pak = fm(c0)
                        pp, ppk = fm(cp)
                        sc.op('dve', lambda e: e.tensor_tensor(t1[:], pa[:, :], cs_t[:], ALU.mult), r=[pak, 'cs_t'], w=['t1'])
                        sc.op('dve', lambda e: e.tensor_tensor(t2[:], pp[:, :], sn_t[:], ALU.mult), r=[ppk, 'sn_t'], w=['t2'])
                        o, ok = nob()
                        sc.op('dve', lambda e: e.tensor_tensor(o[:], t1[:], t2[:], ALU.add), r=['t1', 't2'], w=[ok])
                        sc.dma('sp', dst[:, t0:t0 + 512], o[:], r=[ok], w=[(dk, tb)])
                    for c in range(2):
                        pa, pak = fm(512 + 128 * c)
                        o, ok = nob()
                        sc.op('act', lambda e: e.activation(o[:], pa[:, :], AF.Silu), r=[pak], w=[ok])
                        sc.dma('sp', GATE[128 * c:128 * c + 128, t0:t0 + 512], o[:], r=[ok], w=[('GATE', tb)])
                    for c in range(2):
                        pa, pak, pah, pahk = fm(768 + 128 * c, halo=True)
                        pb_, pbk, pbh, pbhk = fm(1024 + 128 * c, halo=True)
                        u = u_t[c]
                        uk = f'u{c}'
                        sc.op('act', lambda e: e.activation(sg_t[:, 32:544], pb_[:, :], AF.Sigmoid), r=[pbk], w=['sg_t'])
                        sc.op('act', lambda e: e.activation(sg_t[:, 0:32], pbh[:, 0:32], AF.Sigmoid), r=[pbhk, 'sg_t'], w=['sg_t'])
                        sc.op('dve', lambda e: e.tensor_tensor(u[:, 32:544], pa[:, :], sg_t[:, 32:544], ALU.mult), r=[pak, 'sg_t'], w=[uk])
                        sc.op('dve', lambda e: e.tensor_tensor(u[:, 0:32], pah[:, 0:32], sg_t[:, 0:32], ALU.mult), r=[pahk, 'sg_t', uk], w=[uk])
                        ce = 'dve'
                        ca = ca_t[c]
                        cak = f'ca{c}'
                        wc0 = 31 * c
                        sc.op(ce, lambda e: e.tensor_scalar(ca[:], u[:, 2:514], cv[:, wc0:wc0 + 1], cv[:, 68 + c:69 + c], ALU.mult, ALU.add),
                              r=[uk, 'cv'], w=[cak])
                        for j in range(1, 31):
                            sc.op(ce, lambda e, j=j: e.scalar_tensor_tensor(ca[:], u[:, 2 + j:514 + j], cv[:, wc0 + j:wc0 + j + 1], ca[:], ALU.mult, ALU.add),
                                  r=[uk, 'cv', cak], w=[cak])
                    pm, pmk = bank()
                    pe2, pe2k = bank()
                    for c in range(2):
                        mm(pm[:, :], ones256[:], ca_t[c][:], c == 0, c == 1, r=['ones256', f'ca{c}'], w=[pmk])
                    for c in range(2):
                        sc.op('act', lambda e, c=c: e.activation(sq1[:], ca_t[c][:], AF.Square), r=[f'ca{c}'], w=['sq1'])
                        mm(pe2[:, :], ones256[:], sq1[:], c == 0, c == 1, r=['ones256', 'sq1'], w=[pe2k], inc=True)
                    sc.op('act', lambda e: e.copy(mean1[:], pm[:, :]), r=[pmk], w=['mean1'])
                    sc.op('dve', lambda e: e.tensor_tensor(rstd1[:], mean1[:], mean1[:], ALU.mult), r=['mean1'], w=['rstd1'])
                    sc.op('dve', lambda e: e.tensor_tensor(rstd1[:], pe2[:, :], rstd1[:], ALU.subtract), r=[pe2k, 'rstd1'], w=['rstd1'])
                    sc.op('dve', lambda e: e.tensor_scalar(rstd1[:], rstd1[:], 0.0, EPS, ALU.max, ALU.add), r=['rstd1'], w=['rstd1'])
                    sc.op('act', lambda e: e.activation(rstd1[:], rstd1[:], AF.Ln), r=['rstd1'], w=['rstd1'])
                    sc.op('act', lambda e: e.activation(rstd1[:], rstd1[:], AF.Exp, scale=-0.5), r=['rstd1'], w=['rstd1'])
                    for c in range(2):
                        sc.op('dve', lambda e, c=c: e.tensor_tensor(t1[:], ca_t[c][:], mean1[:], ALU.subtract), r=[f'ca{c}', 'mean1'], w=['t1'])
                        sc.op('dve', lambda e: e.tensor_tensor(t1[:], t1[:], rstd1[:], ALU.mult), r=['t1', 'rstd1'], w=['t1'])
                        o, ok = nob()
                        sc.op('act', lambda e, c=c: e.activation(o[:], t1[:], AF.Silu, bias=cv[:, 72 + c:73 + c], scale=cv[:, 70 + c:71 + c]),
                              r=['t1', 'cv'], w=[ok])
                        sc.dma('sp', MIXT[256 + 128 * c:384 + 128 * c, t0:t0 + 512], o[:], r=[ok], w=[('MIXT', tb)])
                    for c in range(2):
                        pc, pck, pch, pchk = fm(1536 + 128 * c, halo=True)
                        ph_, phk_, phh, phhk = fm(1792 + 128 * c, halo=True)
                        u = u_t[c]
                        uk = f'u{c}'
                        sc.op('act', lambda e: e.copy(sg_t[:, 32:544], pc[:, :]), r=[pck], w=['sg_t'])
                        sc.op('act', lambda e: e.copy(sg_t[:, 0:32], pch[:, 0:32]), r=[pchk, 'sg_t'], w=['sg_t'])
                        sc.op('dve', lambda e: e.tensor_tensor(u[:, 32:544], ph_[:, :], sg_t[:, 32:544], ALU.mult), r=[phk_, 'sg_t'], w=[uk])
                        sc.op('dve', lambda e: e.tensor_tensor(u[:, 0:32], phh[:, 0:32], sg_t[:, 0:32], ALU.mult), r=[phhk, 'sg_t', uk], w=[uk])
                        wc0 = 62 + 3 * c
                        sc.op('dve', lambda e: e.tensor_scalar(t1[:], u[:, 30:542], cv[:, wc0:wc0 + 1], None, ALU.mult), r=[uk, 'cv'], w=['t1'])
                        for j in (1, 2):
                            sc.op('dve', lambda e, j=j: e.scalar_tensor_tensor(t1[:], u[:, 30 + j:542 + j], cv[:, wc0 + j:wc0 + j + 1], t1[:], ALU.mult, ALU.add),
                                  r=[uk, 'cv', 't1'], w=['t1'])
                        pbb, pbbk = fm(1280 + 128 * c)
                        o, ok = nob()
                        sc.op('dve', lambda e: e.tensor_tensor(o[:], pbb[:, :], t1[:], ALU.mult), r=[pbbk, 't1'], w=[ok])
                        sc.dma('sp', MIXT[512 + 128 * c:640 + 128 * c, t0:t0 + 512], o[:], r=[ok], w=[('MIXT', tb)])
                    for c in range(2):
                        pa, pak = fm(2048 + 128 * c)
                        o, ok = nob()
                        sc.op('act', lambda e: e.mul(o[:], pa[:, :], 0.125), r=[pak], w=[ok])
                        sc.dma('sp', FQ[128 * c:128 * c + 128, t0:t0 + 512], o[:], r=[ok], w=[('FQ', tb)])
                        pa, pak = fm(2304 + 128 * c)
                        o, ok = nob()
                        sc.op('act', lambda e: e.copy(o[:], pa[:, :]), r=[pak], w=[ok])
                        sc.dma('sp', FK[128 * c:128 * c + 128, t0:t0 + 512], o[:], r=[ok], w=[('FK', tb)])
                    pa, pak = fm(2816, ncols=4)
                    sc.op('act', lambda e: e.copy(ff_t[:], pa[0:4, :]), r=[pak], w=['ff_t'])
                    sc.dma('sp', FF[:, t0:t0 + 512], ff_t[:], r=['ff_t'], w=[('FF', tb)])
                    for sub in range(4):
                        for (c0, dst, dk) in ((256, RV, 'RV'), (2560, FV, 'FV')):
                            pm, pmk = bank()
                            for kc in range(8):
                                mm(pm[:, 0:256], x_[:, kc, 32 + 128 * sub:160 + 128 * sub], win[:, kc, c0:c0 + 256], kc == 0, kc == 7, r=[WK[kc], xk], w=[pmk])
                            vtc[0] ^= 1
                            v = vt[vtc[0]]
                            vk = f'vt{vtc[0]}'
                            sc.op('act', lambda e: e.copy(v[:], pm[:, 0:256]), r=[pmk], w=[vk])
                            sc.dma('sp', dst[t0 + 128 * sub:t0 + 128 * sub + 128, :], v[:], r=[vk], w=[(dk, tb)])
                sc.barrier()
                if upto == 1:
                    stop[0] = True

            with ExitStack() as st2:
              for _once in ([0] if not stop[0] else []):
                rq_t = sb("rq_t", [32, S], BF16, st2)
                rk_t = sb("rk_t", [32, S], BF16, st2)
                qd_t = sb("qd_t", [32, S], BF16, st2)
                v_t = sb("v_t", [128, NT, 64], BF16, st2)
                v64 = sb("v64", [64, NCH, 64], BF16, st2)
                qdec = sb("qdec", [32, 512], F32, st2)
                stt = sb("stt", [32, 64], F32, st2)
                prevb = sb("prevb", [32, NCH, 64], BF16, st2)
                kd = [sb(f"kd{i}", [128, 32], BF16, st2) for i in range(2)]
                sd = [sb(f"sd{i}", [128, 128], BF16, st2) for i in range(2)]
                o_sb = sb("o_sb", [64, 512], F32, st2)
                sq2 = sb("sq2", [64, 512], F32, st2)
                mean2 = sb("mean2", [64, 512], F32, st2)
                rstd2 = sb("rstd2", [64, 512], F32, st2)
                g_t = sb("g_t", [64, 512], BF16, st2)
                ro = [sb(f"ro{i}", [64, 512], BF16, st2) for i in range(2)]
                for h in range(4):
                    gam = 1.0 - 2.0 ** (-5.0 - h)
                    cd = gam ** CHUNK
                    sc.dma('sp', rq_t[:], RQ[32 * h:32 * h + 32, :], w=['rq_t'])
                    sc.dma('sp', rk_t[:], RK[32 * h:32 * h + 32, :], w=['rk_t'])
                    sc.dma('sp', v_t[:], RV[:, 64 * h:64 * h + 64].rearrange("(j p) e -> p j e", p=128), w=['v_t'])
                    sc.dma('sp', qdec[:], cst_d[32 * h:32 * h + 32, 512:1024], w=['qdec'])
                    for tb in range(NB):
                        sc.op('dve', lambda e, tb=tb: e.tensor_tensor(qd_t[:, tb * 512:tb * 512 + 512], rq_t[:, tb * 512:tb * 512 + 512], qdec[:], ALU.mult),
                              r=['rq_t', 'qdec'], w=['qd_t'])
                    sc.op('dve', lambda e: e.memset(stt[:], 0.0), w=['stt'])
                    sc.dma('sp', v64[:], RV[:, 64 * h:64 * h + 64].rearrange("(c p) e -> p c e", p=64), w=['v64'])
                    for c in range(NCH):
                        sc.op('pe', lambda e, c=c: e.transpose(PSB[0:64, 0:32], rk_t[:, 64 * c:64 * c + 64], identb[0:32, 0:32]),
                              r=['rk_t', 'identb'], w=['psb'])
                        k_ = kd[c % 2]
                        kk = f'kd{c % 2}'
                        sc.op('dve', lambda e: e.tensor_scalar(k_[0:64, :], PSB[0:64, 0:32], cst[0:64, 1024 + h:1025 + h], None, ALU.mult), r=['psb', 'cst'], w=[kk])
                        pkv, pkvk = bank()
                        mm(pkv[0:32, 0:64], k_[0:64, :], v64[:, c, :], True, True, r=[kk, 'v64'], w=[pkvk])
                        sc.op('dve', lambda e, c=c: e.tensor_copy(prevb[:, c, :], stt[:]), r=['stt'], w=['prevb'])
                        sc.op('dve', lambda e: e.scalar_tensor_tensor(stt[:], stt[:], cd, pkv[0:32, 0:64], ALU.mult, ALU.add),
                              r=['stt', pkvk], w=['stt'])
                    for tb in range(NB if KN >= 5 else 0):
                        t0 = tb * 512
                        po, pok = bank()
                        for jj in range(4):
                            j = tb * 4 + jj
                            ps_, psk = bank()
                            mm(ps_[:, 0:128], rk_t[:, 128 * j:128 * j + 128], rq_t[:, 128 * j:128 * j + 128], True, True, r=['rk_t', 'rq_t'], w=[psk])
                            s_ = sd[j % 2]
                            sk = f'sd{j % 2}'
                            sc.op('dve', lambda e: e.tensor_tensor(s_[:], ps_[:, 0:128], cst[:, 128 * h:128 * h + 128], ALU.mult), r=[psk, 'cst'], w=[sk])
                            mm(po[0:64, 128 * jj:128 * jj + 128], v_t[:, j, :], s_[:], True, False, r=['v_t', sk], w=[pok], inc=False)
                            for hf in range(2):
                                c0 = 128 * jj + 64 * hf
                                mm(po[0:64, c0:c0 + 64], prevb[:, 2 * j + hf, :], qd_t[:, 128 * j + 64 * hf:128 * j + 64 * hf + 64], False, hf == 1,
                                   r=['prevb', 'qd_t'], w=[pok], inc=(hf == 1))
                        sc.op('act', lambda e: e.copy(o_sb[:], po[0:64, :]), r=[pok], w=['o_sb'])
                        sc.op('act', lambda e: e.activation(sq2[:], po[0:64, :], AF.Square), r=[pok], w=['sq2'])
                        pm, pmk = bank()
                        mm(pm[0:64, :], ones64[:], o_sb[:], True, True, r=['ones64', 'o_sb'], w=[pmk])
                        pe2, pe2k = bank()
                        mm(pe2[0:64, :], ones64[:], sq2[:], True, True, r=['ones64', 'sq2'], w=[pe2k])
                        sc.op('act', lambda e: e.copy(mean2[:], pm[0:64, :]), r=[pmk], w=['mean2'])
                        sc.op('dve', lambda e: e.tensor_tensor(rstd2[:], mean2[:], mean2[:], ALU.mult), r=['mean2'], w=['rstd2'])
                        sc.op('dve', lambda e: e.tensor_tensor(rstd2[:], pe2[0:64, :], rstd2[:], ALU.subtract), r=[pe2k, 'rstd2'], w=['rstd2'])
                        sc.op('dve', lambda e: e.tensor_scalar(rstd2[:], rstd2[:], 0.0, EPS, ALU.max, ALU.add), r=['rstd2'], w=['rstd2'])
                        sc.op('act', lambda e: e.activation(rstd2[:], rstd2[:], AF.Ln), r=['rstd2'], w=['rstd2'])
                        sc.op('act', lambda e: e.activation(rstd2[:], rstd2[:], AF.Exp, scale=-0.5), r=['rstd2'], w=['rstd2'])
                        sc.dma('sp', g_t[:], GATE[64 * h:64 * h + 64, t0:t0 + 512], w=['g_t'])
                        sc.op('dve', lambda e: e.tensor_tensor(o_sb[:], o_sb[:], mean2[:], ALU.subtract), r=['o_sb', 'mean2'], w=['o_sb'])
                        sc.op('dve', lambda e: e.tensor_tensor(o_sb[:], o_sb[:], rstd2[:], ALU.mult), r=['o_sb', 'rstd2'], w=['o_sb'])
                        sc.op('dve', lambda e: e.tensor_scalar(o_sb[:], o_sb[:], cv[0:64, 74 + h:75 + h], None, ALU.mult), r=['o_sb', 'cv'], w=['o_sb'])
                        r_ = ro[tb % 2]
                        rk_ = f'ro{tb % 2}'
                        sc.op('dve', lambda e: e.tensor_tensor(r_[:], o_sb[:], g_t[:], ALU.mult), r=['o_sb', 'g_t'], w=[rk_])
                        sc.dma('sp', MIXT[64 * h:64 * h + 64, t0:t0 + 512], r_[:], r=[rk_], w=[('MIXT2', h, tb)])
                sc.barrier()
                if upto == 2:
                    stop[0] = True

            with ExitStack() as st3:
              for _once in ([0] if not stop[0] else []):
                qa = sb("qa", [70, S], BF16, st3)
                ka = sb("ka", [70, S], BF16, st3)
                fv_t = sb("fv_t", [128, NT, 64], BF16, st3)
                msk = sb("msk", [128, 4 * 512], F32, st3)
                sc.dma('sp', msk[:], msk_d[:, :], w=['msk'])
                fb = sb("fb", [1, 4], F32, st3)
                sc.dma('sp', fb[:], foxb_d[l, :, :], w=['fb'])
                sc.op('dve', lambda e: e.tensor_scalar(fb[:], fb[:], -1.0, None, ALU.mult), r=['fb'], w=['fb'])
                FW = min(S, 2048)
                f_r = sb("f_r", [1, FW], F32, st3)
                F_r = sb("F_r", [1, FW], F32, st3)
                one_r = sb("one_r", [1, FW], F32, st3)
                sc.op('pool', lambda e: e.memset(one_r[:], 1.0), w=['one_r'])
                fr = sb("fr", [1, 7, FW], BF16, st3)
                r1 = sb("r1", [1, FW], F32, st3)
                r2 = sb("r2", [1, FW], F32, st3)
                Fl = sb("Fl", [1, 1], F32, st3)
                pt = [sb(f"pt{i}", [128, 512], BF16, st3) for i in range(3)]
                rden = sb("rden", [64, 512], F32, st3)
                fo = [sb(f"fo{i}", [64, 512], BF16, st3) for i in range(2)]
                for h in range(4):
                    sc.op('dve', lambda e: e.memset(Fl[:], 0.0), w=['Fl'])
                    for pc in range(S // FW):
                        sl = slice(pc * FW, pc * FW + FW)
                        sc.dma('sp', f_r[:], FF[h:h + 1, sl], w=['f_r'])
                        sc.op('act', lambda e: e.activation(f_r[:], f_r[:], AF.Exp, bias=fb[0:1, h:h + 1], scale=-1.0), r=['f_r', 'fb'], w=['f_r'])
                        sc.op('act', lambda e: e.activation(f_r[:], f_r[:], AF.Ln, bias=1.0), r=['f_r'], w=['f_r'])
                        sc.op('dve', lambda e: e.tensor_scalar(f_r[:], f_r[:], -1.0, None, ALU.mult), r=['f_r'], w=['f_r'])
                        sc.op('dve', lambda e: e.tensor_tensor_scan(F_r[:], one_r[:], f_r[:], Fl[0:1, 0:1], ALU.mult, ALU.add), r=['one_r', 'f_r', 'Fl'], w=['F_r'])
                        sc.op('dve', lambda e: e.tensor_copy(Fl[:], F_r[0:1, FW - 1:FW]), r=['F_r'], w=['Fl'])
                        sc.op('dve', lambda e: e.tensor_copy(fr[:, 0, :], F_r[:]), r=['F_r'], w=['fr'])
                        sc.op('dve', lambda e: e.tensor_tensor(r1[:], F_r[:], fr[:, 0, :], ALU.subtract), r=['F_r', 'fr'], w=['r1'])
                        sc.op('dve', lambda e: e.tensor_copy(fr[:, 1, :], r1[:]), r=['r1', 'fr'], w=['fr'])
                        sc.op('dve', lambda e: e.tensor_tensor(r2[:], r1[:], fr[:, 1, :], ALU.subtract), r=['r1', 'fr'], w=['r2'])
                        sc.op('dve', lambda e: e.tensor_copy(fr[:, 2, :], r2[:]), r=['r2', 'fr'], w=['fr'])
                        sc.op('dve', lambda e: e.tensor_copy(fr[:, 3, :], one_r[:]), r=['one_r', 'fr'], w=['fr'])
                        for i in range(3):
                            sc.op('dve', lambda e, i=i: e.tensor_scalar(fr[:, 4 + i, :], fr[:, i, :], -1.0, None, ALU.mult), r=['fr'], w=['fr'])
                        sc.dma('sp', FRD[:, sl].rearrange("(o r) t -> o r t", o=1), fr[:], r=['fr'], w=[('FRD', pc)])
                    FRk = [('FRD', pc) for pc in range(S // FW)]
                    sc.dma('sp', qa[0:64, :], FQ[64 * h:64 * h + 64, :], w=['qa'])
                    sc.dma('sp', qa[64:67, :], FRD[0:3, :], r=FRk + ['qa'], w=['qa'])
                    for i in range(3):
                        sc.dma('sp', qa[67 + i:68 + i, :], FRD[3:4, :], r=FRk + ['qa'], w=['qa'])
                    sc.dma('sp', ka[0:64, :], FK[64 * h:64 * h + 64, :], w=['ka'])
                    for i in range(3):
                        sc.dma('sp', ka[64 + i:65 + i, :], FRD[3:4, :], r=FRk + ['ka'], w=['ka'])
                    sc.dma('sp', ka[67:70, :], FRD[4:7, :], r=FRk + ['ka'], w=['ka'])
                    sc.dma('sp', fv_t[:], FV[:, 64 * h:64 * h + 64].rearrange("(j p) e -> p j e", p=128), w=['fv_t'])
                    pi = 0
                    for qb in range(NB):
                        q0 = qb * 512
                        pn, pnk = PS[5], 'ps5'
                        pd, pdk = PS[6], 'ps6'
                        nk = 4 * qb + 4
                        def score(j):
                            mm(PS[j % 4][:, :], ka[:, 128 * j:128 * j + 128], qa[:, q0:q0 + 512], True, True, r=['ka', 'qa'], w=[f'ps{j % 4}'])

                        score(0)
                        for j in range(nk):
                            if j + 1 < nk:
                                score(j + 1)
                            ps_ = PS[j % 4]
                            psk = f'ps{j % 4}'
                            p_ = pt[pi % 3]
                            pk = f'pt{pi % 3}'
                            pi += 1
                            sc.op('act', lambda e: e.activation(p_[:], ps_[:, :], AF.Exp), r=[psk], w=[pk])
                            if j >= 4 * qb:
                                m = j - 4 * qb
                                sc.op('dve', lambda e: e.tensor_tensor(p_[:], p_[:], msk[:, 512 * m:512 * m + 512], ALU.mult), r=[pk, 'msk'], w=[pk])
                            mm(pn[0:64, :], fv_t[:, j, :], p_[:], j == 0, j == nk - 1, r=['fv_t', pk], w=[pnk], inc=False)
                            mm(pd[0:64, :], onesb[:], p_[:], j == 0, j == nk - 1, r=['onesb', pk], w=[pdk], inc=True)
                        sc.op('dve', lambda e: e.reciprocal(rden[:], pd[0:64, :]), r=[pdk], w=['rden'])
                        f_ = fo[qb % 2]
                        fk_ = f'fo{qb % 2}'
                        sc.op('dve', lambda e: e.tensor_tensor(f_[:], pn[0:64, :], rden[:], ALU.mult), r=[pnk, 'rden'], w=[fk_])
                        sc.dma('sp', MIXT[768 + 64 * h:832 + 64 * h, q0:q0 + 512], f_[:], r=[fk_], w=[('MIXT3', h, qb)])
                sc.barrier()
                if upto == 3:
                    stop[0] = True

            with ExitStack() as st4:
              for _once in ([0] if not stop[0] else []):
                wo = sb("wo", [128, 8, D], BF16, st4)
                for kc in range(8):
                    sc.dma('pool', wo[:, kc, :], w_o_d[l, kc * 128:(kc + 1) * 128, :], w=[('wo', kc)])
                rwt = sb("rwt", [128, 8, NE], F32, st4)
                sc.dma('sp', rwt[:], rw_d[l].rearrange("(kc p) e -> p kc e", p=128), w=['rwt'])
                rbt = sb("rbt", [128, NE], F32, st4)
                sc.dma('sp', rbt[:], rb_d[l, 0:1, :].partition_broadcast(128), w=['rbt'])
                mx = [sb(f"mx{i}", [128, 8, 512], BF16, st4) for i in range(2)]
                xr = [sb(f"xr{i}", [128, 8, 512], F32, st4) for i in range(2)]
                z = sb("z", [128, 8, 512], F32, st4)
                stl = dict(sq=[sb(f"sq4{i}", [128, 512], F32, st4) for i in range(2)], sqk='s4', mean=sb("mean", [128, 512], F32, st4),
                           rstd=sb("rstd", [128, 512], F32, st4), tmp=sb("tmp", [128, 512], F32, st4))
                x1f = z
                x1h = sb("x1h", [128, 8, 512], BF16, st4)
                lg = sb("lg", [128, NE], F32, st4)
                m8 = sb("m8", [128, 8], F32, st4)
                nmx = sb("nmx", [128, 1], F32, st4)
                mk = sb("mk", [128, NE], F32, st4)
                ex = sb("ex", [128, NE], F32, st4)
                ssum = sb("ssum", [128, 1], F32, st4)
                gtt = sb("gtt", [NE, 512], F32, st4)
                for tb in range(NB):
                    t0 = tb * 512
                    p = tb % 2
                    sl = slice(t0, t0 + 512)
                    sc.dma('sp', mx[p][:], chunked(MIXT)[:, :, sl], w=[f'mx{p}'])
                    sc.dma('sp', xr[p][:], chunked(XT)[:, :, sl], w=[f'xr{p}'])
                    for dc in range(8):
                        pm, pmk = bank()
                        for kc in range(8):
                            mm(pm[:, :], wo[:, kc, 128 * dc:128 * dc + 128], mx[p][:, kc, :], kc == 0, kc == 7, r=[('wo', kc), f'mx{p}'], w=[pmk])
                        sc.op('dve', lambda e, dc=dc: e.scalar_tensor_tensor(z[:, dc, :], xr[p][:, dc, :], ALPHA, pm[:, :], ALU.mult, ALU.add),
                              r=[f'xr{p}', pmk], w=['z'])
                    ln_block(z, 'z', 78, 86, stl)
                    sc.op('dve', lambda e: e.tensor_copy(x1h[:], z[:]), r=['z'], w=['x1h'])
                    sc.dma('sp', chunked(X1T)[:, :, sl], x1f[:], r=['z'], w=[('X1T', tb)])
                    sc.dma('sp', chunked(X1B)[:, :, sl], x1h[:], r=['x1h'], w=[('X1B', tb)])
                    for sub in range(4):
                        pm, pmk = bank()
                        for kc in range(8):
                            mm(pm[:, 0:NE], x1f[:, kc, 128 * sub:128 * sub + 128], rwt[:, kc, :], kc == 0, kc == 7, r=['z', 'rwt'], w=[pmk])
                        sc.op('dve', lambda e: e.tensor_tensor(lg[:], pm[:, 0:NE], rbt[:], ALU.add), r=[pmk, 'rbt'], w=['lg'])
                        sc.op('dve', lambda e: e.max(m8[:], lg[:]), r=['lg'], w=['m8'])
                        sc.op('dve', lambda e: e.tensor_scalar(mk[:], lg[:], m8[:, 3:4], None, ALU.is_ge), r=['lg', 'm8'], w=['mk'])
                        sc.op('dve', lambda e: e.tensor_scalar(nmx[:], m8[:, 0:1], -1.0, None, ALU.mult), r=['m8'], w=['nmx'])
                        sc.op('act', lambda e: e.activation(ex[:], lg[:], AF.Exp, bias=nmx[:, 0:1], scale=1.0), r=['lg', 'nmx'], w=['ex'])
                        sc.op('dve', lambda e: e.tensor_tensor(ex[:], ex[:], mk[:], ALU.mult), r=['ex', 'mk'], w=['ex'])
                        sc.op('dve', lambda e: e.tensor_reduce(ssum[:], ex[:], AX.X, ALU.add), r=['ex'], w=['ssum'])
                        sc.op('dve', lambda e: e.reciprocal(ssum[:], ssum[:]), r=['ssum'], w=['ssum'])
                        sc.op('dve', lambda e: e.tensor_scalar(ex[:], ex[:], ssum[:, 0:1], None, ALU.mult), r=['ex', 'ssum'], w=['ex'])
                        pg, pgk = bank()
                        mm(pg[0:NE, 0:128], ex[:], ident, True, True, r=['ex', 'cst'], w=[pgk])
                        sc.op('act', lambda e, sub=sub: e.copy(gtt[:, 128 * sub:128 * sub + 128], pg[0:NE, 0:128]), r=[pgk], w=['gtt'])
                    sc.dma('sp', GT[:, sl], gtt[:], r=['gtt'], w=[('GT', tb)])
                sc.barrier()
                if upto == 4:
                    stop[0] = True

            with ExitStack() as st5:
              for _once in ([0] if not stop[0] else []):
                NBS = SBK // 512
                acc = sb("acc", [128, 8, SBK], F32, st5)
                xs = sb("xs", [128, 8, SBK], BF16, st5)
                w1t = [sb(f"w1t{i}", [128, 8, 2048], BF16, st5) for i in range(2)]
                w2t = [sb(f"w2t{i}", [128, 8, D], BF16, st5) for i in range(2)]
                gb = [sb(f"gb{i}", [128, SBK], F32, st5) for i in range(2)]
                b1t = sb("b1t", [128, NE * 16], F32, st5)
                sc.dma('sp', b1t[:], b1_d[l, :, :], w=['b1t'])
                b2t = sb("b2t", [NE, D], F32, st5)
                sc.dma('sp', b2t[:], b2_d[l, :, :], w=['b2t'])
                gts = sb("gts", [NE, SBK], F32, st5)
                actt = [sb(f"actt{i}", [128, 8, 512], BF16, st5) for i in range(2)]
                blk = [0]
                ga = [sb(f"ga{i}", [128, 512], F32, st5) for i in range(3)]
                sgm = [sb(f"sgm{i}", [128, 512], F32, st5) for i in range(3)]
                li = [sb(f"li{i}", [128, 512], F32, st5) for i in range(3)]
                b1p = sb("b1p", [128, NE * 16], F32, st5)
                sc.op('dve', lambda e: e.tensor_scalar(b1p[:], b1t[:], 1.0, None, ALU.add), r=['b1t'], w=['b1p'])
                hb5 = [sb(f"hb5{i}", [128, 512], BF16, st5) for i in range(2)]
                xr5 = [li[0], li[1]]
                stl = dict(sq=[ga[0], ga[1]], sqk='ga', mean=sgm[0], rstd=sgm[1], tmp=sgm[2], keys=('sgm0', 'sgm1', 'sgm2'))
                wn = 0
                for sbi in range(NSB):
                    s0 = sbi * SBK
                    sc.dma('sp', xs[:], chunked(X1B)[:, :, s0:s0 + SBK], w=['xs'])
                    sc.dma('sp', gts[:], GT[0:NE, s0:s0 + SBK], w=['gts'])
                    for tb in range(NBS):
                        for dc in range(8):
                            pm, pmk = bank()
                            mm(pm[:, :], b2t[:, 128 * dc:128 * dc + 128], gts[:, 512 * tb:512 * tb + 512], True, True, r=['b2t', 'gts'], w=[pmk])
                            sc.op('act', lambda e, dc=dc, tb=tb: e.copy(acc[:, dc, 512 * tb:512 * tb + 512], pm[:, :]), r=[pmk], w=['acc'])
                    for ex_ in range(NE):
                        wp = wn % 2
                        wn += 1
                        for kc in range(8 if (KW or wn <= 2) else 0):
                            sc.dma('pool', w1t[wp][:, kc, :], w1_d[l, ex_, kc * 128:(kc + 1) * 128, :], w=[(f'w1t{wp}', kc)])
                        for kc in range(8 if (KW or wn <= 2) else 0):
                            sc.dma('pool', w2t[wp][:, kc, :], w2_d[l, ex_, kc * 128:(kc + 1) * 128, :], w=[(f'w2t{wp}', kc)])
                        sc.dma('sp', gb[wp][:], GT[ex_:ex_ + 1, s0:s0 + SBK].partition_broadcast(128), w=[f'gb{wp}'])
                        for tb in range(NBS):
                            ab = actt[blk[0] % 2]
                            abk = f'actt{blk[0] % 2}'
                            blk[0] += 1
                            gsl = gb[wp][:, 512 * tb:512 * tb + 512]

                            def X(fc):
                                pg, pgk = bank()
                                for kc in range(8):
                                    mm(pg[:, :], w1t[wp][:, kc, 128 * fc:128 * fc + 128], xs[:, kc, 512 * tb:512 * tb + 512], kc == 0, kc == 7,
                                       r=[(f'w1t{wp}', kc), 'xs'], w=[pgk])
                                pl, plk = bank()
                                for kc in range(8):
                                    mm(pl[:, :], w1t[wp][:, kc, 1024 + 128 * fc:1152 + 128 * fc], xs[:, kc, 512 * tb:512 * tb + 512], kc == 0, kc == 7,
                                       r=[(f'w1t{wp}', kc), 'xs'], w=[plk])
                                q = fc % 3
                                bg = b1t[:, ex_ * 16 + fc:ex_ * 16 + fc + 1]
                                bl = b1p[:, ex_ * 16 + 8 + fc:ex_ * 16 + 8 + fc + 1]
                                sc.op('dve', lambda e: e.tensor_scalar(ga[q][:], pg[:, :], bg, 7.0, ALU.add, ALU.min), r=[pgk, 'b1t'], w=[f'ga{q}'])
                                sc.op('act', lambda e: e.activation(sgm[q][:], ga[q][:], AF.Sigmoid, scale=1.702), r=[f'ga{q}'], w=[f'sgm{q}'])
                                sc.op('dve', lambda e: e.tensor_scalar(li[q][:], pl[:, :], bl, -6.0, ALU.add, ALU.max), r=[plk, 'b1p'], w=[f'li{q}'])

                            def Y(fc):
                                q = fc % 3
                                sc.op('dve', lambda e: e.tensor_tensor(ga[q][:], ga[q][:], sgm[q][:], ALU.mult), r=[f'ga{q}', f'sgm{q}'], w=[f'ga{q}'])
                                sc.op('dve', lambda e: e.scalar_tensor_tensor(ga[q][:], li[q][:], 8.0, ga[q][:], ALU.min, ALU.mult), r=[f'ga{q}', f'li{q}'], w=[f'ga{q}'])
                                sc.op('dve', lambda e: e.tensor_tensor(ab[:, fc, :], ga[q][:], gsl, ALU.mult), r=[f'ga{q}', f'gb{wp}'], w=[(abk, fc)])

                            X(0)
                            X(1)
                            for fc in range(8):
                                if fc + 2 < 8:
                                    X(fc + 2)
                                Y(fc)
                            for dc in range(8):
                                pm, pmk = bank()
                                for fc in range(8):
                                    mm(pm[:, :], w2t[wp][:, fc, 128 * dc:128 * dc + 128], ab[:, fc, :], fc == 0, fc == 7, r=[(f'w2t{wp}', fc), (abk, fc)], w=[pmk])
                                sc.op('dve', lambda e, dc=dc, tb=tb: e.tensor_tensor(acc[:, dc, 512 * tb:512 * tb + 512], acc[:, dc, 512 * tb:512 * tb + 512], pm[:, :], ALU.add),
                                      r=['acc', pmk], w=['acc'])
                    for tb in range(NBS):
                        gtb = sbi * NBS + tb
                        sl = slice(gtb * 512, gtb * 512 + 512)
                        zs = acc[:, :, 512 * tb:512 * tb + 512]
                        for dc in range(8):
                            xr_ = xr5[dc % 2]
                            xk_ = f'li{dc % 2}'
                            sc.dma('sp', xr_[:], X1T[128 * dc:128 * dc + 128, sl], r=[('X1T', gtb)], w=[xk_])
                            sc.op('dve', lambda e, dc=dc: e.scalar_tensor_tensor(zs[:, dc, :], xr_[:], ALPHA, zs[:, dc, :], ALU.mult, ALU.add),
                                  r=[xk_, 'acc'], w=['acc'])
                        ln_block(zs, 'acc', 94, 102, stl)
                        if last:
                            sc.dma('sp', chunked(outT)[:, :, sl], zs, r=['acc'], w=[('outT', gtb)])
                        else:
                            sc.dma('sp', chunked(XT)[:, :, sl], zs, r=['acc'], w=[('XT', gtb)])
                            for dc in range(8):
                                hb_ = hb5[dc % 2]
                                hk_ = f'hb5{dc % 2}'
                                sc.op('act', lambda e, dc=dc: e.copy(hb_[:], zs[:, dc, :]), r=['acc'], w=[hk_])
                                sc.dma('sp', XB[128 * dc:128 * dc + 128, sl], hb_[:], r=[hk_], w=[('XB', gtb, dc)])
                sc.barrier()
                if upto == 5:
                    stop[0] = True
        except _Stop:
            pass
    return nc


def host_consts(S):
    half = 16
    freqs = (10000.0 ** (-np.arange(half, dtype=np.float32) / half)).astype(np.float32)
    pos = np.arange(S, dtype=np.float32)
    ang = pos[None, :] * freqs[:, None]
    cos = np.cos(ang).astype(np.float32)
    sin = np.sin(ang).astype(np.float32)
    ropec = np.tile(np.concatenate([cos, cos], 0), (4, 1))
    ropes = np.tile(np.concatenate([-sin, sin], 0), (4, 1))
    cst = np.zeros((128, 4 * 128 + 512 + 4 + 128), np.float32)
    idx = np.arange(128)
    same = (idx[:, None] // 64) == (idx[None, :] // 64)
    scale = 32 ** -0.5
    for h in range(4):
        g = 1.0 - 2.0 ** (-5.0 - h)
        cst[:, 128 * h:128 * h + 128] = np.where(same, g ** np.abs(idx[:, None] - idx[None, :]), 0.0) * scale
        cst[32 * h:32 * h + 32, 512:1024] = (g ** ((np.arange(512) % 64) + 1.0))[None, :]
        cst[:, 1024 + h] = g ** (63 - (idx % 64)) * scale
    cst[:, 1028:1156] = np.eye(128, dtype=np.float32)
    msk = np.zeros((128, 4 * 512), np.float32)
    s_ = np.arange(128)[:, None]
    t_ = np.arange(512)[None, :]
    for m in range(4):
        msk[:, 512 * m:512 * m + 512] = (t_ >= 128 * m + s_)
    return dict(ropec=np.ascontiguousarray(ropec), ropes=np.ascontiguousarray(ropes), cst=cst, msk=msk)


def host_layout(inp, S, DEPTH, NE):
    L = DEPTH
    perm = np.concatenate([np.arange(16, 32), np.arange(0, 16)])
    pq = np.concatenate([h * 32 + perm for h in range(4)])
    w_in = np.asarray(inp['w_in'])
    w_aug = np.concatenate([w_in, w_in[:, :, pq], w_in[:, :, 128 + pq]], axis=2)
    cv = np.zeros((L, 128, NCV), np.float32)
    cdw = np.asarray(inp['conf_dw'])
    sdw = np.asarray(inp['sc_dw'])
    for c in range(2):
        cv[:, :, 31 * c:31 * c + 31] = cdw[:, :, 128 * c:128 * c + 128].transpose(0, 2, 1)
        cv[:, :, 62 + 3 * c:65 + 3 * c] = sdw[:, :, 128 * c:128 * c + 128].transpose(0, 2, 1)
        cv[:, :, 68 + c] = np.asarray(inp['conf_dw_b'])[:, 128 * c:128 * c + 128]
        cv[:, :, 70 + c] = np.asarray(inp['conf_ln_g'])[:, 128 * c:128 * c + 128]
        cv[:, :, 72 + c] = np.asarray(inp['conf_ln_b'])[:, 128 * c:128 * c + 128]
    cv[:, 0:64, 74:78] = np.asarray(inp['ret_gn_g']).reshape(L, 4, 64).transpose(0, 2, 1)
    for name, c0 in (('ln1_g', 78), ('ln1_b', 86), ('ln2_g', 94), ('ln2_b', 102)):
        cv[:, :, c0:c0 + 8] = np.asarray(inp[name]).reshape(L, 8, 128).transpose(0, 2, 1)
    b1T = np.ascontiguousarray(np.asarray(inp['b1']).reshape(L, NE, 16, 128).transpose(0, 3, 1, 2).reshape(L, 128, NE * 16))
    rw = np.asarray(inp['router_w'])
    rb = np.asarray(inp['router_b'])
    common = dict(w_in=np.ascontiguousarray(w_aug), w_o=np.asarray(inp['w_o']), cvec=cv,
                  foxb=np.asarray(inp['fox_b_f']).reshape(L, 1, 4), rw=rw, rb=rb.reshape(L, 1, -1),
                  w1=np.asarray(inp['w1']), b1T=b1T, w2=np.asarray(inp['w2']), b2=np.asarray(inp['b2']))
    common.update(host_consts(S))
    return common


def run(inp, S, DEPTH, NE, SBK, dbg=None, upto=99, trace=False):
    x = np.asarray(inp['x'])
    B = x.shape[0]
    common = host_layout(inp, S, DEPTH, NE)
    nc = build(S, DEPTH, NE, SBK, dbg, upto)
    in_maps = []
    for b in range(B):
        m = dict(common)
        m['xT'] = np.ascontiguousarray(x[b].T)
        in_maps.append(m)
    res = run_bass_kernel_spmd(nc, in_maps, core_ids=list(range(B)), **({'trace': True} if trace else {}))
    if trace:
        print('EXEC_NS', res.exec_time_ns)
    out = np.stack([np.ascontiguousarray(r['outT'].T) for r in res.results], 0).astype(np.float32)
    if dbg is not None:
        return out, res.results
    return out


def kernel(**inputs):
    return run(inputs, 8192, 4, 32, 1024)
```

```python
import math
import os
KN = int(os.environ.get('KN', '99'))
KW = int(os.environ.get('KW', '1'))
from contextlib import ExitStack
import numpy as np
import concourse.bass as bass
import concourse.mybir as mybir
from concourse.bass_utils import run_bass_kernel_spmd

F32 = mybir.dt.float32
BF16 = mybir.dt.bfloat16
ALU = mybir.AluOpType
AF = mybir.ActivationFunctionType
AX = mybir.AxisListType

D = 1024
CHUNK = 64
ALPHA = 8 ** 0.25
EPS = 1e-5
NCV = 110
WA = 2820 + 256


class Sched:
    def __init__(s, nc, es):
        s.nc = nc
        s.eng = {'pe': nc.tensor, 'act': nc.scalar, 'dve': nc.vector, 'pool': nc.gpsimd, 'sp': nc.sync}
        s.sem = {k: es.enter_context(nc.semaphore('s_' + k)) for k in ('pe', 'act', 'dve', 'pool')}
        s.cnt = {k: 0 for k in s.sem}
        s.seen = {k: {} for k in s.eng}
        s.dq = {}
        for q in ('sp', 'pool', 'act'):
            s.dq[q] = dict(sems=[es.enter_context(nc.semaphore(f'd_{q}{i}')) for i in range(8)], n=0)
        s.res = {}

    def _need(s, en, deps):
        best = {}
        for (k, h, v) in deps:
            if v > best.get(k, (None, 0))[1]:
                best[k] = (h, v)
        for k, (h, v) in best.items():
            if s.seen[en].get(k, 0) < v:
                s.eng[en].wait_ge(h, v)
                s.seen[en][k] = v

    def _collect(s, en, r, w):
        deps = []
        for k in r:
            st = s.res.get(k)
            if st and st[0]:
                deps.append(st[0])
        for k in w:
            st = s.res.get(k)
            if st:
                if st[0]:
                    deps.append(st[0])
                deps.extend(st[1].values())
        if en == 'pe':
            deps = [d for d in deps if d[0] != 'pe']
        return deps

    def _record(s, dep, r, w):
        for k in r:
            st = s.res.setdefault(k, [None, {}])
            o = st[1].get(dep[0])
            if o is None or o[2] < dep[2]:
                st[1][dep[0]] = dep
        for k in w:
            s.res[k] = [dep, {}]

    def op(s, en, fn, r=(), w=(), inc=True):
        assert inc or en == 'pe'
        s._need(en, s._collect(en, r, w))
        ins = fn(s.eng[en])
        if inc:
            s.cnt[en] += 1
            ins.then_inc(s.sem[en], 1)
            dep = (en, s.sem[en], s.cnt[en])
        else:
            dep = (en, s.sem[en], s.cnt[en] + 1)
        s._record(dep, r, w)

    def dma(s, q, out, in_, r=(), w=(), **kw):
        Q = s.dq[q]
        i = Q['n']
        Q['n'] += 1
        K = len(Q['sems'])
        h = Q['sems'][i % K]
        key = f'd_{q}{i % K}'
        deps = s._collect(q, r, w)
        if i >= K:
            deps.append((key, h, 16 * (i // K)))
        s._need(q, deps)
        s.eng[q].dma_start(out=out, in_=in_, **kw).then_inc(h, 16)
        s._record((key, h, 16 * (i // K + 1)), r, w)

    def alldeps(s):
        deps = [(k, s.sem[k], s.cnt[k]) for k in s.sem if s.cnt[k] > 0]
        for q, Q in s.dq.items():
            K = len(Q['sems'])
            for j in range(min(K, Q['n'])):
                tot = (Q['n'] - 1 - j) // K + 1
                deps.append((f'd_{q}{j}', Q['sems'][j], 16 * tot))
        return deps

    def barrier(s):
        deps = s.alldeps()
        for en in s.eng:
            s._need(en, [d for d in deps if not (en == 'pe' and d[0] == 'pe')])
        s.res = {}


class _Stop(Exception):
    pass


def build(S, DEPTH, NE, SBK, dbg=None, upto=99):
    NB = S // 512
    NT = S // 128
    NCH = S // 64
    NSB = S // SBK
    nc = bass.Bass("TRN2", target_bir_lowering=False)
    dt = nc.dram_tensor
    xT_in = dt("xT", [D, S], F32, kind="ExternalInput").ap()
    w_in_d = dt("w_in", [DEPTH, D, WA], F32, kind="ExternalInput").ap()
    w_o_d = dt("w_o", [DEPTH, D, D], F32, kind="ExternalInput").ap()
    cvec_d = dt("cvec", [DEPTH, 128, NCV], F32, kind="ExternalInput").ap()
    foxb_d = dt("foxb", [DEPTH, 1, 4], F32, kind="ExternalInput").ap()
    rw_d = dt("rw", [DEPTH, D, NE], F32, kind="ExternalInput").ap()
    rb_d = dt("rb", [DEPTH, 1, NE], F32, kind="ExternalInput").ap()
    w1_d = dt("w1", [DEPTH, NE, D, 2048], F32, kind="ExternalInput").ap()
    b1_d = dt("b1T", [DEPTH, 128, NE * 16], F32, kind="ExternalInput").ap()
    w2_d = dt("w2", [DEPTH, NE, D, D], F32, kind="ExternalInput").ap()
    b2_d = dt("b2", [DEPTH, NE, D], F32, kind="ExternalInput").ap()
    cos_d = dt("ropec", [128, S], F32, kind="ExternalInput").ap()
    sin_d = dt("ropes", [128, S], F32, kind="ExternalInput").ap()
    cst_d = dt("cst", [128, 4 * 128 + 512 + 4 + 128], F32, kind="ExternalInput").ap()
    msk_d = dt("msk", [128, 4 * 512], F32, kind="ExternalInput").ap()
    outT = dt("outT", [D, S], F32, kind="ExternalOutput").ap()
    okind = {} if dbg is None else {"kind": "ExternalOutput"}
    XT = dt("XT", [D, S], F32).ap()
    XB = dt("XB", [D, S], BF16).ap()
    X1T = dt("X1T", [D, S], F32).ap()
    X1B = dt("X1B", [D, S], BF16).ap()
    MIXT = dt("MIXT", [D, S], BF16, **okind).ap()
    RQ = dt("RQ", [128, S], BF16).ap()
    RK = dt("RK", [128, S], BF16).ap()
    RV = dt("RV", [S, 256], BF16).ap()
    GATE = dt("GATE", [256, S], BF16).ap()
    FQ = dt("FQ", [256, S], BF16).ap()
    FK = dt("FK", [256, S], BF16).ap()
    FV = dt("FV", [S, 256], BF16).ap()
    FF = dt("FF", [4, S], F32).ap()
    FRD = dt("FRD", [7, S], BF16).ap()
    GT = dt("GT", [NE, S], F32, **okind).ap()

    def chunked(ap):
        return ap.rearrange("(kc p) t -> p kc t", p=128)

    with ExitStack() as es:
        sc = Sched(nc, es)
        uid = [0]

        def sb(name, shape, dtp, st=es):
            uid[0] += 1
            return st.enter_context(nc.sbuf_tensor(f"{name}_s{uid[0]}", shape, dtp))
        PS = [es.enter_context(nc.psum_tensor(f"ps{i}", [128, 512], F32)) for i in range(7)]
        PSB = es.enter_context(nc.psum_tensor("psb", [128, 1024], BF16))
        pctr = [0]

        def bank():
            pctr[0] = (pctr[0] + 1) % 7
            return PS[pctr[0]], f"ps{pctr[0]}"

        cst = sb("cst", [128, 4 * 128 + 512 + 4 + 128], F32)
        sc.dma('sp', cst[:], cst_d[:, :], w=['cst'])
        identb = sb("identb", [128, 128], BF16)
        sc.op('dve', lambda e: e.tensor_copy(identb[:], cst[:, 1028:1156]), r=['cst'], w=['identb'])
        ident = cst[:, 1028:1156]
        onesD = sb("onesD", [128, 128], F32)
        sc.op('pool', lambda e: e.memset(onesD[:], 1.0 / D), w=['onesD'])
        ones256 = sb("ones256", [128, 128], F32)
        sc.op('pool', lambda e: e.memset(ones256[:], 1.0 / 256), w=['ones256'])
        ones64 = sb("ones64", [64, 64], F32)
        sc.op('pool', lambda e: e.memset(ones64[:], 1.0 / 64), w=['ones64'])
        onesb = sb("onesb", [128, 64], BF16)
        sc.op('pool', lambda e: e.memset(onesb[:], 1.0), w=['onesb'])
        cv = sb("cv", [128, NCV], F32)

        def mm(out, lhsT, rhs, start, stop, r, w, inc=None):
            sc.op('pe', lambda e: e.matmul(out, lhsT, rhs, start=start, stop=stop), r=r, w=w,
                  inc=(stop if inc is None else inc))

        def ln_block(z, zk, gcol, bcol, st):
            pm, pmk = bank()
            for dc in range(8):
                mm(pm[:, :], onesD[:], z[:, dc, :], dc == 0, dc == 7, r=['onesD', zk], w=[pmk])
            pe2, pe2k = bank()
            for dc in range(8):
                sq = st['sq'][dc % 2]
                sqk = st['sqk'] + str(dc % 2)
                sc.op('act', lambda e, dc=dc: e.activation(sq[:], z[:, dc, :], AF.Square), r=[zk], w=[sqk])
                mm(pe2[:, :], onesD[:], sq[:], dc == 0, dc == 7, r=['onesD', sqk], w=[pe2k], inc=True)
            mean, rstd, tmp = st['mean'], st['rstd'], st['tmp']
            mk_, rk_, tk_ = st.get('keys', (st['sqk'] + 'mean', st['sqk'] + 'rstd', st['sqk'] + 'tmp'))
            sc.op('act', lambda e: e.copy(mean[:], pm[:, :]), r=[pmk], w=[mk_])
            sc.op('dve', lambda e: e.tensor_tensor(rstd[:], mean[:], mean[:], ALU.mult), r=[mk_], w=[rk_])
            sc.op('dve', lambda e: e.tensor_tensor(rstd[:], pe2[:, :], rstd[:], ALU.subtract), r=[pe2k, rk_], w=[rk_])
            sc.op('dve', lambda e: e.tensor_scalar(rstd[:], rstd[:], 0.0, EPS, ALU.max, ALU.add), r=[rk_], w=[rk_])
            sc.op('act', lambda e: e.activation(rstd[:], rstd[:], AF.Ln), r=[rk_], w=[rk_])
            sc.op('act', lambda e: e.activation(rstd[:], rstd[:], AF.Exp, scale=-0.5), r=[rk_], w=[rk_])
            for dc in range(8):
                sc.op('dve', lambda e, dc=dc: e.tensor_tensor(tmp[:], z[:, dc, :], mean[:], ALU.subtract), r=[zk, mk_], w=[tk_])
                sc.op('dve', lambda e, dc=dc: e.tensor_tensor(tmp[:], tmp[:], rstd[:], ALU.mult), r=[tk_, rk_], w=[tk_])
                sc.op('act', lambda e, dc=dc: e.activation(z[:, dc, :], tmp[:], AF.Identity,
                                                          bias=cv[:, bcol + dc:bcol + dc + 1], scale=cv[:, gcol + dc:gcol + dc + 1]),
                      r=[tk_, 'cv'], w=[zk])

        with ExitStack() as st0:
            xf = [sb(f"xf{i}", [128, 8, 512], F32, st0) for i in range(2)]
            xh = [sb(f"xh{i}", [128, 8, 512], BF16, st0) for i in range(2)]
            for tb in range(NB):
                p = tb % 2
                sl = slice(tb * 512, tb * 512 + 512)
                sc.dma('sp', xf[p][:], chunked(xT_in)[:, :, sl], w=[f'xf{p}'])
                sc.op('dve', lambda e, p=p: e.tensor_copy(xh[p][:], xf[p][:]), r=[f'xf{p}'], w=[f'xh{p}'])
                sc.dma('sp', chunked(XT)[:, :, sl], xf[p][:], r=[f'xf{p}'], w=[('XT', tb)])
                sc.dma('sp', chunked(XB)[:, :, sl], xh[p][:], r=[f'xh{p}'], w=[('XB', tb)])
            sc.barrier()

        stop = [False]
        try:
          for l in range(DEPTH):
            last = l == DEPTH - 1
            sc.dma('sp', cv[:], cvec_d[l, :, :], w=['cv'])
            with ExitStack() as st1:
              for _once in ([0] if not stop[0] else []):
                win = sb("win", [128, 8, WA], BF16, st1)
                for kc in range(8):
                    sc.dma('pool', win[:, kc, :], w_in_d[l, kc * 128:(kc + 1) * 128, :], w=[('win', kc)])
                WK = [('win', kc) for kc in range(8)]
                xbt = [sb(f"xbt{i}", [128, 8, 544], BF16, st1) for i in range(2)]
                cs_t = sb("cs_t", [128, 512], F32, st1)
                sn_t = sb("sn_t", [128, 512], F32, st1)
                t1 = sb("t1", [128, 512], F32, st1)
                t2 = sb("t2", [128, 512], F32, st1)
                ob = [sb(f"ob{i}", [128, 512], BF16, st1) for i in range(4)]
                obc = [0]
                u_t = [sb(f"u{i}", [128, 544], F32, st1) for i in range(2)]
                sg_t = sb("sg_t", [128, 544], F32, st1)
                ca_t = [sb(f"ca{i}", [128, 512], F32, st1) for i in range(2)]
                sq1 = sb("sq1", [128, 512], F32, st1)
                mean1 = sb("mean1", [128, 512], F32, st1)
                rstd1 = sb("rstd1", [128, 512], F32, st1)
                ff_t = sb("ff_t", [4, 512], F32, st1)
                vt = [sb(f"vt{i}", [128, 256], BF16, st1) for i in range(2)]
                vtc = [0]

                def nob():
                    obc[0] = (obc[0] + 1) % 4
                    return ob[obc[0]], f"ob{obc[0]}"

                for tb in range(NB):
                    t0 = tb * 512
                    p = tb % 2
                    xk = f'xbt{p}'
                    x_ = xbt[p]
                    if tb == 0:
                        sc.op('pool', lambda e: e.memset(x_[:, :, 0:32], 0.0), w=[xk])
                        sc.dma('sp', x_[:, :, 32:544], chunked(XB)[:, :, 0:512], r=[('XB', 0)], w=[xk])
                    else:
                        sc.dma('sp', x_[:, :, :], chunked(XB)[:, :, t0 - 32:t0 + 512], r=[('XB', tb - 1), ('XB', tb)], w=[xk])
                    sc.dma('sp', cs_t[:], cos_d[:, t0:t0 + 512], w=['cs_t'])
                    sc.dma('sp', sn_t[:], sin_d[:, t0:t0 + 512], w=['sn_t'])

                    def fm(c0, ncols=128, halo=False):
                        pm, pmk = bank()
                        for kc in range(8):
                            mm(pm[0:ncols, :], win[:, kc, c0:c0 + ncols], x_[:, kc, 32:544], kc == 0, kc == 7, r=[WK[kc], xk], w=[pmk])
                        if not halo:
                            return pm, pmk
                        ph, phk = bank()
                        for kc in range(8):
                            mm(ph[0:ncols, 0:32], win[:, kc, c0:c0 + ncols], x_[:, kc, 0:32], kc == 0, kc == 7, r=[WK[kc], xk], w=[phk])
                        return pm, pmk, ph, phk

                    for (c0, cp, dst, dk) in ((0, 2820, RQ, 'RQ'), (128, 2948, RK, 'RK')):
                        pa, pak = fm(c0)
                        pp, ppk = fm(cp)
                        sc.op('dve', lambda e: e.tensor_tensor(t1[:], pa[:, :], cs_t[:], ALU.mult), r=[pak, 'cs_t'], w=['t1'])
                        sc.op('dve', lambda e: e.tensor_tensor(t2[:], pp[:, :], sn_t[:], ALU.mult), r=[ppk, 'sn_t'], w=['t2'])
                        o, ok = nob()
                        sc.op('dve', lambda e: e.tensor_tensor(o[:], t1[:], t2[:], ALU.add), r=['t1', 't2'], w=[ok])
                        sc.dma('sp', dst[:, t0:t0 + 512], o[:], r=[ok], w=[(dk, tb)])
                    for c in range(2):
                        pa, pak = fm(512 + 128 * c)
                        o, ok = nob()
                        sc.op('act', lambda e: e.activation(o[:], pa[:, :], AF.Silu), r=[pak], w=[ok])
                        sc.dma('sp', GATE[128 * c:128 * c + 128, t0:t0 + 512], o[:], r=[ok], w=[('GATE', tb)])
                    for c in range(2):
                        pa, pak, pah, pahk = fm(768 + 128 * c, halo=True)
                        pb_, pbk, pbh, pbhk = fm(1024 + 128 * c, halo=True)
                        u = u_t[c]
                        uk = f'u{c}'
                        sc.op('act', lambda e: e.activation(sg_t[:, 32:544], pb_[:, :], AF.Sigmoid), r=[pbk], w=['sg_t'])
                        sc.op('act', lambda e: e.activation(sg_t[:, 0:32], pbh[:, 0:32], AF.Sigmoid), r=[pbhk, 'sg_t'], w=['sg_t'])
                        sc.op('dve', lambda e: e.tensor_tensor(u[:, 32:544], pa[:, :], sg_t[:, 32:544], ALU.mult), r=[pak, 'sg_t'], w=[uk])
                        sc.op('dve', lambda e: e.tensor_tensor(u[:, 0:32], pah[:, 0:32], sg_t[:, 0:32], ALU.mult), r=[pahk, 'sg_t', uk], w=[uk])
                        ce = 'dve'
                        ca = ca_t[c]
                        cak = f'ca{c}'
                        wc0 = 31 * c
                        sc.op(ce, lambda e: e.tensor_scalar(ca[:], u[:, 2:514], cv[:, wc0:wc0 + 1], cv[:, 68 + c:69 + c], ALU.mult, ALU.add),
                              r=[uk, 'cv'], w=[cak])
                        for j in range(1, 31):
                            sc.op(ce, lambda e, j=j: e.scalar_tensor_tensor(ca[:], u[:, 2 + j:514 + j], cv[:, wc0 + j:wc0 + j + 1], ca[:], ALU.mult, ALU.add),
                                  r=[uk, 'cv', cak], w=[cak])
                    pm, pmk = bank()
                    pe2, pe2k = bank()
                    for c in range(2):
                        mm(pm[:, :], ones256[:], ca_t[c][:], c == 0, c == 1, r=['ones256', f'ca{c}'], w=[pmk])
                    for c in range(2):
                        sc.op('act', lambda e, c=c: e.activation(sq1[:], ca_t[c][:], AF.Square), r=[f'ca{c}'], w=['sq1'])
                        mm(pe2[:, :], ones256[:], sq1[:], c == 0, c == 1, r=['ones256', 'sq1'], w=[pe2k], inc=True)
                    sc.op('act', lambda e: e.copy(mean1[:], pm[:, :]), r=[pmk], w=['mean1'])
                    sc.op('dve', lambda e: e.tensor_tensor(rstd1[:], mean1[:], mean1[:], ALU.mult), r=['mean1'], w=['rstd1'])
                    sc.op('dve', lambda e: e.tensor_tensor(rstd1[:], pe2[:, :], rstd1[:], ALU.subtract), r=[pe2k, 'rstd1'], w=['rstd1'])
                    sc.op('dve', lambda e: e.tensor_scalar(rstd1[:], rstd1[:], 0.0, EPS, ALU.max, ALU.add), r=['rstd1'], w=['rstd1'])
                    sc.op('act', lambda e: e.activation(rstd1[:], rstd1[:], AF.Ln), r=['rstd1'], w=['rstd1'])
                    sc.op('act', lambda e: e.activation(rstd1[:], rstd1[:], AF.Exp, scale=-0.5), r=['rstd1'], w=['rstd1'])
                    for c in range(2):
                        sc.op('dve', lambda e, c=c: e.tensor_tensor(t1[:], ca_t[c][:], mean1[:], ALU.subtract), r=[f'ca{c}', 'mean1'], w=['t1'])
                        sc.op('dve', lambda e: e.tensor_tensor(t1[:], t1[:], rstd1[:], ALU.mult), r=['t1', 'rstd1'], w=['t1'])
                        o, ok = nob()
                        sc.op('act', lambda e, c=c: e.activation(o[:], t1[:], AF.Silu, bias=cv[:, 72 + c:73 + c], scale=cv[:, 70 + c:71 + c]),
                              r=['t1', 'cv'], w=[ok])
                        sc.dma('sp', MIXT[256 + 128 * c:384 + 128 * c, t0:t0 + 512], o[:], r=[ok], w=[('MIXT', tb)])
                    for c in range(2):
                        pc, pck, pch, pchk = fm(1536 + 128 * c, halo=True)
                        ph_, phk_, phh, phhk = fm(1792 + 128 * c, halo=True)
                        u = u_t[c]
                        uk = f'u{c}'
                        sc.op('act', lambda e: e.copy(sg_t[:, 32:544], pc[:, :]), r=[pck], w=['sg_t'])
                        sc.op('act', lambda e: e.copy(sg_t[:, 0:32], pch[:, 0:32]), r=[pchk, 'sg_t'], w=['sg_t'])
                        sc.op('dve', lambda e: e.tensor_tensor(u[:, 32:544], ph_[:, :], sg_t[:, 32:544], ALU.mult), r=[phk_, 'sg_t'], w=[uk])
                        sc.op('dve', lambda e: e.tensor_tensor(u[:, 0:32], phh[:, 0:32], sg_t[:, 0:32], ALU.mult), r=[phhk, 'sg_t', uk], w=[uk])
                        wc0 = 62 + 3 * c
                        sc.op('dve', lambda e: e.tensor_scalar(t1[:], u[:, 30:542], cv[:, wc0:wc0 + 1], None, ALU.mult), r=[uk, 'cv'], w=['t1'])
                        for j in (1, 2):
                            sc.op('dve', lambda e, j=j: e.scalar_tensor_tensor(t1[:], u[:, 30 + j:542 + j], cv[:, wc0 + j:wc0 + j + 1], t1[:], ALU.mult, ALU.add),
                                  r=[uk, 'cv', 't1'], w=['t1'])
                        pbb, pbbk = fm(1280 + 128 * c)
                        o, ok = nob()
                        sc.op('dve', lambda e: e.tensor_tensor(o[:], pbb[:, :], t1[:], ALU.mult), r=[pbbk, 't1'], w=[ok])
                        sc.dma('sp', MIXT[512 + 128 * c:640 + 128 * c, t0:t0 + 512], o[:], r=[ok], w=[('MIXT', tb)])
                    for c in range(2):
                        pa, pak = fm(2048 + 128 * c)
                        o, ok = nob()
                        sc.op('act', lambda e: e.mul(o[:], pa[:, :], 0.125), r=[pak], w=[ok])
                        sc.dma('sp', FQ[128 * c:128 * c + 128, t0:t0 + 512], o[:], r=[ok], w=[('FQ', tb)])
                        pa, pak = fm(2304 + 128 * c)
                        o, ok = nob()
                        sc.op('act', lambda e: e.copy(o[:], pa[:, :]), r=[pak], w=[ok])
                        sc.dma('sp', FK[128 * c:128 * c + 128, t0:t0 + 512], o[:], r=[ok], w=[('FK', tb)])
                    pa, pak = fm(2816, ncols=4)
                    sc.op('act', lambda e: e.copy(ff_t[:], pa[0:4, :]), r=[pak], w=['ff_t'])
                    sc.dma('sp', FF[:, t0:t0 + 512], ff_t[:], r=['ff_t'], w=[('FF', tb)])
                    for sub in range(4):
                        for (c0, dst, dk) in ((256, RV, 'RV'), (2560, FV, 'FV')):
                            pm, pmk = bank()
                            for kc in range(8):
                                mm(pm[:, 0:256], x_[:, kc, 32 + 128 * sub:160 + 128 * sub], win[:, kc, c0:c0 + 256], kc == 0, kc == 7, r=[WK[kc], xk], w=[pmk])
                            vtc[0] ^= 1
                            v = vt[vtc[0]]
                            vk = f'vt{vtc[0]}'
                            sc.op('act', lambda e: e.copy(v[:], pm[:, 0:256]), r=[pmk], w=[vk])
                            sc.dma('sp', dst[t0 + 128 * sub:t0 + 128 * sub + 128, :], v[:], r=[vk], w=[(dk, tb)])
                sc.barrier()
                if upto == 1:
                    stop[0] = True

            with ExitStack() as st2:
              for _once in ([0] if not stop[0] else []):
                rq_t = sb("rq_t", [32, S], BF16, st2)
                rk_t = sb("rk_t", [32, S], BF16, st2)
                qd_t = sb("qd_t", [32, S], BF16, st2)
                v_t = sb("v_t", [128, NT, 64], BF16, st2)
                v64 = sb("v64", [64, NCH, 64], BF16, st2)
                qdec = sb("qdec", [32, 512], F32, st2)
                stt = sb("stt", [32, 64], F32, st2)
                prevb = sb("prevb", [32, NCH, 64], BF16, st2)
                kd = [sb(f"kd{i}", [128, 32], BF16, st2) for i in range(2)]
                sd = [sb(f"sd{i}", [128, 128], BF16, st2) for i in range(2)]
                o_sb = sb("o_sb", [64, 512], F32, st2)
                sq2 = sb("sq2", [64, 512], F32, st2)
                mean2 = sb("mean2", [64, 512], F32, st2)
                rstd2 = sb("rstd2", [64, 512], F32, st2)
                g_t = sb("g_t", [64, 512], BF16, st2)
                ro = [sb(f"ro{i}", [64, 512], BF16, st2) for i in range(2)]
                for h in range(4):
                    gam = 1.0 - 2.0 ** (-5.0 - h)
                    cd = gam ** CHUNK
                    sc.dma('sp', rq_t[:], RQ[32 * h:32 * h + 32, :], w=['rq_t'])
                    sc.dma('sp', rk_t[:], RK[32 * h:32 * h + 32, :], w=['rk_t'])
                    sc.dma('sp', v_t[:], RV[:, 64 * h:64 * h + 64].rearrange("(j p) e -> p j e", p=128), w=['v_t'])
                    sc.dma('sp', qdec[:], cst_d[32 * h:32 * h + 32, 512:1024], w=['qdec'])
                    for tb in range(NB):
                        sc.op('dve', lambda e, tb=tb: e.tensor_tensor(qd_t[:, tb * 512:tb * 512 + 512], rq_t[:, tb * 512:tb * 512 + 512], qdec[:], ALU.mult),
                              r=['rq_t', 'qdec'], w=['qd_t'])
                    sc.op('dve', lambda e: e.memset(stt[:], 0.0), w=['stt'])
                    sc.dma('sp', v64[:], RV[:, 64 * h:64 * h + 64].rearrange("(c p) e -> p c e", p=64), w=['v64'])
                    for c in range(NCH):
                        sc.op('pe', lambda e, c=c: e.transpose(PSB[0:64, 0:32], rk_t[:, 64 * c:64 * c + 64], identb[0:32, 0:32]),
                              r=['rk_t', 'identb'], w=['psb'])
                        k_ = kd[c % 2]
                        kk = f'kd{c % 2}'
                        sc.op('dve', lambda e: e.tensor_scalar(k_[0:64, :], PSB[0:64, 0:32], cst[0:64, 1024 + h:1025 + h], None, ALU.mult), r=['psb', 'cst'], w=[kk])
                        pkv, pkvk = bank()
                        mm(pkv[0:32, 0:64], k_[0:64, :], v64[:, c, :], True, True, r=[kk, 'v64'], w=[pkvk])
                        sc.op('dve', lambda e, c=c: e.tensor_copy(prevb[:, c, :], stt[:]), r=['stt'], w=['prevb'])
                        sc.op('dve', lambda e: e.scalar_tensor_tensor(stt[:], stt[:], cd, pkv[0:32, 0:64], ALU.mult, ALU.add),
                              r=['stt', pkvk], w=['stt'])
                    for tb in range(NB if KN >= 5 else 0):
                        t0 = tb * 512
                        po, pok = bank()
                        for jj in range(4):
                            j = tb * 4 + jj
                            ps_, psk = bank()
                            mm(ps_[:, 0:128], rk_t[:, 128 * j:128 * j + 128], rq_t[:, 128 * j:128 * j + 128], True, True, r=['rk_t', 'rq_t'], w=[psk])
                            s_ = sd[j % 2]
                            sk = f'sd{j % 2}'
                            sc.op('dve', lambda e: e.tensor_tensor(s_[:], ps_[:, 0:128], cst[:, 128 * h:128 * h + 128], ALU.mult), r=[psk, 'cst'], w=[sk])
                            mm(po[0:64, 128 * jj:128 * jj + 128], v_t[:, j, :], s_[:], True, False, r=['v_t', sk], w=[pok], inc=False)
                            for hf in range(2):
                                c0 = 128 * jj + 64 * hf
                                mm(po[0:64, c0:c0 + 64], prevb[:, 2 * j + hf, :], qd_t[:, 128 * j + 64 * hf:128 * j + 64 * hf + 64], False, hf == 1,
                                   r=['prevb', 'qd_t'], w=[pok], inc=(hf == 1))
                        sc.op('act', lambda e: e.copy(o_sb[:], po[0:64, :]), r=[pok], w=['o_sb'])
                        sc.op('act', lambda e: e.activation(sq2[:], po[0:64, :], AF.Square), r=[pok], w=['sq2'])
                        pm, pmk = bank()
                        mm(pm[0:64, :], ones64[:], o_sb[:], True, True, r=['ones64', 'o_sb'], w=[pmk])
                        pe2, pe2k = bank()
                        mm(pe2[0:64, :], ones64[:], sq2[:], True, True, r=['ones64', 'sq2'], w=[pe2k])
                        sc.op('act', lambda e: e.copy(mean2[:], pm[0:64, :]), r=[pmk], w=['mean2'])
                        sc.op('dve', lambda e: e.tensor_tensor(rstd2[:], mean2[:], mean2[:], ALU.mult), r=['mean2'], w=['rstd2'])
                        sc.op('dve', lambda e: e.tensor_tensor(rstd2[:], pe2[0:64, :], rstd2[:], ALU.subtract), r=[pe2k, 'rstd2'], w=['rstd2'])
                        sc.op('dve', lambda e: e.tensor_scalar(rstd2[:], rstd2[:], 0.0, EPS, ALU.max, ALU.add), r=['rstd2'], w=['rstd2'])
                        sc.op('act', lambda e: e.activation(rstd2[:], rstd2[:], AF.Ln), r=['rstd2'], w=['rstd2'])
                        sc.op('act', lambda e: e.activation(rstd2[:], rstd2[:], AF.Exp, scale=-0.5), r=['rstd2'], w=['rstd2'])
                        sc.dma('sp', g_t[:], GATE[64 * h:64 * h + 64, t0:t0 + 512], w=['g_t'])
                        sc.op('dve', lambda e: e.tensor_tensor(o_sb[:], o_sb[:], mean2[:], ALU.subtract), r=['o_sb', 'mean2'], w=['o_sb'])
                        sc.op('dve', lambda e: e.tensor_tensor(o_sb[:], o_sb[:], rstd2[:], ALU.mult), r=['o_sb', 'rstd2'], w=['o_sb'])
                        sc.op('dve', lambda e: e.tensor_scalar(o_sb[:], o_sb[:], cv[0:64, 74 + h:75 + h], None, ALU.mult), r=['o_sb', 'cv'], w=['o_sb'])
                        r_ = ro[tb % 2]
                        rk_ = f'ro{tb % 2}'
                        sc.op('dve', lambda e: e.tensor_tensor(r_[:], o_sb[:], g_t[:], ALU.mult), r=['o_sb', 'g_t'], w=[rk_])
                        sc.dma('sp', MIXT[64 * h:64 * h + 64, t0:t0 + 512], r_[:], r=[rk_], w=[('MIXT2', h, tb)])
                sc.barrier()
                if upto == 2:
                    stop[0] = True

            with ExitStack() as st3:
              for _once in ([0] if not stop[0] else []):
                qa = sb("qa", [70, S], BF16, st3)
                ka = sb("ka", [70, S], BF16, st3)
                fv_t = sb("fv_t", [128, NT, 64], BF16, st3)
                msk = sb("msk", [128, 4 * 512], F32, st3)
                sc.dma('sp', msk[:], msk_d[:, :], w=['msk'])
                fb = sb("fb", [1, 4], F32, st3)
                sc.dma('sp', fb[:], foxb_d[l, :, :], w=['fb'])
                sc.op('dve', lambda e: e.tensor_scalar(fb[:], fb[:], -1.0, None, ALU.mult), r=['fb'], w=['fb'])
                FW = min(S, 2048)
                f_r = sb("f_r", [1, FW], F32, st3)
                F_r = sb("F_r", [1, FW], F32, st3)
                one_r = sb("one_r", [1, FW], F32, st3)
                sc.op('pool', lambda e: e.memset(one_r[:], 1.0), w=['one_r'])
                fr = sb("fr", [1, 7, FW], BF16, st3)
                r1 = sb("r1", [1, FW], F32, st3)
                r2 = sb("r2", [1, FW], F32, st3)
                Fl = sb("Fl", [1, 1], F32, st3)
                pt = [sb(f"pt{i}", [128, 512], BF16, st3) for i in range(3)]
                rden = sb("rden", [64, 512], F32, st3)
                fo = [sb(f"fo{i}", [64, 512], BF16, st3) for i in range(2)]
                for h in range(4):
                    sc.op('dve', lambda e: e.memset(Fl[:], 0.0), w=['Fl'])
                    for pc in range(S // FW):
                        sl = slice(pc * FW, pc * FW + FW)
                        sc.dma('sp', f_r[:], FF[h:h + 1, sl], w=['f_r'])
                        sc.op('act', lambda e: e.activation(f_r[:], f_r[:], AF.Exp, bias=fb[0:1, h:h + 1], scale=-1.0), r=['f_r', 'fb'], w=['f_r'])
                        sc.op('act', lambda e: e.activation(f_r[:], f_r[:], AF.Ln, bias=1.0), r=['f_r'], w=['f_r'])
                        sc.op('dve', lambda e: e.tensor_scalar(f_r[:], f_r[:], -1.0, None, ALU.mult), r=['f_r'], w=['f_r'])
                        sc.op('dve', lambda e: e.tensor_tensor_scan(F_r[:], one_r[:], f_r[:], Fl[0:1, 0:1], ALU.mult, ALU.add), r=['one_r', 'f_r', 'Fl'], w=['F_r'])
                        sc.op('dve', lambda e: e.tensor_copy(Fl[:], F_r[0:1, FW - 1:FW]), r=['F_r'], w=['Fl'])
                        sc.op('dve', lambda e: e.tensor_copy(fr[:, 0, :], F_r[:]), r=['F_r'], w=['fr'])
                        sc.op('dve', lambda e: e.tensor_tensor(r1[:], F_r[:], fr[:, 0, :], ALU.subtract), r=['F_r', 'fr'], w=['r1'])
                        sc.op('dve', lambda e: e.tensor_copy(fr[:, 1, :], r1[:]), r=['r1', 'fr'], w=['fr'])
                        sc.op('dve', lambda e: e.tensor_tensor(r2[:], r1[:], fr[:, 1, :], ALU.subtract), r=['r1', 'fr'], w=['r2'])
                        sc.op('dve', lambda e: e.tensor_copy(fr[:, 2, :], r2[:]), r=['r2', 'fr'], w=['fr'])
                        sc.op('dve', lambda e: e.tensor_copy(fr[:, 3, :], one_r[:]), r=['one_r', 'fr'], w=['fr'])
                        for i in range(3):
                            sc.op('dve', lambda e, i=i: e.tensor_scalar(fr[:, 4 + i, :], fr[:, i, :], -1.0, None, ALU.mult), r=['fr'], w=['fr'])
                        sc.dma('sp', FRD[:, sl].rearrange("(o r) t -> o r t", o=1), fr[:], r=['fr'], w=[('FRD', pc)])
                    FRk = [('FRD', pc) for pc in range(S // FW)]
                    sc.dma('sp', qa[0:64, :], FQ[64 * h:64 * h + 64, :], w=['qa'])
                    sc.dma('sp', qa[64:67, :], FRD[0:3, :], r=FRk + ['qa'], w=['qa'])
                    for i in range(3):
                        sc.dma('sp', qa[67 + i:68 + i, :], FRD[3:4, :], r=FRk + ['qa'], w=['qa'])
                    sc.dma('sp', ka[0:64, :], FK[64 * h:64 * h + 64, :], w=['ka'])
                    for i in range(3):
                        sc.dma('sp', ka[64 + i:65 + i, :], FRD[3:4, :], r=FRk + ['ka'], w=['ka'])
                    sc.dma('sp', ka[67:70, :], FRD[4:7, :], r=FRk + ['ka'], w=['ka'])
                    sc.dma('sp', fv_t[:], FV[:, 64 * h:64 * h + 64].rearrange("(j p) e -> p j e", p=128), w=['fv_t'])
                    pi = 0
                    for qb in range(NB):
                        q0 = qb * 512
                        pn, pnk = PS[5], 'ps5'
                        pd, pdk = PS[6], 'ps6'
                        nk = 4 * qb + 4
                        def score(j):
                            mm(PS[j % 4][:, :], ka[:, 128 * j:128 * j + 128], qa[:, q0:q0 + 512], True, True, r=['ka', 'qa'], w=[f'ps{j % 4}'])

                        score(0)
                        for j in range(nk):
                            if j + 1 < nk:
                                score(j + 1)
                            ps_ = PS[j % 4]
                            psk = f'ps{j % 4}'
                            p_ = pt[pi % 3]
                            pk = f'pt{pi % 3}'
                            pi += 1
                            sc.op('act', lambda e: e.activation(p_[:], ps_[:, :], AF.Exp), r=[psk], w=[pk])
                            if j >= 4 * qb:
                                m = j - 4 * qb
                                sc.op('dve', lambda e: e.tensor_tensor(p_[:], p_[:], msk[:, 512 * m:512 * m + 512], ALU.mult), r=[pk, 'msk'], w=[pk])
                            mm(pn[0:64, :], fv_t[:, j, :], p_[:], j == 0, j == nk - 1, r=['fv_t', pk], w=[pnk], inc=False)
                            mm(pd[0:64, :], onesb[:], p_[:], j == 0, j == nk - 1, r=['onesb', pk], w=[pdk], inc=True)
                        sc.op('dve', lambda e: e.reciprocal(rden[:], pd[0:64, :]), r=[pdk], w=['rden'])
                        f_ = fo[qb % 2]
                        fk_ = f'fo{qb % 2}'
                        sc.op('dve', lambda e: e.tensor_tensor(f_[:], pn[0:64, :], rden[:], ALU.mult), r=[pnk, 'rden'], w=[fk_])
                        sc.dma('sp', MIXT[768 + 64 * h:832 + 64 * h, q0:q0 + 512], f_[:], r=[fk_], w=[('MIXT3', h, qb)])
                sc.barrier()
                if upto == 3:
                    stop[0] = True

            with ExitStack() as st4:
              for _once in ([0] if not stop[0] else []):
                wo = sb("wo", [128, 8, D], BF16, st4)
                for kc in range(8):
                    sc.dma('pool', wo[:, kc, :], w_o_d[l, kc * 128:(kc + 1) * 128, :], w=[('wo', kc)])
                rwt = sb("rwt", [128, 8, NE], F32, st4)
                sc.dma('sp', rwt[:], rw_d[l].rearrange("(kc p) e -> p kc e", p=128), w=['rwt'])
                rbt = sb("rbt", [128, NE], F32, st4)
                sc.dma('sp', rbt[:], rb_d[l, 0:1, :].partition_broadcast(128), w=['rbt'])
                mx = [sb(f"mx{i}", [128, 8, 512], BF16, st4) for i in range(2)]
                xr = [sb(f"xr{i}", [128, 8, 512], F32, st4) for i in range(2)]
                z = sb("z", [128, 8, 512], F32, st4)
                stl = dict(sq=[sb(f"sq4{i}", [128, 512], F32, st4) for i in range(2)], sqk='s4', mean=sb("mean", [128, 512], F32, st4),
                           rstd=sb("rstd", [128, 512], F32, st4), tmp=sb("tmp", [128, 512], F32, st4))
                x1f = z
                x1h = sb("x1h", [128, 8, 512], BF16, st4)
                lg = sb("lg", [128, NE], F32, st4)
                m8 = sb("m8", [128, 8], F32, st4)
                nmx = sb("nmx", [128, 1], F32, st4)
                mk = sb("mk", [128, NE], F32, st4)
                ex = sb("ex", [128, NE], F32, st4)
                ssum = sb("ssum", [128, 1], F32, st4)
                gtt = sb("gtt", [NE, 512], F32, st4)
                for tb in range(NB):
                    t0 = tb * 512
                    p = tb % 2
                    sl = slice(t0, t0 + 512)
                    sc.dma('sp', mx[p][:], chunked(MIXT)[:, :, sl], w=[f'mx{p}'])
                    sc.dma('sp', xr[p][:], chunked(XT)[:, :, sl], w=[f'xr{p}'])
                    for dc in range(8):
                        pm, pmk = bank()
                        for kc in range(8):
                            mm(pm[:, :], wo[:, kc, 128 * dc:128 * dc + 128], mx[p][:, kc, :], kc == 0, kc == 7, r=[('wo', kc), f'mx{p}'], w=[pmk])
                        sc.op('dve', lambda e, dc=dc: e.scalar_tensor_tensor(z[:, dc, :], xr[p][:, dc, :], ALPHA, pm[:, :], ALU.mult, ALU.add),
                              r=[f'xr{p}', pmk], w=['z'])
                    ln_block(z, 'z', 78, 86, stl)
                    sc.op('dve', lambda e: e.tensor_copy(x1h[:], z[:]), r=['z'], w=['x1h'])
                    sc.dma('sp', chunked(X1T)[:, :, sl], x1f[:], r=['z'], w=[('X1T', tb)])
                    sc.dma('sp', chunked(X1B)[:, :, sl], x1h[:], r=['x1h'], w=[('X1B', tb)])
                    for sub in range(4):
                        pm, pmk = bank()
                        for kc in range(8):
                            mm(pm[:, 0:NE], x1f[:, kc, 128 * sub:128 * sub + 128], rwt[:, kc, :], kc == 0, kc == 7, r=['z', 'rwt'], w=[pmk])
                        sc.op('dve', lambda e: e.tensor_tensor(lg[:], pm[:, 0:NE], rbt[:], ALU.add), r=[pmk, 'rbt'], w=['lg'])
                        sc.op('dve', lambda e: e.max(m8[:], lg[:]), r=['lg'], w=['m8'])
                        sc.op('dve', lambda e: e.tensor_scalar(mk[:], lg[:], m8[:, 3:4], None, ALU.is_ge), r=['lg', 'm8'], w=['mk'])
                        sc.op('dve', lambda e: e.tensor_scalar(nmx[:], m8[:, 0:1], -1.0, None, ALU.mult), r=['m8'], w=['nmx'])
                        sc.op('act', lambda e: e.activation(ex[:], lg[:], AF.Exp, bias=nmx[:, 0:1], scale=1.0), r=['lg', 'nmx'], w=['ex'])
                        sc.op('dve', lambda e: e.tensor_tensor(ex[:], ex[:], mk[:], ALU.mult), r=['ex', 'mk'], w=['ex'])
                        sc.op('dve', lambda e: e.tensor_reduce(ssum[:], ex[:], AX.X, ALU.add), r=['ex'], w=['ssum'])
                        sc.op('dve', lambda e: e.reciprocal(ssum[:], ssum[:]), r=['ssum'], w=['ssum'])
                        sc.op('dve', lambda e: e.tensor_scalar(ex[:], ex[:], ssum[:, 0:1], None, ALU.mult), r=['ex', 'ssum'], w=['ex'])
                        pg, pgk = bank()
                        mm(pg[0:NE, 0:128], ex[:], ident, True, True, r=['ex', 'cst'], w=[pgk])
                        sc.op('act', lambda e, sub=sub: e.copy(gtt[:, 128 * sub:128 * sub + 128], pg[0:NE, 0:128]), r=[pgk], w=['gtt'])
                    sc.dma('sp', GT[:, sl], gtt[:], r=['gtt'], w=[('GT', tb)])
                sc.barrier()
                if upto == 4:
                    stop[0] = True

            with ExitStack() as st5:
              for _once in ([0] if not stop[0] else []):
                NBS = SBK // 512
                acc = sb("acc", [128, 8, SBK], F32, st5)
                xs = sb("xs", [128, 8, SBK], BF16, st5)
                w1t = [sb(f"w1t{i}", [128, 8, 2048], BF16, st5) for i in range(2)]
                w2t = [sb(f"w2t{i}", [128, 8, D], BF16, st5) for i in range(2)]
                gb = [sb(f"gb{i}", [128, SBK], F32, st5) for i in range(2)]
                b1t = sb("b1t", [128, NE * 16], F32, st5)
                sc.dma('sp', b1t[:], b1_d[l, :, :], w=['b1t'])
                b2t = sb("b2t", [NE, D], F32, st5)
                sc.dma('sp', b2t[:], b2_d[l, :, :], w=['b2t'])
                gts = sb("gts", [NE, SBK], F32, st5)
                actt = [sb(f"actt{i}", [128, 8, 512], BF16, st5) for i in range(2)]
                blk = [0]
                ga = [sb(f"ga{i}", [128, 512], F32, st5) for i in range(3)]
                sgm = [sb(f"sgm{i}", [128, 512], F32, st5) for i in range(3)]
                li = [sb(f"li{i}", [128, 512], F32, st5) for i in range(3)]
                b1p = sb("b1p", [128, NE * 16], F32, st5)
                sc.op('dve', lambda e: e.tensor_scalar(b1p[:], b1t[:], 1.0, None, ALU.add), r=['b1t'], w=['b1p'])
                b1m = b1t
                sc.op('dve', lambda e: e.tensor_scalar(b1m[:], b1t[:], -1.0, 7.0, ALU.mult, ALU.add), r=['b1t'], w=['b1t'])
                c119 = sb("c119", [128, 1], F32, st5)
                sc.op('pool', lambda e: e.memset(c119[:], 1.702 * 7.0), w=['c119'])
                hb5 = [sb(f"hb5{i}", [128, 512], BF16, st5) for i in range(2)]
                xr5 = [li[0], li[1]]
                stl = dict(sq=[ga[0], ga[1]], sqk='ga', mean=sgm[0], rstd=sgm[1], tmp=sgm[2], keys=('sgm0', 'sgm1', 'sgm2'))
                wn = 0
                for sbi in range(NSB):
                    s0 = sbi * SBK
                    sc.dma('sp', xs[:], chunked(X1B)[:, :, s0:s0 + SBK], w=['xs'])
                    sc.dma('sp', gts[:], GT[0:NE, s0:s0 + SBK], w=['gts'])
                    for tb in range(NBS):
                        for dc in range(8):
                            pm, pmk = bank()
                            mm(pm[:, :], b2t[:, 128 * dc:128 * dc + 128], gts[:, 512 * tb:512 * tb + 512], True, True, r=['b2t', 'gts'], w=[pmk])
                            sc.op('act', lambda e, dc=dc, tb=tb: e.copy(acc[:, dc, 512 * tb:512 * tb + 512], pm[:, :]), r=[pmk], w=['acc'])
                    for ex_ in range(NE):
                        wp = wn % 2
                        wn += 1
                        for kc in range(8 if (KW or wn <= 2) else 0):
                            sc.dma('pool', w1t[wp][:, kc, :], w1_d[l, ex_, kc * 128:(kc + 1) * 128, :], w=[(f'w1t{wp}', kc)])
                        for kc in range(8 if (KW or wn <= 2) else 0):
                            sc.dma('pool', w2t[wp][:, kc, :], w2_d[l, ex_, kc * 128:(kc + 1) * 128, :], w=[(f'w2t{wp}', kc)])
                        sc.dma('sp', gb[wp][:], GT[ex_:ex_ + 1, s0:s0 + SBK].partition_broadcast(128), w=[f'gb{wp}'])
                        sc.op('pool', lambda e: e.tensor_scalar(gb[wp][:], gb[wp][:], 1.0 / 1.702, None, ALU.mult), r=[f'gb{wp}'], w=[f'gb{wp}'])
                        for tb in range(NBS):
                            ab = actt[blk[0] % 2]
                            abk = f'actt{blk[0] % 2}'
                            blk[0] += 1
                            gsl = gb[wp][:, 512 * tb:512 * tb + 512]

                            def X(fc):
                                pg, pgk = bank()
                                for kc in range(8):
                                    mm(pg[:, :], w1t[wp][:, kc, 128 * fc:128 * fc + 128], xs[:, kc, 512 * tb:512 * tb + 512], kc == 0, kc == 7,
                                       r=[(f'w1t{wp}', kc), 'xs'], w=[pgk])
                                pl, plk = bank()
                                for kc in range(8):
                                    mm(pl[:, :], w1t[wp][:, kc, 1024 + 128 * fc:1152 + 128 * fc], xs[:, kc, 512 * tb:512 * tb + 512], kc == 0, kc == 7,
                                       r=[(f'w1t{wp}', kc), 'xs'], w=[plk])
                                q = fc % 3
                                bg = b1t[:, ex_ * 16 + fc:ex_ * 16 + fc + 1]
                                bl = b1p[:, ex_ * 16 + 8 + fc:ex_ * 16 + 8 + fc + 1]
                                bg7 = b1m[:, ex_ * 16 + fc:ex_ * 16 + fc + 1]
                                sc.op('act', lambda e: e.activation(sgm[q][:], pg[:, :], AF.Relu, bias=bg7, scale=-1.0), r=[pgk, 'b1t'], w=[f'sgm{q}'])
                                sc.op('act', lambda e: e.activation(ga[q][:], sgm[q][:], AF.Silu, bias=c119[:, 0:1], scale=-1.702), r=[f'sgm{q}', 'c119'], w=[f'ga{q}'])
                                sc.op('dve', lambda e: e.tensor_scalar(li[q][:], pl[:, :], bl, -6.0, ALU.add, ALU.max), r=[plk, 'b1p'], w=[f'li{q}'])

                            def Y(fc):
                                q = fc % 3
                                sc.op('dve', lambda e: e.scalar_tensor_tensor(ga[q][:], li[q][:], 8.0, ga[q][:], ALU.min, ALU.mult), r=[f'ga{q}', f'li{q}'], w=[f'ga{q}'])
                                sc.op('dve', lambda e: e.tensor_tensor(ab[:, fc, :], ga[q][:], gsl, ALU.mult), r=[f'ga{q}', f'gb{wp}'], w=[(abk, fc)])

                            X(0)
                            X(1)
                            for fc in range(8):
                                if fc + 2 < 8:
                                    X(fc + 2)
                                Y(fc)
                            for dc in range(8):
                                pm, pmk = bank()
                                for fc in range(8):
                                    mm(pm[:, :], w2t[wp][:, fc, 128 * dc:128 * dc + 128], ab[:, fc, :], fc == 0, fc == 7, r=[(f'w2t{wp}', fc), (abk, fc)], w=[pmk])
                                sc.op('dve', lambda e, dc=dc, tb=tb: e.tensor_tensor(acc[:, dc, 512 * tb:512 * tb + 512], acc[:, dc, 512 * tb:512 * tb + 512], pm[:, :], ALU.add),
                                      r=['acc', pmk], w=['acc'])
                    for tb in range(NBS):
                        gtb = sbi * NBS + tb
                        sl = slice(gtb * 512, gtb * 512 + 512)
                        zs = acc[:, :, 512 * tb:512 * tb + 512]
                        for dc in range(8):
                            xr_ = xr5[dc % 2]
                            xk_ = f'li{dc % 2}'
                            sc.dma('sp', xr_[:], X1T[128 * dc:128 * dc + 128, sl], r=[('X1T', gtb)], w=[xk_])
                            sc.op('dve', lambda e, dc=dc: e.scalar_tensor_tensor(zs[:, dc, :], xr_[:], ALPHA, zs[:, dc, :], ALU.mult, ALU.add),
                                  r=[xk_, 'acc'], w=['acc'])
                        ln_block(zs, 'acc', 94, 102, stl)
                        if last:
                            sc.dma('sp', chunked(outT)[:, :, sl], zs, r=['acc'], w=[('outT', gtb)])
                        else:
                            sc.dma('sp', chunked(XT)[:, :, sl], zs, r=['acc'], w=[('XT', gtb)])
                            for dc in range(8):
                                hb_ = hb5[dc % 2]
                                hk_ = f'hb5{dc % 2}'
                                sc.op('act', lambda e, dc=dc: e.copy(hb_[:], zs[:, dc, :]), r=['acc'], w=[hk_])
                                sc.dma('sp', XB[128 * dc:128 * dc + 128, sl], hb_[:], r=[hk_], w=[('XB', gtb, dc)])
                sc.barrier()
                if upto == 5:
                    stop[0] = True
        except _Stop:
            pass
    return nc


def host_consts(S):
    half = 16
    freqs = (10000.0 ** (-np.arange(half, dtype=np.float32) / half)).astype(np.float32)
    pos = np.arange(S, dtype=np.float32)
    ang = pos[None, :] * freqs[:, None]
    cos = np.cos(ang).astype(np.float32)
    sin = np.sin(ang).astype(np.float32)
    ropec = np.tile(np.concatenate([cos, cos], 0), (4, 1))
    ropes = np.tile(np.concatenate([-sin, sin], 0), (4, 1))
    cst = np.zeros((128, 4 * 128 + 512 + 4 + 128), np.float32)
    idx = np.arange(128)
    same = (idx[:, None] // 64) == (idx[None, :] // 64)
    scale = 32 ** -0.5
    for h in range(4):
        g = 1.0 - 2.0 ** (-5.0 - h)
        cst[:, 128 * h:128 * h + 128] = np.where(same, g ** np.abs(idx[:, None] - idx[None, :]), 0.0) * scale
        cst[32 * h:32 * h + 32, 512:1024] = (g ** ((np.arange(512) % 64) + 1.0))[None, :]
        cst[:, 1024 + h] = g ** (63 - (idx % 64)) * scale
    cst[:, 1028:1156] = np.eye(128, dtype=np.float32)
    msk = np.zeros((128, 4 * 512), np.float32)
    s_ = np.arange(128)[:, None]
    t_ = np.arange(512)[None, :]
    for m in range(4):
        msk[:, 512 * m:512 * m + 512] = (t_ >= 128 * m + s_)
    return dict(ropec=np.ascontiguousarray(ropec), ropes=np.ascontiguousarray(ropes), cst=cst, msk=msk)


def host_layout(inp, S, DEPTH, NE):
    L = DEPTH
    perm = np.concatenate([np.arange(16, 32), np.arange(0, 16)])
    pq = np.concatenate([h * 32 + perm for h in range(4)])
    w_in = np.asarray(inp['w_in'])
    w_aug = np.concatenate([w_in, w_in[:, :, pq], w_in[:, :, 128 + pq]], axis=2)
    cv = np.zeros((L, 128, NCV), np.float32)
    cdw = np.asarray(inp['conf_dw'])
    sdw = np.asarray(inp['sc_dw'])
    for c in range(2):
        cv[:, :, 31 * c:31 * c + 31] = cdw[:, :, 128 * c:128 * c + 128].transpose(0, 2, 1)
        cv[:, :, 62 + 3 * c:65 + 3 * c] = sdw[:, :, 128 * c:128 * c + 128].transpose(0, 2, 1)
        cv[:, :, 68 + c] = np.asarray(inp['conf_dw_b'])[:, 128 * c:128 * c + 128]
        cv[:, :, 70 + c] = np.asarray(inp['conf_ln_g'])[:, 128 * c:128 * c + 128]
        cv[:, :, 72 + c] = np.asarray(inp['conf_ln_b'])[:, 128 * c:128 * c + 128]
    cv[:, 0:64, 74:78] = np.asarray(inp['ret_gn_g']).reshape(L, 4, 64).transpose(0, 2, 1)
    for name, c0 in (('ln1_g', 78), ('ln1_b', 86), ('ln2_g', 94), ('ln2_b', 102)):
        cv[:, :, c0:c0 + 8] = np.asarray(inp[name]).reshape(L, 8, 128).transpose(0, 2, 1)
    b1T = np.ascontiguousarray(np.asarray(inp['b1']).reshape(L, NE, 16, 128).transpose(0, 3, 1, 2).reshape(L, 128, NE * 16))
    rw = np.asarray(inp['router_w'])
    rb = np.asarray(inp['router_b'])
    common = dict(w_in=np.ascontiguousarray(w_aug), w_o=np.asarray(inp['w_o']), cvec=cv,
                  foxb=np.asarray(inp['fox_b_f']).reshape(L, 1, 4), rw=rw, rb=rb.reshape(L, 1, -1),
                  w1=np.asarray(inp['w1']), b1T=b1T, w2=np.asarray(inp['w2']), b2=np.asarray(inp['b2']))
    common.update(host_consts(S))
    return common


def run(inp, S, DEPTH, NE, SBK, dbg=None, upto=99, trace=False):
    x = np.asarray(inp['x'])
    B = x.shape[0]
    common = host_layout(inp, S, DEPTH, NE)
    nc = build(S, DEPTH, NE, SBK, dbg, upto)
    in_maps = []
    for b in range(B):
        m = dict(common)
        m['xT'] = np.ascontiguousarray(x[b].T)
        in_maps.append(m)
    res = run_bass_kernel_spmd(nc, in_maps, core_ids=list(range(B)), **({'trace': True} if trace else {}))
    if trace:
        print('EXEC_NS', res.exec_time_ns)
    out = np.stack([np.ascontiguousarray(r['outT'].T) for r in res.results], 0).astype(np.float32)
    if dbg is not None:
        return out, res.results
    return out


def kernel(**inputs):
    return run(inputs, 8192, 4, 32, 1024)
```

```python
import math
import os
KN = int(os.environ.get('KN', '99'))
KW = int(os.environ.get('KW', '1'))
from contextlib import ExitStack
import numpy as np
import concourse.bass as bass
import concourse.mybir as mybir
from concourse.bass_utils import run_bass_kernel_spmd

F32 = mybir.dt.float32
BF16 = mybir.dt.bfloat16
ALU = mybir.AluOpType
AF = mybir.ActivationFunctionType
AX = mybir.AxisListType

D = 1024
CHUNK = 64
ALPHA = 8 ** 0.25
EPS = 1e-5
NCV = 110
WA = 2820 + 256


class Sched:
    def __init__(s, nc, es):
        s.nc = nc
        s.eng = {'pe': nc.tensor, 'act': nc.scalar, 'dve': nc.vector, 'pool': nc.gpsimd, 'sp': nc.sync}
        s.sem = {k: es.enter_context(nc.semaphore('s_' + k)) for k in ('pe', 'act', 'dve', 'pool')}
        s.cnt = {k: 0 for k in s.sem}
        s.seen = {k: {} for k in s.eng}
        s.dq = {}
        for q in ('sp', 'pool', 'act'):
            s.dq[q] = dict(sems=[es.enter_context(nc.semaphore(f'd_{q}{i}')) for i in range(8)], n=0)
        s.res = {}

    def _need(s, en, deps):
        best = {}
        for (k, h, v) in deps:
            if v > best.get(k, (None, 0))[1]:
                best[k] = (h, v)
        for k, (h, v) in best.items():
            if s.seen[en].get(k, 0) < v:
                s.eng[en].wait_ge(h, v)
                s.seen[en][k] = v

    def _collect(s, en, r, w):
        deps = []
        for k in r:
            st = s.res.get(k)
            if st and st[0]:
                deps.append(st[0])
        for k in w:
            st = s.res.get(k)
            if st:
                if st[0]:
                    deps.append(st[0])
                deps.extend(st[1].values())
        if en == 'pe':
            deps = [d for d in deps if d[0] != 'pe']
        return deps

    def _record(s, dep, r, w):
        for k in r:
            st = s.res.setdefault(k, [None, {}])
            o = st[1].get(dep[0])
            if o is None or o[2] < dep[2]:
                st[1][dep[0]] = dep
        for k in w:
            s.res[k] = [dep, {}]

    def op(s, en, fn, r=(), w=(), inc=True):
        assert inc or en == 'pe'
        s._need(en, s._collect(en, r, w))
        ins = fn(s.eng[en])
        if inc:
            s.cnt[en] += 1
            ins.then_inc(s.sem[en], 1)
            dep = (en, s.sem[en], s.cnt[en])
        else:
            dep = (en, s.sem[en], s.cnt[en] + 1)
        s._record(dep, r, w)

    def dma(s, q, out, in_, r=(), w=(), **kw):
        Q = s.dq[q]
        i = Q['n']
        Q['n'] += 1
        K = len(Q['sems'])
        h = Q['sems'][i % K]
        key = f'd_{q}{i % K}'
        deps = s._collect(q, r, w)
        if i >= K:
            deps.append((key, h, 16 * (i // K)))
        s._need(q, deps)
        s.eng[q].dma_start(out=out, in_=in_, **kw).then_inc(h, 16)
        s._record((key, h, 16 * (i // K + 1)), r, w)

    def idma(s, out, out_off, in_, in_off, bound, r=(), w=()):
        q = 'pool'
        Q = s.dq[q]
        i = Q['n']
        Q['n'] += 1
        K = len(Q['sems'])
        h = Q['sems'][i % K]
        key = f'd_{q}{i % K}'
        deps = s._collect(q, r, w)
        if i >= K:
            deps.append((key, h, 16 * (i // K)))
        s._need(q, deps)
        s.eng[q].indirect_dma_start(out=out, out_offset=out_off, in_=in_, in_offset=in_off,
                                    bounds_check=bound, oob_is_err=False).then_inc(h, 16)
        s._record((key, h, 16 * (i // K + 1)), r, w)

    def alldeps(s):
        deps = [(k, s.sem[k], s.cnt[k]) for k in s.sem if s.cnt[k] > 0]
        for q, Q in s.dq.items():
            K = len(Q['sems'])
            for j in range(min(K, Q['n'])):
                tot = (Q['n'] - 1 - j) // K + 1
                deps.append((f'd_{q}{j}', Q['sems'][j], 16 * tot))
        return deps

    def barrier(s):
        deps = s.alldeps()
        for en in s.eng:
            s._need(en, [d for d in deps if not (en == 'pe' and d[0] == 'pe')])
        s.res = {}


class _Stop(Exception):
    pass


def build(S, DEPTH, NE, SBK, dbg=None, upto=99):
    NB = S // 512
    NT = S // 128
    NCH = S // 64
    NSB = S // SBK
    CAPB = -(-int(1.5 * 4 * S / NE) // 512)
    CAP = 512 * CAPB
    U32 = mybir.dt.uint32
    nc = bass.Bass("TRN2", target_bir_lowering=False)
    dt = nc.dram_tensor
    xT_in = dt("xT", [D, S], F32, kind="ExternalInput").ap()
    w_in_d = dt("w_in", [DEPTH, D, WA], F32, kind="ExternalInput").ap()
    w_o_d = dt("w_o", [DEPTH, D, D], F32, kind="ExternalInput").ap()
    cvec_d = dt("cvec", [DEPTH, 128, NCV], F32, kind="ExternalInput").ap()
    foxb_d = dt("foxb", [DEPTH, 1, 4], F32, kind="ExternalInput").ap()
    rw_d = dt("rw", [DEPTH, D, NE], F32, kind="ExternalInput").ap()
    rb_d = dt("rb", [DEPTH, 1, NE], F32, kind="ExternalInput").ap()
    w1_d = dt("w1", [DEPTH, NE, D, 2048], F32, kind="ExternalInput").ap()
    b1_d = dt("b1T", [DEPTH, 128, NE * 16], F32, kind="ExternalInput").ap()
    w2_d = dt("w2", [DEPTH, NE, D, D], F32, kind="ExternalInput").ap()
    b2_d = dt("b2", [DEPTH, NE, D], F32, kind="ExternalInput").ap()
    cos_d = dt("ropec", [128, S], F32, kind="ExternalInput").ap()
    sin_d = dt("ropes", [128, S], F32, kind="ExternalInput").ap()
    cst_d = dt("cst", [128, 4 * 128 + 512 + 4 + 128], F32, kind="ExternalInput").ap()
    msk_d = dt("msk", [128, 4 * 512], F32, kind="ExternalInput").ap()
    io_d = dt("iota", [128, 128 + 32], F32, kind="ExternalInput").ap()
    b2T_d = dt("b2T", [DEPTH, 128, NE * 8], F32, kind="ExternalInput").ap()
    outT = dt("outT", [D, S], F32, kind="ExternalOutput").ap()
    okind = {} if dbg is None else {"kind": "ExternalOutput"}
    XT = dt("XT", [D, S], F32).ap()
    XB = dt("XB", [D, S], BF16).ap()
    X1T = dt("X1T", [D, S], F32).ap()
    X1B = dt("X1B", [D, S], BF16).ap()
    MIXT = dt("MIXT", [D, S], BF16, **okind).ap()
    RQ = dt("RQ", [128, S], BF16).ap()
    RK = dt("RK", [128, S], BF16).ap()
    RV = dt("RV", [S, 256], BF16).ap()
    GATE = dt("GATE", [256, S], BF16).ap()
    FQ = dt("FQ", [256, S], BF16).ap()
    FK = dt("FK", [256, S], BF16).ap()
    FV = dt("FV", [S, 256], BF16).ap()
    FF = dt("FF", [4, S], F32).ap()
    FRD = dt("FRD", [7, S], BF16).ap()
    GT = dt("GT", [NE, S], F32, **okind).ap()
    XG = dt("XG", [NE * CAP, D], BF16, **okind).ap()
    SLOTD = dt("SLOTD", [128, NT * 4], U32, **okind).ap()
    GKD = dt("GKD", [128, NT * 4], F32, **okind).ap()
    YS = dt("YS", [NE * CAP, D], F32).ap()

    def chunked(ap):
        return ap.rearrange("(kc p) t -> p kc t", p=128)

    with ExitStack() as es:
        sc = Sched(nc, es)
        uid = [0]

        def sb(name, shape, dtp, st=es):
            uid[0] += 1
            return st.enter_context(nc.sbuf_tensor(f"{name}_s{uid[0]}", shape, dtp))
        PS = [es.enter_context(nc.psum_tensor(f"ps{i}", [128, 512], F32)) for i in range(7)]
        PSB = es.enter_context(nc.psum_tensor("psb", [128, 1024], BF16))
        pctr = [0]

        def bank():
            pctr[0] = (pctr[0] + 1) % 7
            return PS[pctr[0]], f"ps{pctr[0]}"

        cst = sb("cst", [128, 4 * 128 + 512 + 4 + 128], F32)
        sc.dma('sp', cst[:], cst_d[:, :], w=['cst'])
        identb = sb("identb", [128, 128], BF16)
        sc.op('dve', lambda e: e.tensor_copy(identb[:], cst[:, 1028:1156]), r=['cst'], w=['identb'])
        ident = cst[:, 1028:1156]
        onesD = sb("onesD", [128, 128], F32)
        sc.op('pool', lambda e: e.memset(onesD[:], 1.0 / D), w=['onesD'])
        ones256 = sb("ones256", [128, 128], F32)
        sc.op('pool', lambda e: e.memset(ones256[:], 1.0 / 256), w=['ones256'])
        ones64 = sb("ones64", [64, 64], F32)
        sc.op('pool', lambda e: e.memset(ones64[:], 1.0 / 64), w=['ones64'])
        onesb = sb("onesb", [128, 64], BF16)
        sc.op('pool', lambda e: e.memset(onesb[:], 1.0), w=['onesb'])
        cv = sb("cv", [128, NCV], F32)
        iot = sb("iot", [128, 160], F32)
        sc.dma('sp', iot[:], io_d[:, :], w=['iot'])
        ones1 = sb("ones1", [128, 128], F32)
        sc.op('pool', lambda e: e.memset(ones1[:], 1.0), w=['ones1'])
        SLOT = sb("SLOT", [128, NT, 4], U32)
        bnd_reg = nc.gpsimd.to_reg(NE * CAP - 1)
        GK = sb("GK", [128, NT, 4], F32)

        def mm(out, lhsT, rhs, start, stop, r, w, inc=None):
            sc.op('pe', lambda e: e.matmul(out, lhsT, rhs, start=start, stop=stop), r=r, w=w,
                  inc=(stop if inc is None else inc))

        def ln_block(z, zk, gcol, bcol, st):
            pm, pmk = bank()
            for dc in range(8):
                mm(pm[:, :], onesD[:], z[:, dc, :], dc == 0, dc == 7, r=['onesD', zk], w=[pmk])
            pe2, pe2k = bank()
            for dc in range(8):
                sq = st['sq'][dc % 2]
                sqk = st['sqk'] + str(dc % 2)
                sc.op('act', lambda e, dc=dc: e.activation(sq[:], z[:, dc, :], AF.Square), r=[zk], w=[sqk])
                mm(pe2[:, :], onesD[:], sq[:], dc == 0, dc == 7, r=['onesD', sqk], w=[pe2k], inc=True)
            mean, rstd, tmp = st['mean'], st['rstd'], st['tmp']
            mk_, rk_, tk_ = st.get('keys', (st['sqk'] + 'mean', st['sqk'] + 'rstd', st['sqk'] + 'tmp'))
            sc.op('act', lambda e: e.copy(mean[:], pm[:, :]), r=[pmk], w=[mk_])
            sc.op('dve', lambda e: e.tensor_tensor(rstd[:], mean[:], mean[:], ALU.mult), r=[mk_], w=[rk_])
            sc.op('dve', lambda e: e.tensor_tensor(rstd[:], pe2[:, :], rstd[:], ALU.subtract), r=[pe2k, rk_], w=[rk_])
            sc.op('dve', lambda e: e.tensor_scalar(rstd[:], rstd[:], 0.0, EPS, ALU.max, ALU.add), r=[rk_], w=[rk_])
            sc.op('act', lambda e: e.activation(rstd[:], rstd[:], AF.Ln), r=[rk_], w=[rk_])
            sc.op('act', lambda e: e.activation(rstd[:], rstd[:], AF.Exp, scale=-0.5), r=[rk_], w=[rk_])
            for dc in range(8):
                sc.op('dve', lambda e, dc=dc: e.tensor_tensor(tmp[:], z[:, dc, :], mean[:], ALU.subtract), r=[zk, mk_], w=[tk_])
                sc.op('dve', lambda e, dc=dc: e.tensor_tensor(tmp[:], tmp[:], rstd[:], ALU.mult), r=[tk_, rk_], w=[tk_])
                sc.op('act', lambda e, dc=dc: e.activation(z[:, dc, :], tmp[:], AF.Identity,
                                                          bias=cv[:, bcol + dc:bcol + dc + 1], scale=cv[:, gcol + dc:gcol + dc + 1]),
                      r=[tk_, 'cv'], w=[zk])

        with ExitStack() as st0:
            xf = [sb(f"xf{i}", [128, 8, 512], F32, st0) for i in range(2)]
            zt = sb("zt", [128, 4096], BF16, st0)
            sc.op('pool', lambda e: e.memset(zt[:], 0.0), w=['zt'])
            XGz = XG.rearrange("(n p r) d -> n p (r d)", p=128, r=4)
            for n_ in range(NE * CAP // 512):
                sc.dma('sp', XGz[n_], zt[:], r=['zt'], w=[('XGz', n_)])
            xh = [sb(f"xh{i}", [128, 8, 512], BF16, st0) for i in range(2)]
            for tb in range(NB):
                p = tb % 2
                sl = slice(tb * 512, tb * 512 + 512)
                sc.dma('sp', xf[p][:], chunked(xT_in)[:, :, sl], w=[f'xf{p}'])
                sc.op('dve', lambda e, p=p: e.tensor_copy(xh[p][:], xf[p][:]), r=[f'xf{p}'], w=[f'xh{p}'])
                sc.dma('sp', chunked(XT)[:, :, sl], xf[p][:], r=[f'xf{p}'], w=[('XT', tb)])
                sc.dma('sp', chunked(XB)[:, :, sl], xh[p][:], r=[f'xh{p}'], w=[('XB', tb)])
            sc.barrier()

        stop = [False]
        try:
          for l in range(DEPTH):
            last = l == DEPTH - 1
            sc.dma('sp', cv[:], cvec_d[l, :, :], w=['cv'])
            with ExitStack() as st1:
              for _once in ([0] if not stop[0] else []):
                win = sb("win", [128, 8, WA], BF16, st1)
                for kc in range(8):
                    sc.dma('pool', win[:, kc, :], w_in_d[l, kc * 128:(kc + 1) * 128, :], w=[('win', kc)])
                WK = [('win', kc) for kc in range(8)]
                xbt = [sb(f"xbt{i}", [128, 8, 544], BF16, st1) for i in range(2)]
                cs_t = sb("cs_t", [128, 512], F32, st1)
                sn_t = sb("sn_t", [128, 512], F32, st1)
                t1 = sb("t1", [128, 512], F32, st1)
                t2 = sb("t2", [128, 512], F32, st1)
                ob = [sb(f"ob{i}", [128, 512], BF16, st1) for i in range(4)]
                obc = [0]
                u_t = [sb(f"u{i}", [128, 544], F32, st1) for i in range(2)]
                sg_t = sb("sg_t", [128, 544], F32, st1)
                ca_t = [sb(f"ca{i}", [128, 512], F32, st1) for i in range(2)]
                sq1 = sb("sq1", [128, 512], F32, st1)
                mean1 = sb("mean1", [128, 512], F32, st1)
                rstd1 = sb("rstd1", [128, 512], F32, st1)
                ff_t = sb("ff_t", [4, 512], F32, st1)
                vt = [sb(f"vt{i}", [128, 256], BF16, st1) for i in range(2)]
                vtc = [0]

                def nob():
                    obc[0] = (obc[0] + 1) % 4
                    return ob[obc[0]], f"ob{obc[0]}"

                for tb in range(NB):
                    t0 = tb * 512
                    p = tb % 2
                    xk = f'xbt{p}'
                    x_ = xbt[p]
                    if tb == 0:
                        sc.op('pool', lambda e: e.memset(x_[:, :, 0:32], 0.0), w=[xk])
                        sc.dma('sp', x_[:, :, 32:544], chunked(XB)[:, :, 0:512], r=[('XB', 0)], w=[xk])
                    else:
                        sc.dma('sp', x_[:, :, :], chunked(XB)[:, :, t0 - 32:t0 + 512], r=[('XB', tb - 1), ('XB', tb)], w=[xk])
                    sc.dma('sp', cs_t[:], cos_d[:, t0:t0 + 512], w=['cs_t'])
                    sc.dma('sp', sn_t[:], sin_d[:, t0:t0 + 512], w=['sn_t'])

                    def fm(c0, ncols=128, halo=False):
                        pm, pmk = bank()
                        for kc in range(8):
                            mm(pm[0:ncols, :], win[:, kc, c0:c0 + ncols], x_[:, kc, 32:544], kc == 0, kc == 7, r=[WK[kc], xk], w=[pmk])
                        if not halo:
                            return pm, pmk
                        ph, phk = bank()
                        for kc in range(8):
                            mm(ph[0:ncols, 0:32], win[:, kc, c0:c0 + ncols], x_[:, kc, 0:32], kc == 0, kc == 7, r=[WK[kc], xk], w=[phk])
                        return pm, pmk, ph, phk

                    for (c0, cp, dst, dk) in ((0, 2820, RQ, 'RQ'), (128, 2948, RK, 'RK')):
                        pa, pak = fm(c0)
                        pp, ppk = fm(cp)
                        sc.op('dve', lambda e: e.tensor_tensor(t1[:], pa[:, :], cs_t[:], ALU.mult), r=[pak, 'cs_t'], w=['t1'])
                        sc.op('dve', lambda e: e.tensor_tensor(t2[:], pp[:, :], sn_t[:], ALU.mult), r=[ppk, 'sn_t'], w=['t2'])
                        o, ok = nob()
                        sc.op('dve', lambda e: e.tensor_tensor(o[:], t1[:], t2[:], ALU.add), r=['t1', 't2'], w=[ok])
                        sc.dma('sp', dst[:, t0:t0 + 512], o[:], r=[ok], w=[(dk, tb)])
                    for c in range(2):
                        pa, pak = fm(512 + 128 * c)
                        o, ok = nob()
                        sc.op('act', lambda e: e.activation(o[:], pa[:, :], AF.Silu), r=[pak], w=[ok])
                        sc.dma('sp', GATE[128 * c:128 * c + 128, t0:t0 + 512], o[:], r=[ok], w=[('GATE', tb)])
                    for c in range(2):
                        pa, pak, pah, pahk = fm(768 + 128 * c, halo=True)
                        pb_, pbk, pbh, pbhk = fm(1024 + 128 * c, halo=True)
                        u = u_t[c]
                        uk = f'u{c}'
                        sc.op('act', lambda e: e.activation(sg_t[:, 32:544], pb_[:, :], AF.Sigmoid), r=[pbk], w=['sg_t'])
                        sc.op('act', lambda e: e.activation(sg_t[:, 0:32], pbh[:, 0:32], AF.Sigmoid), r=[pbhk, 'sg_t'], w=['sg_t'])
                        sc.op('dve', lambda e: e.tensor_tensor(u[:, 32:544], pa[:, :], sg_t[:, 32:544], ALU.mult), r=[pak, 'sg_t'], w=[uk])
                        sc.op('dve', lambda e: e.tensor_tensor(u[:, 0:32], pah[:, 0:32], sg_t[:, 0:32], ALU.mult), r=[pahk, 'sg_t', uk], w=[uk])
                        ce = 'dve'
                        ca = ca_t[c]
                        cak = f'ca{c}'
                        wc0 = 31 * c
                        sc.op(ce, lambda e: e.tensor_scalar(ca[:], u[:, 2:514], cv[:, wc0:wc0 + 1], cv[:, 68 + c:69 + c], ALU.mult, ALU.add),
                              r=[uk, 'cv'], w=[cak])
                        for j in range(1, 31):
                            sc.op(ce, lambda e, j=j: e.scalar_tensor_tensor(ca[:], u[:, 2 + j:514 + j], cv[:, wc0 + j:wc0 + j + 1], ca[:], ALU.mult, ALU.add),
                                  r=[uk, 'cv', cak], w=[cak])
                    pm, pmk = bank()
                    pe2, pe2k = bank()
                    for c in range(2):
                        mm(pm[:, :], ones256[:], ca_t[c][:], c == 0, c == 1, r=['ones256', f'ca{c}'], w=[pmk])
                    for c in range(2):
                        sc.op('act', lambda e, c=c: e.activation(sq1[:], ca_t[c][:], AF.Square), r=[f'ca{c}'], w=['sq1'])
                        mm(pe2[:, :], ones256[:], sq1[:], c == 0, c == 1, r=['ones256', 'sq1'], w=[pe2k], inc=True)
                    sc.op('act', lambda e: e.copy(mean1[:], pm[:, :]), r=[pmk], w=['mean1'])
                    sc.op('dve', lambda e: e.tensor_tensor(rstd1[:], mean1[:], mean1[:], ALU.mult), r=['mean1'], w=['rstd1'])
                    sc.op('dve', lambda e: e.tensor_tensor(rstd1[:], pe2[:, :], rstd1[:], ALU.subtract), r=[pe2k, 'rstd1'], w=['rstd1'])
                    sc.op('dve', lambda e: e.tensor_scalar(rstd1[:], rstd1[:], 0.0, EPS, ALU.max, ALU.add), r=['rstd1'], w=['rstd1'])
                    sc.op('act', lambda e: e.activation(rstd1[:], rstd1[:], AF.Ln), r=['rstd1'], w=['rstd1'])
                    sc.op('act', lambda e: e.activation(rstd1[:], rstd1[:], AF.Exp, scale=-0.5), r=['rstd1'], w=['rstd1'])
                    for c in range(2):
                        sc.op('dve', lambda e, c=c: e.tensor_tensor(t1[:], ca_t[c][:], mean1[:], ALU.subtract), r=[f'ca{c}', 'mean1'], w=['t1'])
                        sc.op('dve', lambda e: e.tensor_tensor(t1[:], t1[:], rstd1[:], ALU.mult), r=['t1', 'rstd1'], w=['t1'])
                        o, ok = nob()
                        sc.op('act', lambda e, c=c: e.activation(o[:], t1[:], AF.Silu, bias=cv[:, 72 + c:73 + c], scale=cv[:, 70 + c:71 + c]),
                              r=['t1', 'cv'], w=[ok])
                        sc.dma('sp', MIXT[256 + 128 * c:384 + 128 * c, t0:t0 + 512], o[:], r=[ok], w=[('MIXT', tb)])
                    for c in range(2):
                        pc, pck, pch, pchk = fm(1536 + 128 * c, halo=True)
                        ph_, phk_, phh, phhk = fm(1792 + 128 * c, halo=True)
                        u = u_t[c]
                        uk = f'u{c}'
                        sc.op('act', lambda e: e.copy(sg_t[:, 32:544], pc[:, :]), r=[pck], w=['sg_t'])
                        sc.op('act', lambda e: e.copy(sg_t[:, 0:32], pch[:, 0:32]), r=[pchk, 'sg_t'], w=['sg_t'])
                        sc.op('dve', lambda e: e.tensor_tensor(u[:, 32:544], ph_[:, :], sg_t[:, 32:544], ALU.mult), r=[phk_, 'sg_t'], w=[uk])
                        sc.op('dve', lambda e: e.tensor_tensor(u[:, 0:32], phh[:, 0:32], sg_t[:, 0:32], ALU.mult), r=[phhk, 'sg_t', uk], w=[uk])
                        wc0 = 62 + 3 * c
                        sc.op('dve', lambda e: e.tensor_scalar(t1[:], u[:, 30:542], cv[:, wc0:wc0 + 1], None, ALU.mult), r=[uk, 'cv'], w=['t1'])
                        for j in (1, 2):
                            sc.op('dve', lambda e, j=j: e.scalar_tensor_tensor(t1[:], u[:, 30 + j:542 + j], cv[:, wc0 + j:wc0 + j + 1], t1[:], ALU.mult, ALU.add),
                                  r=[uk, 'cv', 't1'], w=['t1'])
                        pbb, pbbk = fm(1280 + 128 * c)
                        o, ok = nob()
                        sc.op('dve', lambda e: e.tensor_tensor(o[:], pbb[:, :], t1[:], ALU.mult), r=[pbbk, 't1'], w=[ok])
                        sc.dma('sp', MIXT[512 + 128 * c:640 + 128 * c, t0:t0 + 512], o[:], r=[ok], w=[('MIXT', tb)])
                    for c in range(2):
                        pa, pak = fm(2048 + 128 * c)
                        o, ok = nob()
                        sc.op('act', lambda e: e.mul(o[:], pa[:, :], 0.125), r=[pak], w=[ok])
                        sc.dma('sp', FQ[128 * c:128 * c + 128, t0:t0 + 512], o[:], r=[ok], w=[('FQ', tb)])
                        pa, pak = fm(2304 + 128 * c)
                        o, ok = nob()
                        sc.op('act', lambda e: e.copy(o[:], pa[:, :]), r=[pak], w=[ok])
                        sc.dma('sp', FK[128 * c:128 * c + 128, t0:t0 + 512], o[:], r=[ok], w=[('FK', tb)])
                    pa, pak = fm(2816, ncols=4)
                    sc.op('act', lambda e: e.copy(ff_t[:], pa[0:4, :]), r=[pak], w=['ff_t'])
                    sc.dma('sp', FF[:, t0:t0 + 512], ff_t[:], r=['ff_t'], w=[('FF', tb)])
                    for sub in range(4):
                        for (c0, dst, dk) in ((256, RV, 'RV'), (2560, FV, 'FV')):
                            pm, pmk = bank()
                            for kc in range(8):
                                mm(pm[:, 0:256], x_[:, kc, 32 + 128 * sub:160 + 128 * sub], win[:, kc, c0:c0 + 256], kc == 0, kc == 7, r=[WK[kc], xk], w=[pmk])
                            vtc[0] ^= 1
                            v = vt[vtc[0]]
                            vk = f'vt{vtc[0]}'
                            sc.op('act', lambda e: e.copy(v[:], pm[:, 0:256]), r=[pmk], w=[vk])
                            sc.dma('sp', dst[t0 + 128 * sub:t0 + 128 * sub + 128, :], v[:], r=[vk], w=[(dk, tb)])
                sc.barrier()
                if upto == 1:
                    stop[0] = True

            with ExitStack() as st2:
              for _once in ([0] if not stop[0] else []):
                rq_t = sb("rq_t", [32, S], BF16, st2)
                rk_t = sb("rk_t", [32, S], BF16, st2)
                qd_t = sb("qd_t", [32, S], BF16, st2)
                v_t = sb("v_t", [128, NT, 64], BF16, st2)
                v64 = sb("v64", [64, NCH, 64], BF16, st2)
                qdec = sb("qdec", [32, 512], F32, st2)
                stt = sb("stt", [32, 64], F32, st2)
                prevb = sb("prevb", [32, NCH, 64], BF16, st2)
                kd = [sb(f"kd{i}", [128, 32], BF16, st2) for i in range(2)]
                sd = [sb(f"sd{i}", [128, 128], BF16, st2) for i in range(2)]
                o_sb = sb("o_sb", [64, 512], F32, st2)
                sq2 = sb("sq2", [64, 512], F32, st2)
                mean2 = sb("mean2", [64, 512], F32, st2)
                rstd2 = sb("rstd2", [64, 512], F32, st2)
                g_t = sb("g_t", [64, 512], BF16, st2)
                ro = [sb(f"ro{i}", [64, 512], BF16, st2) for i in range(2)]
                for h in range(4):
                    gam = 1.0 - 2.0 ** (-5.0 - h)
                    cd = gam ** CHUNK
                    sc.dma('sp', rq_t[:], RQ[32 * h:32 * h + 32, :], w=['rq_t'])
                    sc.dma('sp', rk_t[:], RK[32 * h:32 * h + 32, :], w=['rk_t'])
                    sc.dma('sp', v_t[:], RV[:, 64 * h:64 * h + 64].rearrange("(j p) e -> p j e", p=128), w=['v_t'])
                    sc.dma('sp', qdec[:], cst_d[32 * h:32 * h + 32, 512:1024], w=['qdec'])
                    for tb in range(NB):
                        sc.op('dve', lambda e, tb=tb: e.tensor_tensor(qd_t[:, tb * 512:tb * 512 + 512], rq_t[:, tb * 512:tb * 512 + 512], qdec[:], ALU.mult),
                              r=['rq_t', 'qdec'], w=['qd_t'])
                    sc.op('dve', lambda e: e.memset(stt[:], 0.0), w=['stt'])
                    sc.dma('sp', v64[:], RV[:, 64 * h:64 * h + 64].rearrange("(c p) e -> p c e", p=64), w=['v64'])
                    for c in range(NCH):
                        sc.op('pe', lambda e, c=c: e.transpose(PSB[0:64, 0:32], rk_t[:, 64 * c:64 * c + 64], identb[0:32, 0:32]),
                              r=['rk_t', 'identb'], w=['psb'])
                        k_ = kd[c % 2]
                        kk = f'kd{c % 2}'
                        sc.op('dve', lambda e: e.tensor_scalar(k_[0:64, :], PSB[0:64, 0:32], cst[0:64, 1024 + h:1025 + h], None, ALU.mult), r=['psb', 'cst'], w=[kk])
                        pkv, pkvk = bank()
                        mm(pkv[0:32, 0:64], k_[0:64, :], v64[:, c, :], True, True, r=[kk, 'v64'], w=[pkvk])
                        sc.op('dve', lambda e, c=c: e.tensor_copy(prevb[:, c, :], stt[:]), r=['stt'], w=['prevb'])
                        sc.op('dve', lambda e: e.scalar_tensor_tensor(stt[:], stt[:], cd, pkv[0:32, 0:64], ALU.mult, ALU.add),
                              r=['stt', pkvk], w=['stt'])
                    for tb in range(NB if KN >= 5 else 0):
                        t0 = tb * 512
                        po, pok = bank()
                        for jj in range(4):
                            j = tb * 4 + jj
                            ps_, psk = bank()
                            mm(ps_[:, 0:128], rk_t[:, 128 * j:128 * j + 128], rq_t[:, 128 * j:128 * j + 128], True, True, r=['rk_t', 'rq_t'], w=[psk])
                            s_ = sd[j % 2]
                            sk = f'sd{j % 2}'
                            sc.op('dve', lambda e: e.tensor_tensor(s_[:], ps_[:, 0:128], cst[:, 128 * h:128 * h + 128], ALU.mult), r=[psk, 'cst'], w=[sk])
                            mm(po[0:64, 128 * jj:128 * jj + 128], v_t[:, j, :], s_[:], True, False, r=['v_t', sk], w=[pok], inc=False)
                            for hf in range(2):
                                c0 = 128 * jj + 64 * hf
                                mm(po[0:64, c0:c0 + 64], prevb[:, 2 * j + hf, :], qd_t[:, 128 * j + 64 * hf:128 * j + 64 * hf + 64], False, hf == 1,
                                   r=['prevb', 'qd_t'], w=[pok], inc=(hf == 1))
                        sc.op('act', lambda e: e.copy(o_sb[:], po[0:64, :]), r=[pok], w=['o_sb'])
                        sc.op('act', lambda e: e.activation(sq2[:], po[0:64, :], AF.Square), r=[pok], w=['sq2'])
                        pm, pmk = bank()
                        mm(pm[0:64, :], ones64[:], o_sb[:], True, True, r=['ones64', 'o_sb'], w=[pmk])
                        pe2, pe2k = bank()
                        mm(pe2[0:64, :], ones64[:], sq2[:], True, True, r=['ones64', 'sq2'], w=[pe2k])
                        sc.op('act', lambda e: e.copy(mean2[:], pm[0:64, :]), r=[pmk], w=['mean2'])
                        sc.op('dve', lambda e: e.tensor_tensor(rstd2[:], mean2[:], mean2[:], ALU.mult), r=['mean2'], w=['rstd2'])
                        sc.op('dve', lambda e: e.tensor_tensor(rstd2[:], pe2[0:64, :], rstd2[:], ALU.subtract), r=[pe2k, 'rstd2'], w=['rstd2'])
                        sc.op('dve', lambda e: e.tensor_scalar(rstd2[:], rstd2[:], 0.0, EPS, ALU.max, ALU.add), r=['rstd2'], w=['rstd2'])
                        sc.op('act', lambda e: e.activation(rstd2[:], rstd2[:], AF.Ln), r=['rstd2'], w=['rstd2'])
                        sc.op('act', lambda e: e.activation(rstd2[:], rstd2[:], AF.Exp, scale=-0.5), r=['rstd2'], w=['rstd2'])
                        sc.dma('sp', g_t[:], GATE[64 * h:64 * h + 64, t0:t0 + 512], w=['g_t'])
                        sc.op('dve', lambda e: e.tensor_tensor(o_sb[:], o_sb[:], mean2[:], ALU.subtract), r=['o_sb', 'mean2'], w=['o_sb'])
                        sc.op('dve', lambda e: e.tensor_tensor(o_sb[:], o_sb[:], rstd2[:], ALU.mult), r=['o_sb', 'rstd2'], w=['o_sb'])
                        sc.op('dve', lambda e: e.tensor_scalar(o_sb[:], o_sb[:], cv[0:64, 74 + h:75 + h], None, ALU.mult), r=['o_sb', 'cv'], w=['o_sb'])
                        r_ = ro[tb % 2]
                        rk_ = f'ro{tb % 2}'
                        sc.op('dve', lambda e: e.tensor_tensor(r_[:], o_sb[:], g_t[:], ALU.mult), r=['o_sb', 'g_t'], w=[rk_])
                        sc.dma('sp', MIXT[64 * h:64 * h + 64, t0:t0 + 512], r_[:], r=[rk_], w=[('MIXT2', h, tb)])
                sc.barrier()
                if upto == 2:
                    stop[0] = True

            with ExitStack() as st3:
              for _once in ([0] if not stop[0] else []):
                qa = sb("qa", [70, S], BF16, st3)
                ka = sb("ka", [70, S], BF16, st3)
                fv_t = sb("fv_t", [128, NT, 64], BF16, st3)
                msk = sb("msk", [128, 4 * 512], F32, st3)
                sc.dma('sp', msk[:], msk_d[:, :], w=['msk'])
                fb = sb("fb", [1, 4], F32, st3)
                sc.dma('sp', fb[:], foxb_d[l, :, :], w=['fb'])
                sc.op('dve', lambda e: e.tensor_scalar(fb[:], fb[:], -1.0, None, ALU.mult), r=['fb'], w=['fb'])
                FW = min(S, 2048)
                f_r = sb("f_r", [1, FW], F32, st3)
                F_r = sb("F_r", [1, FW], F32, st3)
                one_r = sb("one_r", [1, FW], F32, st3)
                sc.op('pool', lambda e: e.memset(one_r[:], 1.0), w=['one_r'])
                fr = sb("fr", [1, 7, FW], BF16, st3)
                r1 = sb("r1", [1, FW], F32, st3)
                r2 = sb("r2", [1, FW], F32, st3)
                Fl = sb("Fl", [1, 1], F32, st3)
                pt = [sb(f"pt{i}", [128, 512], BF16, st3) for i in range(3)]
                rden = sb("rden", [64, 512], F32, st3)
                fo = [sb(f"fo{i}", [64, 512], BF16, st3) for i in range(2)]
                for h in range(4):
                    sc.op('dve', lambda e: e.memset(Fl[:], 0.0), w=['Fl'])
                    for pc in range(S // FW):
                        sl = slice(pc * FW, pc * FW + FW)
                        sc.dma('sp', f_r[:], FF[h:h + 1, sl], w=['f_r'])
                        sc.op('act', lambda e: e.activation(f_r[:], f_r[:], AF.Exp, bias=fb[0:1, h:h + 1], scale=-1.0), r=['f_r', 'fb'], w=['f_r'])
                        sc.op('act', lambda e: e.activation(f_r[:], f_r[:], AF.Ln, bias=1.0), r=['f_r'], w=['f_r'])
                        sc.op('dve', lambda e: e.tensor_scalar(f_r[:], f_r[:], -1.0, None, ALU.mult), r=['f_r'], w=['f_r'])
                        sc.op('dve', lambda e: e.tensor_tensor_scan(F_r[:], one_r[:], f_r[:], Fl[0:1, 0:1], ALU.mult, ALU.add), r=['one_r', 'f_r', 'Fl'], w=['F_r'])
                        sc.op('dve', lambda e: e.tensor_copy(Fl[:], F_r[0:1, FW - 1:FW]), r=['F_r'], w=['Fl'])
                        sc.op('dve', lambda e: e.tensor_copy(fr[:, 0, :], F_r[:]), r=['F_r'], w=['fr'])
                        sc.op('dve', lambda e: e.tensor_tensor(r1[:], F_r[:], fr[:, 0, :], ALU.subtract), r=['F_r', 'fr'], w=['r1'])
                        sc.op('dve', lambda e: e.tensor_copy(fr[:, 1, :], r1[:]), r=['r1', 'fr'], w=['fr'])
                        sc.op('dve', lambda e: e.tensor_tensor(r2[:], r1[:], fr[:, 1, :], ALU.subtract), r=['r1', 'fr'], w=['r2'])
                        sc.op('dve', lambda e: e.tensor_copy(fr[:, 2, :], r2[:]), r=['r2', 'fr'], w=['fr'])
                        sc.op('dve', lambda e: e.tensor_copy(fr[:, 3, :], one_r[:]), r=['one_r', 'fr'], w=['fr'])
                        for i in range(3):
                            sc.op('dve', lambda e, i=i: e.tensor_scalar(fr[:, 4 + i, :], fr[:, i, :], -1.0, None, ALU.mult), r=['fr'], w=['fr'])
                        sc.dma('sp', FRD[:, sl].rearrange("(o r) t -> o r t", o=1), fr[:], r=['fr'], w=[('FRD', pc)])
                    FRk = [('FRD', pc) for pc in range(S // FW)]
                    sc.dma('sp', qa[0:64, :], FQ[64 * h:64 * h + 64, :], w=['qa'])
                    sc.dma('sp', qa[64:67, :], FRD[0:3, :], r=FRk + ['qa'], w=['qa'])
                    for i in range(3):
                        sc.dma('sp', qa[67 + i:68 + i, :], FRD[3:4, :], r=FRk + ['qa'], w=['qa'])
                    sc.dma('sp', ka[0:64, :], FK[64 * h:64 * h + 64, :], w=['ka'])
                    for i in range(3):
                        sc.dma('sp', ka[64 + i:65 + i, :], FRD[3:4, :], r=FRk + ['ka'], w=['ka'])
                    sc.dma('sp', ka[67:70, :], FRD[4:7, :], r=FRk + ['ka'], w=['ka'])
                    sc.dma('sp', fv_t[:], FV[:, 64 * h:64 * h + 64].rearrange("(j p) e -> p j e", p=128), w=['fv_t'])
                    pi = 0
                    for qb in range(NB):
                        q0 = qb * 512
                        pn, pnk = PS[5], 'ps5'
                        pd, pdk = PS[6], 'ps6'
                        nk = 4 * qb + 4
                        def score(j):
                            mm(PS[j % 4][:, :], ka[:, 128 * j:128 * j + 128], qa[:, q0:q0 + 512], True, True, r=['ka', 'qa'], w=[f'ps{j % 4}'])

                        score(0)
                        for j in range(nk):
                            if j + 1 < nk:
                                score(j + 1)
                            ps_ = PS[j % 4]
                            psk = f'ps{j % 4}'
                            p_ = pt[pi % 3]
                            pk = f'pt{pi % 3}'
                            pi += 1
                            sc.op('act', lambda e: e.activation(p_[:], ps_[:, :], AF.Exp), r=[psk], w=[pk])
                            if j >= 4 * qb:
                                m = j - 4 * qb
                                sc.op('dve', lambda e: e.tensor_tensor(p_[:], p_[:], msk[:, 512 * m:512 * m + 512], ALU.mult), r=[pk, 'msk'], w=[pk])
                            mm(pn[0:64, :], fv_t[:, j, :], p_[:], j == 0, j == nk - 1, r=['fv_t', pk], w=[pnk], inc=False)
                            mm(pd[0:64, :], onesb[:], p_[:], j == 0, j == nk - 1, r=['onesb', pk], w=[pdk], inc=True)
                        sc.op('dve', lambda e: e.reciprocal(rden[:], pd[0:64, :]), r=[pdk], w=['rden'])
                        f_ = fo[qb % 2]
                        fk_ = f'fo{qb % 2}'
                        sc.op('dve', lambda e: e.tensor_tensor(f_[:], pn[0:64, :], rden[:], ALU.mult), r=[pnk, 'rden'], w=[fk_])
                        sc.dma('sp', MIXT[768 + 64 * h:832 + 64 * h, q0:q0 + 512], f_[:], r=[fk_], w=[('MIXT3', h, qb)])
                sc.barrier()
                if upto == 3:
                    stop[0] = True

            with ExitStack() as st4:
              for _once in ([0] if not stop[0] else []):
                wo = sb("wo", [128, 8, D], BF16, st4)
                for kc in range(8):
                    sc.dma('pool', wo[:, kc, :], w_o_d[l, kc * 128:(kc + 1) * 128, :], w=[('wo', kc)])
                rwt = sb("rwt", [128, 8, NE], F32, st4)
                sc.dma('sp', rwt[:], rw_d[l].rearrange("(kc p) e -> p kc e", p=128), w=['rwt'])
                rbt = sb("rbt", [128, NE], F32, st4)
                sc.dma('sp', rbt[:], rb_d[l, 0:1, :].partition_broadcast(128), w=['rbt'])
                mx = [sb(f"mx{i}", [128, 8, 512], BF16, st4) for i in range(2)]
                xr = [sb(f"xr{i}", [128, 8, 512], F32, st4) for i in range(2)]
                z = sb("z", [128, 8, 512], F32, st4)
                stl = dict(sq=[sb(f"sq4{i}", [128, 512], F32, st4) for i in range(2)], sqk='s4', mean=sb("mean", [128, 512], F32, st4),
                           rstd=sb("rstd", [128, 512], F32, st4), tmp=sb("tmp", [128, 512], F32, st4))
                x1f = z
                x1h = sb("x1h", [128, 8, 512], BF16, st4)
                lg = sb("lg", [128, NE], F32, st4)
                m8 = sb("m8", [128, 8], F32, st4)
                nmx = sb("nmx", [128, 1], F32, st4)
                mk = sb("mk", [128, NE], F32, st4)
                ex = sb("ex", [128, NE], F32, st4)
                ssum = sb("ssum", [128, 1], F32, st4)
                gtt = sb("gtt", [NE, 512], F32, st4)
                xtm = [sb(f"xtm{i}", [128, D], BF16, st4) for i in range(2)]
                cnt = sb("cnt", [128, NE], F32, st4)
                sc.op('dve', lambda e: e.memset(cnt[:], 0.0), w=['cnt'])
                posf = sb("posf", [128, NE], F32, st4)
                ohf = sb("ohf", [128, NE], F32, st4)
                idx8 = sb("idx8", [128, 8], U32, st4)
                idxf = sb("idxf", [128, 8], F32, st4)
                pos4 = sb("pos4", [128, 4], F32, st4)
                ov4 = sb("ov4", [128, 4], F32, st4)
                e4 = sb("e4", [128, 4], F32, st4)
                for tb in range(NB):
                    t0 = tb * 512
                    p = tb % 2
                    sl = slice(t0, t0 + 512)
                    sc.dma('sp', mx[p][:], chunked(MIXT)[:, :, sl], w=[f'mx{p}'])
                    sc.dma('sp', xr[p][:], chunked(XT)[:, :, sl], w=[f'xr{p}'])
                    for dc in range(8):
                        pm, pmk = bank()
                        for kc in range(8):
                            mm(pm[:, :], wo[:, kc, 128 * dc:128 * dc + 128], mx[p][:, kc, :], kc == 0, kc == 7, r=[('wo', kc), f'mx{p}'], w=[pmk])
                        sc.op('dve', lambda e, dc=dc: e.scalar_tensor_tensor(z[:, dc, :], xr[p][:, dc, :], ALPHA, pm[:, :], ALU.mult, ALU.add),
                              r=[f'xr{p}', pmk], w=['z'])
                    ln_block(z, 'z', 78, 86, stl)
                    sc.op('dve', lambda e: e.tensor_copy(x1h[:], z[:]), r=['z'], w=['x1h'])
                    sc.dma('sp', chunked(X1T)[:, :, sl], x1f[:], r=['z'], w=[('X1T', tb)])
                    sc.dma('sp', chunked(X1B)[:, :, sl], x1h[:], r=['x1h'], w=[('X1B', tb)])
                    for sub in range(4):
                        pm, pmk = bank()
                        for kc in range(8):
                            mm(pm[:, 0:NE], x1f[:, kc, 128 * sub:128 * sub + 128], rwt[:, kc, :], kc == 0, kc == 7, r=['z', 'rwt'], w=[pmk])
                        sc.op('dve', lambda e: e.tensor_tensor(lg[:], pm[:, 0:NE], rbt[:], ALU.add), r=[pmk, 'rbt'], w=['lg'])
                        sc.op('dve', lambda e: e.max(m8[:], lg[:]), r=['lg'], w=['m8'])
                        sc.op('dve', lambda e: e.tensor_scalar(mk[:], lg[:], m8[:, 3:4], None, ALU.is_ge), r=['lg', 'm8'], w=['mk'])
                        sc.op('dve', lambda e: e.tensor_scalar(nmx[:], m8[:, 0:1], -1.0, None, ALU.mult), r=['m8'], w=['nmx'])
                        sc.op('act', lambda e: e.activation(ex[:], lg[:], AF.Exp, bias=nmx[:, 0:1], scale=1.0), r=['lg', 'nmx'], w=['ex'])
                        sc.op('dve', lambda e: e.tensor_tensor(ex[:], ex[:], mk[:], ALU.mult), r=['ex', 'mk'], w=['ex'])
                        sc.op('dve', lambda e: e.tensor_reduce(ssum[:], ex[:], AX.X, ALU.add), r=['ex'], w=['ssum'])
                        sc.op('dve', lambda e: e.reciprocal(ssum[:], ssum[:]), r=['ssum'], w=['ssum'])
                        sc.op('dve', lambda e: e.tensor_scalar(ex[:], ex[:], ssum[:, 0:1], None, ALU.mult), r=['ex', 'ssum'], w=['ex'])
                        tt_ = tb * 4 + sub
                        sc.op('dve', lambda e: e.max_index(idx8[:], m8[:], lg[:]), r=['m8', 'lg'], w=['idx8'])
                        sc.op('dve', lambda e: e.tensor_copy(idxf[:], idx8[:]), r=['idx8'], w=['idxf'])
                        sc.op('act', lambda e: e.activation(e4[:], m8[:, 0:4], AF.Exp, bias=nmx[:, 0:1], scale=1.0), r=['m8', 'nmx'], w=['e4'])
                        sc.op('dve', lambda e: e.tensor_scalar(GK[:, tt_, :], e4[:], ssum[:, 0:1], None, ALU.mult), r=['e4', 'ssum'], w=['GK'])
                        pp, ppk = bank()
                        mm(pp[:, 0:NE], iot[:, 0:128], mk[:], True, True, r=['iot', 'mk'], w=[ppk])
                        sc.op('dve', lambda e: e.tensor_tensor(posf[:], pp[:, 0:NE], cnt[:], ALU.add), r=[ppk, 'cnt'], w=['posf'])
                        pt_, ptk = bank()
                        mm(pt_[:, 0:NE], ones1[:], mk[:], True, True, r=['ones1', 'mk'], w=[ptk])
                        sc.op('dve', lambda e: e.tensor_tensor(cnt[:], cnt[:], pt_[:, 0:NE], ALU.add), r=['cnt', ptk], w=['cnt'])
                        for k in range(4):
                            sc.op('dve', lambda e, k=k: e.tensor_scalar(ohf[:], iot[:, 128:128 + NE], idxf[:, k:k + 1], None, ALU.is_equal), r=['iot', 'idxf'], w=['ohf'])
                            sc.op('dve', lambda e: e.tensor_tensor(ohf[:], ohf[:], posf[:], ALU.mult), r=['ohf', 'posf'], w=['ohf'])
                            sc.op('dve', lambda e, k=k: e.tensor_reduce(pos4[:, k:k + 1], ohf[:], AX.X, ALU.add), r=['ohf'], w=['pos4'])
                        sc.op('dve', lambda e: e.tensor_scalar(ov4[:], pos4[:], float(CAP), 1.0e6, ALU.is_ge, ALU.mult), r=['pos4'], w=['ov4'])
                        sc.op('dve', lambda e: e.scalar_tensor_tensor(pos4[:], idxf[:, 0:4], float(CAP), pos4[:], ALU.mult, ALU.add), r=['idxf', 'pos4'], w=['pos4'])
                        sc.op('dve', lambda e: e.tensor_tensor(pos4[:], pos4[:], ov4[:], ALU.add), r=['pos4', 'ov4'], w=['pos4'])
                        sc.op('dve', lambda e: e.tensor_copy(SLOT[:, tt_, :], pos4[:]), r=['pos4'], w=['SLOT'])
                        for kc in range(8):
                            sc.op('pe', lambda e, kc=kc: e.transpose(PSB[:, 128 * kc:128 * kc + 128], x1h[:, kc, 128 * sub:128 * sub + 128], identb[:, :]),
                                  r=['x1h', 'identb'], w=['psb'], inc=(kc == 7))
                        xt_ = xtm[sub % 2]
                        xtk = f'xtm{sub % 2}'
                        sc.op('act', lambda e: e.copy(xt_[:], PSB[:, :]), r=['psb'], w=[xtk])
                        for k in range(4):
                            sc.idma(XG[:, :], bass.IndirectOffsetOnAxis(ap=SLOT[:, tt_, k:k + 1], axis=0), xt_[:, :], None, bnd_reg,
                                    r=[xtk, 'SLOT'], w=[('XG', tt_, k)])
                        pg, pgk = bank()
                        mm(pg[0:NE, 0:128], ex[:], ident, True, True, r=['ex', 'cst'], w=[pgk])
                        sc.op('act', lambda e, sub=sub: e.copy(gtt[:, 128 * sub:128 * sub + 128], pg[0:NE, 0:128]), r=[pgk], w=['gtt'])
                    sc.dma('sp', GT[:, sl], gtt[:], r=['gtt'], w=[('GT', tb)])
                if dbg is not None:
                    sc.dma('sp', SLOTD[:, :], SLOT[:].rearrange('p a b -> p (a b)'), r=['SLOT'], w=['SLOTD'])
                    sc.dma('sp', GKD[:, :], GK[:].rearrange('p a b -> p (a b)'), r=['GK'], w=['GKD'])
                sc.barrier()
                if upto == 4:
                    stop[0] = True

            with ExitStack() as st5:
              for _once in ([0] if not stop[0] else []):
                w1t = [sb(f"w1t{i}", [128, 8, 2048], BF16, st5) for i in range(2)]
                w2t = [sb(f"w2t{i}", [128, 8, D], BF16, st5) for i in range(2)]
                b1t = sb("b1t", [128, NE * 16], F32, st5)
                sc.dma('sp', b1t[:], b1_d[l, :, :], w=['b1t'])
                b2T = sb("b2T", [128, NE * 8], F32, st5)
                sc.dma('sp', b2T[:], b2T_d[l, :, :], w=['b2T'])
                b1p = sb("b1p", [128, NE * 16], F32, st5)
                sc.op('dve', lambda e: e.tensor_scalar(b1p[:], b1t[:], 1.0, None, ALU.add), r=['b1t'], w=['b1p'])
                b1m = b1t
                sc.op('dve', lambda e: e.tensor_scalar(b1m[:], b1t[:], -1.0, 7.0, ALU.mult, ALU.add), r=['b1t'], w=['b1t'])
                c119 = sb("c119", [128, 1], F32, st5)
                sc.op('pool', lambda e: e.memset(c119[:], 1.702 * 7.0), w=['c119'])
                xgt = sb("xgt", [128, 4, D], BF16, st5)
                xs = sb("xs", [128, 8, 512], BF16, st5)
                actt = [sb("actt0", [128, 8, 512], BF16, st5)] * 2
                blk = [0]
                ga = [sb(f"ga{i}", [128, 512], F32, st5) for i in range(3)]
                sgm = [sb(f"sgm{i}", [128, 512], F32, st5) for i in range(3)]
                li = [sb(f"li{i}", [128, 512], F32, st5) for i in range(3)]
                yfm = sb("yfm", [128, 8, 512], F32, st5)
                yk = [sb(f"yk{i}", [128, D], F32, st5) for i in range(4)]
                ytm = [yk[0], yk[1]]
                ycomb = sb("ycomb", [128, D], F32, st5)
                acc = sb("acc", [128, 8, 512], F32, st5)
                hb5 = [xs[:, 0, :], xs[:, 1, :]]
                xr5 = [li[0], li[1]]
                stl = dict(sq=[ga[0], ga[1]], sqk='ga', mean=sgm[0], rstd=sgm[1], tmp=sgm[2], keys=('sgm0', 'sgm1', 'sgm2'))
                XGK = [('XG', t_, k) for t_ in range(NT) for k in range(4)]
                wn = 0
                ytc = [0]
                for ex_ in range(NE):
                    wp = wn % 2
                    wn += 1
                    for kc in range(8):
                        sc.dma('pool', w1t[wp][:, kc, :], w1_d[l, ex_, kc * 128:(kc + 1) * 128, :], w=[(f'w1t{wp}', kc)])
                    for kc in range(8):
                        sc.dma('pool', w2t[wp][:, kc, :], w2_d[l, ex_, kc * 128:(kc + 1) * 128, :], w=[(f'w2t{wp}', kc)])
                    for cb in range(CAPB):
                        r0 = ex_ * CAP + 512 * cb
                        sc.dma('sp', xgt[:], XG[r0:r0 + 512, :].rearrange("(j p) d -> p j d", p=128), r=XGK if (ex_ == 0 and cb == 0) else [], w=['xgt'])
                        for kp in range(4):
                            for kk in range(2):
                                kc = 2 * kp + kk
                                for j in range(4):
                                    sc.op('pe', lambda e, kc=kc, j=j, kk=kk: e.transpose(PSB[:, 512 * kk + 128 * j:512 * kk + 128 * j + 128], xgt[:, j, 128 * kc:128 * kc + 128], identb[:, :]),
                                          r=['xgt', 'identb'], w=['psb'], inc=(kk == 1 and j == 3))
                            sc.op('act', lambda e, kp=kp: e.copy(xs[:, 2 * kp:2 * kp + 2, :].rearrange("p a b -> p (a b)"), PSB[:, :]), r=['psb'], w=[('xs', kp)])
                        XSK = [('xs', kc // 2) for kc in range(8)]
                        ab = actt[blk[0] % 2]
                        abk = 'actt0'
                        blk[0] += 1

                        def X(fc):
                            pg, pgk = bank()
                            for kc in range(8):
                                mm(pg[:, :], w1t[wp][:, kc, 128 * fc:128 * fc + 128], xs[:, kc, :], kc == 0, kc == 7, r=[(f'w1t{wp}', kc), XSK[kc]], w=[pgk])
                            pl, plk = bank()
                            for kc in range(8):
                                mm(pl[:, :], w1t[wp][:, kc, 1024 + 128 * fc:1152 + 128 * fc], xs[:, kc, :], kc == 0, kc == 7, r=[(f'w1t{wp}', kc), XSK[kc]], w=[plk])
                            q = fc % 3
                            bl = b1p[:, ex_ * 16 + 8 + fc:ex_ * 16 + 8 + fc + 1]
                            bg7 = b1m[:, ex_ * 16 + fc:ex_ * 16 + fc + 1]
                            sc.op('act', lambda e: e.activation(sgm[q][:], pg[:, :], AF.Relu, bias=bg7, scale=-1.0), r=[pgk, 'b1t'], w=[f'sgm{q}'])
                            sc.op('act', lambda e: e.activation(ga[q][:], sgm[q][:], AF.Silu, bias=c119[:, 0:1], scale=-1.702), r=[f'sgm{q}', 'c119'], w=[f'ga{q}'])
                            sc.op('dve', lambda e: e.tensor_scalar(li[q][:], pl[:, :], bl, -6.0, ALU.add, ALU.max), r=[plk, 'b1p'], w=[f'li{q}'])

                        def Y(fc):
                            q = fc % 3
                            sc.op('dve', lambda e: e.scalar_tensor_tensor(ab[:, fc, :], li[q][:], 8.0, ga[q][:], ALU.min, ALU.mult), r=[f'ga{q}', f'li{q}'], w=[(abk, fc)])

                        X(0)
                        X(1)
                        for fc in range(8):
                            if fc + 2 < 8:
                                X(fc + 2)
                            Y(fc)
                        for dc in range(8):
                            pm, pmk = bank()
                            for fc in range(8):
                                mm(pm[:, :], w2t[wp][:, fc, 128 * dc:128 * dc + 128], ab[:, fc, :], fc == 0, fc == 7, r=[(f'w2t{wp}', fc), (abk, fc)], w=[pmk])
                            sc.op('act', lambda e, dc=dc: e.activation(yfm[:, dc, :], pm[:, :], AF.Identity, bias=b2T[:, ex_ * 8 + dc:ex_ * 8 + dc + 1], scale=1.0 / 1.702),
                                  r=[pmk, 'b2T'], w=[('yfm', dc)])
                        for j in range(4):
                            for half in range(2):
                                pT, pTk = bank()
                                for d4 in range(4):
                                    dc = 4 * half + d4
                                    mm(pT[:, 128 * d4:128 * d4 + 128], yfm[:, dc, 128 * j:128 * j + 128], ident, True, True, r=[('yfm', dc), 'cst'], w=[pTk], inc=(d4 == 3))
                                y_ = ytm[ytc[0] % 2]
                                yk_ = f'yk{ytc[0] % 2}'
                                sc.op('act' if half else 'dve', (lambda e, half=half: e.copy(y_[:, 512 * half:512 * half + 512], pT[:, :])) if half else
                                      (lambda e, half=half: e.tensor_copy(y_[:, 512 * half:512 * half + 512], pT[:, :])), r=[pTk], w=[yk_])
                            sc.dma('sp', YS[r0 + 128 * j:r0 + 128 * j + 128, :], y_[:], r=[yk_], w=[('YS', ex_, cb, j)])
                            ytc[0] += 1
                YSK = [('YS', e_, c_, j_) for e_ in range(NE) for c_ in range(CAPB) for j_ in range(4)]
                for tb in range(NB):
                    sl = slice(tb * 512, tb * 512 + 512)
                    for sub in range(4):
                        tt_ = tb * 4 + sub
                        for k in range(4):
                            sc.idma(yk[k][:, :], None, YS[:, :], bass.IndirectOffsetOnAxis(ap=SLOT[:, tt_, k:k + 1], axis=0), bnd_reg,
                                    r=(YSK if (tt_ == 0 and k == 0) else []) + ['SLOT'], w=[f'yk{k}'])
                        sc.op('dve', lambda e: e.tensor_scalar(ycomb[:], yk[0][:], GK[:, tt_, 0:1], None, ALU.mult), r=['yk0', 'GK'], w=['ycomb'])
                        for k in range(1, 4):
                            sc.op('dve', lambda e, k=k: e.scalar_tensor_tensor(ycomb[:], yk[k][:], GK[:, tt_, k:k + 1], ycomb[:], ALU.mult, ALU.add),
                                  r=[f'yk{k}', 'GK', 'ycomb'], w=['ycomb'])
                        for half in range(2):
                            pT, pTk = bank()
                            for d4 in range(4):
                                dc = 4 * half + d4
                                mm(pT[:, 128 * d4:128 * d4 + 128], ycomb[:, 128 * dc:128 * dc + 128], ident, True, True, r=['ycomb', 'cst'], w=[pTk], inc=(d4 == 3))
                            sc.op('act', lambda e, half=half, sub=sub: e.copy(acc[:, 4 * half:4 * half + 4, 128 * sub:128 * sub + 128],
                                                                              pT[:, :].rearrange("p (a b) -> p a b", a=4)), r=[pTk], w=['acc'])
                    zs = acc
                    for dc in range(8):
                        xr_ = xr5[dc % 2]
                        xk_ = f'li{dc % 2}'
                        sc.dma('sp', xr_[:], X1T[128 * dc:128 * dc + 128, sl], r=[('X1T', tb)], w=[xk_])
                        sc.op('dve', lambda e, dc=dc: e.scalar_tensor_tensor(zs[:, dc, :], xr_[:], ALPHA, zs[:, dc, :], ALU.mult, ALU.add),
                              r=[xk_, 'acc'], w=['acc'])
                    ln_block(zs, 'acc', 94, 102, stl)
                    if last:
                        sc.dma('sp', chunked(outT)[:, :, sl], zs[:], r=['acc'], w=[('outT', tb)])
                    else:
                        sc.dma('sp', chunked(XT)[:, :, sl], zs[:], r=['acc'], w=[('XT', tb)])
                        for dc in range(8):
                            hb_ = hb5[dc % 2]
                            hk_ = ('xs', 0)
                            sc.op('act', lambda e, dc=dc: e.copy(hb_, zs[:, dc, :]), r=['acc'], w=[hk_])
                            sc.dma('sp', XB[128 * dc:128 * dc + 128, sl], hb_, r=[hk_], w=[('XB', tb, dc)])
                sc.barrier()
                if upto == 5:
                    stop[0] = True
        except _Stop:
            pass
    return nc


def host_consts(S):
    half = 16
    freqs = (10000.0 ** (-np.arange(half, dtype=np.float32) / half)).astype(np.float32)
    pos = np.arange(S, dtype=np.float32)
    ang = pos[None, :] * freqs[:, None]
    cos = np.cos(ang).astype(np.float32)
    sin = np.sin(ang).astype(np.float32)
    ropec = np.tile(np.concatenate([cos, cos], 0), (4, 1))
    ropes = np.tile(np.concatenate([-sin, sin], 0), (4, 1))
    cst = np.zeros((128, 4 * 128 + 512 + 4 + 128), np.float32)
    idx = np.arange(128)
    same = (idx[:, None] // 64) == (idx[None, :] // 64)
    scale = 32 ** -0.5
    for h in range(4):
        g = 1.0 - 2.0 ** (-5.0 - h)
        cst[:, 128 * h:128 * h + 128] = np.where(same, g ** np.abs(idx[:, None] - idx[None, :]), 0.0) * scale
        cst[32 * h:32 * h + 32, 512:1024] = (g ** ((np.arange(512) % 64) + 1.0))[None, :]
        cst[:, 1024 + h] = g ** (63 - (idx % 64)) * scale
    cst[:, 1028:1156] = np.eye(128, dtype=np.float32)
    msk = np.zeros((128, 4 * 512), np.float32)
    s_ = np.arange(128)[:, None]
    t_ = np.arange(512)[None, :]
    for m in range(4):
        msk[:, 512 * m:512 * m + 512] = (t_ >= 128 * m + s_)
    io = np.zeros((128, 160), np.float32)
    io[:, 0:128] = (np.arange(128)[:, None] < np.arange(128)[None, :])
    io[:, 128:160] = np.arange(32, dtype=np.float32)[None, :]
    return dict(ropec=np.ascontiguousarray(ropec), ropes=np.ascontiguousarray(ropes), cst=cst, msk=msk, iota=io)


def host_layout(inp, S, DEPTH, NE):
    L = DEPTH
    perm = np.concatenate([np.arange(16, 32), np.arange(0, 16)])
    pq = np.concatenate([h * 32 + perm for h in range(4)])
    w_in = np.asarray(inp['w_in'])
    w_aug = np.concatenate([w_in, w_in[:, :, pq], w_in[:, :, 128 + pq]], axis=2)
    cv = np.zeros((L, 128, NCV), np.float32)
    cdw = np.asarray(inp['conf_dw'])
    sdw = np.asarray(inp['sc_dw'])
    for c in range(2):
        cv[:, :, 31 * c:31 * c + 31] = cdw[:, :, 128 * c:128 * c + 128].transpose(0, 2, 1)
        cv[:, :, 62 + 3 * c:65 + 3 * c] = sdw[:, :, 128 * c:128 * c + 128].transpose(0, 2, 1)
        cv[:, :, 68 + c] = np.asarray(inp['conf_dw_b'])[:, 128 * c:128 * c + 128]
        cv[:, :, 70 + c] = np.asarray(inp['conf_ln_g'])[:, 128 * c:128 * c + 128]
        cv[:, :, 72 + c] = np.asarray(inp['conf_ln_b'])[:, 128 * c:128 * c + 128]
    cv[:, 0:64, 74:78] = np.asarray(inp['ret_gn_g']).reshape(L, 4, 64).transpose(0, 2, 1)
    for name, c0 in (('ln1_g', 78), ('ln1_b', 86), ('ln2_g', 94), ('ln2_b', 102)):
        cv[:, :, c0:c0 + 8] = np.asarray(inp[name]).reshape(L, 8, 128).transpose(0, 2, 1)
    b1T = np.ascontiguousarray(np.asarray(inp['b1']).reshape(L, NE, 16, 128).transpose(0, 3, 1, 2).reshape(L, 128, NE * 16))
    b2T = np.ascontiguousarray(np.asarray(inp['b2']).reshape(L, NE, 8, 128).transpose(0, 3, 1, 2).reshape(L, 128, NE * 8))
    rw = np.asarray(inp['router_w'])
    rb = np.asarray(inp['router_b'])
    common = dict(w_in=np.ascontiguousarray(w_aug), w_o=np.asarray(inp['w_o']), cvec=cv,
                  foxb=np.asarray(inp['fox_b_f']).reshape(L, 1, 4), rw=rw, rb=rb.reshape(L, 1, -1),
                  w1=np.asarray(inp['w1']), b1T=b1T, w2=np.asarray(inp['w2']), b2=np.asarray(inp['b2']), b2T=b2T)
    common.update(host_consts(S))
    return common


def run(inp, S, DEPTH, NE, SBK, dbg=None, upto=99, trace=False):
    x = np.asarray(inp['x'])
    B = x.shape[0]
    common = host_layout(inp, S, DEPTH, NE)
    nc = build(S, DEPTH, NE, SBK, dbg, upto)
    in_maps = []
    for b in range(B):
        m = dict(common)
        m['xT'] = np.ascontiguousarray(x[b].T)
        in_maps.append(m)
    res = run_bass_kernel_spmd(nc, in_maps, core_ids=list(range(B)), **({'trace': True} if trace else {}))
    if trace:
        print('EXEC_NS', res.exec_time_ns)
    out = np.stack([np.ascontiguousarray(r['outT'].T) for r in res.results], 0).astype(np.float32)
    if dbg is not None:
        return out, res.results
    return out


def kernel(**inputs):
    return run(inputs, 8192, 4, 32, 1024)
```

```python
import math
import os
KN = int(os.environ.get('KN', '99'))
KW = int(os.environ.get('KW', '1'))
from contextlib import ExitStack
import numpy as np
import concourse.bass as bass
import concourse.mybir as mybir
from concourse.bass_utils import run_bass_kernel_spmd

F32 = mybir.dt.float32
BF16 = mybir.dt.bfloat16
ALU = mybir.AluOpType
AF = mybir.ActivationFunctionType
AX = mybir.AxisListType

D = 1024
CHUNK = 64
ALPHA = 8 ** 0.25
EPS = 1e-5
NCV = 110
WA = 2820 + 256


class Sched:
    def __init__(s, nc, es):
        s.nc = nc
        s.eng = {'pe': nc.tensor, 'act': nc.scalar, 'dve': nc.vector, 'pool': nc.gpsimd, 'sp': nc.sync}
        s.sem = {k: es.enter_context(nc.semaphore('s_' + k)) for k in ('pe', 'act', 'dve', 'pool')}
        s.cnt = {k: 0 for k in s.sem}
        s.seen = {k: {} for k in s.eng}
        s.dq = {}
        for q in ('sp', 'pool', 'act'):
            s.dq[q] = dict(sems=[es.enter_context(nc.semaphore(f'd_{q}{i}')) for i in range(8)], n=0)
        s.res = {}

    def _need(s, en, deps):
        best = {}
        for (k, h, v) in deps:
            if v > best.get(k, (None, 0))[1]:
                best[k] = (h, v)
        for k, (h, v) in best.items():
            if s.seen[en].get(k, 0) < v:
                s.eng[en].wait_ge(h, v)
                s.seen[en][k] = v

    def _collect(s, en, r, w):
        deps = []
        for k in r:
            st = s.res.get(k)
            if st and st[0]:
                deps.append(st[0])
        for k in w:
            st = s.res.get(k)
            if st:
                if st[0]:
                    deps.append(st[0])
                deps.extend(st[1].values())
        if en == 'pe':
            deps = [d for d in deps if d[0] != 'pe']
        return deps

    def _record(s, dep, r, w):
        for k in r:
            st = s.res.setdefault(k, [None, {}])
            o = st[1].get(dep[0])
            if o is None or o[2] < dep[2]:
                st[1][dep[0]] = dep
        for k in w:
            s.res[k] = [dep, {}]

    def op(s, en, fn, r=(), w=(), inc=True):
        assert inc or en == 'pe'
        s._need(en, s._collect(en, r, w))
        ins = fn(s.eng[en])
        if inc:
            s.cnt[en] += 1
            ins.then_inc(s.sem[en], 1)
            dep = (en, s.sem[en], s.cnt[en])
        else:
            dep = (en, s.sem[en], s.cnt[en] + 1)
        s._record(dep, r, w)

    def dma(s, q, out, in_, r=(), w=(), **kw):
        Q = s.dq[q]
        i = Q['n']
        Q['n'] += 1
        K = len(Q['sems'])
        h = Q['sems'][i % K]
        key = f'd_{q}{i % K}'
        deps = s._collect(q, r, w)
        if i >= K:
            deps.append((key, h, 16 * (i // K)))
        s._need(q, deps)
        s.eng[q].dma_start(out=out, in_=in_, **kw).then_inc(h, 16)
        s._record((key, h, 16 * (i // K + 1)), r, w)

    def idma(s, out, out_off, in_, in_off, bound, r=(), w=()):
        q = 'pool'
        Q = s.dq[q]
        i = Q['n']
        Q['n'] += 1
        K = len(Q['sems'])
        h = Q['sems'][i % K]
        key = f'd_{q}{i % K}'
        deps = s._collect(q, r, w)
        if i >= K:
            deps.append((key, h, 16 * (i // K)))
        s._need(q, deps)
        s.eng[q].indirect_dma_start(out=out, out_offset=out_off, in_=in_, in_offset=in_off,
                                    bounds_check=bound, oob_is_err=False).then_inc(h, 16)
        s._record((key, h, 16 * (i // K + 1)), r, w)

    def alldeps(s):
        deps = [(k, s.sem[k], s.cnt[k]) for k in s.sem if s.cnt[k] > 0]
        for q, Q in s.dq.items():
            K = len(Q['sems'])
            for j in range(min(K, Q['n'])):
                tot = (Q['n'] - 1 - j) // K + 1
                deps.append((f'd_{q}{j}', Q['sems'][j], 16 * tot))
        return deps

    def barrier(s):
        deps = s.alldeps()
        for en in s.eng:
            s._need(en, [d for d in deps if not (en == 'pe' and d[0] == 'pe')])
        s.res = {}


class _Stop(Exception):
    pass


def build(S, DEPTH, NE, SBK, dbg=None, upto=99):
    NB = S // 512
    NT = S // 128
    NCH = S // 64
    NSB = S // SBK
    CAPB = -(-int(1.5 * 4 * S / NE) // 512)
    CAP = 512 * CAPB
    U32 = mybir.dt.uint32
    nc = bass.Bass("TRN2", target_bir_lowering=False)
    dt = nc.dram_tensor
    xT_in = dt("xT", [D, S], F32, kind="ExternalInput").ap()
    w_in_d = dt("w_in", [DEPTH, D, WA], F32, kind="ExternalInput").ap()
    w_o_d = dt("w_o", [DEPTH, D, D], F32, kind="ExternalInput").ap()
    cvec_d = dt("cvec", [DEPTH, 128, NCV], F32, kind="ExternalInput").ap()
    foxb_d = dt("foxb", [DEPTH, 1, 4], F32, kind="ExternalInput").ap()
    rw_d = dt("rw", [DEPTH, D, NE], F32, kind="ExternalInput").ap()
    rb_d = dt("rb", [DEPTH, 1, NE], F32, kind="ExternalInput").ap()
    w1_d = dt("w1", [DEPTH, NE, D, 2048], F32, kind="ExternalInput").ap()
    b1_d = dt("b1T", [DEPTH, 128, NE * 16], F32, kind="ExternalInput").ap()
    w2_d = dt("w2", [DEPTH, NE, D, D], F32, kind="ExternalInput").ap()
    b2_d = dt("b2", [DEPTH, NE, D], F32, kind="ExternalInput").ap()
    cos_d = dt("ropec", [128, S], F32, kind="ExternalInput").ap()
    sin_d = dt("ropes", [128, S], F32, kind="ExternalInput").ap()
    cst_d = dt("cst", [128, 4 * 128 + 512 + 4 + 128], F32, kind="ExternalInput").ap()
    msk_d = dt("msk", [128, 4 * 512], F32, kind="ExternalInput").ap()
    io_d = dt("iota", [128, 128 + 32], F32, kind="ExternalInput").ap()
    b2T_d = dt("b2T", [DEPTH, 128, NE * 8], F32, kind="ExternalInput").ap()
    outT = dt("outT", [D, S], F32, kind="ExternalOutput").ap()
    okind = {} if dbg is None else {"kind": "ExternalOutput"}
    XT = dt("XT", [D, S], F32).ap()
    XB = dt("XB", [D, S], BF16).ap()
    X1T = dt("X1T", [D, S], F32).ap()
    X1B = dt("X1B", [D, S], BF16).ap()
    MIXT = dt("MIXT", [D, S], BF16, **okind).ap()
    RQ = dt("RQ", [128, S], BF16).ap()
    RK = dt("RK", [128, S], BF16).ap()
    RV = dt("RV", [S, 256], BF16).ap()
    GATE = dt("GATE", [256, S], BF16).ap()
    FQ = dt("FQ", [256, S], BF16).ap()
    FK = dt("FK", [256, S], BF16).ap()
    FV = dt("FV", [S, 256], BF16).ap()
    FF = dt("FF", [4, S], F32).ap()
    FRD = dt("FRD", [7, S], BF16).ap()
    GT = dt("GT", [NE, S], F32, **okind).ap()
    XG = dt("XG", [NE * CAP, D], BF16, **okind).ap()
    SLOTD = dt("SLOTD", [128, NT * 4], U32, **okind).ap()
    GKD = dt("GKD", [128, NT * 4], F32, **okind).ap()
    YS = dt("YS", [NE * CAP, D], F32).ap()

    def chunked(ap):
        return ap.rearrange("(kc p) t -> p kc t", p=128)

    with ExitStack() as es:
        sc = Sched(nc, es)
        uid = [0]

        def sb(name, shape, dtp, st=es):
            uid[0] += 1
            return st.enter_context(nc.sbuf_tensor(f"{name}_s{uid[0]}", shape, dtp))
        PS = [es.enter_context(nc.psum_tensor(f"ps{i}", [128, 512], F32)) for i in range(7)]
        PSB = es.enter_context(nc.psum_tensor("psb", [128, 1024], BF16))
        pctr = [0]

        def bank():
            pctr[0] = (pctr[0] + 1) % 7
            return PS[pctr[0]], f"ps{pctr[0]}"

        cst = sb("cst", [128, 4 * 128 + 512 + 4 + 128], F32)
        sc.dma('sp', cst[:], cst_d[:, :], w=['cst'])
        identb = sb("identb", [128, 128], BF16)
        sc.op('dve', lambda e: e.tensor_copy(identb[:], cst[:, 1028:1156]), r=['cst'], w=['identb'])
        ident = cst[:, 1028:1156]
        onesD = sb("onesD", [128, 128], F32)
        sc.op('pool', lambda e: e.memset(onesD[:], 1.0 / D), w=['onesD'])
        ones256 = sb("ones256", [128, 128], F32)
        sc.op('pool', lambda e: e.memset(ones256[:], 1.0 / 256), w=['ones256'])
        ones64 = sb("ones64", [64, 64], F32)
        sc.op('pool', lambda e: e.memset(ones64[:], 1.0 / 64), w=['ones64'])
        onesb = sb("onesb", [128, 64], BF16)
        sc.op('pool', lambda e: e.memset(onesb[:], 1.0), w=['onesb'])
        cv = sb("cv", [128, NCV], F32)
        iot = sb("iot", [128, 160], F32)
        sc.dma('sp', iot[:], io_d[:, :], w=['iot'])
        ones1 = sb("ones1", [128, 128], F32)
        sc.op('pool', lambda e: e.memset(ones1[:], 1.0), w=['ones1'])
        SLOT = sb("SLOT", [128, NT, 4], U32)
        bnd_reg = nc.gpsimd.to_reg(NE * CAP - 1)
        GK = sb("GK", [128, NT, 4], F32)

        def mm(out, lhsT, rhs, start, stop, r, w, inc=None):
            sc.op('pe', lambda e: e.matmul(out, lhsT, rhs, start=start, stop=stop), r=r, w=w,
                  inc=(stop if inc is None else inc))

        def ln_block(z, zk, gcol, bcol, st):
            pm, pmk = bank()
            for dc in range(8):
                mm(pm[:, :], onesD[:], z[:, dc, :], dc == 0, dc == 7, r=['onesD', zk], w=[pmk])
            pe2, pe2k = bank()
            for dc in range(8):
                sq = st['sq'][dc % 2]
                sqk = st['sqk'] + str(dc % 2)
                sc.op('act', lambda e, dc=dc: e.activation(sq[:], z[:, dc, :], AF.Square), r=[zk], w=[sqk])
                mm(pe2[:, :], onesD[:], sq[:], dc == 0, dc == 7, r=['onesD', sqk], w=[pe2k], inc=True)
            mean, rstd, tmp = st['mean'], st['rstd'], st['tmp']
            mk_, rk_, tk_ = st.get('keys', (st['sqk'] + 'mean', st['sqk'] + 'rstd', st['sqk'] + 'tmp'))
            sc.op('act', lambda e: e.copy(mean[:], pm[:, :]), r=[pmk], w=[mk_])
            sc.op('dve', lambda e: e.tensor_tensor(rstd[:], mean[:], mean[:], ALU.mult), r=[mk_], w=[rk_])
            sc.op('dve', lambda e: e.tensor_tensor(rstd[:], pe2[:, :], rstd[:], ALU.subtract), r=[pe2k, rk_], w=[rk_])
            sc.op('dve', lambda e: e.tensor_scalar(rstd[:], rstd[:], 0.0, EPS, ALU.max, ALU.add), r=[rk_], w=[rk_])
            sc.op('act', lambda e: e.activation(rstd[:], rstd[:], AF.Ln), r=[rk_], w=[rk_])
            sc.op('act', lambda e: e.activation(rstd[:], rstd[:], AF.Exp, scale=-0.5), r=[rk_], w=[rk_])
            for dc in range(8):
                sc.op('dve', lambda e, dc=dc: e.tensor_tensor(tmp[:], z[:, dc, :], mean[:], ALU.subtract), r=[zk, mk_], w=[tk_])
                sc.op('dve', lambda e, dc=dc: e.tensor_tensor(tmp[:], tmp[:], rstd[:], ALU.mult), r=[tk_, rk_], w=[tk_])
                sc.op('act', lambda e, dc=dc: e.activation(z[:, dc, :], tmp[:], AF.Identity,
                                                          bias=cv[:, bcol + dc:bcol + dc + 1], scale=cv[:, gcol + dc:gcol + dc + 1]),
                      r=[tk_, 'cv'], w=[zk])

        with ExitStack() as st0:
            xf = [sb(f"xf{i}", [128, 8, 512], F32, st0) for i in range(2)]
            zt = sb("zt", [128, 4096], BF16, st0)
            sc.op('pool', lambda e: e.memset(zt[:], 0.0), w=['zt'])
            XGz = XG.rearrange("(n p r) d -> n p (r d)", p=128, r=4)
            for n_ in range(NE * CAP // 512):
                sc.dma('sp', XGz[n_], zt[:], r=['zt'], w=[('XGz', n_)])
            xh = [sb(f"xh{i}", [128, 8, 512], BF16, st0) for i in range(2)]
            for tb in range(NB):
                p = tb % 2
                sl = slice(tb * 512, tb * 512 + 512)
                sc.dma('sp', xf[p][:], chunked(xT_in)[:, :, sl], w=[f'xf{p}'])
                sc.op('dve', lambda e, p=p: e.tensor_copy(xh[p][:], xf[p][:]), r=[f'xf{p}'], w=[f'xh{p}'])
                sc.dma('sp', chunked(XT)[:, :, sl], xf[p][:], r=[f'xf{p}'], w=[('XT', tb)])
                sc.dma('sp', chunked(XB)[:, :, sl], xh[p][:], r=[f'xh{p}'], w=[('XB', tb)])
            sc.barrier()

        stop = [False]
        try:
          for l in range(DEPTH):
            last = l == DEPTH - 1
            sc.dma('sp', cv[:], cvec_d[l, :, :], w=['cv'])
            with ExitStack() as st1:
              for _once in ([0] if not stop[0] else []):
                win = sb("win", [128, 8, WA], BF16, st1)
                for kc in range(8):
                    sc.dma('pool', win[:, kc, :], w_in_d[l, kc * 128:(kc + 1) * 128, :], w=[('win', kc)])
                WK = [('win', kc) for kc in range(8)]
                xbt = [sb(f"xbt{i}", [128, 8, 544], BF16, st1) for i in range(2)]
                cs_t = sb("cs_t", [128, 512], F32, st1)
                sn_t = sb("sn_t", [128, 512], F32, st1)
                t1 = sb("t1", [128, 512], F32, st1)
                t2 = sb("t2", [128, 512], F32, st1)
                ob = [sb(f"ob{i}", [128, 512], BF16, st1) for i in range(4)]
                obc = [0]
                u_t = [sb(f"u{i}", [128, 544], F32, st1) for i in range(2)]
                sg_t = sb("sg_t", [128, 544], F32, st1)
                ca_t = [sb(f"ca{i}", [128, 512], F32, st1) for i in range(2)]
                sq1 = sb("sq1", [128, 512], F32, st1)
                mean1 = sb("mean1", [128, 512], F32, st1)
                rstd1 = sb("rstd1", [128, 512], F32, st1)
                ff_t = sb("ff_t", [4, 512], F32, st1)
                vt = [sb(f"vt{i}", [128, 256], BF16, st1) for i in range(2)]
                vtc = [0]

                def nob():
                    obc[0] = (obc[0] + 1) % 4
                    return ob[obc[0]], f"ob{obc[0]}"

                for tb in range(NB):
                    t0 = tb * 512
                    p = tb % 2
                    xk = f'xbt{p}'
                    x_ = xbt[p]
                    if tb == 0:
                        sc.op('pool', lambda e: e.memset(x_[:, :, 0:32], 0.0), w=[xk])
                        sc.dma('sp', x_[:, :, 32:544], chunked(XB)[:, :, 0:512], r=[('XB', 0)], w=[xk])
                    else:
                        sc.dma('sp', x_[:, :, :], chunked(XB)[:, :, t0 - 32:t0 + 512], r=[('XB', tb - 1), ('XB', tb)], w=[xk])
                    sc.dma('sp', cs_t[:], cos_d[:, t0:t0 + 512], w=['cs_t'])
                    sc.dma('sp', sn_t[:], sin_d[:, t0:t0 + 512], w=['sn_t'])

                    def fm(c0, ncols=128, halo=False):
                        pm, pmk = bank()
                        for kc in range(8):
                            mm(pm[0:ncols, :], win[:, kc, c0:c0 + ncols], x_[:, kc, 32:544], kc == 0, kc == 7, r=[WK[kc], xk], w=[pmk])
                        if not halo:
                            return pm, pmk
                        ph, phk = bank()
                        for kc in range(8):
                            mm(ph[0:ncols, 0:32], win[:, kc, c0:c0 + ncols], x_[:, kc, 0:32], kc == 0, kc == 7, r=[WK[kc], xk], w=[phk])
                        return pm, pmk, ph, phk

                    for (c0, cp, dst, dk) in ((0, 2820, RQ, 'RQ'), (128, 2948, RK, 'RK')):
                        pa, pak = fm(c0)
                        pp, ppk = fm(cp)
                        sc.op('dve', lambda e: e.tensor_tensor(t1[:], pa[:, :], cs_t[:], ALU.mult), r=[pak, 'cs_t'], w=['t1'])
                        sc.op('dve', lambda e: e.tensor_tensor(t2[:], pp[:, :], sn_t[:], ALU.mult), r=[ppk, 'sn_t'], w=['t2'])
                        o, ok = nob()
                        sc.op('dve', lambda e: e.tensor_tensor(o[:], t1[:], t2[:], ALU.add), r=['t1', 't2'], w=[ok])
                        sc.dma('sp', dst[:, t0:t0 + 512], o[:], r=[ok], w=[(dk, tb)])
                    for c in range(2):
                        pa, pak = fm(512 + 128 * c)
                        o, ok = nob()
                        sc.op('act', lambda e: e.activation(o[:], pa[:, :], AF.Silu), r=[pak], w=[ok])
                        sc.dma('sp', GATE[128 * c:128 * c + 128, t0:t0 + 512], o[:], r=[ok], w=[('GATE', tb)])
                    for c in range(2):
                        pa, pak, pah, pahk = fm(768 + 128 * c, halo=True)
                        pb_, pbk, pbh, pbhk = fm(1024 + 128 * c, halo=True)
                        u = u_t[c]
                        uk = f'u{c}'
                        sc.op('act', lambda e: e.activation(sg_t[:, 32:544], pb_[:, :], AF.Sigmoid), r=[pbk], w=['sg_t'])
                        sc.op('act', lambda e: e.activation(sg_t[:, 0:32], pbh[:, 0:32], AF.Sigmoid), r=[pbhk, 'sg_t'], w=['sg_t'])
                        sc.op('dve', lambda e: e.tensor_tensor(u[:, 32:544], pa[:, :], sg_t[:, 32:544], ALU.mult), r=[pak, 'sg_t'], w=[uk])
                        sc.op('dve', lambda e: e.tensor_tensor(u[:, 0:32], pah[:, 0:32], sg_t[:, 0:32], ALU.mult), r=[pahk, 'sg_t', uk], w=[uk])
                        ce = 'dve'
                        ca = ca_t[c]
                        cak = f'ca{c}'
                        wc0 = 31 * c
                        sc.op(ce, lambda e: e.tensor_scalar(ca[:], u[:, 2:514], cv[:, wc0:wc0 + 1], cv[:, 68 + c:69 + c], ALU.mult, ALU.add),
                              r=[uk, 'cv'], w=[cak])
                        for j in range(1, 31):
                            sc.op(ce, lambda e, j=j: e.scalar_tensor_tensor(ca[:], u[:, 2 + j:514 + j], cv[:, wc0 + j:wc0 + j + 1], ca[:], ALU.mult, ALU.add),
                                  r=[uk, 'cv', cak], w=[cak])
                    pm, pmk = bank()
                    pe2, pe2k = bank()
                    for c in range(2):
                        mm(pm[:, :], ones256[:], ca_t[c][:], c == 0, c == 1, r=['ones256', f'ca{c}'], w=[pmk])
                    for c in range(2):
                        sc.op('act', lambda e, c=c: e.activation(sq1[:], ca_t[c][:], AF.Square), r=[f'ca{c}'], w=['sq1'])
                        mm(pe2[:, :], ones256[:], sq1[:], c == 0, c == 1, r=['ones256', 'sq1'], w=[pe2k], inc=True)
                    sc.op('act', lambda e: e.copy(mean1[:], pm[:, :]), r=[pmk], w=['mean1'])
                    sc.op('dve', lambda e: e.tensor_tensor(rstd1[:], mean1[:], mean1[:], ALU.mult), r=['mean1'], w=['rstd1'])
                    sc.op('dve', lambda e: e.tensor_tensor(rstd1[:], pe2[:, :], rstd1[:], ALU.subtract), r=[pe2k, 'rstd1'], w=['rstd1'])
                    sc.op('dve', lambda e: e.tensor_scalar(rstd1[:], rstd1[:], 0.0, EPS, ALU.max, ALU.add), r=['rstd1'], w=['rstd1'])
                    sc.op('act', lambda e: e.activation(rstd1[:], rstd1[:], AF.Ln), r=['rstd1'], w=['rstd1'])
                    sc.op('act', lambda e: e.activation(rstd1[:], rstd1[:], AF.Exp, scale=-0.5), r=['rstd1'], w=['rstd1'])
                    for c in range(2):
                        sc.op('dve', lambda e, c=c: e.tensor_tensor(t1[:], ca_t[c][:], mean1[:], ALU.subtract), r=[f'ca{c}', 'mean1'], w=['t1'])
                        sc.op('dve', lambda e: e.tensor_tensor(t1[:], t1[:], rstd1[:], ALU.mult), r=['t1', 'rstd1'], w=['t1'])
                        o, ok = nob()
                        sc.op('act', lambda e, c=c: e.activation(o[:], t1[:], AF.Silu, bias=cv[:, 72 + c:73 + c], scale=cv[:, 70 + c:71 + c]),
                              r=['t1', 'cv'], w=[ok])
                        sc.dma('sp', MIXT[256 + 128 * c:384 + 128 * c, t0:t0 + 512], o[:], r=[ok], w=[('MIXT', tb)])
                    for c in range(2):
                        pc, pck, pch, pchk = fm(1536 + 128 * c, halo=True)
                        ph_, phk_, phh, phhk = fm(1792 + 128 * c, halo=True)
                        u = u_t[c]
                        uk = f'u{c}'
                        sc.op('act', lambda e: e.copy(sg_t[:, 32:544], pc[:, :]), r=[pck], w=['sg_t'])
                        sc.op('act', lambda e: e.copy(sg_t[:, 0:32], pch[:, 0:32]), r=[pchk, 'sg_t'], w=['sg_t'])
                        sc.op('dve', lambda e: e.tensor_tensor(u[:, 32:544], ph_[:, :], sg_t[:, 32:544], ALU.mult), r=[phk_, 'sg_t'], w=[uk])
                        sc.op('dve', lambda e: e.tensor_tensor(u[:, 0:32], phh[:, 0:32], sg_t[:, 0:32], ALU.mult), r=[phhk, 'sg_t', uk], w=[uk])
                        wc0 = 62 + 3 * c
                        sc.op('dve', lambda e: e.tensor_scalar(t1[:], u[:, 30:542], cv[:, wc0:wc0 + 1], None, ALU.mult), r=[uk, 'cv'], w=['t1'])
                        for j in (1, 2):
                            sc.op('dve', lambda e, j=j: e.scalar_tensor_tensor(t1[:], u[:, 30 + j:542 + j], cv[:, wc0 + j:wc0 + j + 1], t1[:], ALU.mult, ALU.add),
                                  r=[uk, 'cv', 't1'], w=['t1'])
                        pbb, pbbk = fm(1280 + 128 * c)
                        o, ok = nob()
                        sc.op('dve', lambda e: e.tensor_tensor(o[:], pbb[:, :], t1[:], ALU.mult), r=[pbbk, 't1'], w=[ok])
                        sc.dma('sp', MIXT[512 + 128 * c:640 + 128 * c, t0:t0 + 512], o[:], r=[ok], w=[('MIXT', tb)])
                    for c in range(2):
                        pa, pak = fm(2048 + 128 * c)
                        o, ok = nob()
                        sc.op('act', lambda e: e.mul(o[:], pa[:, :], 0.125), r=[pak], w=[ok])
                        sc.dma('sp', FQ[128 * c:128 * c + 128, t0:t0 + 512], o[:], r=[ok], w=[('FQ', tb)])
                        pa, pak = fm(2304 + 128 * c)
                        o, ok = nob()
                        sc.op('act', lambda e: e.copy(o[:], pa[:, :]), r=[pak], w=[ok])
                        sc.dma('sp', FK[128 * c:128 * c + 128, t0:t0 + 512], o[:], r=[ok], w=[('FK', tb)])
                    pa, pak = fm(2816, ncols=4)
                    sc.op('act', lambda e: e.copy(ff_t[:], pa[0:4, :]), r=[pak], w=['ff_t'])
                    sc.dma('sp', FF[:, t0:t0 + 512], ff_t[:], r=['ff_t'], w=[('FF', tb)])
                    for sub in range(4):
                        for (c0, dst, dk) in ((256, RV, 'RV'), (2560, FV, 'FV')):
                            pm, pmk = bank()
                            for kc in range(8):
                                mm(pm[:, 0:256], x_[:, kc, 32 + 128 * sub:160 + 128 * sub], win[:, kc, c0:c0 + 256], kc == 0, kc == 7, r=[WK[kc], xk], w=[pmk])
                            vtc[0] ^= 1
                            v = vt[vtc[0]]
                            vk = f'vt{vtc[0]}'
                            sc.op('act', lambda e: e.copy(v[:], pm[:, 0:256]), r=[pmk], w=[vk])
                            sc.dma('sp', dst[t0 + 128 * sub:t0 + 128 * sub + 128, :], v[:], r=[vk], w=[(dk, tb)])
                sc.barrier()
                if upto == 1:
                    stop[0] = True

            with ExitStack() as st2:
              for _once in ([0] if not stop[0] else []):
                rq_t = sb("rq_t", [32, S], BF16, st2)
                rk_t = sb("rk_t", [32, S], BF16, st2)
                qd_t = sb("qd_t", [32, S], BF16, st2)
                v_t = sb("v_t", [128, NT, 64], BF16, st2)
                v64 = sb("v64", [64, NCH, 64], BF16, st2)
                qdec = sb("qdec", [32, 512], F32, st2)
                stt = sb("stt", [32, 64], F32, st2)
                prevb = sb("prevb", [32, NCH, 64], BF16, st2)
                kd = [sb(f"kd{i}", [128, 32], BF16, st2) for i in range(2)]
                sd = [sb(f"sd{i}", [128, 128], BF16, st2) for i in range(2)]
                o_sb = sb("o_sb", [64, 512], F32, st2)
                sq2 = sb("sq2", [64, 512], F32, st2)
                mean2 = sb("mean2", [64, 512], F32, st2)
                rstd2 = sb("rstd2", [64, 512], F32, st2)
                g_t = sb("g_t", [64, 512], BF16, st2)
                ro = [sb(f"ro{i}", [64, 512], BF16, st2) for i in range(2)]
                for h in range(4):
                    gam = 1.0 - 2.0 ** (-5.0 - h)
                    cd = gam ** CHUNK
                    sc.dma('sp', rq_t[:], RQ[32 * h:32 * h + 32, :], w=['rq_t'])
                    sc.dma('sp', rk_t[:], RK[32 * h:32 * h + 32, :], w=['rk_t'])
                    sc.dma('sp', v_t[:], RV[:, 64 * h:64 * h + 64].rearrange("(j p) e -> p j e", p=128), w=['v_t'])
                    sc.dma('sp', qdec[:], cst_d[32 * h:32 * h + 32, 512:1024], w=['qdec'])
                    for tb in range(NB):
                        sc.op('dve', lambda e, tb=tb: e.tensor_tensor(qd_t[:, tb * 512:tb * 512 + 512], rq_t[:, tb * 512:tb * 512 + 512], qdec[:], ALU.mult),
                              r=['rq_t', 'qdec'], w=['qd_t'])
                    sc.op('dve', lambda e: e.memset(stt[:], 0.0), w=['stt'])
                    sc.dma('sp', v64[:], RV[:, 64 * h:64 * h + 64].rearrange("(c p) e -> p c e", p=64), w=['v64'])
                    for c in range(NCH):
                        sc.op('pe', lambda e, c=c: e.transpose(PSB[0:64, 0:32], rk_t[:, 64 * c:64 * c + 64], identb[0:32, 0:32]),
                              r=['rk_t', 'identb'], w=['psb'])
                        k_ = kd[c % 2]
                        kk = f'kd{c % 2}'
                        sc.op('dve', lambda e: e.tensor_scalar(k_[0:64, :], PSB[0:64, 0:32], cst[0:64, 1024 + h:1025 + h], None, ALU.mult), r=['psb', 'cst'], w=[kk])
                        pkv, pkvk = bank()
                        mm(pkv[0:32, 0:64], k_[0:64, :], v64[:, c, :], True, True, r=[kk, 'v64'], w=[pkvk])
                        sc.op('dve', lambda e, c=c: e.tensor_copy(prevb[:, c, :], stt[:]), r=['stt'], w=['prevb'])
                        sc.op('dve', lambda e: e.scalar_tensor_tensor(stt[:], stt[:], cd, pkv[0:32, 0:64], ALU.mult, ALU.add),
                              r=['stt', pkvk], w=['stt'])
                    for tb in range(NB if KN >= 5 else 0):
                        t0 = tb * 512
                        po, pok = bank()
                        for jj in range(4):
                            j = tb * 4 + jj
                            ps_, psk = bank()
                            mm(ps_[:, 0:128], rk_t[:, 128 * j:128 * j + 128], rq_t[:, 128 * j:128 * j + 128], True, True, r=['rk_t', 'rq_t'], w=[psk])
                            s_ = sd[j % 2]
                            sk = f'sd{j % 2}'
                            sc.op('dve', lambda e: e.tensor_tensor(s_[:], ps_[:, 0:128], cst[:, 128 * h:128 * h + 128], ALU.mult), r=[psk, 'cst'], w=[sk])
                            mm(po[0:64, 128 * jj:128 * jj + 128], v_t[:, j, :], s_[:], True, False, r=['v_t', sk], w=[pok], inc=False)
                            for hf in range(2):
                                c0 = 128 * jj + 64 * hf
                                mm(po[0:64, c0:c0 + 64], prevb[:, 2 * j + hf, :], qd_t[:, 128 * j + 64 * hf:128 * j + 64 * hf + 64], False, hf == 1,
                                   r=['prevb', 'qd_t'], w=[pok], inc=(hf == 1))
                        sc.op('act', lambda e: e.copy(o_sb[:], po[0:64, :]), r=[pok], w=['o_sb'])
                        sc.op('act', lambda e: e.activation(sq2[:], po[0:64, :], AF.Square), r=[pok], w=['sq2'])
                        pm, pmk = bank()
                        mm(pm[0:64, :], ones64[:], o_sb[:], True, True, r=['ones64', 'o_sb'], w=[pmk])
                        pe2, pe2k = bank()
                        mm(pe2[0:64, :], ones64[:], sq2[:], True, True, r=['ones64', 'sq2'], w=[pe2k])
                        sc.op('act', lambda e: e.copy(mean2[:], pm[0:64, :]), r=[pmk], w=['mean2'])
                        sc.op('dve', lambda e: e.tensor_tensor(rstd2[:], mean2[:], mean2[:], ALU.mult), r=['mean2'], w=['rstd2'])
                        sc.op('dve', lambda e: e.tensor_tensor(rstd2[:], pe2[0:64, :], rstd2[:], ALU.subtract), r=[pe2k, 'rstd2'], w=['rstd2'])
                        sc.op('dve', lambda e: e.tensor_scalar(rstd2[:], rstd2[:], 0.0, EPS, ALU.max, ALU.add), r=['rstd2'], w=['rstd2'])
                        sc.op('act', lambda e: e.activation(rstd2[:], rstd2[:], AF.Ln), r=['rstd2'], w=['rstd2'])
                        sc.op('act', lambda e: e.activation(rstd2[:], rstd2[:], AF.Exp, scale=-0.5), r=['rstd2'], w=['rstd2'])
                        sc.dma('sp', g_t[:], GATE[64 * h:64 * h + 64, t0:t0 + 512], w=['g_t'])
                        sc.op('dve', lambda e: e.tensor_tensor(o_sb[:], o_sb[:], mean2[:], ALU.subtract), r=['o_sb', 'mean2'], w=['o_sb'])
                        sc.op('dve', lambda e: e.tensor_tensor(o_sb[:], o_sb[:], rstd2[:], ALU.mult), r=['o_sb', 'rstd2'], w=['o_sb'])
                        sc.op('dve', lambda e: e.tensor_scalar(o_sb[:], o_sb[:], cv[0:64, 74 + h:75 + h], None, ALU.mult), r=['o_sb', 'cv'], w=['o_sb'])
                        r_ = ro[tb % 2]
                        rk_ = f'ro{tb % 2}'
                        sc.op('dve', lambda e: e.tensor_tensor(r_[:], o_sb[:], g_t[:], ALU.mult), r=['o_sb', 'g_t'], w=[rk_])
                        sc.dma('sp', MIXT[64 * h:64 * h + 64, t0:t0 + 512], r_[:], r=[rk_], w=[('MIXT2', h, tb)])
                sc.barrier()
                if upto == 2:
                    stop[0] = True

            with ExitStack() as st3:
              for _once in ([0] if not stop[0] else []):
                qa = sb("qa", [70, S], BF16, st3)
                ka = sb("ka", [70, S], BF16, st3)
                fv_t = sb("fv_t", [128, NT, 128], BF16, st3)
                sc.op('pool', lambda e: e.memset(fv_t[:], 1.0), w=['fv_t'])
                nd = sb("nd", [128, 512], F32, st3)
                msk = sb("msk", [128, 4 * 512], F32, st3)
                sc.dma('sp', msk[:], msk_d[:, :], w=['msk'])
                fb = sb("fb", [1, 4], F32, st3)
                sc.dma('sp', fb[:], foxb_d[l, :, :], w=['fb'])
                sc.op('dve', lambda e: e.tensor_scalar(fb[:], fb[:], -1.0, None, ALU.mult), r=['fb'], w=['fb'])
                FW = min(S, 2048)
                f_r = sb("f_r", [1, FW], F32, st3)
                F_r = sb("F_r", [1, FW], F32, st3)
                one_r = sb("one_r", [1, FW], F32, st3)
                sc.op('pool', lambda e: e.memset(one_r[:], 1.0), w=['one_r'])
                fr = sb("fr", [1, 7, FW], BF16, st3)
                r1 = sb("r1", [1, FW], F32, st3)
                r2 = sb("r2", [1, FW], F32, st3)
                Fl = sb("Fl", [1, 1], F32, st3)
                pt = [sb(f"pt{i}", [128, 512], BF16, st3) for i in range(3)]
                rden = sb("rden", [64, 512], F32, st3)
                fo = [sb(f"fo{i}", [64, 512], BF16, st3) for i in range(2)]
                for h in range(4):
                    sc.op('dve', lambda e: e.memset(Fl[:], 0.0), w=['Fl'])
                    for pc in range(S // FW):
                        sl = slice(pc * FW, pc * FW + FW)
                        sc.dma('sp', f_r[:], FF[h:h + 1, sl], w=['f_r'])
                        sc.op('act', lambda e: e.activation(f_r[:], f_r[:], AF.Exp, bias=fb[0:1, h:h + 1], scale=-1.0), r=['f_r', 'fb'], w=['f_r'])
                        sc.op('act', lambda e: e.activation(f_r[:], f_r[:], AF.Ln, bias=1.0), r=['f_r'], w=['f_r'])
                        sc.op('dve', lambda e: e.tensor_scalar(f_r[:], f_r[:], -1.0, None, ALU.mult), r=['f_r'], w=['f_r'])
                        sc.op('dve', lambda e: e.tensor_tensor_scan(F_r[:], one_r[:], f_r[:], Fl[0:1, 0:1], ALU.mult, ALU.add), r=['one_r', 'f_r', 'Fl'], w=['F_r'])
                        sc.op('dve', lambda e: e.tensor_copy(Fl[:], F_r[0:1, FW - 1:FW]), r=['F_r'], w=['Fl'])
                        sc.op('dve', lambda e: e.tensor_copy(fr[:, 0, :], F_r[:]), r=['F_r'], w=['fr'])
                        sc.op('dve', lambda e: e.tensor_tensor(r1[:], F_r[:], fr[:, 0, :], ALU.subtract), r=['F_r', 'fr'], w=['r1'])
                        sc.op('dve', lambda e: e.tensor_copy(fr[:, 1, :], r1[:]), r=['r1', 'fr'], w=['fr'])
                        sc.op('dve', lambda e: e.tensor_tensor(r2[:], r1[:], fr[:, 1, :], ALU.subtract), r=['r1', 'fr'], w=['r2'])
                        sc.op('dve', lambda e: e.tensor_copy(fr[:, 2, :], r2[:]), r=['r2', 'fr'], w=['fr'])
                        sc.op('dve', lambda e: e.tensor_copy(fr[:, 3, :], one_r[:]), r=['one_r', 'fr'], w=['fr'])
                        for i in range(3):
                            sc.op('dve', lambda e, i=i: e.tensor_scalar(fr[:, 4 + i, :], fr[:, i, :], -1.0, None, ALU.mult), r=['fr'], w=['fr'])
                        sc.dma('sp', FRD[:, sl].rearrange("(o r) t -> o r t", o=1), fr[:], r=['fr'], w=[('FRD', pc)])
                    FRk = [('FRD', pc) for pc in range(S // FW)]
                    sc.dma('sp', qa[0:64, :], FQ[64 * h:64 * h + 64, :], w=['qa'])
                    sc.dma('sp', qa[64:67, :], FRD[0:3, :], r=FRk + ['qa'], w=['qa'])
                    for i in range(3):
                        sc.dma('sp', qa[67 + i:68 + i, :], FRD[3:4, :], r=FRk + ['qa'], w=['qa'])
                    sc.dma('sp', ka[0:64, :], FK[64 * h:64 * h + 64, :], w=['ka'])
                    for i in range(3):
                        sc.dma('sp', ka[64 + i:65 + i, :], FRD[3:4, :], r=FRk + ['ka'], w=['ka'])
                    sc.dma('sp', ka[67:70, :], FRD[4:7, :], r=FRk + ['ka'], w=['ka'])
                    sc.dma('sp', fv_t[:, :, 0:64], FV[:, 64 * h:64 * h + 64].rearrange("(j p) e -> p j e", p=128), w=['fv_t'])
                    pi = 0
                    for qb in range(NB):
                        q0 = qb * 512
                        pn, pnk = PS[5], 'ps5'
                        pd, pdk = PS[6], 'ps6'
                        nk = 4 * qb + 4
                        def score(j):
                            mm(PS[j % 4][:, :], ka[:, 128 * j:128 * j + 128], qa[:, q0:q0 + 512], True, True, r=['ka', 'qa'], w=[f'ps{j % 4}'])

                        score(0)
                        if nk > 1:
                            score(1)
                        for j in range(nk):
                            if j + 2 < nk:
                                score(j + 2)
                            ps_ = PS[j % 4]
                            psk = f'ps{j % 4}'
                            p_ = pt[pi % 3]
                            pk = f'pt{pi % 3}'
                            pi += 1
                            sc.op('act', lambda e: e.activation(p_[:], ps_[:, :], AF.Exp), r=[psk], w=[pk])
                            if j >= 4 * qb:
                                m = j - 4 * qb
                                sc.op('dve', lambda e: e.tensor_tensor(p_[:], p_[:], msk[:, 512 * m:512 * m + 512], ALU.mult), r=[pk, 'msk'], w=[pk])
                            mm(pn[:, :], fv_t[:, j, :], p_[:], j == 0, j == nk - 1, r=['fv_t', pk], w=[pnk], inc=True)
                        sc.op('act', lambda e: e.copy(nd[:], pn[:, :]), r=[pnk], w=['nd'])
                        mm(pd[0:64, :], ident[:, 64:128], nd[:], True, True, r=['cst', 'nd'], w=[pdk])
                        sc.op('dve', lambda e: e.reciprocal(rden[:], pd[0:64, :]), r=[pdk], w=['rden'])
                        f_ = fo[qb % 2]
                        fk_ = f'fo{qb % 2}'
                        sc.op('dve', lambda e: e.tensor_tensor(f_[:], nd[0:64, :], rden[:], ALU.mult), r=['nd', 'rden'], w=[fk_])
                        sc.dma('sp', MIXT[768 + 64 * h:832 + 64 * h, q0:q0 + 512], f_[:], r=[fk_], w=[('MIXT3', h, qb)])
                sc.barrier()
                if upto == 3:
                    stop[0] = True

            with ExitStack() as st4:
              for _once in ([0] if not stop[0] else []):
                wo = sb("wo", [128, 8, D], BF16, st4)
                for kc in range(8):
                    sc.dma('pool', wo[:, kc, :], w_o_d[l, kc * 128:(kc + 1) * 128, :], w=[('wo', kc)])
                rwt = sb("rwt", [128, 8, NE], F32, st4)
                sc.dma('sp', rwt[:], rw_d[l].rearrange("(kc p) e -> p kc e", p=128), w=['rwt'])
                rbt = sb("rbt", [128, NE], F32, st4)
                sc.dma('sp', rbt[:], rb_d[l, 0:1, :].partition_broadcast(128), w=['rbt'])
                mx = [sb(f"mx{i}", [128, 8, 512], BF16, st4) for i in range(2)]
                xr = [sb(f"xr{i}", [128, 8, 512], F32, st4) for i in range(2)]
                z = sb("z", [128, 8, 512], F32, st4)
                stl = dict(sq=[sb(f"sq4{i}", [128, 512], F32, st4) for i in range(2)], sqk='s4', mean=sb("mean", [128, 512], F32, st4),
                           rstd=sb("rstd", [128, 512], F32, st4), tmp=sb("tmp", [128, 512], F32, st4))
                x1f = z
                x1h = sb("x1h", [128, 8, 512], BF16, st4)
                lg = sb("lg", [128, NE], F32, st4)
                m8 = sb("m8", [128, 8], F32, st4)
                nmx = sb("nmx", [128, 1], F32, st4)
                mk = sb("mk", [128, NE], F32, st4)
                ex = sb("ex", [128, NE], F32, st4)
                ssum = sb("ssum", [128, 1], F32, st4)
                gtt = sb("gtt", [NE, 512], F32, st4)
                xtm = [sb(f"xtm{i}", [128, D], BF16, st4) for i in range(2)]
                cnt = sb("cnt", [128, NE], F32, st4)
                sc.op('dve', lambda e: e.memset(cnt[:], 0.0), w=['cnt'])
                posf = sb("posf", [128, NE], F32, st4)
                ohf = sb("ohf", [128, NE], F32, st4)
                idx8 = sb("idx8", [128, 8], U32, st4)
                idxf = sb("idxf", [128, 8], F32, st4)
                pos4 = sb("pos4", [128, 4], F32, st4)
                ov4 = sb("ov4", [128, 4], F32, st4)
                e4 = sb("e4", [128, 4], F32, st4)
                for tb in range(NB):
                    t0 = tb * 512
                    p = tb % 2
                    sl = slice(t0, t0 + 512)
                    sc.dma('sp', mx[p][:], chunked(MIXT)[:, :, sl], w=[f'mx{p}'])
                    sc.dma('sp', xr[p][:], chunked(XT)[:, :, sl], w=[f'xr{p}'])
                    for dc in range(8):
                        pm, pmk = bank()
                        for kc in range(8):
                            mm(pm[:, :], wo[:, kc, 128 * dc:128 * dc + 128], mx[p][:, kc, :], kc == 0, kc == 7, r=[('wo', kc), f'mx{p}'], w=[pmk])
                        sc.op('dve', lambda e, dc=dc: e.scalar_tensor_tensor(z[:, dc, :], xr[p][:, dc, :], ALPHA, pm[:, :], ALU.mult, ALU.add),
                              r=[f'xr{p}', pmk], w=['z'])
                    ln_block(z, 'z', 78, 86, stl)
                    sc.op('dve', lambda e: e.tensor_copy(x1h[:], z[:]), r=['z'], w=['x1h'])
                    sc.dma('sp', chunked(X1T)[:, :, sl], x1f[:], r=['z'], w=[('X1T', tb)])
                    sc.dma('sp', chunked(X1B)[:, :, sl], x1h[:], r=['x1h'], w=[('X1B', tb)])
                    for sub in range(4):
                        pm, pmk = bank()
                        for kc in range(8):
                            mm(pm[:, 0:NE], x1f[:, kc, 128 * sub:128 * sub + 128], rwt[:, kc, :], kc == 0, kc == 7, r=['z', 'rwt'], w=[pmk])
                        sc.op('dve', lambda e: e.tensor_tensor(lg[:], pm[:, 0:NE], rbt[:], ALU.add), r=[pmk, 'rbt'], w=['lg'])
                        sc.op('dve', lambda e: e.max(m8[:], lg[:]), r=['lg'], w=['m8'])
                        sc.op('dve', lambda e: e.tensor_scalar(mk[:], lg[:], m8[:, 3:4], None, ALU.is_ge), r=['lg', 'm8'], w=['mk'])
                        sc.op('dve', lambda e: e.tensor_scalar(nmx[:], m8[:, 0:1], -1.0, None, ALU.mult), r=['m8'], w=['nmx'])
                        sc.op('act', lambda e: e.activation(ex[:], lg[:], AF.Exp, bias=nmx[:, 0:1], scale=1.0), r=['lg', 'nmx'], w=['ex'])
                        sc.op('dve', lambda e: e.tensor_tensor(ex[:], ex[:], mk[:], ALU.mult), r=['ex', 'mk'], w=['ex'])
                        sc.op('dve', lambda e: e.tensor_reduce(ssum[:], ex[:], AX.X, ALU.add), r=['ex'], w=['ssum'])
                        sc.op('dve', lambda e: e.reciprocal(ssum[:], ssum[:]), r=['ssum'], w=['ssum'])
                        sc.op('dve', lambda e: e.tensor_scalar(ex[:], ex[:], ssum[:, 0:1], None, ALU.mult), r=['ex', 'ssum'], w=['ex'])
                        tt_ = tb * 4 + sub
                        sc.op('dve', lambda e: e.max_index(idx8[:], m8[:], lg[:]), r=['m8', 'lg'], w=['idx8'])
                        sc.op('dve', lambda e: e.tensor_copy(idxf[:], idx8[:]), r=['idx8'], w=['idxf'])
                        sc.op('act', lambda e: e.activation(e4[:], m8[:, 0:4], AF.Exp, bias=nmx[:, 0:1], scale=1.0), r=['m8', 'nmx'], w=['e4'])
                        sc.op('dve', lambda e: e.tensor_scalar(GK[:, tt_, :], e4[:], ssum[:, 0:1], None, ALU.mult), r=['e4', 'ssum'], w=['GK'])
                        pp, ppk = bank()
                        mm(pp[:, 0:NE], iot[:, 0:128], mk[:], True, True, r=['iot', 'mk'], w=[ppk])
                        sc.op('dve', lambda e: e.tensor_tensor(posf[:], pp[:, 0:NE], cnt[:], ALU.add), r=[ppk, 'cnt'], w=['posf'])
                        pt_, ptk = bank()
                        mm(pt_[:, 0:NE], ones1[:], mk[:], True, True, r=['ones1', 'mk'], w=[ptk])
                        sc.op('dve', lambda e: e.tensor_tensor(cnt[:], cnt[:], pt_[:, 0:NE], ALU.add), r=['cnt', ptk], w=['cnt'])
                        for k in range(4):
                            sc.op('dve', lambda e, k=k: e.tensor_scalar(ohf[:], iot[:, 128:128 + NE], idxf[:, k:k + 1], None, ALU.is_equal), r=['iot', 'idxf'], w=['ohf'])
                            sc.op('dve', lambda e: e.tensor_tensor(ohf[:], ohf[:], posf[:], ALU.mult), r=['ohf', 'posf'], w=['ohf'])
                            sc.op('dve', lambda e, k=k: e.tensor_reduce(pos4[:, k:k + 1], ohf[:], AX.X, ALU.add), r=['ohf'], w=['pos4'])
                        sc.op('dve', lambda e: e.tensor_scalar(ov4[:], pos4[:], float(CAP), 1.0e6, ALU.is_ge, ALU.mult), r=['pos4'], w=['ov4'])
                        sc.op('dve', lambda e: e.scalar_tensor_tensor(pos4[:], idxf[:, 0:4], float(CAP), pos4[:], ALU.mult, ALU.add), r=['idxf', 'pos4'], w=['pos4'])
                        sc.op('dve', lambda e: e.tensor_tensor(pos4[:], pos4[:], ov4[:], ALU.add), r=['pos4', 'ov4'], w=['pos4'])
                        sc.op('dve', lambda e: e.tensor_copy(SLOT[:, tt_, :], pos4[:]), r=['pos4'], w=['SLOT'])
                        for kc in range(8):
                            sc.op('pe', lambda e, kc=kc: e.transpose(PSB[:, 128 * kc:128 * kc + 128], x1h[:, kc, 128 * sub:128 * sub + 128], identb[:, :]),
                                  r=['x1h', 'identb'], w=['psb'], inc=(kc == 7))
                        xt_ = xtm[sub % 2]
                        xtk = f'xtm{sub % 2}'
                        sc.op('act', lambda e: e.copy(xt_[:], PSB[:, :]), r=['psb'], w=[xtk])
                        for k in range(4):
                            sc.idma(XG[:, :], bass.IndirectOffsetOnAxis(ap=SLOT[:, tt_, k:k + 1], axis=0), xt_[:, :], None, bnd_reg,
                                    r=[xtk, 'SLOT'], w=[('XG', tt_, k)])
                        pg, pgk = bank()
                        mm(pg[0:NE, 0:128], ex[:], ident, True, True, r=['ex', 'cst'], w=[pgk])
                        sc.op('act', lambda e, sub=sub: e.copy(gtt[:, 128 * sub:128 * sub + 128], pg[0:NE, 0:128]), r=[pgk], w=['gtt'])
                    sc.dma('sp', GT[:, sl], gtt[:], r=['gtt'], w=[('GT', tb)])
                if dbg is not None:
                    sc.dma('sp', SLOTD[:, :], SLOT[:].rearrange('p a b -> p (a b)'), r=['SLOT'], w=['SLOTD'])
                    sc.dma('sp', GKD[:, :], GK[:].rearrange('p a b -> p (a b)'), r=['GK'], w=['GKD'])
                sc.barrier()
                if upto == 4:
                    stop[0] = True

            with ExitStack() as st5:
              for _once in ([0] if not stop[0] else []):
                w1t = [sb(f"w1t{i}", [128, 8, 2048], BF16, st5) for i in range(2)]
                w2t = [sb(f"w2t{i}", [128, 8, D], BF16, st5) for i in range(2)]
                b1t = sb("b1t", [128, NE * 16], F32, st5)
                sc.dma('sp', b1t[:], b1_d[l, :, :], w=['b1t'])
                b2T = sb("b2T", [128, NE * 8], F32, st5)
                sc.dma('sp', b2T[:], b2T_d[l, :, :], w=['b2T'])
                b1p = sb("b1p", [128, NE * 16], F32, st5)
                sc.op('dve', lambda e: e.tensor_scalar(b1p[:], b1t[:], 1.0, None, ALU.add), r=['b1t'], w=['b1p'])
                b1m = b1t
                sc.op('dve', lambda e: e.tensor_scalar(b1m[:], b1t[:], -1.0, 7.0, ALU.mult, ALU.add), r=['b1t'], w=['b1t'])
                c119 = sb("c119", [128, 1], F32, st5)
                sc.op('pool', lambda e: e.memset(c119[:], 1.702 * 7.0), w=['c119'])
                xgt = sb("xgt", [128, 4, D], BF16, st5)
                xs = sb("xs", [128, 8, 512], BF16, st5)
                actt = [sb("actt0", [128, 8, 512], BF16, st5)] * 2
                blk = [0]
                ga = [sb(f"ga{i}", [128, 512], F32, st5) for i in range(3)]
                sgm = [sb(f"sgm{i}", [128, 512], F32, st5) for i in range(3)]
                li = [sb(f"li{i}", [128, 512], F32, st5) for i in range(3)]
                yfm = sb("yfm", [128, 8, 512], F32, st5)
                yk = [sb(f"yk{i}", [128, D], F32, st5) for i in range(4)]
                ytm = [yk[0], yk[1]]
                ycomb = sb("ycomb", [128, D], F32, st5)
                acc = sb("acc", [128, 8, 512], F32, st5)
                hb5 = [xs[:, 0, :], xs[:, 1, :]]
                xr5 = [li[0], li[1]]
                stl = dict(sq=[ga[0], ga[1]], sqk='ga', mean=sgm[0], rstd=sgm[1], tmp=sgm[2], keys=('sgm0', 'sgm1', 'sgm2'))
                XGK = [('XG', t_, k) for t_ in range(NT) for k in range(4)]
                wn = 0
                ytc = [0]
                for ex_ in range(NE):
                    wp = wn % 2
                    wn += 1
                    for kc in range(8):
                        sc.dma('pool', w1t[wp][:, kc, :], w1_d[l, ex_, kc * 128:(kc + 1) * 128, :], w=[(f'w1t{wp}', kc)])
                    for kc in range(8):
                        sc.dma('pool', w2t[wp][:, kc, :], w2_d[l, ex_, kc * 128:(kc + 1) * 128, :], w=[(f'w2t{wp}', kc)])
                    for cb in range(CAPB):
                        r0 = ex_ * CAP + 512 * cb
                        sc.dma('sp', xgt[:], XG[r0:r0 + 512, :].rearrange("(j p) d -> p j d", p=128), r=XGK if (ex_ == 0 and cb == 0) else [], w=['xgt'])
                        for kp in range(4):
                            for kk in range(2):
                                kc = 2 * kp + kk
                                for j in range(4):
                                    sc.op('pe', lambda e, kc=kc, j=j, kk=kk: e.transpose(PSB[:, 512 * kk + 128 * j:512 * kk + 128 * j + 128], xgt[:, j, 128 * kc:128 * kc + 128], identb[:, :]),
                                          r=['xgt', 'identb'], w=['psb'], inc=(kk == 1 and j == 3))
                            sc.op('act', lambda e, kp=kp: e.copy(xs[:, 2 * kp:2 * kp + 2, :].rearrange("p a b -> p (a b)"), PSB[:, :]), r=['psb'], w=[('xs', kp)])
                        XSK = [('xs', kc // 2) for kc in range(8)]
                        ab = actt[blk[0] % 2]
                        abk = 'actt0'
                        blk[0] += 1

                        def X(fc):
                            pg, pgk = bank()
                            for kc in range(8):
                                mm(pg[:, :], w1t[wp][:, kc, 128 * fc:128 * fc + 128], xs[:, kc, :], kc == 0, kc == 7, r=[(f'w1t{wp}', kc), XSK[kc]], w=[pgk])
                            pl, plk = bank()
                            for kc in range(8):
                                mm(pl[:, :], w1t[wp][:, kc, 1024 + 128 * fc:1152 + 128 * fc], xs[:, kc, :], kc == 0, kc == 7, r=[(f'w1t{wp}', kc), XSK[kc]], w=[plk])
                            q = fc % 3
                            bl = b1p[:, ex_ * 16 + 8 + fc:ex_ * 16 + 8 + fc + 1]
                            bg7 = b1m[:, ex_ * 16 + fc:ex_ * 16 + fc + 1]
                            sc.op('act', lambda e: e.activation(sgm[q][:], pg[:, :], AF.Relu, bias=bg7, scale=-1.0), r=[pgk, 'b1t'], w=[f'sgm{q}'])
                            sc.op('act', lambda e: e.activation(ga[q][:], sgm[q][:], AF.Silu, bias=c119[:, 0:1], scale=-1.702), r=[f'sgm{q}', 'c119'], w=[f'ga{q}'])
                            sc.op('dve', lambda e: e.tensor_scalar(li[q][:], pl[:, :], bl, -6.0, ALU.add, ALU.max), r=[plk, 'b1p'], w=[f'li{q}'])

                        def Y(fc):
                            q = fc % 3
                            sc.op('dve', lambda e: e.scalar_tensor_tensor(ab[:, fc, :], li[q][:], 8.0, ga[q][:], ALU.min, ALU.mult), r=[f'ga{q}', f'li{q}'], w=[(abk, fc)])

                        X(0)
                        X(1)
                        for fc in range(8):
                            if fc + 2 < 8:
                                X(fc + 2)
                            Y(fc)
                        for dc in range(8):
                            pm, pmk = bank()
                            for fc in range(8):
                                mm(pm[:, :], w2t[wp][:, fc, 128 * dc:128 * dc + 128], ab[:, fc, :], fc == 0, fc == 7, r=[(f'w2t{wp}', fc), (abk, fc)], w=[pmk])
                            sc.op('act', lambda e, dc=dc: e.activation(yfm[:, dc, :], pm[:, :], AF.Identity, bias=b2T[:, ex_ * 8 + dc:ex_ * 8 + dc + 1], scale=1.0 / 1.702),
                                  r=[pmk, 'b2T'], w=[('yfm', dc)])
                        for j in range(4):
                            for half in range(2):
                                pT, pTk = bank()
                                for d4 in range(4):
                                    dc = 4 * half + d4
                                    mm(pT[:, 128 * d4:128 * d4 + 128], yfm[:, dc, 128 * j:128 * j + 128], ident, True, True, r=[('yfm', dc), 'cst'], w=[pTk], inc=(d4 == 3))
                                y_ = ytm[ytc[0] % 2]
                                yk_ = f'yk{ytc[0] % 2}'
                                sc.op('act' if half else 'dve', (lambda e, half=half: e.copy(y_[:, 512 * half:512 * half + 512], pT[:, :])) if half else
                                      (lambda e, half=half: e.tensor_copy(y_[:, 512 * half:512 * half + 512], pT[:, :])), r=[pTk], w=[yk_])
                            sc.dma('sp', YS[r0 + 128 * j:r0 + 128 * j + 128, :], y_[:], r=[yk_], w=[('YS', ex_, cb, j)])
                            ytc[0] += 1
                YSK = [('YS', e_, c_, j_) for e_ in range(NE) for c_ in range(CAPB) for j_ in range(4)]
                for tb in range(NB):
                    sl = slice(tb * 512, tb * 512 + 512)
                    for sub in range(4):
                        tt_ = tb * 4 + sub
                        for k in range(4):
                            sc.idma(yk[k][:, :], None, YS[:, :], bass.IndirectOffsetOnAxis(ap=SLOT[:, tt_, k:k + 1], axis=0), bnd_reg,
                                    r=(YSK if (tt_ == 0 and k == 0) else []) + ['SLOT'], w=[f'yk{k}'])
                        sc.op('dve', lambda e: e.tensor_scalar(ycomb[:], yk[0][:], GK[:, tt_, 0:1], None, ALU.mult), r=['yk0', 'GK'], w=['ycomb'])
                        for k in range(1, 4):
                            sc.op('dve', lambda e, k=k: e.scalar_tensor_tensor(ycomb[:], yk[k][:], GK[:, tt_, k:k + 1], ycomb[:], ALU.mult, ALU.add),
                                  r=[f'yk{k}', 'GK', 'ycomb'], w=['ycomb'])
                        for half in range(2):
                            pT, pTk = bank()
                            for d4 in range(4):
                                dc = 4 * half + d4
                                mm(pT[:, 128 * d4:128 * d4 + 128], ycomb[:, 128 * dc:128 * dc + 128], ident, True, True, r=['ycomb', 'cst'], w=[pTk], inc=(d4 == 3))
                            sc.op('act', lambda e, half=half, sub=sub: e.copy(acc[:, 4 * half:4 * half + 4, 128 * sub:128 * sub + 128],
                                                                              pT[:, :].rearrange("p (a b) -> p a b", a=4)), r=[pTk], w=['acc'])
                    zs = acc
                    for dc in range(8):
                        xr_ = xr5[dc % 2]
                        xk_ = f'li{dc % 2}'
                        sc.dma('sp', xr_[:], X1T[128 * dc:128 * dc + 128, sl], r=[('X1T', tb)], w=[xk_])
                        sc.op('dve', lambda e, dc=dc: e.scalar_tensor_tensor(zs[:, dc, :], xr_[:], ALPHA, zs[:, dc, :], ALU.mult, ALU.add),
                              r=[xk_, 'acc'], w=['acc'])
                    ln_block(zs, 'acc', 94, 102, stl)
                    if last:
                        sc.dma('sp', chunked(outT)[:, :, sl], zs[:], r=['acc'], w=[('outT', tb)])
                    else:
                        sc.dma('sp', chunked(XT)[:, :, sl], zs[:], r=['acc'], w=[('XT', tb)])
                        for dc in range(8):
                            hb_ = hb5[dc % 2]
                            hk_ = ('xs', 0)
                            sc.op('act', lambda e, dc=dc: e.copy(hb_, zs[:, dc, :]), r=['acc'], w=[hk_])
                            sc.dma('sp', XB[128 * dc:128 * dc + 128, sl], hb_, r=[hk_], w=[('XB', tb, dc)])
                sc.barrier()
                if upto == 5:
                    stop[0] = True
        except _Stop:
            pass
    return nc


def host_consts(S):
    half = 16
    freqs = (10000.0 ** (-np.arange(half, dtype=np.float32) / half)).astype(np.float32)
    pos = np.arange(S, dtype=np.float32)
    ang = pos[None, :] * freqs[:, None]
    cos = np.cos(ang).astype(np.float32)
    sin = np.sin(ang).astype(np.float32)
    ropec = np.tile(np.concatenate([cos, cos], 0), (4, 1))
    ropes = np.tile(np.concatenate([-sin, sin], 0), (4, 1))
    cst = np.zeros((128, 4 * 128 + 512 + 4 + 128), np.float32)
    idx = np.arange(128)
    same = (idx[:, None] // 64) == (idx[None, :] // 64)
    scale = 32 ** -0.5
    for h in range(4):
        g = 1.0 - 2.0 ** (-5.0 - h)
        cst[:, 128 * h:128 * h + 128] = np.where(same, g ** np.abs(idx[:, None] - idx[None, :]), 0.0) * scale
        cst[32 * h:32 * h + 32, 512:1024] = (g ** ((np.arange(512) % 64) + 1.0))[None, :]
        cst[:, 1024 + h] = g ** (63 - (idx % 64)) * scale
    cst[:, 1028:1156] = np.eye(128, dtype=np.float32)
    msk = np.zeros((128, 4 * 512), np.float32)
    s_ = np.arange(128)[:, None]
    t_ = np.arange(512)[None, :]
    for m in range(4):
        msk[:, 512 * m:512 * m + 512] = (t_ >= 128 * m + s_)
    io = np.zeros((128, 160), np.float32)
    io[:, 0:128] = (np.arange(128)[:, None] < np.arange(128)[None, :])
    io[:, 128:160] = np.arange(32, dtype=np.float32)[None, :]
    return dict(ropec=np.ascontiguousarray(ropec), ropes=np.ascontiguousarray(ropes), cst=cst, msk=msk, iota=io)


def host_layout(inp, S, DEPTH, NE):
    L = DEPTH
    perm = np.concatenate([np.arange(16, 32), np.arange(0, 16)])
    pq = np.concatenate([h * 32 + perm for h in range(4)])
    w_in = np.asarray(inp['w_in'])
    w_aug = np.concatenate([w_in, w_in[:, :, pq], w_in[:, :, 128 + pq]], axis=2)
    cv = np.zeros((L, 128, NCV), np.float32)
    cdw = np.asarray(inp['conf_dw'])
    sdw = np.asarray(inp['sc_dw'])
    for c in range(2):
        cv[:, :, 31 * c:31 * c + 31] = cdw[:, :, 128 * c:128 * c + 128].transpose(0, 2, 1)
        cv[:, :, 62 + 3 * c:65 + 3 * c] = sdw[:, :, 128 * c:128 * c + 128].transpose(0, 2, 1)
        cv[:, :, 68 + c] = np.asarray(inp['conf_dw_b'])[:, 128 * c:128 * c + 128]
        cv[:, :, 70 + c] = np.asarray(inp['conf_ln_g'])[:, 128 * c:128 * c + 128]
        cv[:, :, 72 + c] = np.asarray(inp['conf_ln_b'])[:, 128 * c:128 * c + 128]
    cv[:, 0:64, 74:78] = np.asarray(inp['ret_gn_g']).reshape(L, 4, 64).transpose(0, 2, 1)
    for name, c0 in (('ln1_g', 78), ('ln1_b', 86), ('ln2_g', 94), ('ln2_b', 102)):
        cv[:, :, c0:c0 + 8] = np.asarray(inp[name]).reshape(L, 8, 128).transpose(0, 2, 1)
    b1T = np.ascontiguousarray(np.asarray(inp['b1']).reshape(L, NE, 16, 128).transpose(0, 3, 1, 2).reshape(L, 128, NE * 16))
    b2T = np.ascontiguousarray(np.asarray(inp['b2']).reshape(L, NE, 8, 128).transpose(0, 3, 1, 2).reshape(L, 128, NE * 8))
    rw = np.asarray(inp['router_w'])
    rb = np.asarray(inp['router_b'])
    common = dict(w_in=np.ascontiguousarray(w_aug), w_o=np.asarray(inp['w_o']), cvec=cv,
                  foxb=np.asarray(inp['fox_b_f']).reshape(L, 1, 4), rw=rw, rb=rb.reshape(L, 1, -1),
                  w1=np.asarray(inp['w1']), b1T=b1T, w2=np.asarray(inp['w2']), b2=np.asarray(inp['b2']), b2T=b2T)
    common.update(host_consts(S))
    return common


def run(inp, S, DEPTH, NE, SBK, dbg=None, upto=99, trace=False):
    x = np.asarray(inp['x'])
    B = x.shape[0]
    common = host_layout(inp, S, DEPTH, NE)
    nc = build(S, DEPTH, NE, SBK, dbg, upto)
    in_maps = []
    for b in range(B):
        m = dict(common)
        m['xT'] = np.ascontiguousarray(x[b].T)
        in_maps.append(m)
    res = run_bass_kernel_spmd(nc, in_maps, core_ids=list(range(B)), **({'trace': True} if trace else {}))
    if trace:
        print('EXEC_NS', res.exec_time_ns)
    out = np.stack([np.ascontiguousarray(r['outT'].T) for r in res.results], 0).astype(np.float32)
    if dbg is not None:
        return out, res.results
    return out


def kernel(**inputs):
    return run(inputs, 8192, 4, 32, 1024)
```

```python
import math
import os
KN = int(os.environ.get('KN', '99'))
KW = int(os.environ.get('KW', '1'))
from contextlib import ExitStack
import numpy as np
import concourse.bass as bass
import concourse.mybir as mybir
from concourse.bass_utils import run_bass_kernel_spmd

F32 = mybir.dt.float32
BF16 = mybir.dt.bfloat16
ALU = mybir.AluOpType
AF = mybir.ActivationFunctionType
AX = mybir.AxisListType

D = 1024
CHUNK = 64
ALPHA = 8 ** 0.25
EPS = 1e-5
NCV = 110
WA = 2820 + 256


class Sched:
    def __init__(s, nc, es):
        s.nc = nc
        s.eng = {'pe': nc.tensor, 'act': nc.scalar, 'dve': nc.vector, 'pool': nc.gpsimd, 'sp': nc.sync}
        s.sem = {k: es.enter_context(nc.semaphore('s_' + k)) for k in ('pe', 'act', 'dve', 'pool')}
        s.cnt = {k: 0 for k in s.sem}
        s.seen = {k: {} for k in s.eng}
        s.dq = {}
        for q in ('sp', 'pool', 'act'):
            s.dq[q] = dict(sems=[es.enter_context(nc.semaphore(f'd_{q}{i}')) for i in range(8)], n=0)
        s.res = {}

    def _need(s, en, deps):
        best = {}
        for (k, h, v) in deps:
            if v > best.get(k, (None, 0))[1]:
                best[k] = (h, v)
        for k, (h, v) in best.items():
            if s.seen[en].get(k, 0) < v:
                s.eng[en].wait_ge(h, v)
                s.seen[en][k] = v

    def _collect(s, en, r, w):
        deps = []
        for k in r:
            st = s.res.get(k)
            if st and st[0]:
                deps.append(st[0])
        for k in w:
            st = s.res.get(k)
            if st:
                if st[0]:
                    deps.append(st[0])
                deps.extend(st[1].values())
        if en == 'pe':
            deps = [d for d in deps if d[0] != 'pe']
        return deps

    def _record(s, dep, r, w):
        for k in r:
            st = s.res.setdefault(k, [None, {}])
            o = st[1].get(dep[0])
            if o is None or o[2] < dep[2]:
                st[1][dep[0]] = dep
        for k in w:
            s.res[k] = [dep, {}]

    def op(s, en, fn, r=(), w=(), inc=True):
        assert inc or en == 'pe'
        s._need(en, s._collect(en, r, w))
        ins = fn(s.eng[en])
        if inc:
            s.cnt[en] += 1
            ins.then_inc(s.sem[en], 1)
            dep = (en, s.sem[en], s.cnt[en])
        else:
            dep = (en, s.sem[en], s.cnt[en] + 1)
        s._record(dep, r, w)

    def dma(s, q, out, in_, r=(), w=(), **kw):
        Q = s.dq[q]
        i = Q['n']
        Q['n'] += 1
        K = len(Q['sems'])
        h = Q['sems'][i % K]
        key = f'd_{q}{i % K}'
        deps = s._collect(q, r, w)
        if i >= K:
            deps.append((key, h, 16 * (i // K)))
        s._need(q, deps)
        s.eng[q].dma_start(out=out, in_=in_, **kw).then_inc(h, 16)
        s._record((key, h, 16 * (i // K + 1)), r, w)

    def idma(s, out, out_off, in_, in_off, bound, r=(), w=()):
        q = 'pool'
        Q = s.dq[q]
        i = Q['n']
        Q['n'] += 1
        K = len(Q['sems'])
        h = Q['sems'][i % K]
        key = f'd_{q}{i % K}'
        deps = s._collect(q, r, w)
        if i >= K:
            deps.append((key, h, 16 * (i // K)))
        s._need(q, deps)
        s.eng[q].indirect_dma_start(out=out, out_offset=out_off, in_=in_, in_offset=in_off,
                                    bounds_check=bound, oob_is_err=False).then_inc(h, 16)
        s._record((key, h, 16 * (i // K + 1)), r, w)

    def alldeps(s):
        deps = [(k, s.sem[k], s.cnt[k]) for k in s.sem if s.cnt[k] > 0]
        for q, Q in s.dq.items():
            K = len(Q['sems'])
            for j in range(min(K, Q['n'])):
                tot = (Q['n'] - 1 - j) // K + 1
                deps.append((f'd_{q}{j}', Q['sems'][j], 16 * tot))
        return deps

    def barrier(s):
        deps = s.alldeps()
        for en in s.eng:
            s._need(en, [d for d in deps if not (en == 'pe' and d[0] == 'pe')])
        s.res = {}


class _Stop(Exception):
    pass


def build(S, DEPTH, NE, SBK, dbg=None, upto=99):
    NB = S // 512
    NT = S // 128
    NCH = S // 64
    NSB = S // SBK
    CAPB = -(-int(1.5 * 4 * S / NE) // 512)
    CAP = 512 * CAPB
    U32 = mybir.dt.uint32
    nc = bass.Bass("TRN2", target_bir_lowering=False)
    dt = nc.dram_tensor
    xT_in = dt("xT", [D, S], F32, kind="ExternalInput").ap()
    w_in_d = dt("w_in", [DEPTH, D, WA], F32, kind="ExternalInput").ap()
    w_o_d = dt("w_o", [DEPTH, D, D], F32, kind="ExternalInput").ap()
    cvec_d = dt("cvec", [DEPTH, 128, NCV], F32, kind="ExternalInput").ap()
    foxb_d = dt("foxb", [DEPTH, 1, 4], F32, kind="ExternalInput").ap()
    rw_d = dt("rw", [DEPTH, D, NE], F32, kind="ExternalInput").ap()
    rb_d = dt("rb", [DEPTH, 1, NE], F32, kind="ExternalInput").ap()
    w1_d = dt("w1", [DEPTH, NE, D, 2048], F32, kind="ExternalInput").ap()
    b1_d = dt("b1T", [DEPTH, 128, NE * 16], F32, kind="ExternalInput").ap()
    w2_d = dt("w2", [DEPTH, NE, D, D], F32, kind="ExternalInput").ap()
    b2_d = dt("b2", [DEPTH, NE, D], F32, kind="ExternalInput").ap()
    cos_d = dt("ropec", [128, S], F32, kind="ExternalInput").ap()
    sin_d = dt("ropes", [128, S], F32, kind="ExternalInput").ap()
    cst_d = dt("cst", [128, 4 * 128 + 512 + 4 + 128], F32, kind="ExternalInput").ap()
    msk_d = dt("msk", [128, 4 * 512], F32, kind="ExternalInput").ap()
    io_d = dt("iota", [128, 128 + 32], F32, kind="ExternalInput").ap()
    b2T_d = dt("b2T", [DEPTH, 128, NE * 8], F32, kind="ExternalInput").ap()
    outT = dt("outT", [D, S], F32, kind="ExternalOutput").ap()
    okind = {} if dbg is None else {"kind": "ExternalOutput"}
    XT = dt("XT", [D, S], F32).ap()
    XB = dt("XB", [D, S], BF16).ap()
    X1T = dt("X1T", [D, S], F32).ap()
    X1B = dt("X1B", [D, S], BF16).ap()
    MIXT = dt("MIXT", [D, S], BF16, **okind).ap()
    RQ = dt("RQ", [128, S], BF16).ap()
    RK = dt("RK", [128, S], BF16).ap()
    RV = dt("RV", [S, 256], BF16).ap()
    GATE = dt("GATE", [256, S], BF16).ap()
    FQ = dt("FQ", [256, S], BF16).ap()
    FK = dt("FK", [256, S], BF16).ap()
    FV = dt("FV", [S, 256], BF16).ap()
    FF = dt("FF", [4, S], F32).ap()
    FRD = dt("FRD", [7, S], BF16).ap()
    GT = dt("GT", [NE, S], F32, **okind).ap()
    XG = dt("XG", [NE * CAP, D], BF16, **okind).ap()
    SLOTD = dt("SLOTD", [128, NT * 4], U32, **okind).ap()
    GKD = dt("GKD", [128, NT * 4], F32, **okind).ap()
    YS = dt("YS", [NE * CAP, D], F32).ap()

    def chunked(ap):
        return ap.rearrange("(kc p) t -> p kc t", p=128)

    with ExitStack() as es:
        sc = Sched(nc, es)
        uid = [0]

        def sb(name, shape, dtp, st=es):
            uid[0] += 1
            return st.enter_context(nc.sbuf_tensor(f"{name}_s{uid[0]}", shape, dtp))
        PS = [es.enter_context(nc.psum_tensor(f"ps{i}", [128, 512], F32)) for i in range(7)]
        PSB = es.enter_context(nc.psum_tensor("psb", [128, 1024], BF16))
        pctr = [0]

        def bank():
            pctr[0] = (pctr[0] + 1) % 7
            return PS[pctr[0]], f"ps{pctr[0]}"

        cst = sb("cst", [128, 4 * 128 + 512 + 4 + 128], F32)
        sc.dma('sp', cst[:], cst_d[:, :], w=['cst'])
        identb = sb("identb", [128, 128], BF16)
        sc.op('dve', lambda e: e.tensor_copy(identb[:], cst[:, 1028:1156]), r=['cst'], w=['identb'])
        ident = cst[:, 1028:1156]
        onesD = sb("onesD", [128, 128], F32)
        sc.op('pool', lambda e: e.memset(onesD[:], 1.0 / D), w=['onesD'])
        ones256 = sb("ones256", [128, 128], F32)
        sc.op('pool', lambda e: e.memset(ones256[:], 1.0 / 256), w=['ones256'])
        ones64 = sb("ones64", [64, 64], F32)
        sc.op('pool', lambda e: e.memset(ones64[:], 1.0 / 64), w=['ones64'])
        onesb = sb("onesb", [128, 64], BF16)
        sc.op('pool', lambda e: e.memset(onesb[:], 1.0), w=['onesb'])
        cv = sb("cv", [128, NCV], F32)
        iot = sb("iot", [128, 160], F32)
        sc.dma('sp', iot[:], io_d[:, :], w=['iot'])
        ones1 = sb("ones1", [128, 128], F32)
        sc.op('pool', lambda e: e.memset(ones1[:], 1.0), w=['ones1'])
        SLOT = sb("SLOT", [128, NT, 4], U32)
        bnd_reg = nc.gpsimd.to_reg(NE * CAP - 1)
        GK = sb("GK", [128, NT, 4], F32)

        def mm(out, lhsT, rhs, start, stop, r, w, inc=None):
            sc.op('pe', lambda e: e.matmul(out, lhsT, rhs, start=start, stop=stop), r=r, w=w,
                  inc=(stop if inc is None else inc))

        def ln_block(z, zk, gcol, bcol, st):
            pm, pmk = bank()
            for dc in range(8):
                mm(pm[:, :], onesD[:], z[:, dc, :], dc == 0, dc == 7, r=['onesD', zk], w=[pmk])
            pe2, pe2k = bank()
            for dc in range(8):
                sq = st['sq'][dc % 2]
                sqk = st['sqk'] + str(dc % 2)
                sc.op('act', lambda e, dc=dc: e.activation(sq[:], z[:, dc, :], AF.Square), r=[zk], w=[sqk])
                mm(pe2[:, :], onesD[:], sq[:], dc == 0, dc == 7, r=['onesD', sqk], w=[pe2k], inc=True)
            mean, rstd, tmp = st['mean'], st['rstd'], st['tmp']
            mk_, rk_, tk_ = st.get('keys', (st['sqk'] + 'mean', st['sqk'] + 'rstd', st['sqk'] + 'tmp'))
            sc.op('act', lambda e: e.copy(mean[:], pm[:, :]), r=[pmk], w=[mk_])
            sc.op('dve', lambda e: e.tensor_tensor(rstd[:], mean[:], mean[:], ALU.mult), r=[mk_], w=[rk_])
            sc.op('dve', lambda e: e.tensor_tensor(rstd[:], pe2[:, :], rstd[:], ALU.subtract), r=[pe2k, rk_], w=[rk_])
            sc.op('dve', lambda e: e.tensor_scalar(rstd[:], rstd[:], 0.0, EPS, ALU.max, ALU.add), r=[rk_], w=[rk_])
            sc.op('act', lambda e: e.activation(rstd[:], rstd[:], AF.Ln), r=[rk_], w=[rk_])
            sc.op('act', lambda e: e.activation(rstd[:], rstd[:], AF.Exp, scale=-0.5), r=[rk_], w=[rk_])
            for dc in range(8):
                sc.op('dve', lambda e, dc=dc: e.tensor_tensor(tmp[:], z[:, dc, :], mean[:], ALU.subtract), r=[zk, mk_], w=[tk_])
                sc.op('dve', lambda e, dc=dc: e.tensor_tensor(tmp[:], tmp[:], rstd[:], ALU.mult), r=[tk_, rk_], w=[tk_])
                sc.op('act', lambda e, dc=dc: e.activation(z[:, dc, :], tmp[:], AF.Identity,
                                                          bias=cv[:, bcol + dc:bcol + dc + 1], scale=cv[:, gcol + dc:gcol + dc + 1]),
                      r=[tk_, 'cv'], w=[zk])

        with ExitStack() as st0:
            xf = [sb(f"xf{i}", [128, 8, 512], F32, st0) for i in range(2)]
            zt = sb("zt", [128, 4096], BF16, st0)
            sc.op('pool', lambda e: e.memset(zt[:], 0.0), w=['zt'])
            XGz = XG.rearrange("(n p r) d -> n p (r d)", p=128, r=4)
            for n_ in range(NE * CAP // 512):
                sc.dma('sp', XGz[n_], zt[:], r=['zt'], w=[('XGz', n_)])
            xh = [sb(f"xh{i}", [128, 8, 512], BF16, st0) for i in range(2)]
            for tb in range(NB):
                p = tb % 2
                sl = slice(tb * 512, tb * 512 + 512)
                sc.dma('sp', xf[p][:], chunked(xT_in)[:, :, sl], w=[f'xf{p}'])
                sc.op('dve', lambda e, p=p: e.tensor_copy(xh[p][:], xf[p][:]), r=[f'xf{p}'], w=[f'xh{p}'])
                sc.dma('sp', chunked(XT)[:, :, sl], xf[p][:], r=[f'xf{p}'], w=[('XT', tb)])
                sc.dma('sp', chunked(XB)[:, :, sl], xh[p][:], r=[f'xh{p}'], w=[('XB', tb)])
            sc.barrier()

        stop = [False]
        try:
          for l in range(DEPTH):
            last = l == DEPTH - 1
            sc.dma('sp', cv[:], cvec_d[l, :, :], w=['cv'])
            with ExitStack() as st1:
              for _once in ([0] if not stop[0] else []):
                win = sb("win", [128, 8, WA], BF16, st1)
                for kc in range(8):
                    sc.dma('pool', win[:, kc, :], w_in_d[l, kc * 128:(kc + 1) * 128, :], w=[('win', kc)])
                WK = [('win', kc) for kc in range(8)]
                xbt = [sb(f"xbt{i}", [128, 8, 544], BF16, st1) for i in range(2)]
                cs_t = sb("cs_t", [128, 512], F32, st1)
                sn_t = sb("sn_t", [128, 512], F32, st1)
                t1 = sb("t1", [128, 512], F32, st1)
                t2 = sb("t2", [128, 512], F32, st1)
                ob = [sb(f"ob{i}", [128, 512], BF16, st1) for i in range(4)]
                obc = [0]
                u_t = [sb(f"u{i}", [128, 544], F32, st1) for i in range(2)]
                sg_t = sb("sg_t", [128, 544], F32, st1)
                ca_t = [sb(f"ca{i}", [128, 512], F32, st1) for i in range(2)]
                sq1 = sb("sq1", [128, 512], F32, st1)
                mean1 = sb("mean1", [128, 512], F32, st1)
                rstd1 = sb("rstd1", [128, 512], F32, st1)
                ff_t = sb("ff_t", [4, 512], F32, st1)
                vt = [sb(f"vt{i}", [128, 256], BF16, st1) for i in range(2)]
                vtc = [0]

                def nob():
                    obc[0] = (obc[0] + 1) % 4
                    return ob[obc[0]], f"ob{obc[0]}"

                for tb in range(NB):
                    t0 = tb * 512
                    p = tb % 2
                    xk = f'xbt{p}'
                    x_ = xbt[p]
                    if tb == 0:
                        sc.op('pool', lambda e: e.memset(x_[:, :, 0:32], 0.0), w=[xk])
                        sc.dma('sp', x_[:, :, 32:544], chunked(XB)[:, :, 0:512], r=[('XB', 0)], w=[xk])
                    else:
                        sc.dma('sp', x_[:, :, :], chunked(XB)[:, :, t0 - 32:t0 + 512], r=[('XB', tb - 1), ('XB', tb)], w=[xk])
                    sc.dma('sp', cs_t[:], cos_d[:, t0:t0 + 512], w=['cs_t'])
                    sc.dma('sp', sn_t[:], sin_d[:, t0:t0 + 512], w=['sn_t'])

                    def fm(c0, ncols=128, halo=False):
                        pm, pmk = bank()
                        for kc in range(8):
                            mm(pm[0:ncols, :], win[:, kc, c0:c0 + ncols], x_[:, kc, 32:544], kc == 0, kc == 7, r=[WK[kc], xk], w=[pmk])
                        if not halo:
                            return pm, pmk
                        ph, phk = bank()
                        for kc in range(8):
                            mm(ph[0:ncols, 0:32], win[:, kc, c0:c0 + ncols], x_[:, kc, 0:32], kc == 0, kc == 7, r=[WK[kc], xk], w=[phk])
                        return pm, pmk, ph, phk

                    for (c0, cp, dst, dk) in ((0, 2820, RQ, 'RQ'), (128, 2948, RK, 'RK')):
                        pa, pak = fm(c0)
                        pp, ppk = fm(cp)
                        sc.op('dve', lambda e: e.tensor_tensor(t1[:], pa[:, :], cs_t[:], ALU.mult), r=[pak, 'cs_t'], w=['t1'])
                        sc.op('dve', lambda e: e.tensor_tensor(t2[:], pp[:, :], sn_t[:], ALU.mult), r=[ppk, 'sn_t'], w=['t2'])
                        o, ok = nob()
                        sc.op('dve', lambda e: e.tensor_tensor(o[:], t1[:], t2[:], ALU.add), r=['t1', 't2'], w=[ok])
                        sc.dma('sp', dst[:, t0:t0 + 512], o[:], r=[ok], w=[(dk, tb)])
                    for c in range(2):
                        pa, pak = fm(512 + 128 * c)
                        o, ok = nob()
                        sc.op('act', lambda e: e.activation(o[:], pa[:, :], AF.Silu), r=[pak], w=[ok])
                        sc.dma('sp', GATE[128 * c:128 * c + 128, t0:t0 + 512], o[:], r=[ok], w=[('GATE', tb)])
                    for c in range(2):
                        pa, pak, pah, pahk = fm(768 + 128 * c, halo=True)
                        pb_, pbk, pbh, pbhk = fm(1024 + 128 * c, halo=True)
                        u = u_t[c]
                        uk = f'u{c}'
                        sc.op('act', lambda e: e.activation(sg_t[:, 32:544], pb_[:, :], AF.Sigmoid), r=[pbk], w=['sg_t'])
                        sc.op('act', lambda e: e.activation(sg_t[:, 0:32], pbh[:, 0:32], AF.Sigmoid), r=[pbhk, 'sg_t'], w=['sg_t'])
                        sc.op('dve', lambda e: e.tensor_tensor(u[:, 32:544], pa[:, :], sg_t[:, 32:544], ALU.mult), r=[pak, 'sg_t'], w=[uk])
                        sc.op('dve', lambda e: e.tensor_tensor(u[:, 0:32], pah[:, 0:32], sg_t[:, 0:32], ALU.mult), r=[pahk, 'sg_t', uk], w=[uk])
                        ce = 'dve'
                        ca = ca_t[c]
                        cak = f'ca{c}'
                        wc0 = 31 * c
                        sc.op(ce, lambda e: e.tensor_scalar(ca[:], u[:, 2:514], cv[:, wc0:wc0 + 1], cv[:, 68 + c:69 + c], ALU.mult, ALU.add),
                              r=[uk, 'cv'], w=[cak])
                        for j in range(1, 31):
                            sc.op(ce, lambda e, j=j: e.scalar_tensor_tensor(ca[:], u[:, 2 + j:514 + j], cv[:, wc0 + j:wc0 + j + 1], ca[:], ALU.mult, ALU.add),
                                  r=[uk, 'cv', cak], w=[cak])
                    pm, pmk = bank()
                    pe2, pe2k = bank()
                    for c in range(2):
                        mm(pm[:, :], ones256[:], ca_t[c][:], c == 0, c == 1, r=['ones256', f'ca{c}'], w=[pmk])
                    for c in range(2):
                        sc.op('act', lambda e, c=c: e.activation(sq1[:], ca_t[c][:], AF.Square), r=[f'ca{c}'], w=['sq1'])
                        mm(pe2[:, :], ones256[:], sq1[:], c == 0, c == 1, r=['ones256', 'sq1'], w=[pe2k], inc=True)
                    sc.op('act', lambda e: e.copy(mean1[:], pm[:, :]), r=[pmk], w=['mean1'])
                    sc.op('dve', lambda e: e.tensor_tensor(rstd1[:], mean1[:], mean1[:], ALU.mult), r=['mean1'], w=['rstd1'])
                    sc.op('dve', lambda e: e.tensor_tensor(rstd1[:], pe2[:, :], rstd1[:], ALU.subtract), r=[pe2k, 'rstd1'], w=['rstd1'])
                    sc.op('dve', lambda e: e.tensor_scalar(rstd1[:], rstd1[:], 0.0, EPS, ALU.max, ALU.add), r=['rstd1'], w=['rstd1'])
                    sc.op('act', lambda e: e.activation(rstd1[:], rstd1[:], AF.Ln), r=['rstd1'], w=['rstd1'])
                    sc.op('act', lambda e: e.activation(rstd1[:], rstd1[:], AF.Exp, scale=-0.5), r=['rstd1'], w=['rstd1'])
                    for c in range(2):
                        sc.op('dve', lambda e, c=c: e.tensor_tensor(t1[:], ca_t[c][:], mean1[:], ALU.subtract), r=[f'ca{c}', 'mean1'], w=['t1'])
                        sc.op('dve', lambda e: e.tensor_tensor(t1[:], t1[:], rstd1[:], ALU.mult), r=['t1', 'rstd1'], w=['t1'])
                        o, ok = nob()
                        sc.op('act', lambda e, c=c: e.activation(o[:], t1[:], AF.Silu, bias=cv[:, 72 + c:73 + c], scale=cv[:, 70 + c:71 + c]),
                              r=['t1', 'cv'], w=[ok])
                        sc.dma('sp', MIXT[256 + 128 * c:384 + 128 * c, t0:t0 + 512], o[:], r=[ok], w=[('MIXT', tb)])
                    for c in range(2):
                        pc, pck, pch, pchk = fm(1536 + 128 * c, halo=True)
                        ph_, phk_, phh, phhk = fm(1792 + 128 * c, halo=True)
                        u = u_t[c]
                        uk = f'u{c}'
                        sc.op('act', lambda e: e.copy(sg_t[:, 32:544], pc[:, :]), r=[pck], w=['sg_t'])
                        sc.op('act', lambda e: e.copy(sg_t[:, 0:32], pch[:, 0:32]), r=[pchk, 'sg_t'], w=['sg_t'])
                        sc.op('dve', lambda e: e.tensor_tensor(u[:, 32:544], ph_[:, :], sg_t[:, 32:544], ALU.mult), r=[phk_, 'sg_t'], w=[uk])
                        sc.op('dve', lambda e: e.tensor_tensor(u[:, 0:32], phh[:, 0:32], sg_t[:, 0:32], ALU.mult), r=[phhk, 'sg_t', uk], w=[uk])
                        wc0 = 62 + 3 * c
                        sc.op('dve', lambda e: e.tensor_scalar(t1[:], u[:, 30:542], cv[:, wc0:wc0 + 1], None, ALU.mult), r=[uk, 'cv'], w=['t1'])
                        for j in (1, 2):
                            sc.op('dve', lambda e, j=j: e.scalar_tensor_tensor(t1[:], u[:, 30 + j:542 + j], cv[:, wc0 + j:wc0 + j + 1], t1[:], ALU.mult, ALU.add),
                                  r=[uk, 'cv', 't1'], w=['t1'])
                        pbb, pbbk = fm(1280 + 128 * c)
                        o, ok = nob()
                        sc.op('dve', lambda e: e.tensor_tensor(o[:], pbb[:, :], t1[:], ALU.mult), r=[pbbk, 't1'], w=[ok])
                        sc.dma('sp', MIXT[512 + 128 * c:640 + 128 * c, t0:t0 + 512], o[:], r=[ok], w=[('MIXT', tb)])
                    for c in range(2):
                        pa, pak = fm(2048 + 128 * c)
                        o, ok = nob()
                        sc.op('act', lambda e: e.mul(o[:], pa[:, :], 0.125), r=[pak], w=[ok])
                        sc.dma('sp', FQ[128 * c:128 * c + 128, t0:t0 + 512], o[:], r=[ok], w=[('FQ', tb)])
                        pa, pak = fm(2304 + 128 * c)
                        o, ok = nob()
                        sc.op('act', lambda e: e.copy(o[:], pa[:, :]), r=[pak], w=[ok])
                        sc.dma('sp', FK[128 * c:128 * c + 128, t0:t0 + 512], o[:], r=[ok], w=[('FK', tb)])
                    pa, pak = fm(2816, ncols=4)
                    sc.op('act', lambda e: e.copy(ff_t[:], pa[0:4, :]), r=[pak], w=['ff_t'])
                    sc.dma('sp', FF[:, t0:t0 + 512], ff_t[:], r=['ff_t'], w=[('FF', tb)])
                    for sub in range(4):
                        for (c0, dst, dk) in ((256, RV, 'RV'), (2560, FV, 'FV')):
                            pm, pmk = bank()
                            for kc in range(8):
                                mm(pm[:, 0:256], x_[:, kc, 32 + 128 * sub:160 + 128 * sub], win[:, kc, c0:c0 + 256], kc == 0, kc == 7, r=[WK[kc], xk], w=[pmk])
                            vtc[0] ^= 1
                            v = vt[vtc[0]]
                            vk = f'vt{vtc[0]}'
                            sc.op('act', lambda e: e.copy(v[:], pm[:, 0:256]), r=[pmk], w=[vk])
                            sc.dma('sp', dst[t0 + 128 * sub:t0 + 128 * sub + 128, :], v[:], r=[vk], w=[(dk, tb)])
                sc.barrier()
                if upto == 1:
                    stop[0] = True

            with ExitStack() as st2:
              for _once in ([0] if not stop[0] else []):
                rq_t = sb("rq_t", [32, S], BF16, st2)
                rk_t = sb("rk_t", [32, S], BF16, st2)
                qd_t = sb("qd_t", [32, S], BF16, st2)
                v_t = sb("v_t", [128, NT, 64], BF16, st2)
                v64 = sb("v64", [64, NCH, 64], BF16, st2)
                qdec = sb("qdec", [32, 512], F32, st2)
                stt = sb("stt", [32, 64], F32, st2)
                prevb = sb("prevb", [32, NCH, 64], BF16, st2)
                kd = [sb(f"kd{i}", [128, 32], BF16, st2) for i in range(2)]
                sd = [sb(f"sd{i}", [128, 128], BF16, st2) for i in range(2)]
                o_sb = sb("o_sb", [64, 512], F32, st2)
                sq2 = sb("sq2", [64, 512], F32, st2)
                mean2 = sb("mean2", [64, 512], F32, st2)
                rstd2 = sb("rstd2", [64, 512], F32, st2)
                g_t = sb("g_t", [64, 512], BF16, st2)
                ro = [sb(f"ro{i}", [64, 512], BF16, st2) for i in range(2)]
                for h in range(4):
                    gam = 1.0 - 2.0 ** (-5.0 - h)
                    cd = gam ** CHUNK
                    sc.dma('sp', rq_t[:], RQ[32 * h:32 * h + 32, :], w=['rq_t'])
                    sc.dma('sp', rk_t[:], RK[32 * h:32 * h + 32, :], w=['rk_t'])
                    sc.dma('sp', v_t[:], RV[:, 64 * h:64 * h + 64].rearrange("(j p) e -> p j e", p=128), w=['v_t'])
                    sc.dma('sp', qdec[:], cst_d[32 * h:32 * h + 32, 512:1024], w=['qdec'])
                    for tb in range(NB):
                        sc.op('dve', lambda e, tb=tb: e.tensor_tensor(qd_t[:, tb * 512:tb * 512 + 512], rq_t[:, tb * 512:tb * 512 + 512], qdec[:], ALU.mult),
                              r=['rq_t', 'qdec'], w=['qd_t'])
                    sc.op('dve', lambda e: e.memset(stt[:], 0.0), w=['stt'])
                    sc.dma('sp', v64[:], RV[:, 64 * h:64 * h + 64].rearrange("(c p) e -> p c e", p=64), w=['v64'])
                    for c in range(NCH):
                        sc.op('pe', lambda e, c=c: e.transpose(PSB[0:64, 0:32], rk_t[:, 64 * c:64 * c + 64], identb[0:32, 0:32]),
                              r=['rk_t', 'identb'], w=['psb'])
                        k_ = kd[c % 2]
                        kk = f'kd{c % 2}'
                        sc.op('dve', lambda e: e.tensor_scalar(k_[0:64, :], PSB[0:64, 0:32], cst[0:64, 1024 + h:1025 + h], None, ALU.mult), r=['psb', 'cst'], w=[kk])
                        pkv, pkvk = bank()
                        mm(pkv[0:32, 0:64], k_[0:64, :], v64[:, c, :], True, True, r=[kk, 'v64'], w=[pkvk])
                        sc.op('dve', lambda e, c=c: e.tensor_copy(prevb[:, c, :], stt[:]), r=['stt'], w=['prevb'])
                        sc.op('dve', lambda e: e.scalar_tensor_tensor(stt[:], stt[:], cd, pkv[0:32, 0:64], ALU.mult, ALU.add),
                              r=['stt', pkvk], w=['stt'])
                    for tb in range(NB if KN >= 5 else 0):
                        t0 = tb * 512
                        po, pok = bank()
                        for jj in range(4):
                            j = tb * 4 + jj
                            ps_, psk = bank()
                            mm(ps_[:, 0:128], rk_t[:, 128 * j:128 * j + 128], rq_t[:, 128 * j:128 * j + 128], True, True, r=['rk_t', 'rq_t'], w=[psk])
                            s_ = sd[j % 2]
                            sk = f'sd{j % 2}'
                            sc.op('dve', lambda e: e.tensor_tensor(s_[:], ps_[:, 0:128], cst[:, 128 * h:128 * h + 128], ALU.mult), r=[psk, 'cst'], w=[sk])
                            mm(po[0:64, 128 * jj:128 * jj + 128], v_t[:, j, :], s_[:], True, False, r=['v_t', sk], w=[pok], inc=False)
                            for hf in range(2):
                                c0 = 128 * jj + 64 * hf
                                mm(po[0:64, c0:c0 + 64], prevb[:, 2 * j + hf, :], qd_t[:, 128 * j + 64 * hf:128 * j + 64 * hf + 64], False, hf == 1,
                                   r=['prevb', 'qd_t'], w=[pok], inc=(hf == 1))
                        sc.op('act', lambda e: e.copy(o_sb[:], po[0:64, :]), r=[pok], w=['o_sb'])
                        sc.op('act', lambda e: e.activation(sq2[:], po[0:64, :], AF.Square), r=[pok], w=['sq2'])
                        pm, pmk = bank()
                        mm(pm[0:64, :], ones64[:], o_sb[:], True, True, r=['ones64', 'o_sb'], w=[pmk])
                        pe2, pe2k = bank()
                        mm(pe2[0:64, :], ones64[:], sq2[:], True, True, r=['ones64', 'sq2'], w=[pe2k])
                        sc.op('act', lambda e: e.copy(mean2[:], pm[0:64, :]), r=[pmk], w=['mean2'])
                        sc.op('dve', lambda e: e.tensor_tensor(rstd2[:], mean2[:], mean2[:], ALU.mult), r=['mean2'], w=['rstd2'])
                        sc.op('dve', lambda e: e.tensor_tensor(rstd2[:], pe2[0:64, :], rstd2[:], ALU.subtract), r=[pe2k, 'rstd2'], w=['rstd2'])
                        sc.op('dve', lambda e: e.tensor_scalar(rstd2[:], rstd2[:], 0.0, EPS, ALU.max, ALU.add), r=['rstd2'], w=['rstd2'])
                        sc.op('act', lambda e: e.activation(rstd2[:], rstd2[:], AF.Ln), r=['rstd2'], w=['rstd2'])
                        sc.op('act', lambda e: e.activation(rstd2[:], rstd2[:], AF.Exp, scale=-0.5), r=['rstd2'], w=['rstd2'])
                        sc.dma('sp', g_t[:], GATE[64 * h:64 * h + 64, t0:t0 + 512], w=['g_t'])
                        sc.op('dve', lambda e: e.tensor_tensor(o_sb[:], o_sb[:], mean2[:], ALU.subtract), r=['o_sb', 'mean2'], w=['o_sb'])
                        sc.op('dve', lambda e: e.tensor_tensor(o_sb[:], o_sb[:], rstd2[:], ALU.mult), r=['o_sb', 'rstd2'], w=['o_sb'])
                        sc.op('dve', lambda e: e.tensor_scalar(o_sb[:], o_sb[:], cv[0:64, 74 + h:75 + h], None, ALU.mult), r=['o_sb', 'cv'], w=['o_sb'])
                        r_ = ro[tb % 2]
                        rk_ = f'ro{tb % 2}'
                        sc.op('dve', lambda e: e.tensor_tensor(r_[:], o_sb[:], g_t[:], ALU.mult), r=['o_sb', 'g_t'], w=[rk_])
                        sc.dma('sp', MIXT[64 * h:64 * h + 64, t0:t0 + 512], r_[:], r=[rk_], w=[('MIXT2', h, tb)])
                sc.barrier()
                if upto == 2:
                    stop[0] = True

            with ExitStack() as st3:
              for _once in ([0] if not stop[0] else []):
                qa = sb("qa", [70, S], BF16, st3)
                ka = sb("ka", [70, S], BF16, st3)
                fv_t = sb("fv_t", [128, NT, 128], BF16, st3)
                sc.op('pool', lambda e: e.memset(fv_t[:], 1.0), w=['fv_t'])
                nd = sb("nd", [128, 512], F32, st3)
                msk = sb("msk", [128, 4 * 512], F32, st3)
                sc.dma('sp', msk[:], msk_d[:, :], w=['msk'])
                fb = sb("fb", [1, 4], F32, st3)
                sc.dma('sp', fb[:], foxb_d[l, :, :], w=['fb'])
                sc.op('dve', lambda e: e.tensor_scalar(fb[:], fb[:], -1.0, None, ALU.mult), r=['fb'], w=['fb'])
                FW = min(S, 2048)
                f_r = sb("f_r", [1, FW], F32, st3)
                F_r = sb("F_r", [1, FW], F32, st3)
                one_r = sb("one_r", [1, FW], F32, st3)
                sc.op('pool', lambda e: e.memset(one_r[:], 1.0), w=['one_r'])
                fr = sb("fr", [1, 7, FW], BF16, st3)
                r1 = sb("r1", [1, FW], F32, st3)
                r2 = sb("r2", [1, FW], F32, st3)
                Fl = sb("Fl", [1, 1], F32, st3)
                pt = [sb(f"pt{i}", [128, 512], BF16, st3) for i in range(3)]
                rden = sb("rden", [64, 512], F32, st3)
                fo = [sb(f"fo{i}", [64, 512], BF16, st3) for i in range(2)]
                for h in range(4):
                    sc.op('dve', lambda e: e.memset(Fl[:], 0.0), w=['Fl'])
                    for pc in range(S // FW):
                        sl = slice(pc * FW, pc * FW + FW)
                        sc.dma('sp', f_r[:], FF[h:h + 1, sl], w=['f_r'])
                        sc.op('act', lambda e: e.activation(f_r[:], f_r[:], AF.Exp, bias=fb[0:1, h:h + 1], scale=-1.0), r=['f_r', 'fb'], w=['f_r'])
                        sc.op('act', lambda e: e.activation(f_r[:], f_r[:], AF.Ln, bias=1.0), r=['f_r'], w=['f_r'])
                        sc.op('dve', lambda e: e.tensor_scalar(f_r[:], f_r[:], -1.0, None, ALU.mult), r=['f_r'], w=['f_r'])
                        sc.op('dve', lambda e: e.tensor_tensor_scan(F_r[:], one_r[:], f_r[:], Fl[0:1, 0:1], ALU.mult, ALU.add), r=['one_r', 'f_r', 'Fl'], w=['F_r'])
                        sc.op('dve', lambda e: e.tensor_copy(Fl[:], F_r[0:1, FW - 1:FW]), r=['F_r'], w=['Fl'])
                        sc.op('dve', lambda e: e.tensor_copy(fr[:, 0, :], F_r[:]), r=['F_r'], w=['fr'])
                        sc.op('dve', lambda e: e.tensor_tensor(r1[:], F_r[:], fr[:, 0, :], ALU.subtract), r=['F_r', 'fr'], w=['r1'])
                        sc.op('dve', lambda e: e.tensor_copy(fr[:, 1, :], r1[:]), r=['r1', 'fr'], w=['fr'])
                        sc.op('dve', lambda e: e.tensor_tensor(r2[:], r1[:], fr[:, 1, :], ALU.subtract), r=['r1', 'fr'], w=['r2'])
                        sc.op('dve', lambda e: e.tensor_copy(fr[:, 2, :], r2[:]), r=['r2', 'fr'], w=['fr'])
                        sc.op('dve', lambda e: e.tensor_copy(fr[:, 3, :], one_r[:]), r=['one_r', 'fr'], w=['fr'])
                        for i in range(3):
                            sc.op('dve', lambda e, i=i: e.tensor_scalar(fr[:, 4 + i, :], fr[:, i, :], -1.0, None, ALU.mult), r=['fr'], w=['fr'])
                        sc.dma('sp', FRD[:, sl].rearrange("(o r) t -> o r t", o=1), fr[:], r=['fr'], w=[('FRD', pc)])
                    FRk = [('FRD', pc) for pc in range(S // FW)]
                    sc.dma('sp', qa[0:64, :], FQ[64 * h:64 * h + 64, :], w=['qa'])
                    sc.dma('sp', qa[64:67, :], FRD[0:3, :], r=FRk + ['qa'], w=['qa'])
                    for i in range(3):
                        sc.dma('sp', qa[67 + i:68 + i, :], FRD[3:4, :], r=FRk + ['qa'], w=['qa'])
                    sc.dma('sp', ka[0:64, :], FK[64 * h:64 * h + 64, :], w=['ka'])
                    for i in range(3):
                        sc.dma('sp', ka[64 + i:65 + i, :], FRD[3:4, :], r=FRk + ['ka'], w=['ka'])
                    sc.dma('sp', ka[67:70, :], FRD[4:7, :], r=FRk + ['ka'], w=['ka'])
                    sc.dma('sp', fv_t[:, :, 0:64], FV[:, 64 * h:64 * h + 64].rearrange("(j p) e -> p j e", p=128), w=['fv_t'])
                    pi = 0
                    for qb in range(NB):
                        q0 = qb * 512
                        pn, pnk = PS[5], 'ps5'
                        pd, pdk = PS[6], 'ps6'
                        nk = 4 * qb + 4
                        def score(j):
                            mm(PS[j % 4][:, :], ka[:, 128 * j:128 * j + 128], qa[:, q0:q0 + 512], True, True, r=['ka', 'qa'], w=[f'ps{j % 4}'])

                        score(0)
                        if nk > 1:
                            score(1)
                        for j in range(nk):
                            if j + 2 < nk:
                                score(j + 2)
                            ps_ = PS[j % 4]
                            psk = f'ps{j % 4}'
                            p_ = pt[pi % 3]
                            pk = f'pt{pi % 3}'
                            pi += 1
                            sc.op('act', lambda e: e.activation(p_[:], ps_[:, :], AF.Exp), r=[psk], w=[pk])
                            if j >= 4 * qb:
                                m = j - 4 * qb
                                sc.op('dve', lambda e: e.tensor_tensor(p_[:], p_[:], msk[:, 512 * m:512 * m + 512], ALU.mult), r=[pk, 'msk'], w=[pk])
                            mm(pn[:, :], fv_t[:, j, :], p_[:], j == 0, j == nk - 1, r=['fv_t', pk], w=[pnk], inc=True)
                        sc.op('act', lambda e: e.copy(nd[:], pn[:, :]), r=[pnk], w=['nd'])
                        mm(pd[0:64, :], ident[:, 64:128], nd[:], True, True, r=['cst', 'nd'], w=[pdk])
                        sc.op('dve', lambda e: e.reciprocal(rden[:], pd[0:64, :]), r=[pdk], w=['rden'])
                        f_ = fo[qb % 2]
                        fk_ = f'fo{qb % 2}'
                        sc.op('dve', lambda e: e.tensor_tensor(f_[:], nd[0:64, :], rden[:], ALU.mult), r=['nd', 'rden'], w=[fk_])
                        sc.dma('sp', MIXT[768 + 64 * h:832 + 64 * h, q0:q0 + 512], f_[:], r=[fk_], w=[('MIXT3', h, qb)])
                sc.barrier()
                if upto == 3:
                    stop[0] = True

            with ExitStack() as st4:
              for _once in ([0] if not stop[0] else []):
                wo = sb("wo", [128, 8, D], BF16, st4)
                for kc in range(8):
                    sc.dma('pool', wo[:, kc, :], w_o_d[l, kc * 128:(kc + 1) * 128, :], w=[('wo', kc)])
                rwt = sb("rwt", [128, 8, NE], F32, st4)
                sc.dma('sp', rwt[:], rw_d[l].rearrange("(kc p) e -> p kc e", p=128), w=['rwt'])
                rbt = sb("rbt", [128, NE], F32, st4)
                sc.dma('sp', rbt[:], rb_d[l, 0:1, :].partition_broadcast(128), w=['rbt'])
                mx = [sb(f"mx{i}", [128, 8, 512], BF16, st4) for i in range(2)]
                xr = [sb(f"xr{i}", [128, 8, 512], F32, st4) for i in range(2)]
                z = sb("z", [128, 8, 512], F32, st4)
                stl = dict(sq=[sb(f"sq4{i}", [128, 512], F32, st4) for i in range(2)], sqk='s4', mean=sb("mean", [128, 512], F32, st4),
                           rstd=sb("rstd", [128, 512], F32, st4), tmp=sb("tmp", [128, 512], F32, st4))
                x1f = z
                x1h = sb("x1h", [128, 8, 512], BF16, st4)
                lg = sb("lg", [128, NE], F32, st4)
                m8 = sb("m8", [128, 8], F32, st4)
                nmx = sb("nmx", [128, 1], F32, st4)
                mk = sb("mk", [128, NE], F32, st4)
                ex = sb("ex", [128, NE], F32, st4)
                ssum = sb("ssum", [128, 1], F32, st4)
                gtt = sb("gtt", [NE, 512], F32, st4)
                xtm = [sb(f"xtm{i}", [128, D], BF16, st4) for i in range(2)]
                cnt = sb("cnt", [128, NE], F32, st4)
                sc.op('dve', lambda e: e.memset(cnt[:], 0.0), w=['cnt'])
                posf = sb("posf", [128, NE], F32, st4)
                ohf = sb("ohf", [128, NE], F32, st4)
                idx8 = sb("idx8", [128, 8], U32, st4)
                idxf = sb("idxf", [128, 8], F32, st4)
                pos4 = sb("pos4", [128, 4], F32, st4)
                ov4 = sb("ov4", [128, 4], F32, st4)
                e4 = sb("e4", [128, 4], F32, st4)
                for tb in range(NB):
                    t0 = tb * 512
                    p = tb % 2
                    sl = slice(t0, t0 + 512)
                    sc.dma('sp', mx[p][:], chunked(MIXT)[:, :, sl], w=[f'mx{p}'])
                    sc.dma('sp', xr[p][:], chunked(XT)[:, :, sl], w=[f'xr{p}'])
                    for dc in range(8):
                        pm, pmk = bank()
                        for kc in range(8):
                            mm(pm[:, :], wo[:, kc, 128 * dc:128 * dc + 128], mx[p][:, kc, :], kc == 0, kc == 7, r=[('wo', kc), f'mx{p}'], w=[pmk])
                        sc.op('dve', lambda e, dc=dc: e.scalar_tensor_tensor(z[:, dc, :], xr[p][:, dc, :], ALPHA, pm[:, :], ALU.mult, ALU.add),
                              r=[f'xr{p}', pmk], w=['z'])
                    ln_block(z, 'z', 78, 86, stl)
                    sc.op('dve', lambda e: e.tensor_copy(x1h[:], z[:]), r=['z'], w=['x1h'])
                    sc.dma('sp', chunked(X1T)[:, :, sl], x1f[:], r=['z'], w=[('X1T', tb)])
                    sc.dma('sp', chunked(X1B)[:, :, sl], x1h[:], r=['x1h'], w=[('X1B', tb)])
                    for sub in range(4):
                        pm, pmk = bank()
                        for kc in range(8):
                            mm(pm[:, 0:NE], x1f[:, kc, 128 * sub:128 * sub + 128], rwt[:, kc, :], kc == 0, kc == 7, r=['z', 'rwt'], w=[pmk])
                        sc.op('dve', lambda e: e.tensor_tensor(lg[:], pm[:, 0:NE], rbt[:], ALU.add), r=[pmk, 'rbt'], w=['lg'])
                        sc.op('dve', lambda e: e.max(m8[:], lg[:]), r=['lg'], w=['m8'])
                        sc.op('dve', lambda e: e.tensor_scalar(mk[:], lg[:], m8[:, 3:4], None, ALU.is_ge), r=['lg', 'm8'], w=['mk'])
                        sc.op('dve', lambda e: e.tensor_scalar(nmx[:], m8[:, 0:1], -1.0, None, ALU.mult), r=['m8'], w=['nmx'])
                        sc.op('act', lambda e: e.activation(ex[:], lg[:], AF.Exp, bias=nmx[:, 0:1], scale=1.0), r=['lg', 'nmx'], w=['ex'])
                        sc.op('dve', lambda e: e.tensor_tensor(ex[:], ex[:], mk[:], ALU.mult), r=['ex', 'mk'], w=['ex'])
                        sc.op('dve', lambda e: e.tensor_reduce(ssum[:], ex[:], AX.X, ALU.add), r=['ex'], w=['ssum'])
                        sc.op('dve', lambda e: e.reciprocal(ssum[:], ssum[:]), r=['ssum'], w=['ssum'])
                        sc.op('dve', lambda e: e.tensor_scalar(ex[:], ex[:], ssum[:, 0:1], None, ALU.mult), r=['ex', 'ssum'], w=['ex'])
                        tt_ = tb * 4 + sub
                        sc.op('dve', lambda e: e.max_index(idx8[:], m8[:], lg[:]), r=['m8', 'lg'], w=['idx8'])
                        sc.op('dve', lambda e: e.tensor_copy(idxf[:], idx8[:]), r=['idx8'], w=['idxf'])
                        sc.op('act', lambda e: e.activation(e4[:], m8[:, 0:4], AF.Exp, bias=nmx[:, 0:1], scale=1.0), r=['m8', 'nmx'], w=['e4'])
                        sc.op('dve', lambda e: e.tensor_scalar(GK[:, tt_, :], e4[:], ssum[:, 0:1], None, ALU.mult), r=['e4', 'ssum'], w=['GK'])
                        pp, ppk = bank()
                        mm(pp[:, 0:NE], iot[:, 0:128], mk[:], True, True, r=['iot', 'mk'], w=[ppk])
                        sc.op('dve', lambda e: e.tensor_tensor(posf[:], pp[:, 0:NE], cnt[:], ALU.add), r=[ppk, 'cnt'], w=['posf'])
                        pt_, ptk = bank()
                        mm(pt_[:, 0:NE], ones1[:], mk[:], True, True, r=['ones1', 'mk'], w=[ptk])
                        sc.op('dve', lambda e: e.tensor_tensor(cnt[:], cnt[:], pt_[:, 0:NE], ALU.add), r=['cnt', ptk], w=['cnt'])
                        for k in range(4):
                            sc.op('dve', lambda e, k=k: e.tensor_scalar(ohf[:], iot[:, 128:128 + NE], idxf[:, k:k + 1], None, ALU.is_equal), r=['iot', 'idxf'], w=['ohf'])
                            sc.op('dve', lambda e: e.tensor_tensor(ohf[:], ohf[:], posf[:], ALU.mult), r=['ohf', 'posf'], w=['ohf'])
                            sc.op('dve', lambda e, k=k: e.tensor_reduce(pos4[:, k:k + 1], ohf[:], AX.X, ALU.add), r=['ohf'], w=['pos4'])
                        sc.op('dve', lambda e: e.tensor_scalar(ov4[:], pos4[:], float(CAP), 1.0e6, ALU.is_ge, ALU.mult), r=['pos4'], w=['ov4'])
                        sc.op('dve', lambda e: e.scalar_tensor_tensor(pos4[:], idxf[:, 0:4], float(CAP), pos4[:], ALU.mult, ALU.add), r=['idxf', 'pos4'], w=['pos4'])
                        sc.op('dve', lambda e: e.tensor_tensor(pos4[:], pos4[:], ov4[:], ALU.add), r=['pos4', 'ov4'], w=['pos4'])
                        sc.op('dve', lambda e: e.tensor_copy(SLOT[:, tt_, :], pos4[:]), r=['pos4'], w=['SLOT'])
                        for kc in range(8):
                            sc.op('pe', lambda e, kc=kc: e.transpose(PSB[:, 128 * kc:128 * kc + 128], x1h[:, kc, 128 * sub:128 * sub + 128], identb[:, :]),
                                  r=['x1h', 'identb'], w=['psb'], inc=(kc == 7))
                        xt_ = xtm[sub % 2]
                        xtk = f'xtm{sub % 2}'
                        sc.op('act', lambda e: e.copy(xt_[:], PSB[:, :]), r=['psb'], w=[xtk])
                        for k in range(4):
                            sc.idma(XG[:, :], bass.IndirectOffsetOnAxis(ap=SLOT[:, tt_, k:k + 1], axis=0), xt_[:, :], None, bnd_reg,
                                    r=[xtk, 'SLOT'], w=[('XG', tt_, k)])
                        pg, pgk = bank()
                        mm(pg[0:NE, 0:128], ex[:], ident, True, True, r=['ex', 'cst'], w=[pgk])
                        sc.op('act', lambda e, sub=sub: e.copy(gtt[:, 128 * sub:128 * sub + 128], pg[0:NE, 0:128]), r=[pgk], w=['gtt'])
                    sc.dma('sp', GT[:, sl], gtt[:], r=['gtt'], w=[('GT', tb)])
                if dbg is not None:
                    sc.dma('sp', SLOTD[:, :], SLOT[:].rearrange('p a b -> p (a b)'), r=['SLOT'], w=['SLOTD'])
                    sc.dma('sp', GKD[:, :], GK[:].rearrange('p a b -> p (a b)'), r=['GK'], w=['GKD'])
                sc.barrier()
                if upto == 4:
                    stop[0] = True

            with ExitStack() as st5:
              for _once in ([0] if not stop[0] else []):
                w1t = [sb(f"w1t{i}", [128, 8, 2048], BF16, st5) for i in range(2)]
                w2t = [sb(f"w2t{i}", [128, 8, D], BF16, st5) for i in range(2)]
                b1t = sb("b1t", [128, NE * 16], F32, st5)
                sc.dma('sp', b1t[:], b1_d[l, :, :], w=['b1t'])
                b2T = sb("b2T", [128, NE * 8], F32, st5)
                sc.dma('sp', b2T[:], b2T_d[l, :, :], w=['b2T'])
                b1p = sb("b1p", [128, NE * 16], F32, st5)
                sc.op('dve', lambda e: e.tensor_scalar(b1p[:], b1t[:], 1.0, None, ALU.add), r=['b1t'], w=['b1p'])
                b1m = b1t
                sc.op('dve', lambda e: e.tensor_scalar(b1m[:], b1t[:], -1.0, 7.0, ALU.mult, ALU.add), r=['b1t'], w=['b1t'])
                c119 = sb("c119", [128, 1], F32, st5)
                sc.op('pool', lambda e: e.memset(c119[:], 1.702 * 7.0), w=['c119'])
                xgt = sb("xgt", [128, 4, D], BF16, st5)
                xs = sb("xs", [128, 8, 512], BF16, st5)
                actt = [sb("actt0", [128, 8, 512], BF16, st5)] * 2
                blk = [0]
                ga = [sb(f"ga{i}", [128, 512], F32, st5) for i in range(3)]
                sgm = [sb(f"sgm{i}", [128, 512], F32, st5) for i in range(3)]
                li = [sb(f"li{i}", [128, 512], F32, st5) for i in range(3)]
                b2b = [sb(f"b2b{i}", [128, D], F32, st5) for i in range(2)]
                yk = [sb(f"yk{i}", [128, D], F32, st5) for i in range(6)]
                ytm = [yk[0], yk[1]]
                ycomb = sb("ycomb", [128, D], F32, st5)
                acc = sb("acc", [128, 8, 512], F32, st5)
                hb5 = [xs[:, 0, :], xs[:, 1, :]]
                xr5 = [li[0], li[1]]
                stl = dict(sq=[ga[0], ga[1]], sqk='ga', mean=sgm[0], rstd=sgm[1], tmp=sgm[2], keys=('sgm0', 'sgm1', 'sgm2'))
                XGK = [('XG', t_, k) for t_ in range(NT) for k in range(4)]
                wn = 0
                ytc = [0]
                for ex_ in range(NE):
                    wp = wn % 2
                    wn += 1
                    for kc in range(8):
                        sc.dma('pool', w1t[wp][:, kc, :], w1_d[l, ex_, kc * 128:(kc + 1) * 128, :], w=[(f'w1t{wp}', kc)])
                    for kc in range(8):
                        sc.dma('pool', w2t[wp][:, kc, :], w2_d[l, ex_, kc * 128:(kc + 1) * 128, :], w=[(f'w2t{wp}', kc)])
                    sc.dma('sp', b2b[wp][:], b2_d[l, ex_:ex_ + 1, :].partition_broadcast(128), w=[f'b2b{wp}'])
                    for cb in range(CAPB):
                        r0 = ex_ * CAP + 512 * cb
                        if ex_ == 0 and cb == 0:
                            sc.dma('sp', xgt[:], XG[r0:r0 + 512, :].rearrange("(j p) d -> p j d", p=128), r=XGK, w=['xgt'])
                        for kp in range(4):
                            for kk in range(2):
                                kc = 2 * kp + kk
                                for j in range(4):
                                    sc.op('pe', lambda e, kc=kc, j=j, kk=kk: e.transpose(PSB[:, 512 * kk + 128 * j:512 * kk + 128 * j + 128], xgt[:, j, 128 * kc:128 * kc + 128], identb[:, :]),
                                          r=['xgt', 'identb'], w=['psb'], inc=(kk == 1 and j == 3))
                            sc.op('act', lambda e, kp=kp: e.copy(xs[:, 2 * kp:2 * kp + 2, :].rearrange("p a b -> p (a b)"), PSB[:, :]), r=['psb'], w=[('xs', kp)])
                        nxt = ex_ * CAPB + cb + 1
                        if nxt < NE * CAPB:
                            rn = (nxt // CAPB) * CAP + 512 * (nxt % CAPB)
                            sc.dma('sp', xgt[:], XG[rn:rn + 512, :].rearrange("(j p) d -> p j d", p=128), w=['xgt'])
                        XSK = [('xs', kc // 2) for kc in range(8)]
                        ab = actt[blk[0] % 2]
                        abk = 'actt0'
                        blk[0] += 1

                        def X(fc):
                            pg, pgk = bank()
                            for kc in range(8):
                                mm(pg[:, :], w1t[wp][:, kc, 128 * fc:128 * fc + 128], xs[:, kc, :], kc == 0, kc == 7, r=[(f'w1t{wp}', kc), XSK[kc]], w=[pgk])
                            pl, plk = bank()
                            for kc in range(8):
                                mm(pl[:, :], w1t[wp][:, kc, 1024 + 128 * fc:1152 + 128 * fc], xs[:, kc, :], kc == 0, kc == 7, r=[(f'w1t{wp}', kc), XSK[kc]], w=[plk])
                            q = fc % 3
                            bl = b1p[:, ex_ * 16 + 8 + fc:ex_ * 16 + 8 + fc + 1]
                            bg7 = b1m[:, ex_ * 16 + fc:ex_ * 16 + fc + 1]
                            sc.op('act', lambda e: e.activation(sgm[q][:], pg[:, :], AF.Relu, bias=bg7, scale=-1.0), r=[pgk, 'b1t'], w=[f'sgm{q}'])
                            sc.op('act', lambda e: e.activation(ga[q][:], sgm[q][:], AF.Silu, bias=c119[:, 0:1], scale=-1.702), r=[f'sgm{q}', 'c119'], w=[f'ga{q}'])
                            sc.op('dve', lambda e: e.tensor_scalar(li[q][:], pl[:, :], bl, -6.0, ALU.add, ALU.max), r=[plk, 'b1p'], w=[f'li{q}'])

                        def Y(fc):
                            q = fc % 3
                            sc.op('dve', lambda e: e.scalar_tensor_tensor(ab[:, fc, :], li[q][:], 8.0, ga[q][:], ALU.min, ALU.mult), r=[f'ga{q}', f'li{q}'], w=[(abk, fc)])

                        X(0)
                        X(1)
                        for fc in range(8):
                            if fc + 2 < 8:
                                X(fc + 2)
                            Y(fc)
                        for j in range(4):
                            y_ = ytm[ytc[0] % 2]
                            yk_ = f'yk{ytc[0] % 2}'
                            ytc[0] += 1
                            for dh in range(2):
                                py, pyk = bank()
                                for fc in range(8):
                                    mm(py[:, :], ab[:, fc, 128 * j:128 * j + 128], w2t[wp][:, fc, 512 * dh:512 * dh + 512], fc == 0, fc == 7,
                                       r=[(f'w2t{wp}', fc), (abk, fc)], w=[pyk])
                                sc.op('dve', lambda e, dh=dh: e.scalar_tensor_tensor(y_[:, 512 * dh:512 * dh + 512], py[:, :], 1.0 / 1.702, b2b[wp][:, 512 * dh:512 * dh + 512], ALU.mult, ALU.add),
                                      r=[pyk, f'b2b{wp}'], w=[yk_])
                            sc.dma('sp', YS[r0 + 128 * j:r0 + 128 * j + 128, :], y_[:], r=[yk_], w=[('YS', ex_, cb, j)])
                YSK = [('YS', e_, c_, j_) for e_ in range(NE) for c_ in range(CAPB) for j_ in range(4)]
                for tb in range(NB):
                    sl = slice(tb * 512, tb * 512 + 512)
                    for sub in range(4):
                        tt_ = tb * 4 + sub
                        ks = [(4 * tt_ + k) % 6 for k in range(4)]
                        for k in range(4):
                            sc.idma(yk[ks[k]][:, :], None, YS[:, :], bass.IndirectOffsetOnAxis(ap=SLOT[:, tt_, k:k + 1], axis=0), bnd_reg,
                                    r=(YSK if (tt_ == 0 and k == 0) else []) + ['SLOT'], w=[f'yk{ks[k]}'])
                        sc.op('dve', lambda e: e.tensor_scalar(ycomb[:], yk[ks[0]][:], GK[:, tt_, 0:1], None, ALU.mult), r=[f'yk{ks[0]}', 'GK'], w=['ycomb'])
                        for k in range(1, 4):
                            sc.op('dve', lambda e, k=k: e.scalar_tensor_tensor(ycomb[:], yk[ks[k]][:], GK[:, tt_, k:k + 1], ycomb[:], ALU.mult, ALU.add),
                                  r=[f'yk{ks[k]}', 'GK', 'ycomb'], w=['ycomb'])
                        for half in range(2):
                            pT, pTk = bank()
                            for d4 in range(4):
                                dc = 4 * half + d4
                                mm(pT[:, 128 * d4:128 * d4 + 128], ycomb[:, 128 * dc:128 * dc + 128], ident, True, True, r=['ycomb', 'cst'], w=[pTk], inc=(d4 == 3))
                            sc.op('act', lambda e, half=half, sub=sub: e.copy(acc[:, 4 * half:4 * half + 4, 128 * sub:128 * sub + 128],
                                                                              pT[:, :].rearrange("p (a b) -> p a b", a=4)), r=[pTk], w=['acc'])
                    zs = acc
                    for dc in range(8):
                        xr_ = xr5[dc % 2]
                        xk_ = f'li{dc % 2}'
                        sc.dma('sp', xr_[:], X1T[128 * dc:128 * dc + 128, sl], r=[('X1T', tb)], w=[xk_])
                        sc.op('dve', lambda e, dc=dc: e.scalar_tensor_tensor(zs[:, dc, :], xr_[:], ALPHA, zs[:, dc, :], ALU.mult, ALU.add),
                              r=[xk_, 'acc'], w=['acc'])
                    ln_block(zs, 'acc', 94, 102, stl)
                    if last:
                        sc.dma('sp', chunked(outT)[:, :, sl], zs[:], r=['acc'], w=[('outT', tb)])
                    else:
                        sc.dma('sp', chunked(XT)[:, :, sl], zs[:], r=['acc'], w=[('XT', tb)])
                        for dc in range(8):
                            hb_ = hb5[dc % 2]
                            hk_ = ('xs', 0)
                            sc.op('act', lambda e, dc=dc: e.copy(hb_, zs[:, dc, :]), r=['acc'], w=[hk_])
                            sc.dma('sp', XB[128 * dc:128 * dc + 128, sl], hb_, r=[hk_], w=[('XB', tb, dc)])
                sc.barrier()
                if upto == 5:
                    stop[0] = True
        except _Stop:
            pass
    return nc


def host_consts(S):
    half = 16
    freqs = (10000.0 ** (-np.arange(half, dtype=np.float32) / half)).astype(np.float32)
    pos = np.arange(S, dtype=np.float32)
    ang = pos[None, :] * freqs[:, None]
    cos = np.cos(ang).astype(np.float32)
    sin = np.sin(ang).astype(np.float32)
    ropec = np.tile(np.concatenate([cos, cos], 0), (4, 1))
    ropes = np.tile(np.concatenate([-sin, sin], 0), (4, 1))
    cst = np.zeros((128, 4 * 128 + 512 + 4 + 128), np.float32)
    idx = np.arange(128)
    same = (idx[:, None] // 64) == (idx[None, :] // 64)
    scale = 32 ** -0.5
    for h in range(4):
        g = 1.0 - 2.0 ** (-5.0 - h)
        cst[:, 128 * h:128 * h + 128] = np.where(same, g ** np.abs(idx[:, None] - idx[None, :]), 0.0) * scale
        cst[32 * h:32 * h + 32, 512:1024] = (g ** ((np.arange(512) % 64) + 1.0))[None, :]
        cst[:, 1024 + h] = g ** (63 - (idx % 64)) * scale
    cst[:, 1028:1156] = np.eye(128, dtype=np.float32)
    msk = np.zeros((128, 4 * 512), np.float32)
    s_ = np.arange(128)[:, None]
    t_ = np.arange(512)[None, :]
    for m in range(4):
        msk[:, 512 * m:512 * m + 512] = (t_ >= 128 * m + s_)
    io = np.zeros((128, 160), np.float32)
    io[:, 0:128] = (np.arange(128)[:, None] < np.arange(128)[None, :])
    io[:, 128:160] = np.arange(32, dtype=np.float32)[None, :]
    return dict(ropec=np.ascontiguousarray(ropec), ropes=np.ascontiguousarray(ropes), cst=cst, msk=msk, iota=io)


def host_layout(inp, S, DEPTH, NE):
    L = DEPTH
    perm = np.concatenate([np.arange(16, 32), np.arange(0, 16)])
    pq = np.concatenate([h * 32 + perm for h in range(4)])
    w_in = np.asarray(inp['w_in'])
    w_aug = np.concatenate([w_in, w_in[:, :, pq], w_in[:, :, 128 + pq]], axis=2)
    cv = np.zeros((L, 128, NCV), np.float32)
    cdw = np.asarray(inp['conf_dw'])
    sdw = np.asarray(inp['sc_dw'])
    for c in range(2):
        cv[:, :, 31 * c:31 * c + 31] = cdw[:, :, 128 * c:128 * c + 128].transpose(0, 2, 1)
        cv[:, :, 62 + 3 * c:65 + 3 * c] = sdw[:, :, 128 * c:128 * c + 128].transpose(0, 2, 1)
        cv[:, :, 68 + c] = np.asarray(inp['conf_dw_b'])[:, 128 * c:128 * c + 128]
        cv[:, :, 70 + c] = np.asarray(inp['conf_ln_g'])[:, 128 * c:128 * c + 128]
        cv[:, :, 72 + c] = np.asarray(inp['conf_ln_b'])[:, 128 * c:128 * c + 128]
    cv[:, 0:64, 74:78] = np.asarray(inp['ret_gn_g']).reshape(L, 4, 64).transpose(0, 2, 1)
    for name, c0 in (('ln1_g', 78), ('ln1_b', 86), ('ln2_g', 94), ('ln2_b', 102)):
        cv[:, :, c0:c0 + 8] = np.asarray(inp[name]).reshape(L, 8, 128).transpose(0, 2, 1)
    b1T = np.ascontiguousarray(np.asarray(inp['b1']).reshape(L, NE, 16, 128).transpose(0, 3, 1, 2).reshape(L, 128, NE * 16))
    b2T = np.ascontiguousarray(np.asarray(inp['b2']).reshape(L, NE, 8, 128).transpose(0, 3, 1, 2).reshape(L, 128, NE * 8))
    rw = np.asarray(inp['router_w'])
    rb = np.asarray(inp['router_b'])
    common = dict(w_in=np.ascontiguousarray(w_aug), w_o=np.asarray(inp['w_o']), cvec=cv,
                  foxb=np.asarray(inp['fox_b_f']).reshape(L, 1, 4), rw=rw, rb=rb.reshape(L, 1, -1),
                  w1=np.asarray(inp['w1']), b1T=b1T, w2=np.asarray(inp['w2']), b2=np.asarray(inp['b2']), b2T=b2T)
    common.update(host_consts(S))
    return common


def run(inp, S, DEPTH, NE, SBK, dbg=None, upto=99, trace=False):
    x = np.asarray(inp['x'])
    B = x.shape[0]
    common = host_layout(inp, S, DEPTH, NE)
    nc = build(S, DEPTH, NE, SBK, dbg, upto)
    in_maps = []
    for b in range(B):
        m = dict(common)
        m['xT'] = np.ascontiguousarray(x[b].T)
        in_maps.append(m)
    res = run_bass_kernel_spmd(nc, in_maps, core_ids=list(range(B)), **({'trace': True} if trace else {}))
    if trace:
        print('EXEC_NS', res.exec_time_ns)
    out = np.stack([np.ascontiguousarray(r['outT'].T) for r in res.results], 0).astype(np.float32)
    if dbg is not None:
        return out, res.results
    return out


def kernel(**inputs):
    return run(inputs, 8192, 4, 32, 1024)
```

```python
import math
import os
KN = int(os.environ.get('KN', '99'))
KW = int(os.environ.get('KW', '1'))
from contextlib import ExitStack
import numpy as np
import concourse.bass as bass
import concourse.mybir as mybir
from concourse.bass_utils import run_bass_kernel_spmd

F32 = mybir.dt.float32
BF16 = mybir.dt.bfloat16
ALU = mybir.AluOpType
AF = mybir.ActivationFunctionType
AX = mybir.AxisListType

D = 1024
CHUNK = 64
ALPHA = 8 ** 0.25
EPS = 1e-5
NCV = 110
WA = 2820 + 256


class Sched:
    def __init__(s, nc, es):
        s.nc = nc
        s.eng = {'pe': nc.tensor, 'act': nc.scalar, 'dve': nc.vector, 'pool': nc.gpsimd, 'sp': nc.sync}
        s.sem = {k: es.enter_context(nc.semaphore('s_' + k)) for k in ('pe', 'act', 'dve', 'pool')}
        s.cnt = {k: 0 for k in s.sem}
        s.seen = {k: {} for k in s.eng}
        s.dq = {}
        for q in ('sp', 'pool', 'act'):
            s.dq[q] = dict(sems=[es.enter_context(nc.semaphore(f'd_{q}{i}')) for i in range(8)], n=0)
        s.res = {}

    def _need(s, en, deps):
        best = {}
        for (k, h, v) in deps:
            if v > best.get(k, (None, 0))[1]:
                best[k] = (h, v)
        for k, (h, v) in best.items():
            if s.seen[en].get(k, 0) < v:
                s.eng[en].wait_ge(h, v)
                s.seen[en][k] = v

    def _collect(s, en, r, w):
        deps = []
        for k in r:
            st = s.res.get(k)
            if st and st[0]:
                deps.append(st[0])
        for k in w:
            st = s.res.get(k)
            if st:
                if st[0]:
                    deps.append(st[0])
                deps.extend(st[1].values())
        if en == 'pe':
            deps = [d for d in deps if d[0] != 'pe']
        return deps

    def _record(s, dep, r, w):
        for k in r:
            st = s.res.setdefault(k, [None, {}])
            o = st[1].get(dep[0])
            if o is None or o[2] < dep[2]:
                st[1][dep[0]] = dep
        for k in w:
            s.res[k] = [dep, {}]

    def op(s, en, fn, r=(), w=(), inc=True):
        assert inc or en == 'pe'
        s._need(en, s._collect(en, r, w))
        ins = fn(s.eng[en])
        if inc:
            s.cnt[en] += 1
            ins.then_inc(s.sem[en], 1)
            dep = (en, s.sem[en], s.cnt[en])
        else:
            dep = (en, s.sem[en], s.cnt[en] + 1)
        s._record(dep, r, w)

    def dma(s, q, out, in_, r=(), w=(), **kw):
        Q = s.dq[q]
        i = Q['n']
        Q['n'] += 1
        K = len(Q['sems'])
        h = Q['sems'][i % K]
        key = f'd_{q}{i % K}'
        deps = s._collect(q, r, w)
        if i >= K:
            deps.append((key, h, 16 * (i // K)))
        s._need(q, deps)
        s.eng[q].dma_start(out=out, in_=in_, **kw).then_inc(h, 16)
        s._record((key, h, 16 * (i // K + 1)), r, w)

    def idma(s, out, out_off, in_, in_off, bound, r=(), w=()):
        q = 'pool'
        Q = s.dq[q]
        i = Q['n']
        Q['n'] += 1
        K = len(Q['sems'])
        h = Q['sems'][i % K]
        key = f'd_{q}{i % K}'
        deps = s._collect(q, r, w)
        if i >= K:
            deps.append((key, h, 16 * (i // K)))
        s._need(q, deps)
        s.eng[q].indirect_dma_start(out=out, out_offset=out_off, in_=in_, in_offset=in_off,
                                    bounds_check=bound, oob_is_err=False).then_inc(h, 16)
        s._record((key, h, 16 * (i // K + 1)), r, w)

    def alldeps(s):
        deps = [(k, s.sem[k], s.cnt[k]) for k in s.sem if s.cnt[k] > 0]
        for q, Q in s.dq.items():
            K = len(Q['sems'])
            for j in range(min(K, Q['n'])):
                tot = (Q['n'] - 1 - j) // K + 1
                deps.append((f'd_{q}{j}', Q['sems'][j], 16 * tot))
        return deps

    def barrier(s):
        deps = s.alldeps()
        for en in s.eng:
            s._need(en, [d for d in deps if not (en == 'pe' and d[0] == 'pe')])
        s.res = {}


class _Stop(Exception):
    pass


def build(S, DEPTH, NE, SBK, dbg=None, upto=99):
    NB = S // 512
    NT = S // 128
    NCH = S // 64
    NSB = S // SBK
    CAPB = -(-int(1.5 * 4 * S / NE) // 512)
    CAP = 512 * CAPB
    U32 = mybir.dt.uint32
    nc = bass.Bass("TRN2", target_bir_lowering=False)
    dt = nc.dram_tensor
    xT_in = dt("xT", [D, S], F32, kind="ExternalInput").ap()
    w_in_d = dt("w_in", [DEPTH, D, WA], F32, kind="ExternalInput").ap()
    w_o_d = dt("w_o", [DEPTH, D, D], F32, kind="ExternalInput").ap()
    cvec_d = dt("cvec", [DEPTH, 128, NCV], F32, kind="ExternalInput").ap()
    foxb_d = dt("foxb", [DEPTH, 1, 4], F32, kind="ExternalInput").ap()
    rw_d = dt("rw", [DEPTH, D, NE], F32, kind="ExternalInput").ap()
    rb_d = dt("rb", [DEPTH, 1, NE], F32, kind="ExternalInput").ap()
    w1_d = dt("w1", [DEPTH, NE, D, 2048], F32, kind="ExternalInput").ap()
    b1_d = dt("b1T", [DEPTH, 128, NE * 16], F32, kind="ExternalInput").ap()
    w2_d = dt("w2", [DEPTH, NE, D, D], F32, kind="ExternalInput").ap()
    b2_d = dt("b2", [DEPTH, NE, D], F32, kind="ExternalInput").ap()
    cos_d = dt("ropec", [128, S], F32, kind="ExternalInput").ap()
    sin_d = dt("ropes", [128, S], F32, kind="ExternalInput").ap()
    cst_d = dt("cst", [128, 4 * 128 + 512 + 4 + 128], F32, kind="ExternalInput").ap()
    msk_d = dt("msk", [128, 4 * 512], F32, kind="ExternalInput").ap()
    io_d = dt("iota", [128, 128 + 32], F32, kind="ExternalInput").ap()
    b2T_d = dt("b2T", [DEPTH, 128, NE * 8], F32, kind="ExternalInput").ap()
    outT = dt("outT", [D, S], F32, kind="ExternalOutput").ap()
    okind = {} if dbg is None else {"kind": "ExternalOutput"}
    XT = dt("XT", [D, S], F32).ap()
    XB = dt("XB", [D, S], BF16).ap()
    X1T = dt("X1T", [D, S], F32).ap()
    X1B = dt("X1B", [D, S], BF16).ap()
    MIXT = dt("MIXT", [D, S], BF16, **okind).ap()
    RQ = dt("RQ", [128, S], BF16).ap()
    RK = dt("RK", [128, S], BF16).ap()
    RV = dt("RV", [S, 256], BF16).ap()
    GATE = dt("GATE", [256, S], BF16).ap()
    FQ = dt("FQ", [256, S], BF16).ap()
    FK = dt("FK", [256, S], BF16).ap()
    FV = dt("FV", [S, 256], BF16).ap()
    FF = dt("FF", [4, S], F32).ap()
    FRD = dt("FRD", [7, S], BF16).ap()
    GT = dt("GT", [NE, S], F32, **okind).ap()
    XG = dt("XG", [NE * CAP, D], BF16, **okind).ap()
    SLOTD = dt("SLOTD", [128, NT * 4], U32, **okind).ap()
    GKD = dt("GKD", [128, NT * 4], F32, **okind).ap()
    YS = dt("YS", [NE * CAP, D], F32).ap()

    def chunked(ap):
        return ap.rearrange("(kc p) t -> p kc t", p=128)

    with ExitStack() as es:
        sc = Sched(nc, es)
        uid = [0]

        def sb(name, shape, dtp, st=es):
            uid[0] += 1
            return st.enter_context(nc.sbuf_tensor(f"{name}_s{uid[0]}", shape, dtp))
        PS = [es.enter_context(nc.psum_tensor(f"ps{i}", [128, 512], F32)) for i in range(7)]
        PSB = es.enter_context(nc.psum_tensor("psb", [128, 1024], BF16))
        pctr = [0]

        def bank():
            pctr[0] = (pctr[0] + 1) % 7
            return PS[pctr[0]], f"ps{pctr[0]}"

        cst = sb("cst", [128, 4 * 128 + 512 + 4 + 128], F32)
        sc.dma('sp', cst[:], cst_d[:, :], w=['cst'])
        identb = sb("identb", [128, 128], BF16)
        sc.op('dve', lambda e: e.tensor_copy(identb[:], cst[:, 1028:1156]), r=['cst'], w=['identb'])
        ident = cst[:, 1028:1156]
        onesD = sb("onesD", [128, 128], F32)
        sc.op('pool', lambda e: e.memset(onesD[:], 1.0 / D), w=['onesD'])
        ones256 = sb("ones256", [128, 128], F32)
        sc.op('pool', lambda e: e.memset(ones256[:], 1.0 / 256), w=['ones256'])
        ones64 = sb("ones64", [64, 64], F32)
        sc.op('pool', lambda e: e.memset(ones64[:], 1.0 / 64), w=['ones64'])
        onesb = sb("onesb", [128, 64], BF16)
        sc.op('pool', lambda e: e.memset(onesb[:], 1.0), w=['onesb'])
        cv = sb("cv", [128, NCV], F32)
        iot = sb("iot", [128, 160], F32)
        sc.dma('sp', iot[:], io_d[:, :], w=['iot'])
        ones1 = sb("ones1", [128, 128], F32)
        sc.op('pool', lambda e: e.memset(ones1[:], 1.0), w=['ones1'])
        SLOT = sb("SLOT", [128, NT, 4], U32)
        bnd_reg = nc.gpsimd.to_reg(NE * CAP - 1)
        GK = sb("GK", [128, NT, 4], F32)

        def mm(out, lhsT, rhs, start, stop, r, w, inc=None):
            sc.op('pe', lambda e: e.matmul(out, lhsT, rhs, start=start, stop=stop), r=r, w=w,
                  inc=(stop if inc is None else inc))

        def ln_block(z, zk, gcol, bcol, st):
            pm, pmk = bank()
            for dc in range(8):
                mm(pm[:, :], onesD[:], z[:, dc, :], dc == 0, dc == 7, r=['onesD', zk], w=[pmk])
            pe2, pe2k = bank()
            for dc in range(8):
                sq = st['sq'][dc % 2]
                sqk = st['sqk'] + str(dc % 2)
                sc.op('act', lambda e, dc=dc: e.activation(sq[:], z[:, dc, :], AF.Square), r=[zk], w=[sqk])
                mm(pe2[:, :], onesD[:], sq[:], dc == 0, dc == 7, r=['onesD', sqk], w=[pe2k], inc=True)
            mean, rstd, tmp = st['mean'], st['rstd'], st['tmp']
            mk_, rk_, tk_ = st.get('keys', (st['sqk'] + 'mean', st['sqk'] + 'rstd', st['sqk'] + 'tmp'))
            sc.op('act', lambda e: e.copy(mean[:], pm[:, :]), r=[pmk], w=[mk_])
            sc.op('dve', lambda e: e.tensor_tensor(rstd[:], mean[:], mean[:], ALU.mult), r=[mk_], w=[rk_])
            sc.op('dve', lambda e: e.tensor_tensor(rstd[:], pe2[:, :], rstd[:], ALU.subtract), r=[pe2k, rk_], w=[rk_])
            sc.op('dve', lambda e: e.tensor_scalar(rstd[:], rstd[:], 0.0, EPS, ALU.max, ALU.add), r=[rk_], w=[rk_])
            sc.op('act', lambda e: e.activation(rstd[:], rstd[:], AF.Ln), r=[rk_], w=[rk_])
            sc.op('act', lambda e: e.activation(rstd[:], rstd[:], AF.Exp, scale=-0.5), r=[rk_], w=[rk_])
            for dc in range(8):
                sc.op('dve', lambda e, dc=dc: e.tensor_tensor(tmp[:], z[:, dc, :], mean[:], ALU.subtract), r=[zk, mk_], w=[tk_])
                sc.op('dve', lambda e, dc=dc: e.tensor_tensor(tmp[:], tmp[:], rstd[:], ALU.mult), r=[tk_, rk_], w=[tk_])
                sc.op('act', lambda e, dc=dc: e.activation(z[:, dc, :], tmp[:], AF.Identity,
                                                          bias=cv[:, bcol + dc:bcol + dc + 1], scale=cv[:, gcol + dc:gcol + dc + 1]),
                      r=[tk_, 'cv'], w=[zk])

        with ExitStack() as st0:
            xf = [sb(f"xf{i}", [128, 8, 512], F32, st0) for i in range(2)]
            zt = sb("zt", [128, 4096], BF16, st0)
            sc.op('pool', lambda e: e.memset(zt[:], 0.0), w=['zt'])
            XGz = XG.rearrange("(n p r) d -> n p (r d)", p=128, r=4)
            for n_ in range(NE * CAP // 512):
                sc.dma('sp', XGz[n_], zt[:], r=['zt'], w=[('XGz', n_)])
            xh = [sb(f"xh{i}", [128, 8, 512], BF16, st0) for i in range(2)]
            for tb in range(NB):
                p = tb % 2
                sl = slice(tb * 512, tb * 512 + 512)
                sc.dma('sp', xf[p][:], chunked(xT_in)[:, :, sl], w=[f'xf{p}'])
                sc.op('dve', lambda e, p=p: e.tensor_copy(xh[p][:], xf[p][:]), r=[f'xf{p}'], w=[f'xh{p}'])
                sc.dma('sp', chunked(XT)[:, :, sl], xf[p][:], r=[f'xf{p}'], w=[('XT', tb)])
                sc.dma('sp', chunked(XB)[:, :, sl], xh[p][:], r=[f'xh{p}'], w=[('XB', tb)])
            sc.barrier()

        stop = [False]
        try:
          for l in range(DEPTH):
            last = l == DEPTH - 1
            sc.dma('sp', cv[:], cvec_d[l, :, :], w=['cv'])
            with ExitStack() as st1:
              for _once in ([0] if not stop[0] else []):
                win = sb("win", [128, 8, WA], BF16, st1)
                for kc in range(8):
                    sc.dma('pool', win[:, kc, :], w_in_d[l, kc * 128:(kc + 1) * 128, :], w=[('win', kc)])
                WK = [('win', kc) for kc in range(8)]
                xbt = [sb(f"xbt{i}", [128, 8, 544], BF16, st1) for i in range(2)]
                cs_t = sb("cs_t", [128, 512], F32, st1)
                sn_t = sb("sn_t", [128, 512], F32, st1)
                t1 = sb("t1", [128, 512], F32, st1)
                t2 = sb("t2", [128, 512], F32, st1)
                ob = [sb(f"ob{i}", [128, 512], BF16, st1) for i in range(4)]
                obc = [0]
                u_t = [sb(f"u{i}", [128, 544], F32, st1) for i in range(2)]
                sg_t = sb("sg_t", [128, 544], F32, st1)
                ca_t = [sb(f"ca{i}", [128, 512], F32, st1) for i in range(2)]
                sq1 = sb("sq1", [128, 512], F32, st1)
                mean1 = sb("mean1", [128, 512], F32, st1)
                rstd1 = sb("rstd1", [128, 512], F32, st1)
                ff_t = sb("ff_t", [4, 512], F32, st1)
                vt = [sb(f"vt{i}", [128, 256], BF16, st1) for i in range(2)]
                vtc = [0]

                def nob():
                    obc[0] = (obc[0] + 1) % 4
                    return ob[obc[0]], f"ob{obc[0]}"

                for tb in range(NB):
                    t0 = tb * 512
                    p = tb % 2
                    xk = f'xbt{p}'
                    x_ = xbt[p]
                    def loads_x(tb_):
                        t0_ = tb_ * 512
                        xn = xbt[tb_ % 2]
                        xnk = f'xbt{tb_ % 2}'
                        if tb_ == 0:
                            sc.op('pool', lambda e: e.memset(xn[:, :, 0:32], 0.0), w=[xnk])
                            sc.dma('sp', xn[:, :, 32:544], chunked(XB)[:, :, 0:512], r=[('XB', 0)], w=[xnk])
                        else:
                            sc.dma('sp', xn[:, :, :], chunked(XB)[:, :, t0_ - 32:t0_ + 512], r=[('XB', tb_ - 1), ('XB', tb_)], w=[xnk])

                    def loads_cs(tb_):
                        t0_ = tb_ * 512
                        sc.dma('sp', cs_t[:], cos_d[:, t0_:t0_ + 512], w=['cs_t'])
                        sc.dma('sp', sn_t[:], sin_d[:, t0_:t0_ + 512], w=['sn_t'])

                    if tb == 0:
                        loads_x(0)
                        loads_cs(0)
                    if tb + 1 < NB:
                        loads_x(tb + 1)

                    def fm(c0, ncols=128, halo=False):
                        pm, pmk = bank()
                        for kc in range(8):
                            mm(pm[0:ncols, :], win[:, kc, c0:c0 + ncols], x_[:, kc, 32:544], kc == 0, kc == 7, r=[WK[kc], xk], w=[pmk])
                        if not halo:
                            return pm, pmk
                        ph, phk = bank()
                        for kc in range(8):
                            mm(ph[0:ncols, 0:32], win[:, kc, c0:c0 + ncols], x_[:, kc, 0:32], kc == 0, kc == 7, r=[WK[kc], xk], w=[phk])
                        return pm, pmk, ph, phk

                    for (c0, cp, dst, dk) in ((0, 2820, RQ, 'RQ'), (128, 2948, RK, 'RK')):
                        pa, pak = fm(c0)
                        pp, ppk = fm(cp)
                        sc.op('dve', lambda e: e.tensor_tensor(t1[:], pa[:, :], cs_t[:], ALU.mult), r=[pak, 'cs_t'], w=['t1'])
                        sc.op('dve', lambda e: e.tensor_tensor(t2[:], pp[:, :], sn_t[:], ALU.mult), r=[ppk, 'sn_t'], w=['t2'])
                        o, ok = nob()
                        sc.op('dve', lambda e: e.tensor_tensor(o[:], t1[:], t2[:], ALU.add), r=['t1', 't2'], w=[ok])
                        sc.dma('sp', dst[:, t0:t0 + 512], o[:], r=[ok], w=[(dk, tb)])
                    if tb + 1 < NB:
                        loads_cs(tb + 1)
                    for c in range(2):
                        pa, pak = fm(512 + 128 * c)
                        o, ok = nob()
                        sc.op('act', lambda e: e.activation(o[:], pa[:, :], AF.Silu), r=[pak], w=[ok])
                        sc.dma('sp', GATE[128 * c:128 * c + 128, t0:t0 + 512], o[:], r=[ok], w=[('GATE', tb)])
                    for c in range(2):
                        pa, pak, pah, pahk = fm(768 + 128 * c, halo=True)
                        pb_, pbk, pbh, pbhk = fm(1024 + 128 * c, halo=True)
                        u = u_t[c]
                        uk = f'u{c}'
                        sc.op('act', lambda e: e.activation(sg_t[:, 32:544], pb_[:, :], AF.Sigmoid), r=[pbk], w=['sg_t'])
                        sc.op('act', lambda e: e.activation(sg_t[:, 0:32], pbh[:, 0:32], AF.Sigmoid), r=[pbhk, 'sg_t'], w=['sg_t'])
                        sc.op('dve', lambda e: e.tensor_tensor(u[:, 32:544], pa[:, :], sg_t[:, 32:544], ALU.mult), r=[pak, 'sg_t'], w=[uk])
                        sc.op('dve', lambda e: e.tensor_tensor(u[:, 0:32], pah[:, 0:32], sg_t[:, 0:32], ALU.mult), r=[pahk, 'sg_t', uk], w=[uk])
                        ce = 'dve'
                        ca = ca_t[c]
                        cak = f'ca{c}'
                        wc0 = 31 * c
                        sc.op(ce, lambda e: e.tensor_scalar(ca[:], u[:, 2:514], cv[:, wc0:wc0 + 1], cv[:, 68 + c:69 + c], ALU.mult, ALU.add),
                              r=[uk, 'cv'], w=[cak])
                        for j in range(1, 31):
                            sc.op(ce, lambda e, j=j: e.scalar_tensor_tensor(ca[:], u[:, 2 + j:514 + j], cv[:, wc0 + j:wc0 + j + 1], ca[:], ALU.mult, ALU.add),
                                  r=[uk, 'cv', cak], w=[cak])
                    pm, pmk = bank()
                    pe2, pe2k = bank()
                    for c in range(2):
                        mm(pm[:, :], ones256[:], ca_t[c][:], c == 0, c == 1, r=['ones256', f'ca{c}'], w=[pmk])
                    for c in range(2):
                        sc.op('act', lambda e, c=c: e.activation(sq1[:], ca_t[c][:], AF.Square), r=[f'ca{c}'], w=['sq1'])
                        mm(pe2[:, :], ones256[:], sq1[:], c == 0, c == 1, r=['ones256', 'sq1'], w=[pe2k], inc=True)
                    sc.op('act', lambda e: e.copy(mean1[:], pm[:, :]), r=[pmk], w=['mean1'])
                    sc.op('dve', lambda e: e.tensor_tensor(rstd1[:], mean1[:], mean1[:], ALU.mult), r=['mean1'], w=['rstd1'])
                    sc.op('dve', lambda e: e.tensor_tensor(rstd1[:], pe2[:, :], rstd1[:], ALU.subtract), r=[pe2k, 'rstd1'], w=['rstd1'])
                    sc.op('dve', lambda e: e.tensor_scalar(rstd1[:], rstd1[:], 0.0, EPS, ALU.max, ALU.add), r=['rstd1'], w=['rstd1'])
                    sc.op('act', lambda e: e.activation(rstd1[:], rstd1[:], AF.Ln), r=['rstd1'], w=['rstd1'])
                    sc.op('act', lambda e: e.activation(rstd1[:], rstd1[:], AF.Exp, scale=-0.5), r=['rstd1'], w=['rstd1'])
                    for c in range(2):
                        sc.op('dve', lambda e, c=c: e.tensor_tensor(t1[:], ca_t[c][:], mean1[:], ALU.subtract), r=[f'ca{c}', 'mean1'], w=['t1'])
                        sc.op('dve', lambda e: e.tensor_tensor(t1[:], t1[:], rstd1[:], ALU.mult), r=['t1', 'rstd1'], w=['t1'])
                        o, ok = nob()
                        sc.op('act', lambda e, c=c: e.activation(o[:], t1[:], AF.Silu, bias=cv[:, 72 + c:73 + c], scale=cv[:, 70 + c:71 + c]),
                              r=['t1', 'cv'], w=[ok])
                        sc.dma('sp', MIXT[256 + 128 * c:384 + 128 * c, t0:t0 + 512], o[:], r=[ok], w=[('MIXT', tb)])
                    for c in range(2):
                        pc, pck, pch, pchk = fm(1536 + 128 * c, halo=True)
                        ph_, phk_, phh, phhk = fm(1792 + 128 * c, halo=True)
                        u = u_t[c]
                        uk = f'u{c}'
                        sc.op('act', lambda e: e.copy(sg_t[:, 32:544], pc[:, :]), r=[pck], w=['sg_t'])
                        sc.op('act', lambda e: e.copy(sg_t[:, 0:32], pch[:, 0:32]), r=[pchk, 'sg_t'], w=['sg_t'])
                        sc.op('dve', lambda e: e.tensor_tensor(u[:, 32:544], ph_[:, :], sg_t[:, 32:544], ALU.mult), r=[phk_, 'sg_t'], w=[uk])
                        sc.op('dve', lambda e: e.tensor_tensor(u[:, 0:32], phh[:, 0:32], sg_t[:, 0:32], ALU.mult), r=[phhk, 'sg_t', uk], w=[uk])
                        wc0 = 62 + 3 * c
                        sc.op('dve', lambda e: e.tensor_scalar(t1[:], u[:, 30:542], cv[:, wc0:wc0 + 1], None, ALU.mult), r=[uk, 'cv'], w=['t1'])
                        for j in (1, 2):
                            sc.op('dve', lambda e, j=j: e.scalar_tensor_tensor(t1[:], u[:, 30 + j:542 + j], cv[:, wc0 + j:wc0 + j + 1], t1[:], ALU.mult, ALU.add),
                                  r=[uk, 'cv', 't1'], w=['t1'])
                        pbb, pbbk = fm(1280 + 128 * c)
                        o, ok = nob()
                        sc.op('dve', lambda e: e.tensor_tensor(o[:], pbb[:, :], t1[:], ALU.mult), r=[pbbk, 't1'], w=[ok])
                        sc.dma('sp', MIXT[512 + 128 * c:640 + 128 * c, t0:t0 + 512], o[:], r=[ok], w=[('MIXT', tb)])
                    for c in range(2):
                        pa, pak = fm(2048 + 128 * c)
                        o, ok = nob()
                        sc.op('act', lambda e: e.mul(o[:], pa[:, :], 0.125), r=[pak], w=[ok])
                        sc.dma('sp', FQ[128 * c:128 * c + 128, t0:t0 + 512], o[:], r=[ok], w=[('FQ', tb)])
                        pa, pak = fm(2304 + 128 * c)
                        o, ok = nob()
                        sc.op('act', lambda e: e.copy(o[:], pa[:, :]), r=[pak], w=[ok])
                        sc.dma('sp', FK[128 * c:128 * c + 128, t0:t0 + 512], o[:], r=[ok], w=[('FK', tb)])
                    pa, pak = fm(2816, ncols=4)
                    sc.op('act', lambda e: e.copy(ff_t[:], pa[0:4, :]), r=[pak], w=['ff_t'])
                    sc.dma('sp', FF[:, t0:t0 + 512], ff_t[:], r=['ff_t'], w=[('FF', tb)])
                    for sub in range(4):
                        for (c0, dst, dk) in ((256, RV, 'RV'), (2560, FV, 'FV')):
                            pm, pmk = bank()
                            for kc in range(8):
                                mm(pm[:, 0:256], x_[:, kc, 32 + 128 * sub:160 + 128 * sub], win[:, kc, c0:c0 + 256], kc == 0, kc == 7, r=[WK[kc], xk], w=[pmk])
                            vtc[0] ^= 1
                            v = vt[vtc[0]]
                            vk = f'vt{vtc[0]}'
                            sc.op('act', lambda e: e.copy(v[:], pm[:, 0:256]), r=[pmk], w=[vk])
                            sc.dma('sp', dst[t0 + 128 * sub:t0 + 128 * sub + 128, :], v[:], r=[vk], w=[(dk, tb)])
                sc.barrier()
                if upto == 1:
                    stop[0] = True

            with ExitStack() as st2:
              for _once in ([0] if not stop[0] else []):
                rq_t = sb("rq_t", [32, S], BF16, st2)
                rk_t = sb("rk_t", [32, S], BF16, st2)
                qd_t = sb("qd_t", [32, S], BF16, st2)
                v_t = sb("v_t", [128, NT, 64], BF16, st2)
                v64 = sb("v64", [64, NCH, 64], BF16, st2)
                qdec = sb("qdec", [32, 512], F32, st2)
                stt = sb("stt", [32, 64], F32, st2)
                prevb = sb("prevb", [32, NCH, 64], BF16, st2)
                kd = [sb(f"kd{i}", [128, 32], BF16, st2) for i in range(2)]
                sd = [sb(f"sd{i}", [128, 128], BF16, st2) for i in range(2)]
                o_sb = sb("o_sb", [64, 512], F32, st2)
                sq2 = sb("sq2", [64, 512], F32, st2)
                mean2 = sb("mean2", [64, 512], F32, st2)
                rstd2 = sb("rstd2", [64, 512], F32, st2)
                g_t = sb("g_t", [64, 512], BF16, st2)
                ro = [sb(f"ro{i}", [64, 512], BF16, st2) for i in range(2)]
                for h in range(4):
                    gam = 1.0 - 2.0 ** (-5.0 - h)
                    cd = gam ** CHUNK
                    sc.dma('sp', rq_t[:], RQ[32 * h:32 * h + 32, :], w=['rq_t'])
                    sc.dma('sp', rk_t[:], RK[32 * h:32 * h + 32, :], w=['rk_t'])
                    sc.dma('sp', v_t[:], RV[:, 64 * h:64 * h + 64].rearrange("(j p) e -> p j e", p=128), w=['v_t'])
                    sc.dma('sp', qdec[:], cst_d[32 * h:32 * h + 32, 512:1024], w=['qdec'])
                    for tb in range(NB):
                        sc.op('dve', lambda e, tb=tb: e.tensor_tensor(qd_t[:, tb * 512:tb * 512 + 512], rq_t[:, tb * 512:tb * 512 + 512], qdec[:], ALU.mult),
                              r=['rq_t', 'qdec'], w=['qd_t'])
                    sc.op('dve', lambda e: e.memset(stt[:], 0.0), w=['stt'])
                    sc.dma('sp', v64[:], RV[:, 64 * h:64 * h + 64].rearrange("(c p) e -> p c e", p=64), w=['v64'])
                    for c in range(NCH):
                        sc.op('pe', lambda e, c=c: e.transpose(PSB[0:64, 0:32], rk_t[:, 64 * c:64 * c + 64], identb[0:32, 0:32]),
                              r=['rk_t', 'identb'], w=['psb'])
                        k_ = kd[c % 2]
                        kk = f'kd{c % 2}'
                        sc.op('dve', lambda e: e.tensor_scalar(k_[0:64, :], PSB[0:64, 0:32], cst[0:64, 1024 + h:1025 + h], None, ALU.mult), r=['psb', 'cst'], w=[kk])
                        pkv, pkvk = bank()
                        mm(pkv[0:32, 0:64], k_[0:64, :], v64[:, c, :], True, True, r=[kk, 'v64'], w=[pkvk])
                        sc.op('dve', lambda e, c=c: e.tensor_copy(prevb[:, c, :], stt[:]), r=['stt'], w=['prevb'])
                        sc.op('dve', lambda e: e.scalar_tensor_tensor(stt[:], stt[:], cd, pkv[0:32, 0:64], ALU.mult, ALU.add),
                              r=['stt', pkvk], w=['stt'])
                    for tb in range(NB if KN >= 5 else 0):
                        t0 = tb * 512
                        po, pok = bank()
                        for jj in range(4):
                            j = tb * 4 + jj
                            ps_, psk = bank()
                            mm(ps_[:, 0:128], rk_t[:, 128 * j:128 * j + 128], rq_t[:, 128 * j:128 * j + 128], True, True, r=['rk_t', 'rq_t'], w=[psk])
                            s_ = sd[j % 2]
                            sk = f'sd{j % 2}'
                            sc.op('dve', lambda e: e.tensor_tensor(s_[:], ps_[:, 0:128], cst[:, 128 * h:128 * h + 128], ALU.mult), r=[psk, 'cst'], w=[sk])
                            mm(po[0:64, 128 * jj:128 * jj + 128], v_t[:, j, :], s_[:], True, False, r=['v_t', sk], w=[pok], inc=False)
                            for hf in range(2):
                                c0 = 128 * jj + 64 * hf
                                mm(po[0:64, c0:c0 + 64], prevb[:, 2 * j + hf, :], qd_t[:, 128 * j + 64 * hf:128 * j + 64 * hf + 64], False, hf == 1,
                                   r=['prevb', 'qd_t'], w=[pok], inc=(hf == 1))
                        sc.op('act', lambda e: e.copy(o_sb[:], po[0:64, :]), r=[pok], w=['o_sb'])
                        sc.op('act', lambda e: e.activation(sq2[:], po[0:64, :], AF.Square), r=[pok], w=['sq2'])
                        pm, pmk = bank()
                        mm(pm[0:64, :], ones64[:], o_sb[:], True, True, r=['ones64', 'o_sb'], w=[pmk])
                        pe2, pe2k = bank()
                        mm(pe2[0:64, :], ones64[:], sq2[:], True, True, r=['ones64', 'sq2'], w=[pe2k])
                        sc.op('act', lambda e: e.copy(mean2[:], pm[0:64, :]), r=[pmk], w=['mean2'])
                        sc.op('dve', lambda e: e.tensor_tensor(rstd2[:], mean2[:], mean2[:], ALU.mult), r=['mean2'], w=['rstd2'])
                        sc.op('dve', lambda e: e.tensor_tensor(rstd2[:], pe2[0:64, :], rstd2[:], ALU.subtract), r=[pe2k, 'rstd2'], w=['rstd2'])
                        sc.op('dve', lambda e: e.tensor_scalar(rstd2[:], rstd2[:], 0.0, EPS, ALU.max, ALU.add), r=['rstd2'], w=['rstd2'])
                        sc.op('act', lambda e: e.activation(rstd2[:], rstd2[:], AF.Ln), r=['rstd2'], w=['rstd2'])
                        sc.op('act', lambda e: e.activation(rstd2[:], rstd2[:], AF.Exp, scale=-0.5), r=['rstd2'], w=['rstd2'])
                        sc.dma('sp', g_t[:], GATE[64 * h:64 * h + 64, t0:t0 + 512], w=['g_t'])
                        sc.op('dve', lambda e: e.tensor_tensor(o_sb[:], o_sb[:], mean2[:], ALU.subtract), r=['o_sb', 'mean2'], w=['o_sb'])
                        sc.op('dve', lambda e: e.tensor_tensor(o_sb[:], o_sb[:], rstd2[:], ALU.mult), r=['o_sb', 'rstd2'], w=['o_sb'])
                        sc.op('dve', lambda e: e.tensor_scalar(o_sb[:], o_sb[:], cv[0:64, 74 + h:75 + h], None, ALU.mult), r=['o_sb', 'cv'], w=['o_sb'])
                        r_ = ro[tb % 2]
                        rk_ = f'ro{tb % 2}'
                        sc.op('dve', lambda e: e.tensor_tensor(r_[:], o_sb[:], g_t[:], ALU.mult), r=['o_sb', 'g_t'], w=[rk_])
                        sc.dma('sp', MIXT[64 * h:64 * h + 64, t0:t0 + 512], r_[:], r=[rk_], w=[('MIXT2', h, tb)])
                sc.barrier()
                if upto == 2:
                    stop[0] = True

            with ExitStack() as st3:
              for _once in ([0] if not stop[0] else []):
                qa = sb("qa", [70, S], BF16, st3)
                ka = sb("ka", [70, S], BF16, st3)
                fv_t = sb("fv_t", [128, NT, 128], BF16, st3)
                sc.op('pool', lambda e: e.memset(fv_t[:], 1.0), w=['fv_t'])
                nd = sb("nd", [128, 512], F32, st3)
                msk = sb("msk", [128, 4 * 512], F32, st3)
                sc.dma('sp', msk[:], msk_d[:, :], w=['msk'])
                fb = sb("fb", [1, 4], F32, st3)
                sc.dma('sp', fb[:], foxb_d[l, :, :], w=['fb'])
                sc.op('dve', lambda e: e.tensor_scalar(fb[:], fb[:], -1.0, None, ALU.mult), r=['fb'], w=['fb'])
                FW = min(S, 2048)
                f_r = sb("f_r", [1, FW], F32, st3)
                F_r = sb("F_r", [1, FW], F32, st3)
                one_r = sb("one_r", [1, FW], F32, st3)
                sc.op('pool', lambda e: e.memset(one_r[:], 1.0), w=['one_r'])
                fr = sb("fr", [1, 7, FW], BF16, st3)
                r1 = sb("r1", [1, FW], F32, st3)
                r2 = sb("r2", [1, FW], F32, st3)
                Fl = sb("Fl", [1, 1], F32, st3)
                pt = [sb(f"pt{i}", [128, 512], BF16, st3) for i in range(3)]
                rden = sb("rden", [64, 512], F32, st3)
                fo = [sb(f"fo{i}", [64, 512], BF16, st3) for i in range(2)]
                for h in range(4):
                    sc.op('dve', lambda e: e.memset(Fl[:], 0.0), w=['Fl'])
                    for pc in range(S // FW):
                        sl = slice(pc * FW, pc * FW + FW)
                        sc.dma('sp', f_r[:], FF[h:h + 1, sl], w=['f_r'])
                        sc.op('act', lambda e: e.activation(f_r[:], f_r[:], AF.Exp, bias=fb[0:1, h:h + 1], scale=-1.0), r=['f_r', 'fb'], w=['f_r'])
                        sc.op('act', lambda e: e.activation(f_r[:], f_r[:], AF.Ln, bias=1.0), r=['f_r'], w=['f_r'])
                        sc.op('dve', lambda e: e.tensor_scalar(f_r[:], f_r[:], -1.0, None, ALU.mult), r=['f_r'], w=['f_r'])
                        sc.op('dve', lambda e: e.tensor_tensor_scan(F_r[:], one_r[:], f_r[:], Fl[0:1, 0:1], ALU.mult, ALU.add), r=['one_r', 'f_r', 'Fl'], w=['F_r'])
                        sc.op('dve', lambda e: e.tensor_copy(Fl[:], F_r[0:1, FW - 1:FW]), r=['F_r'], w=['Fl'])
                        sc.op('dve', lambda e: e.tensor_copy(fr[:, 0, :], F_r[:]), r=['F_r'], w=['fr'])
                        sc.op('dve', lambda e: e.tensor_tensor(r1[:], F_r[:], fr[:, 0, :], ALU.subtract), r=['F_r', 'fr'], w=['r1'])
                        sc.op('dve', lambda e: e.tensor_copy(fr[:, 1, :], r1[:]), r=['r1', 'fr'], w=['fr'])
                        sc.op('dve', lambda e: e.tensor_tensor(r2[:], r1[:], fr[:, 1, :], ALU.subtract), r=['r1', 'fr'], w=['r2'])
                        sc.op('dve', lambda e: e.tensor_copy(fr[:, 2, :], r2[:]), r=['r2', 'fr'], w=['fr'])
                        sc.op('dve', lambda e: e.tensor_copy(fr[:, 3, :], one_r[:]), r=['one_r', 'fr'], w=['fr'])
                        for i in range(3):
                            sc.op('dve', lambda e, i=i: e.tensor_scalar(fr[:, 4 + i, :], fr[:, i, :], -1.0, None, ALU.mult), r=['fr'], w=['fr'])
                        sc.dma('sp', FRD[:, sl].rearrange("(o r) t -> o r t", o=1), fr[:], r=['fr'], w=[('FRD', pc)])
                    FRk = [('FRD', pc) for pc in range(S // FW)]
                    sc.dma('sp', qa[0:64, :], FQ[64 * h:64 * h + 64, :], w=['qa'])
                    sc.dma('sp', qa[64:67, :], FRD[0:3, :], r=FRk + ['qa'], w=['qa'])
                    for i in range(3):
                        sc.dma('sp', qa[67 + i:68 + i, :], FRD[3:4, :], r=FRk + ['qa'], w=['qa'])
                    sc.dma('sp', ka[0:64, :], FK[64 * h:64 * h + 64, :], w=['ka'])
                    for i in range(3):
                        sc.dma('sp', ka[64 + i:65 + i, :], FRD[3:4, :], r=FRk + ['ka'], w=['ka'])
                    sc.dma('sp', ka[67:70, :], FRD[4:7, :], r=FRk + ['ka'], w=['ka'])
                    sc.dma('sp', fv_t[:, :, 0:64], FV[:, 64 * h:64 * h + 64].rearrange("(j p) e -> p j e", p=128), w=['fv_t'])
                    pi = 0
                    for qb in range(NB):
                        q0 = qb * 512
                        pn, pnk = PS[5], 'ps5'
                        pd, pdk = PS[6], 'ps6'
                        nk = 4 * qb + 4
                        def score(j):
                            mm(PS[j % 4][:, :], ka[:, 128 * j:128 * j + 128], qa[:, q0:q0 + 512], True, True, r=['ka', 'qa'], w=[f'ps{j % 4}'])

                        score(0)
                        if nk > 1:
                            score(1)
                        for j in range(nk):
                            if j + 2 < nk:
                                score(j + 2)
                            ps_ = PS[j % 4]
                            psk = f'ps{j % 4}'
                            p_ = pt[pi % 3]
                            pk = f'pt{pi % 3}'
                            pi += 1
                            sc.op('act', lambda e: e.activation(p_[:], ps_[:, :], AF.Exp), r=[psk], w=[pk])
                            if j >= 4 * qb:
                                m = j - 4 * qb
                                sc.op('dve', lambda e: e.tensor_tensor(p_[:], p_[:], msk[:, 512 * m:512 * m + 512], ALU.mult), r=[pk, 'msk'], w=[pk])
                            mm(pn[:, :], fv_t[:, j, :], p_[:], j == 0, j == nk - 1, r=['fv_t', pk], w=[pnk], inc=True)
                        sc.op('act', lambda e: e.copy(nd[:], pn[:, :]), r=[pnk], w=['nd'])
                        mm(pd[0:64, :], ident[:, 64:128], nd[:], True, True, r=['cst', 'nd'], w=[pdk])
                        sc.op('dve', lambda e: e.reciprocal(rden[:], pd[0:64, :]), r=[pdk], w=['rden'])
                        f_ = fo[qb % 2]
                        fk_ = f'fo{qb % 2}'
                        sc.op('dve', lambda e: e.tensor_tensor(f_[:], nd[0:64, :], rden[:], ALU.mult), r=['nd', 'rden'], w=[fk_])
                        sc.dma('sp', MIXT[768 + 64 * h:832 + 64 * h, q0:q0 + 512], f_[:], r=[fk_], w=[('MIXT3', h, qb)])
                sc.barrier()
                if upto == 3:
                    stop[0] = True

            with ExitStack() as st4:
              for _once in ([0] if not stop[0] else []):
                wo = sb("wo", [128, 8, D], BF16, st4)
                for kc in range(8):
                    sc.dma('pool', wo[:, kc, :], w_o_d[l, kc * 128:(kc + 1) * 128, :], w=[('wo', kc)])
                rwt = sb("rwt", [128, 8, NE], F32, st4)
                sc.dma('sp', rwt[:], rw_d[l].rearrange("(kc p) e -> p kc e", p=128), w=['rwt'])
                rbt = sb("rbt", [128, NE], F32, st4)
                sc.dma('sp', rbt[:], rb_d[l, 0:1, :].partition_broadcast(128), w=['rbt'])
                mx = [sb(f"mx{i}", [128, 8, 512], BF16, st4) for i in range(2)]
                xr = [sb(f"xr{i}", [128, 8, 512], F32, st4) for i in range(2)]
                z = sb("z", [128, 8, 512], F32, st4)
                stl = dict(sq=[sb(f"sq4{i}", [128, 512], F32, st4) for i in range(2)], sqk='s4', mean=sb("mean", [128, 512], F32, st4),
                           rstd=sb("rstd", [128, 512], F32, st4), tmp=sb("tmp", [128, 512], F32, st4))
                x1f = z
                x1h = sb("x1h", [128, 8, 512], BF16, st4)
                lg = sb("lg", [128, NE], F32, st4)
                m8 = sb("m8", [128, 8], F32, st4)
                nmx = sb("nmx", [128, 1], F32, st4)
                mk = sb("mk", [128, NE], F32, st4)
                ex = sb("ex", [128, NE], F32, st4)
                ssum = sb("ssum", [128, 1], F32, st4)
                gtt = sb("gtt", [NE, 512], F32, st4)
                xtm = [sb(f"xtm{i}", [128, D], BF16, st4) for i in range(2)]
                cnt = sb("cnt", [128, NE], F32, st4)
                sc.op('dve', lambda e: e.memset(cnt[:], 0.0), w=['cnt'])
                posf = sb("posf", [128, NE], F32, st4)
                ohf = sb("ohf", [128, NE], F32, st4)
                idx8 = sb("idx8", [128, 8], U32, st4)
                idxf = sb("idxf", [128, 8], F32, st4)
                pos4 = sb("pos4", [128, 4], F32, st4)
                ov4 = sb("ov4", [128, 4], F32, st4)
                e4 = sb("e4", [128, 4], F32, st4)
                for tb in range(NB):
                    t0 = tb * 512
                    p = tb % 2
                    sl = slice(t0, t0 + 512)
                    def loads4(tb_):
                        p_ = tb_ % 2
                        sl_ = slice(tb_ * 512, tb_ * 512 + 512)
                        sc.dma('sp', mx[p_][:], chunked(MIXT)[:, :, sl_], w=[f'mx{p_}'])
                        sc.dma('sp', xr[p_][:], chunked(XT)[:, :, sl_], w=[f'xr{p_}'])

                    if tb == 0:
                        loads4(0)
                    if tb + 1 < NB:
                        loads4(tb + 1)
                    for dc in range(8):
                        pm, pmk = bank()
                        for kc in range(8):
                            mm(pm[:, :], wo[:, kc, 128 * dc:128 * dc + 128], mx[p][:, kc, :], kc == 0, kc == 7, r=[('wo', kc), f'mx{p}'], w=[pmk])
                        sc.op('dve', lambda e, dc=dc: e.scalar_tensor_tensor(z[:, dc, :], xr[p][:, dc, :], ALPHA, pm[:, :], ALU.mult, ALU.add),
                              r=[f'xr{p}', pmk], w=['z'])
                    ln_block(z, 'z', 78, 86, stl)
                    sc.op('dve', lambda e: e.tensor_copy(x1h[:], z[:]), r=['z'], w=['x1h'])
                    sc.dma('sp', chunked(X1T)[:, :, sl], x1f[:], r=['z'], w=[('X1T', tb)])
                    sc.dma('sp', chunked(X1B)[:, :, sl], x1h[:], r=['x1h'], w=[('X1B', tb)])
                    for sub in range(4):
                        pm, pmk = bank()
                        for kc in range(8):
                            mm(pm[:, 0:NE], x1f[:, kc, 128 * sub:128 * sub + 128], rwt[:, kc, :], kc == 0, kc == 7, r=['z', 'rwt'], w=[pmk])
                        sc.op('dve', lambda e: e.tensor_tensor(lg[:], pm[:, 0:NE], rbt[:], ALU.add), r=[pmk, 'rbt'], w=['lg'])
                        sc.op('dve', lambda e: e.max(m8[:], lg[:]), r=['lg'], w=['m8'])
                        sc.op('dve', lambda e: e.tensor_scalar(mk[:], lg[:], m8[:, 3:4], None, ALU.is_ge), r=['lg', 'm8'], w=['mk'])
                        sc.op('dve', lambda e: e.tensor_scalar(nmx[:], m8[:, 0:1], -1.0, None, ALU.mult), r=['m8'], w=['nmx'])
                        sc.op('act', lambda e: e.activation(ex[:], lg[:], AF.Exp, bias=nmx[:, 0:1], scale=1.0), r=['lg', 'nmx'], w=['ex'])
                        sc.op('dve', lambda e: e.tensor_tensor(ex[:], ex[:], mk[:], ALU.mult), r=['ex', 'mk'], w=['ex'])
                        sc.op('dve', lambda e: e.tensor_reduce(ssum[:], ex[:], AX.X, ALU.add), r=['ex'], w=['ssum'])
                        sc.op('dve', lambda e: e.reciprocal(ssum[:], ssum[:]), r=['ssum'], w=['ssum'])
                        sc.op('dve', lambda e: e.tensor_scalar(ex[:], ex[:], ssum[:, 0:1], None, ALU.mult), r=['ex', 'ssum'], w=['ex'])
                        tt_ = tb * 4 + sub
                        sc.op('dve', lambda e: e.max_index(idx8[:], m8[:], lg[:]), r=['m8', 'lg'], w=['idx8'])
                        sc.op('dve', lambda e: e.tensor_copy(idxf[:], idx8[:]), r=['idx8'], w=['idxf'])
                        sc.op('act', lambda e: e.activation(e4[:], m8[:, 0:4], AF.Exp, bias=nmx[:, 0:1], scale=1.0), r=['m8', 'nmx'], w=['e4'])
                        sc.op('dve', lambda e: e.tensor_scalar(GK[:, tt_, :], e4[:], ssum[:, 0:1], None, ALU.mult), r=['e4', 'ssum'], w=['GK'])
                        pp, ppk = bank()
                        mm(pp[:, 0:NE], iot[:, 0:128], mk[:], True, True, r=['iot', 'mk'], w=[ppk])
                        sc.op('dve', lambda e: e.tensor_tensor(posf[:], pp[:, 0:NE], cnt[:], ALU.add), r=[ppk, 'cnt'], w=['posf'])
                        pt_, ptk = bank()
                        mm(pt_[:, 0:NE], ones1[:], mk[:], True, True, r=['ones1', 'mk'], w=[ptk])
                        sc.op('dve', lambda e: e.tensor_tensor(cnt[:], cnt[:], pt_[:, 0:NE], ALU.add), r=['cnt', ptk], w=['cnt'])
                        for k in range(4):
                            sc.op('dve', lambda e, k=k: e.tensor_scalar(ohf[:], iot[:, 128:128 + NE], idxf[:, k:k + 1], None, ALU.is_equal), r=['iot', 'idxf'], w=['ohf'])
                            sc.op('dve', lambda e: e.tensor_tensor(ohf[:], ohf[:], posf[:], ALU.mult), r=['ohf', 'posf'], w=['ohf'])
                            sc.op('dve', lambda e, k=k: e.tensor_reduce(pos4[:, k:k + 1], ohf[:], AX.X, ALU.add), r=['ohf'], w=['pos4'])
                        sc.op('dve', lambda e: e.tensor_scalar(ov4[:], pos4[:], float(CAP), 1.0e6, ALU.is_ge, ALU.mult), r=['pos4'], w=['ov4'])
                        sc.op('dve', lambda e: e.scalar_tensor_tensor(pos4[:], idxf[:, 0:4], float(CAP), pos4[:], ALU.mult, ALU.add), r=['idxf', 'pos4'], w=['pos4'])
                        sc.op('dve', lambda e: e.tensor_tensor(pos4[:], pos4[:], ov4[:], ALU.add), r=['pos4', 'ov4'], w=['pos4'])
                        sc.op('dve', lambda e: e.tensor_copy(SLOT[:, tt_, :], pos4[:]), r=['pos4'], w=['SLOT'])
                        for kc in range(8):
                            sc.op('pe', lambda e, kc=kc: e.transpose(PSB[:, 128 * kc:128 * kc + 128], x1h[:, kc, 128 * sub:128 * sub + 128], identb[:, :]),
                                  r=['x1h', 'identb'], w=['psb'], inc=(kc == 7))
                        xt_ = xtm[sub % 2]
                        xtk = f'xtm{sub % 2}'
                        sc.op('act', lambda e: e.copy(xt_[:], PSB[:, :]), r=['psb'], w=[xtk])
                        for k in range(4):
                            sc.idma(XG[:, :], bass.IndirectOffsetOnAxis(ap=SLOT[:, tt_, k:k + 1], axis=0), xt_[:, :], None, bnd_reg,
                                    r=[xtk, 'SLOT'], w=[('XG', tt_, k)])
                        pg, pgk = bank()
                        mm(pg[0:NE, 0:128], ex[:], ident, True, True, r=['ex', 'cst'], w=[pgk])
                        sc.op('act', lambda e, sub=sub: e.copy(gtt[:, 128 * sub:128 * sub + 128], pg[0:NE, 0:128]), r=[pgk], w=['gtt'])
                    sc.dma('sp', GT[:, sl], gtt[:], r=['gtt'], w=[('GT', tb)])
                if dbg is not None:
                    sc.dma('sp', SLOTD[:, :], SLOT[:].rearrange('p a b -> p (a b)'), r=['SLOT'], w=['SLOTD'])
                    sc.dma('sp', GKD[:, :], GK[:].rearrange('p a b -> p (a b)'), r=['GK'], w=['GKD'])
                sc.barrier()
                if upto == 4:
                    stop[0] = True

            with ExitStack() as st5:
              for _once in ([0] if not stop[0] else []):
                w1t = [sb(f"w1t{i}", [128, 8, 2048], BF16, st5) for i in range(2)]
                w2t = [sb(f"w2t{i}", [128, 8, D], BF16, st5) for i in range(2)]
                b1t = sb("b1t", [128, NE * 16], F32, st5)
                sc.dma('sp', b1t[:], b1_d[l, :, :], w=['b1t'])
                b2T = sb("b2T", [128, NE * 8], F32, st5)
                sc.dma('sp', b2T[:], b2T_d[l, :, :], w=['b2T'])
                b1p = sb("b1p", [128, NE * 16], F32, st5)
                sc.op('dve', lambda e: e.tensor_scalar(b1p[:], b1t[:], 1.0, None, ALU.add), r=['b1t'], w=['b1p'])
                b1m = b1t
                sc.op('dve', lambda e: e.tensor_scalar(b1m[:], b1t[:], -1.0, 7.0, ALU.mult, ALU.add), r=['b1t'], w=['b1t'])
                c119 = sb("c119", [128, 1], F32, st5)
                sc.op('pool', lambda e: e.memset(c119[:], 1.702 * 7.0), w=['c119'])
                xgt = sb("xgt", [128, 4, D], BF16, st5)
                xs = sb("xs", [128, 8, 512], BF16, st5)
                actt = [sb("actt0", [128, 8, 512], BF16, st5)] * 2
                blk = [0]
                ga = [sb(f"ga{i}", [128, 512], F32, st5) for i in range(3)]
                sgm = [sb(f"sgm{i}", [128, 512], F32, st5) for i in range(3)]
                li = [sb(f"li{i}", [128, 512], F32, st5) for i in range(3)]
                b2b = [sb(f"b2b{i}", [128, D], F32, st5) for i in range(2)]
                yk = [sb(f"yk{i}", [128, D], F32, st5) for i in range(6)]
                ytm = [yk[0], yk[1]]
                ycomb = sb("ycomb", [128, D], F32, st5)
                acc = sb("acc", [128, 8, 512], F32, st5)
                hb5 = [xs[:, 0, :], xs[:, 1, :]]
                xr5 = [li[0], li[1]]
                stl = dict(sq=[ga[0], ga[1]], sqk='ga', mean=sgm[0], rstd=sgm[1], tmp=sgm[2], keys=('sgm0', 'sgm1', 'sgm2'))
                XGK = [('XG', t_, k) for t_ in range(NT) for k in range(4)]
                wn = 0
                ytc = [0]
                for ex_ in range(NE):
                    wp = wn % 2
                    wn += 1
                    for kc in range(8):
                        sc.dma('pool', w1t[wp][:, kc, :], w1_d[l, ex_, kc * 128:(kc + 1) * 128, :], w=[(f'w1t{wp}', kc)])
                    for kc in range(8):
                        sc.dma('pool', w2t[wp][:, kc, :], w2_d[l, ex_, kc * 128:(kc + 1) * 128, :], w=[(f'w2t{wp}', kc)])
                    sc.dma('sp', b2b[wp][:], b2_d[l, ex_:ex_ + 1, :].partition_broadcast(128), w=[f'b2b{wp}'])
                    for cb in range(CAPB):
                        r0 = ex_ * CAP + 512 * cb
                        if ex_ == 0 and cb == 0:
                            sc.dma('sp', xgt[:], XG[r0:r0 + 512, :].rearrange("(j p) d -> p j d", p=128), r=XGK, w=['xgt'])
                        for kp in range(4):
                            for kk in range(2):
                                kc = 2 * kp + kk
                                for j in range(4):
                                    sc.op('pe', lambda e, kc=kc, j=j, kk=kk: e.transpose(PSB[:, 512 * kk + 128 * j:512 * kk + 128 * j + 128], xgt[:, j, 128 * kc:128 * kc + 128], identb[:, :]),
                                          r=['xgt', 'identb'], w=['psb'], inc=(kk == 1 and j == 3))
                            sc.op('act', lambda e, kp=kp: e.copy(xs[:, 2 * kp:2 * kp + 2, :].rearrange("p a b -> p (a b)"), PSB[:, :]), r=['psb'], w=[('xs', kp)])
                        nxt = ex_ * CAPB + cb + 1
                        if nxt < NE * CAPB:
                            rn = (nxt // CAPB) * CAP + 512 * (nxt % CAPB)
                            sc.dma('sp', xgt[:], XG[rn:rn + 512, :].rearrange("(j p) d -> p j d", p=128), w=['xgt'])
                        XSK = [('xs', kc // 2) for kc in range(8)]
                        ab = actt[blk[0] % 2]
                        abk = 'actt0'
                        blk[0] += 1

                        def X(fc):
                            pg, pgk = bank()
                            for kc in range(8):
                                mm(pg[:, :], w1t[wp][:, kc, 128 * fc:128 * fc + 128], xs[:, kc, :], kc == 0, kc == 7, r=[(f'w1t{wp}', kc), XSK[kc]], w=[pgk])
                            pl, plk = bank()
                            for kc in range(8):
                                mm(pl[:, :], w1t[wp][:, kc, 1024 + 128 * fc:1152 + 128 * fc], xs[:, kc, :], kc == 0, kc == 7, r=[(f'w1t{wp}', kc), XSK[kc]], w=[plk])
                            q = fc % 3
                            bl = b1p[:, ex_ * 16 + 8 + fc:ex_ * 16 + 8 + fc + 1]
                            bg7 = b1m[:, ex_ * 16 + fc:ex_ * 16 + fc + 1]
                            sc.op('act', lambda e: e.activation(sgm[q][:], pg[:, :], AF.Relu, bias=bg7, scale=-1.0), r=[pgk, 'b1t'], w=[f'sgm{q}'])
                            sc.op('act', lambda e: e.activation(ga[q][:], sgm[q][:], AF.Silu, bias=c119[:, 0:1], scale=-1.702), r=[f'sgm{q}', 'c119'], w=[f'ga{q}'])
                            sc.op('dve', lambda e: e.tensor_scalar(li[q][:], pl[:, :], bl, -6.0, ALU.add, ALU.max), r=[plk, 'b1p'], w=[f'li{q}'])

                        def Y(fc):
                            q = fc % 3
                            sc.op('dve', lambda e: e.scalar_tensor_tensor(ab[:, fc, :], li[q][:], 8.0, ga[q][:], ALU.min, ALU.mult), r=[f'ga{q}', f'li{q}'], w=[(abk, fc)])

                        X(0)
                        X(1)
                        for fc in range(8):
                            if fc + 2 < 8:
                                X(fc + 2)
                            Y(fc)
                        for j in range(4):
                            y_ = ytm[ytc[0] % 2]
                            yk_ = f'yk{ytc[0] % 2}'
                            ytc[0] += 1
                            for dh in range(2):
                                py, pyk = bank()
                                for fc in range(8):
                                    mm(py[:, :], ab[:, fc, 128 * j:128 * j + 128], w2t[wp][:, fc, 512 * dh:512 * dh + 512], fc == 0, fc == 7,
                                       r=[(f'w2t{wp}', fc), (abk, fc)], w=[pyk])
                                sc.op('dve', lambda e, dh=dh: e.scalar_tensor_tensor(y_[:, 512 * dh:512 * dh + 512], py[:, :], 1.0 / 1.702, b2b[wp][:, 512 * dh:512 * dh + 512], ALU.mult, ALU.add),
                                      r=[pyk, f'b2b{wp}'], w=[yk_])
                            sc.dma('sp', YS[r0 + 128 * j:r0 + 128 * j + 128, :], y_[:], r=[yk_], w=[('YS', ex_, cb, j)])
                YSK = [('YS', e_, c_, j_) for e_ in range(NE) for c_ in range(CAPB) for j_ in range(4)]
                for tb in range(NB):
                    sl = slice(tb * 512, tb * 512 + 512)
                    for sub in range(4):
                        tt_ = tb * 4 + sub
                        ks = [(4 * tt_ + k) % 6 for k in range(4)]
                        for k in range(4):
                            sc.idma(yk[ks[k]][:, :], None, YS[:, :], bass.IndirectOffsetOnAxis(ap=SLOT[:, tt_, k:k + 1], axis=0), bnd_reg,
                                    r=(YSK if (tt_ == 0 and k == 0) else []) + ['SLOT'], w=[f'yk{ks[k]}'])
                        sc.op('dve', lambda e: e.tensor_scalar(ycomb[:], yk[ks[0]][:], GK[:, tt_, 0:1], None, ALU.mult), r=[f'yk{ks[0]}', 'GK'], w=['ycomb'])
                        for k in range(1, 4):
                            sc.op('dve', lambda e, k=k: e.scalar_tensor_tensor(ycomb[:], yk[ks[k]][:], GK[:, tt_, k:k + 1], ycomb[:], ALU.mult, ALU.add),
                                  r=[f'yk{ks[k]}', 'GK', 'ycomb'], w=['ycomb'])
                        for half in range(2):
                            pT, pTk = bank()
                            for d4 in range(4):
                                dc = 4 * half + d4
                                mm(pT[:, 128 * d4:128 * d4 + 128], ycomb[:, 128 * dc:128 * dc + 128], ident, True, True, r=['ycomb', 'cst'], w=[pTk], inc=(d4 == 3))
                            sc.op('act', lambda e, half=half, sub=sub: e.copy(acc[:, 4 * half:4 * half + 4, 128 * sub:128 * sub + 128],
                                                                              pT[:, :].rearrange("p (a b) -> p a b", a=4)), r=[pTk], w=['acc'])
                    zs = acc
                    for dc in range(8):
                        xr_ = xr5[dc % 2]
                        xk_ = f'li{dc % 2}'
                        sc.dma('sp', xr_[:], X1T[128 * dc:128 * dc + 128, sl], r=[('X1T', tb)], w=[xk_])
                        sc.op('dve', lambda e, dc=dc: e.scalar_tensor_tensor(zs[:, dc, :], xr_[:], ALPHA, zs[:, dc, :], ALU.mult, ALU.add),
                              r=[xk_, 'acc'], w=['acc'])
                    ln_block(zs, 'acc', 94, 102, stl)
                    if last:
                        sc.dma('sp', chunked(outT)[:, :, sl], zs[:], r=['acc'], w=[('outT', tb)])
                    else:
                        sc.dma('sp', chunked(XT)[:, :, sl], zs[:], r=['acc'], w=[('XT', tb)])
                        for dc in range(8):
                            hb_ = hb5[dc % 2]
                            hk_ = ('xs', 0)
                            sc.op('act', lambda e, dc=dc: e.copy(hb_, zs[:, dc, :]), r=['acc'], w=[hk_])
                            sc.dma('sp', XB[128 * dc:128 * dc + 128, sl], hb_, r=[hk_], w=[('XB', tb, dc)])
                sc.barrier()
                if upto == 5:
                    stop[0] = True
        except _Stop:
            pass
    return nc


def host_consts(S):
    half = 16
    freqs = (10000.0 ** (-np.arange(half, dtype=np.float32) / half)).astype(np.float32)
    pos = np.arange(S, dtype=np.float32)
    ang = pos[None, :] * freqs[:, None]
    cos = np.cos(ang).astype(np.float32)
    sin = np.sin(ang).astype(np.float32)
    ropec = np.tile(np.concatenate([cos, cos], 0), (4, 1))
    ropes = np.tile(np.concatenate([-sin, sin], 0), (4, 1))
    cst = np.zeros((128, 4 * 128 + 512 + 4 + 128), np.float32)
    idx = np.arange(128)
    same = (idx[:, None] // 64) == (idx[None, :] // 64)
    scale = 32 ** -0.5
    for h in range(4):
        g = 1.0 - 2.0 ** (-5.0 - h)
        cst[:, 128 * h:128 * h + 128] = np.where(same, g ** np.abs(idx[:, None] - idx[None, :]), 0.0) * scale
        cst[32 * h:32 * h + 32, 512:1024] = (g ** ((np.arange(512) % 64) + 1.0))[None, :]
        cst[:, 1024 + h] = g ** (63 - (idx % 64)) * scale
    cst[:, 1028:1156] = np.eye(128, dtype=np.float32)
    msk = np.zeros((128, 4 * 512), np.float32)
    s_ = np.arange(128)[:, None]
    t_ = np.arange(512)[None, :]
    for m in range(4):
        msk[:, 512 * m:512 * m + 512] = (t_ >= 128 * m + s_)
    io = np.zeros((128, 160), np.float32)
    io[:, 0:128] = (np.arange(128)[:, None] < np.arange(128)[None, :])
    io[:, 128:160] = np.arange(32, dtype=np.float32)[None, :]
    return dict(ropec=np.ascontiguousarray(ropec), ropes=np.ascontiguousarray(ropes), cst=cst, msk=msk, iota=io)


def host_layout(inp, S, DEPTH, NE):
    L = DEPTH
    perm = np.concatenate([np.arange(16, 32), np.arange(0, 16)])
    pq = np.concatenate([h * 32 + perm for h in range(4)])
    w_in = np.asarray(inp['w_in'])
    w_aug = np.concatenate([w_in, w_in[:, :, pq], w_in[:, :, 128 + pq]], axis=2)
    cv = np.zeros((L, 128, NCV), np.float32)
    cdw = np.asarray(inp['conf_dw'])
    sdw = np.asarray(inp['sc_dw'])
    for c in range(2):
        cv[:, :, 31 * c:31 * c + 31] = cdw[:, :, 128 * c:128 * c + 128].transpose(0, 2, 1)
        cv[:, :, 62 + 3 * c:65 + 3 * c] = sdw[:, :, 128 * c:128 * c + 128].transpose(0, 2, 1)
        cv[:, :, 68 + c] = np.asarray(inp['conf_dw_b'])[:, 128 * c:128 * c + 128]
        cv[:, :, 70 + c] = np.asarray(inp['conf_ln_g'])[:, 128 * c:128 * c + 128]
        cv[:, :, 72 + c] = np.asarray(inp['conf_ln_b'])[:, 128 * c:128 * c + 128]
    cv[:, 0:64, 74:78] = np.asarray(inp['ret_gn_g']).reshape(L, 4, 64).transpose(0, 2, 1)
    for name, c0 in (('ln1_g', 78), ('ln1_b', 86), ('ln2_g', 94), ('ln2_b', 102)):
        cv[:, :, c0:c0 + 8] = np.asarray(inp[name]).reshape(L, 8, 128).transpose(0, 2, 1)
    b1T = np.ascontiguousarray(np.asarray(inp['b1']).reshape(L, NE, 16, 128).transpose(0, 3, 1, 2).reshape(L, 128, NE * 16))
    b2T = np.ascontiguousarray(np.asarray(inp['b2']).reshape(L, NE, 8, 128).transpose(0, 3, 1, 2).reshape(L, 128, NE * 8))
    rw = np.asarray(inp['router_w'])
    rb = np.asarray(inp['router_b'])
    common = dict(w_in=np.ascontiguousarray(w_aug), w_o=np.asarray(inp['w_o']), cvec=cv,
                  foxb=np.asarray(inp['fox_b_f']).reshape(L, 1, 4), rw=rw, rb=rb.reshape(L, 1, -1),
                  w1=np.asarray(inp['w1']), b1T=b1T, w2=np.asarray(inp['w2']), b2=np.asarray(inp['b2']), b2T=b2T)
    common.update(host_consts(S))
    return common


def run(inp, S, DEPTH, NE, SBK, dbg=None, upto=99, trace=False):
    x = np.asarray(inp['x'])
    B = x.shape[0]
    common = host_layout(inp, S, DEPTH, NE)
    nc = build(S, DEPTH, NE, SBK, dbg, upto)
    in_maps = []
    for b in range(B):
        m = dict(common)
        m['xT'] = np.ascontiguousarray(x[b].T)
        in_maps.append(m)
    res = run_bass_kernel_spmd(nc, in_maps, core_ids=list(range(B)), **({'trace': True} if trace else {}))
    if trace:
        print('EXEC_NS', res.exec_time_ns)
    out = np.stack([np.ascontiguousarray(r['outT'].T) for r in res.results], 0).astype(np.float32)
    if dbg is not None:
        return out, res.results
    return out


def kernel(**inputs):
    return run(inputs, 8192, 4, 32, 1024)
```
